# Optimizing a Trainium2 kernel written in Bass

```python
import math
import jax
import jax.numpy as jnp
from jax import lax
import numpy as np

D_MODEL = 1024
BATCH = 16
SEQ = 4096
DEPTH = 4

GRID_W = 64
CTX_LEN = 256
MIX_WIDTH = D_MODEL
GROUP_WIDTH = MIX_WIDTH // 4
Q_BLOCK = 128
ROPE_BASE = 10000.0
EPS = 1e-6

MLA_HEADS = 4
MLA_NOPE = 64
MLA_ROPE = 32
MLA_V = GROUP_WIDTH // MLA_HEADS
MLA_Q_RANK = 192
MLA_KV_RANK = 128
GQA_HEADS = 4
GQA_KV_HEADS = 2
GQA_HEAD_DIM = GROUP_WIDTH // GQA_HEADS
SSM_GROUP = 16
SSM_GROUPS = GROUP_WIDTH // SSM_GROUP
SSM_STATE = 64
HY_WIDTH = GROUP_WIDTH
HY_ORDER = 2
HY_EMB = 33
HY_BANDS = (HY_EMB - 1) // 2
HY_HIDDEN = 64
HY_CONV = 3
HY_FAST_DECAY = 0.3
HY_SLOW_DECAY = 1.5
HY_DECAY_TARGET = 1e-2

MLA_COLS = MLA_Q_RANK + MLA_KV_RANK + MLA_ROPE + GROUP_WIDTH
GQA_COLS = (GQA_HEADS + 2 * GQA_KV_HEADS) * GQA_HEAD_DIM + GROUP_WIDTH
SSM_COLS = 2 * GROUP_WIDTH
HY_COLS = (HY_ORDER + 1) * HY_WIDTH + GROUP_WIDTH
IN_COLS = MLA_COLS + GQA_COLS + SSM_COLS + HY_COLS

kernel_name = 'hybrid_parallel_group_dit_block'


def rms_norm(x, g):
    xf = x.astype(jnp.float32)
    y = xf * lax.rsqrt(jnp.mean(xf * xf, axis=-1, keepdims=True) + EPS)
    return (y * g.astype(jnp.float32)).astype(x.dtype)


def grid_positions(n_rows):
    row = jnp.repeat(jnp.arange(n_rows, dtype=jnp.float32), GRID_W)
    col = jnp.tile(jnp.arange(GRID_W, dtype=jnp.float32), n_rows)
    return row, col


def _rotate(x, pos):
    h = x.shape[-1]
    inv = ROPE_BASE ** (-jnp.arange(0, h, 2, dtype=jnp.float32) / h)
    ang = pos[:, None] * inv[None, :]
    cos = jnp.cos(ang)[None, :, None, :].astype(x.dtype)
    sin = jnp.sin(ang)[None, :, None, :].astype(x.dtype)
    x1, x2 = x[..., :h // 2], x[..., h // 2:]
    return jnp.concatenate([x1 * cos - x2 * sin, x1 * sin + x2 * cos], axis=-1)


def axial_rope(x, row, col):
    half = x.shape[-1] // 2
    return jnp.concatenate([_rotate(x[..., :half], row), _rotate(x[..., half:], col)], axis=-1)


def block_attention(q, k, v, scale):
    b, n, h, dk = q.shape
    hk = k.shape[2]
    dv = v.shape[-1]
    nb = n // Q_BLOCK
    qb = q.reshape(b, nb, Q_BLOCK, hk, h // hk, dk).transpose(1, 0, 2, 3, 4, 5)

    def attend(qblk):
        s = jnp.einsum('bqhgd,bkhd->bhgqk', qblk, k, preferred_element_type=jnp.float32) * scale
        p = jax.nn.softmax(s, axis=-1).astype(v.dtype)
        return jnp.einsum('bhgqk,bkhd->bqhgd', p, v)

    o = lax.map(attend, qb)
    return o.transpose(1, 0, 2, 3, 4, 5).reshape(b, n, h, dv)


def mla_mixer(p_lat, p_ctx, g_cq, w_uq, g_ckv, w_ukv, row, col, need_ctx):
    r1 = MLA_Q_RANK
    r2 = r1 + MLA_KV_RANK
    r3 = r2 + MLA_ROPE
    scale = (MLA_NOPE + MLA_ROPE) ** -0.5

    def keys(p, rotary):
        b, n, _ = p.shape
        kv = (rms_norm(p[..., r1:r2], g_ckv) @ w_ukv).reshape(b, n, MLA_HEADS, MLA_NOPE + MLA_V)
        k_rope = p[..., r2:r3][:, :, None, :]
        if rotary:
            k_rope = axial_rope(k_rope, row, col)
        k = jnp.concatenate([kv[..., :MLA_NOPE], jnp.broadcast_to(k_rope, (b, n, MLA_HEADS, MLA_ROPE))], axis=-1)
        return k, kv[..., MLA_NOPE:]

    def attend(p, k, v, rotary):
        b, n, _ = p.shape
        q = (rms_norm(p[..., :r1], g_cq) @ w_uq).reshape(b, n, MLA_HEADS, MLA_NOPE + MLA_ROPE)
        q_rope = q[..., MLA_NOPE:]
        if rotary:
            q_rope = axial_rope(q_rope, row, col)
        q = jnp.concatenate([q[..., :MLA_NOPE], q_rope], axis=-1)
        o = block_attention(q, k, v, scale).reshape(b, n, GROUP_WIDTH)
        return o * jax.nn.silu(p[..., r3:])

    k_c, v_c = keys(p_ctx, False)
    k_l, v_l = keys(p_lat, True)
    o_l = attend(p_lat, jnp.concatenate([k_l, k_c], axis=1), jnp.concatenate([v_l, v_c], axis=1), True)
    o_c = attend(p_ctx, k_c, v_c, False) if need_ctx else None
    return o_l, o_c


def gqa_mixer(p_lat, p_ctx, g_q, g_k, row, col, need_ctx):
    qd = GQA_HEADS * GQA_HEAD_DIM
    kd = GQA_KV_HEADS * GQA_HEAD_DIM
    scale = GQA_HEAD_DIM ** -0.5

    def keys(p, rotary):
        b, n, _ = p.shape
        k = rms_norm(p[..., qd:qd + kd].reshape(b, n, GQA_KV_HEADS, GQA_HEAD_DIM), g_k)
        v = p[..., qd + kd:qd + 2 * kd].reshape(b, n, GQA_KV_HEADS, GQA_HEAD_DIM)
        if rotary:
            k = axial_rope(k, row, col)
        return k, v

    def attend(p, k, v, rotary):
        b, n, _ = p.shape
        q = rms_norm(p[..., :qd].reshape(b, n, GQA_HEADS, GQA_HEAD_DIM), g_q)
        if rotary:
            q = axial_rope(q, row, col)
        o = block_attention(q, k, v, scale).reshape(b, n, GROUP_WIDTH)
        return o * jax.nn.silu(p[..., qd + 2 * kd:])

    k_c, v_c = keys(p_ctx, False)
    k_l, v_l = keys(p_lat, True)
    o_l = attend(p_lat, jnp.concatenate([k_l, k_c], axis=1), jnp.concatenate([v_l, v_c], axis=1), True)
    o_c = attend(p_ctx, k_c, v_c, False) if need_ctx else None
    return o_l, o_c


def ssm_discretise(lam_re, lam_im, log_step, b_re, b_im):
    lam = lax.complex(lam_re.astype(jnp.float32), lam_im.astype(jnp.float32))
    step = jnp.exp(log_step.astype(jnp.float32))[:, None]
    a_bar = jnp.exp(lam * step)
    b_mat = lax.complex(b_re.astype(jnp.float32), b_im.astype(jnp.float32))
    b_bar = ((a_bar - 1.0) / lam)[..., None] * b_mat
    return a_bar, b_bar


def linear_scan(a_bar, bu, s0, reverse):
    a = jnp.broadcast_to(a_bar, bu.shape)

    def combine(e1, e2):
        return e1[0] * e2[0], e2[0] * e1[1] + e2[1]

    a_cum, h = lax.associative_scan(combine, (a, bu), axis=1, reverse=reverse)
    if s0 is not None:
        h = h + a_cum * s0[:, None]
    return h


def ssm_mixer(p_lat, p_ctx, lam_re, lam_im, log_step, b_re, b_im, c_re, c_im, d_skip, glu_w, glu_b, need_ctx):
    w = GROUP_WIDTH

    def drive(p, b_bar):
        b, n, _ = p.shape
        u = p[..., :w].astype(jnp.float32).reshape(b, n, SSM_GROUPS, SSM_GROUP)
        return jnp.einsum('bngh,gph->bngp', u.astype(jnp.complex64), b_bar)

    def readout(states, c_mat):
        return jnp.einsum('bngp,ghp->bngh', states, c_mat).real

    def finish(p, y):
        b, n, _ = p.shape
        u = p[..., :w].astype(jnp.float32)
        y = y.reshape(b, n, w) + d_skip.astype(jnp.float32) * u
        z = jax.nn.gelu(y)
        z = z * jax.nn.sigmoid(z @ glu_w.astype(jnp.float32) + glu_b.astype(jnp.float32))
        return z.astype(p.dtype) * jax.nn.silu(p[..., w:])

    y_lat = []
    y_ctx = []
    for direction, reverse in ((0, False), (1, True)):
        a_bar, b_bar = ssm_discretise(lam_re[direction], lam_im[direction], log_step[direction],
                                      b_re[direction], b_im[direction])
        c_mat = lax.complex(c_re[direction].astype(jnp.float32), c_im[direction].astype(jnp.float32))
        s_ctx = linear_scan(a_bar, drive(p_ctx, b_bar), None, reverse)
        s_end = s_ctx[:, 0] if reverse else s_ctx[:, -1]
        s_lat = linear_scan(a_bar, drive(p_lat, b_bar), s_end, reverse)
        y_lat.append(readout(s_lat, c_mat))
        if need_ctx:
            y_ctx.append(readout(s_ctx, c_mat))
    o_l = finish(p_lat, y_lat[0] + y_lat[1])
    o_c = finish(p_ctx, y_ctx[0] + y_ctx[1]) if need_ctx else None
    return o_l, o_c


def short_conv(x, w, b):
    n = x.shape[1]
    xp = jnp.pad(x, ((0, 0), (1, 1), (0, 0)))
    return xp[:, :n] * w[0] + xp[:, 1:n + 1] * w[1] + xp[:, 2:] * w[2] + b


def hyena_kernel_freq(n, w1, b1, fr1, w2, b2, fr2, w3):
    f32 = jnp.float32
    t = jnp.linspace(0.0, 1.0, n, dtype=f32)[:, None]
    omega = 2.0 * math.pi * jnp.arange(n, dtype=f32)[:, None] / n
    bands = jnp.linspace(1e-4, HY_BANDS - 1, HY_BANDS, dtype=f32)[None, :]
    z = jnp.concatenate([t, jnp.cos(bands * omega), -jnp.sin(bands * omega)], axis=-1)
    h = jnp.sin(fr1.astype(f32) * (z @ w1.astype(f32) + b1.astype(f32)))
    h = jnp.sin(fr2.astype(f32) * (h @ w2.astype(f32) + b2.astype(f32)))
    h = (h @ w3.astype(f32)).reshape(n, HY_ORDER, 2, HY_WIDTH)
    max_decay = math.log(HY_DECAY_TARGET) / HY_FAST_DECAY
    min_decay = math.log(HY_DECAY_TARGET) / HY_SLOW_DECAY
    deltas = jnp.linspace(min_decay, max_decay, HY_WIDTH, dtype=f32)
    h = h * jnp.exp(-t[:, :, None, None] * jnp.abs(deltas))
    h = h * lax.rsqrt(jnp.sum(h * h, axis=(0, 2), keepdims=True) + EPS)
    fwd, bwd = h[:, :, 0], h[:, :, 1]
    zero = jnp.zeros((1, HY_ORDER, HY_WIDTH), f32)
    k_circ = jnp.concatenate([fwd, zero, bwd[1:][::-1]], axis=0)
    return jnp.fft.rfft(k_circ, axis=0)


def long_conv(u, k_f, bias):
    n = u.shape[1]
    uf = jnp.fft.rfft(u, n=2 * n, axis=1)
    y = jnp.fft.irfft(uf * k_f[None], n=2 * n, axis=1)[:, :n]
    return y + u * bias.astype(jnp.float32)


def hyena_mixer(p_lat, p_ctx, conv_w, conv_b, w1, b1, fr1, w2, b2, fr2, w3, bias, need_ctx):
    w = HY_WIDTH

    def run(p):
        n = p.shape[1]
        proj = short_conv(p[..., :(HY_ORDER + 1) * w], conv_w, conv_b)
        v = proj[..., :w].astype(jnp.float32)
        x1 = proj[..., w:2 * w].astype(jnp.float32)
        x2 = proj[..., 2 * w:3 * w].astype(jnp.float32)
        k_f = hyena_kernel_freq(n, w1, b1, fr1, w2, b2, fr2, w3)
        z = x1 * long_conv(v, k_f[:, 0], bias[0])
        z = x2 * long_conv(z, k_f[:, 1], bias[1])
        return z.astype(p.dtype) * jax.nn.silu(p[..., (HY_ORDER + 1) * w:])

    o_l = run(p_lat)
    o_c = run(p_ctx) if need_ctx else None
    return o_l, o_c


def modulation(cvec, w_mod, b_mod):
    m = jax.nn.silu(cvec) @ w_mod + b_mod
    return jnp.split(m, 3, axis=-1)


def setup_inputs(seed: int = 0) -> dict:
    key = jax.random.key(seed)
    keys = iter(jax.random.split(key, 48))
    f32 = jnp.float32

    def normal(shape, scale):
        return jax.random.normal(next(keys), shape, f32) * scale

    def gain(shape):
        return 1.0 + normal(shape, 0.02)

    w = GROUP_WIDTH
    ssm_shape = (DEPTH, 2, SSM_GROUPS, SSM_STATE)
    x = normal((BATCH, SEQ, D_MODEL), 1.0)
    c = normal((BATCH, D_MODEL), 1.0)
    ctx = normal((BATCH, CTX_LEN, D_MODEL), 1.0)
    c_ctx = normal((D_MODEL,), 1.0)
    w_mod = normal((DEPTH, D_MODEL, 3 * D_MODEL), D_MODEL ** -0.5)
    b_mod = normal((DEPTH, 3 * D_MODEL), 0.02)
    g_pre = gain((DEPTH, D_MODEL))
    g_post = gain((DEPTH, D_MODEL))
    w_in = normal((DEPTH, D_MODEL, IN_COLS), D_MODEL ** -0.5)
    w_out = normal((DEPTH, MIX_WIDTH, D_MODEL), MIX_WIDTH ** -0.5)
    mla_g_cq = gain((DEPTH, MLA_Q_RANK))
    mla_w_uq = normal((DEPTH, MLA_Q_RANK, MLA_HEADS * (MLA_NOPE + MLA_ROPE)), MLA_Q_RANK ** -0.5)
    mla_g_ckv = gain((DEPTH, MLA_KV_RANK))
    mla_w_ukv = normal((DEPTH, MLA_KV_RANK, MLA_HEADS * (MLA_NOPE + MLA_V)), MLA_KV_RANK ** -0.5)
    gqa_g_q = gain((DEPTH, GQA_HEAD_DIM))
    gqa_g_k = gain((DEPTH, GQA_HEAD_DIM))
    ssm_lambda_re = -0.5 * jnp.exp(normal(ssm_shape, 0.05))
    ssm_lambda_im = math.pi * jnp.arange(SSM_STATE, dtype=f32) + normal(ssm_shape, 0.01)
    ssm_log_step = jax.random.uniform(next(keys), (DEPTH, 2, SSM_GROUPS), f32, math.log(1e-3), math.log(1e-1))
    ssm_b_re = normal((DEPTH, 2, SSM_GROUPS, SSM_STATE, SSM_GROUP), (2 * SSM_GROUP) ** -0.5)
    ssm_b_im = normal((DEPTH, 2, SSM_GROUPS, SSM_STATE, SSM_GROUP), (2 * SSM_GROUP) ** -0.5)
    ssm_c_re = normal((DEPTH, 2, SSM_GROUPS, SSM_GROUP, SSM_STATE), SSM_STATE ** -0.5)
    ssm_c_im = normal((DEPTH, 2, SSM_GROUPS, SSM_GROUP, SSM_STATE), SSM_STATE ** -0.5)
    ssm_d = normal((DEPTH, w), 1.0)
    ssm_glu_w = normal((DEPTH, w, w), w ** -0.5)
    ssm_glu_b = normal((DEPTH, w), 0.02)
    hy_conv_w = normal((DEPTH, HY_CONV, (HY_ORDER + 1) * HY_WIDTH), HY_CONV ** -0.5)
    hy_conv_b = normal((DEPTH, (HY_ORDER + 1) * HY_WIDTH), 0.02)
    hy_f_w1 = normal((DEPTH, HY_EMB, HY_HIDDEN), HY_EMB ** -0.5)
    hy_f_b1 = normal((DEPTH, HY_HIDDEN), 0.02)
    hy_f_freq1 = gain((DEPTH, HY_HIDDEN))
    hy_f_w2 = normal((DEPTH, HY_HIDDEN, HY_HIDDEN), HY_HIDDEN ** -0.5)
    hy_f_b2 = normal((DEPTH, HY_HIDDEN), 0.02)
    hy_f_freq2 = gain((DEPTH, HY_HIDDEN))
    hy_f_w3 = normal((DEPTH, HY_HIDDEN, HY_ORDER * 2 * HY_WIDTH), HY_HIDDEN ** -0.5)
    hy_bias = normal((DEPTH, HY_ORDER, HY_WIDTH), 0.5)
    return {'x': x, 'c': c, 'ctx': ctx, 'c_ctx': c_ctx, 'w_mod': w_mod, 'b_mod': b_mod,
            'g_pre': g_pre, 'g_post': g_post, 'w_in': w_in, 'w_out': w_out,
            'mla_g_cq': mla_g_cq, 'mla_w_uq': mla_w_uq, 'mla_g_ckv': mla_g_ckv, 'mla_w_ukv': mla_w_ukv,
            'gqa_g_q': gqa_g_q, 'gqa_g_k': gqa_g_k,
            'ssm_lambda_re': ssm_lambda_re, 'ssm_lambda_im': ssm_lambda_im, 'ssm_log_step': ssm_log_step,
            'ssm_b_re': ssm_b_re, 'ssm_b_im': ssm_b_im, 'ssm_c_re': ssm_c_re, 'ssm_c_im': ssm_c_im,
            'ssm_d': ssm_d, 'ssm_glu_w': ssm_glu_w, 'ssm_glu_b': ssm_glu_b,
            'hy_conv_w': hy_conv_w, 'hy_conv_b': hy_conv_b, 'hy_f_w1': hy_f_w1, 'hy_f_b1': hy_f_b1,
            'hy_f_freq1': hy_f_freq1, 'hy_f_w2': hy_f_w2, 'hy_f_b2': hy_f_b2, 'hy_f_freq2': hy_f_freq2,
            'hy_f_w3': hy_f_w3, 'hy_bias': hy_bias}


def reference(x, c, ctx, c_ctx, w_mod, b_mod, g_pre, g_post, w_in, w_out,
              mla_g_cq, mla_w_uq, mla_g_ckv, mla_w_ukv, gqa_g_q, gqa_g_k,
              ssm_lambda_re, ssm_lambda_im, ssm_log_step, ssm_b_re, ssm_b_im, ssm_c_re, ssm_c_im,
              ssm_d, ssm_glu_w, ssm_glu_b,
              hy_conv_w, hy_conv_b, hy_f_w1, hy_f_b1, hy_f_freq1, hy_f_w2, hy_f_b2, hy_f_freq2,
              hy_f_w3, hy_bias):
    n_rows = x.shape[1] // GRID_W
    row, col = grid_positions(n_rows)
    o1 = MLA_COLS
    o2 = o1 + GQA_COLS
    o3 = o2 + SSM_COLS
    for l in range(DEPTH):
        need_ctx = l < DEPTH - 1
        sh_l, sc_l, gt_l = [m[:, None, :] for m in modulation(c, w_mod[l], b_mod[l])]
        sh_c, sc_c, gt_c = modulation(c_ctx, w_mod[l], b_mod[l])
        h_l = rms_norm(x, g_pre[l]) * (1 + sc_l) + sh_l
        h_c = rms_norm(ctx, g_pre[l]) * (1 + sc_c) + sh_c
        p_l = h_l @ w_in[l]
        p_c = h_c @ w_in[l]
        a_l, a_c = mla_mixer(p_l[..., :o1], p_c[..., :o1], mla_g_cq[l], mla_w_uq[l],
                             mla_g_ckv[l], mla_w_ukv[l], row, col, need_ctx)
        g_l, g_c = gqa_mixer(p_l[..., o1:o2], p_c[..., o1:o2], gqa_g_q[l], gqa_g_k[l], row, col, need_ctx)
        s_l, s_c = ssm_mixer(p_l[..., o2:o3], p_c[..., o2:o3], ssm_lambda_re[l], ssm_lambda_im[l],
                             ssm_log_step[l], ssm_b_re[l], ssm_b_im[l], ssm_c_re[l], ssm_c_im[l],
                             ssm_d[l], ssm_glu_w[l], ssm_glu_b[l], need_ctx)
        y_l, y_c = hyena_mixer(p_l[..., o3:], p_c[..., o3:], hy_conv_w[l], hy_conv_b[l],
                               hy_f_w1[l], hy_f_b1[l], hy_f_freq1[l], hy_f_w2[l], hy_f_b2[l],
                               hy_f_freq2[l], hy_f_w3[l], hy_bias[l], need_ctx)
        out_l = jnp.concatenate([a_l, g_l, s_l, y_l], axis=-1) @ w_out[l]
        x = x + gt_l * rms_norm(out_l, g_post[l])
        if need_ctx:
            out_c = jnp.concatenate([a_c, g_c, s_c, y_c], axis=-1) @ w_out[l]
            ctx = ctx + gt_c * rms_norm(out_c, g_post[l])
    return x
```

```python
import math
import numpy as np
from contextlib import ExitStack
import concourse.bass as bass
import concourse.mybir as mybir
from concourse.bass_utils import run_bass_kernel_spmd

F32 = mybir.dt.float32
BF16 = mybir.dt.bfloat16
AF = mybir.ActivationFunctionType
ALU = mybir.AluOpType
AX = mybir.AxisListType

D = 1024
L = 4096
C = 256
T = L + C
NT = T // 128
DEPTH = 4
EPS = 1e-6
NCORES = 8

O_CQ, O_CKV, O_KR, O_GM = 0, 192, 320, 352
O1 = 608
O_GQ, O_GK, O_GV, O_GG = O1, O1 + 256, O1 + 384, O1 + 512
O2 = O1 + 768
O_SU, O_SG = O2, O2 + 256
O3 = O2 + 512
O_HY, O_HG = O3, O3 + 768
NIN = 2912
O_KRP = NIN
O_QM = O_KRP + 32
O_QP = O_QM + 256
O_KP = O_QP + 256
NCB = O_KP + 128

EPOCH = 30000
NDMASLOT = 8


class Tok:
    __slots__ = ("w", "r", "excl")

    def __init__(self, excl=False):
        self.w = []
        self.r = []
        self.excl = excl


class KB:
    def __init__(self, nc, es):
        self.nc = nc
        self.es = es
        self.eng = {"pe": nc.tensor, "act": nc.scalar, "dve": nc.vector, "pool": nc.gpsimd, "sp": nc.sync}
        self.cnt = {e: 0 for e in self.eng}
        self.epoch = {e: 0 for e in self.eng}
        self.sems = {}
        self.seen = {e: {} for e in self.eng}
        self.dma_slots = {}
        self.dma_rr = {e: 0 for e in self.eng}
        self.ninst = 0

    def _sem(self, key):
        if key not in self.sems:
            self.sems[key] = self.es.enter_context(self.nc.semaphore("s_%s_%s" % key))
        return self.sems[key]

    def _wait(self, e, ev):
        key, val = ev
        if self.seen[e].get(key, 0) >= val:
            return
        self.eng[e].wait_ge(self._sem(key), val)
        self.seen[e][key] = val

    def _deps(self, e, reads, writes):
        best = {}
        for t in reads:
            for k_, v in t.w:
                if best.get(k_, 0) < v:
                    best[k_] = v
            if t.excl:
                for k_, v in t.r:
                    if k_[0] != e and best.get(k_, 0) < v:
                        best[k_] = v
        for t in writes:
            for k_, v in t.w:
                if best.get(k_, 0) < v:
                    best[k_] = v
            for k_, v in t.r:
                if best.get(k_, 0) < v:
                    best[k_] = v
        for k_, v in best.items():
            if e == "pe" and k_[0] == "pe":
                continue
            self._wait(e, (k_, v))

    def _record(self, ev, reads, writes):
        for t in reads:
            t.r.append(ev)
            if len(t.r) > 16:
                best = {}
                for k_, v in t.r:
                    if best.get(k_, 0) < v:
                        best[k_] = v
                t.r = list(best.items())
        for t in writes:
            t.w = [ev]
            t.r = []

    def op(self, e, fn, reads=(), writes=()):
        self._deps(e, reads, writes)
        if self.cnt[e] >= EPOCH:
            self.epoch[e] += 1
            self.cnt[e] = 0
        key = (e, self.epoch[e])
        ins = fn(self.eng[e])
        self.cnt[e] += 1
        ins.then_inc(self._sem(key), 1)
        ev = (key, self.cnt[e])
        self._record(ev, reads, writes)
        self.ninst += 1
        return ev

    def dma(self, e, out, in_, reads=(), writes=(), **kw):
        self._deps(e, reads, writes)
        if e not in self.dma_slots:
            self.dma_slots[e] = [[("d" + e, i), 0] for i in range(NDMASLOT)]
        i = self.dma_rr[e]
        self.dma_rr[e] = (i + 1) % NDMASLOT
        slot = self.dma_slots[e][i]
        key = slot[0]
        if slot[1] > 0:
            self._wait(e, (key, 16 * slot[1]))
        slot[1] += 1
        ins = self.eng[e].dma_start(out=out, in_=in_, **kw)
        ins.then_inc(self._sem(key), 16)
        ev = (key, 16 * slot[1])
        self._record(ev, reads, writes)
        self.ninst += 1
        return ev

    def all_events(self):
        evs = []
        for e in self.eng:
            for ep in range(self.epoch[e] + 1):
                v = self.cnt[e] if ep == self.epoch[e] else EPOCH
                if v > 0:
                    evs.append(((e, ep), v))
        for e, slots in self.dma_slots.items():
            for key, uses in slots:
                if uses:
                    evs.append((key, 16 * uses))
        return evs

    def barrier(self):
        evs = self.all_events()
        for e in ("pe", "act", "dve", "pool", "sp"):
            for ev in evs:
                if ev[0][0] == e:
                    continue
                self._wait(e, ev)

    def drain(self, e="sp"):
        for ev in self.all_events():
            self._wait(e, ev)


_NMC = [0]


def _nm(n):
    _NMC[0] += 1
    return "%s_%d" % (n, _NMC[0])


class Rot:
    def __init__(self, tiles, excl=False):
        self.tiles = [(t, Tok(excl)) for t in tiles]
        self.i = 0

    def next(self):
        r = self.tiles[self.i]
        self.i = (self.i + 1) % len(self.tiles)
        return r


def _rope_table(d):
    hh = d // 2
    qq = hh // 2
    inv = (np.float32(10000.0) ** (-np.arange(0, hh, 2, dtype=np.float32) / np.float32(hh))).astype(np.float32)
    t = np.arange(L)
    row = (t // 64).astype(np.float32)
    col = (t % 64).astype(np.float32)
    cos = np.ones((d, T), np.float32)
    sin = np.zeros((d, T), np.float32)
    for i in range(d):
        hf, within = divmod(i, hh)
        fi = within % qq
        pos = row if hf == 0 else col
        ang = (pos * inv[fi]).astype(np.float32)
        cos[i, C:] = np.cos(ang).astype(np.float32)
        sin[i, C:] = np.sin(ang).astype(np.float32)
    return cos, sin


def _partner_index(d):
    hh = d // 2
    qq = hh // 2
    idx = np.zeros(d, np.int64)
    sg = np.zeros(d, np.float32)
    for i in range(d):
        hf, within = divmod(i, hh)
        if within < qq:
            idx[i] = i + qq
            sg[i] = -1.0
        else:
            idx[i] = i - qq
            sg[i] = 1.0
    return idx, sg


def host_constants():
    cst = {}
    cst["k_ident"] = np.eye(128, dtype=np.float32)
    c32, s32 = _rope_table(32)
    c64, s64 = _rope_table(64)
    mc = np.ones((128, T), np.float32)
    ms = np.zeros((128, T), np.float32)
    mc[64:96] = c32
    ms[64:96] = s32
    cst["k_ropeM"] = np.stack([mc, ms], 0)
    cst["k_ropeG"] = np.stack([np.concatenate([c64, c64], 0), np.concatenate([s64, s64], 0)], 0)
    cst.update(hyena_constants())
    return cst


class G:
    pass


def build_program(layers, final_lat_only, dbg=None):
    nc = bass.Bass("TRN2", target_bir_lowering=False)
    g = G()
    g.nc = nc
    g.dbg = dbg or {}

    def din(name, shape, dt=F32):
        return nc.dram_tensor(name, list(shape), dt, kind="ExternalInput").ap()

    def dscr(name, shape, dt=F32):
        return nc.dram_tensor(name, list(shape), dt).ap()

    g.xs = din("xs", [2, T, D])
    g.cT = din("cT", [128, 8, 3])
    W = {}
    W["w_mod"] = din("w_mod", [DEPTH, D, 3 * D])
    W["b_mod"] = din("b_mod", [DEPTH, 3 * D])
    W["g_pre"] = din("g_pre", [DEPTH, D])
    W["g_post"] = din("g_post", [DEPTH, D])
    W["w_in"] = din("w_in", [DEPTH, D, NIN])
    W["w_out"] = din("w_out", [DEPTH, D, D])
    W["mla_g_cq"] = din("mla_g_cq", [DEPTH, 192])
    W["mla_w_uq"] = din("mla_w_uq", [DEPTH, 192, 384])
    W["mla_g_ckv"] = din("mla_g_ckv", [DEPTH, 128])
    W["mla_w_ukv"] = din("mla_w_ukv", [DEPTH, 128, 512])
    W["gq_cols"] = din("gq_cols", [DEPTH, 128, 4])
    for nm, shp in (("hy_conv_w", [DEPTH, 3, 768]), ("hy_conv_b", [DEPTH, 768]), ("hy_f_w1", [DEPTH, 33, 64]), ("hy_f_b1", [DEPTH, 64]),
                    ("hy_f_freq1", [DEPTH, 64]), ("hy_f_w2", [DEPTH, 64, 64]), ("hy_f_b2", [DEPTH, 64]), ("hy_f_freq2", [DEPTH, 64]),
                    ("hy_f_w3", [DEPTH, 64, 1024]), ("hy_bias", [DEPTH, 2, 256])):
        W[nm] = din(nm, shp)
    for nm, shp in (("ssm_lam", [DEPTH, 128, 2, 16]), ("ssm_ls", [DEPTH, 128, 16]), ("ssm_Bp", [DEPTH, 128, 2, 16, 64]), ("ssm_Cp", [DEPTH, 128, 2, 16, 64]),
                    ("ssm_cols", [DEPTH, 128, 2, 2]), ("ssm_glu_w", [DEPTH, 256, 256])):
        W[nm] = din(nm, shp)
    g.W = W
    K = {}
    K["k_ident"] = din("k_ident", [128, 128])
    K["k_ropeM"] = din("k_ropeM", [2, 128, T])
    K["k_ropeG"] = din("k_ropeG", [2, 128, T])
    for nm, arr in hyena_constants().items():
        K[nm] = din(nm, list(arr.shape))
    g.K = K
    if final_lat_only:
        g.y = nc.dram_tensor("y", [2, L, D], F32, kind="ExternalOutput").ap()
    else:
        g.y = nc.dram_tensor("y", [2, T, D], F32, kind="ExternalOutput").ap()
    for name, (shape, dt) in g.dbg.items():
        g.dbg[name] = nc.dram_tensor(name, list(shape), dt, kind="ExternalOutput").ap()

    g.XS = dscr("XS", [2, T, D]) if len(layers) > 1 else None
    g.WINB = dscr("WINB", [128, 8, NCB], BF16)
    g.WOUTB = dscr("WOUTB", [128, 8, D], BF16)
    g.MODROWS = dscr("MODROWS", [3, 3 * D])
    g.HT = dscr("HT", [128, 8, T], BF16)
    g.CATT = dscr("CATT", [128, 8, T], BF16)
    g.KHAT = {"L": dscr("KHATL", [2, HY_LAT.ng, 128, 512]), "C": dscr("KHATC", [2, HY_CTX.ng, 128, 512])}
    g.T_KHAT = Tok()
    g.SSM_L3 = dscr("SSM_L3", [128, 2, 16, 17, 64], BF16)
    g.SSM_L1 = dscr("SSM_L1", [128, 2, 16, 2, 128], BF16)
    g.SSM_SG = dscr("SSM_SG", [128, 2, 16, 2, 2, 2, 128], BF16)
    g.T_SSMW = Tok()
    g.T_XS = [Tok(), Tok()]
    g.T_WINB = Tok()
    g.T_WOUTB = Tok()
    g.T_MOD = Tok()
    g.T_HT = Tok()
    g.T_CATT = Tok()
    g.T_Y = Tok()

    with ExitStack() as es:
        k = KB(nc, es)
        g.k = k
        g.es = es
        g.ident = es.enter_context(nc.sbuf_tensor("ident", [128, 128], BF16))
        g.T_ident = Tok()
        g.onesf = es.enter_context(nc.sbuf_tensor("onesf", [128, 128], F32))
        g.ones128 = es.enter_context(nc.sbuf_tensor("ones128", [128, 128], BF16))
        g.ones192 = es.enter_context(nc.sbuf_tensor("ones192", [128, 128], BF16))
        g.blk64 = es.enter_context(nc.sbuf_tensor("blk64", [128, 128], BF16))
        g.T_const = Tok()
        with ExitStack() as es2:
            tmp = es2.enter_context(nc.sbuf_tensor("idtmp", [128, 128], F32))
            tt = Tok()
            k.dma("sp", tmp[:], K["k_ident"], writes=[tt])
            k.op("dve", lambda e: e.tensor_copy(out=g.ident[:], in_=tmp[:]), reads=[tt], writes=[g.T_ident])
            k.op("dve", lambda e: e.memset(g.onesf[:], 1.0), writes=[g.T_const])
            k.op("dve", lambda e: e.memset(g.ones128[:], 1.0 / 128), writes=[g.T_const])
            k.op("dve", lambda e: e.memset(g.ones192[:], 1.0 / 192), writes=[g.T_const])
            k.op("dve", lambda e: e.memset(g.blk64[:], 0.0), writes=[g.T_const])
            k.op("dve", lambda e: e.memset(g.blk64[0:64, 0:64], 1.0 / 64), reads=[g.T_const], writes=[g.T_const])
            k.op("dve", lambda e: e.memset(g.blk64[64:128, 64:128], 1.0 / 64), reads=[g.T_const], writes=[g.T_const])
            k.barrier()

        for li, l in enumerate(layers):
            need_ctx = l < DEPTH - 1
            src = g.xs if li == 0 else g.XS
            last = li == len(layers) - 1
            dst = g.y if last else g.XS
            phase_weights(g, l)
            k.barrier()
            phase_ssm_weights(g, l)
            phase_hy_filter(g, l, HY_LAT)
            if need_ctx:
                phase_hy_filter(g, l, HY_CTX)
            for s in range(2):
                phase_P1(g, l, s, src)
                k.barrier()
                phase_hyena(g, l, s, HY_LAT, C)
                if need_ctx:
                    phase_hyena(g, l, s, HY_CTX, 0)
                phase_ssm(g, l, s, need_ctx)
                phase_attn(g, l, s, need_ctx)
                k.barrier()
                phase_P6(g, l, s, src, dst, need_ctx, last and final_lat_only)
                k.barrier()
        k.drain("sp")
    return nc, g


def phase_weights(g, l):
    nc, k, W = g.nc, g.k, g.W
    with ExitStack() as es:
        sb = lambda n, s, d=F32: es.enter_context(nc.sbuf_tensor(_nm(n), s, d))
        ps = lambda n, s, d=F32: es.enter_context(nc.psum_tensor(_nm(n), s, d))
        p32, s32 = _partner_index(32)
        p64, s64 = _partner_index(64)
        fin = Rot([sb("wf%d" % i, [128, NIN]) for i in range(2)])
        fob = Rot([sb("wb%d" % i, [128, NCB], BF16) for i in range(2)])
        qorder = (0, 2, 1, 3)
        engs = ["act", "pool", "dve"]
        ei = 0

        def cp(dst, src, neg=False):
            nonlocal ei
            e = engs[ei % 3]
            ei += 1
            if e == "act":
                k.op("act", lambda en: en.activation(out=dst, in_=src, func=AF.Copy, scale=(-1.0 if neg else 1.0)), reads=[tf], writes=[tb])
            else:
                k.op(e, lambda en: en.tensor_scalar(out=dst, in0=src, scalar1=(-1.0 if neg else 1.0), scalar2=None, op0=ALU.mult), reads=[tf], writes=[tb])

        def partner_cols(dstb, srcb, d):
            hh, qq = d // 2, d // 4
            for hf in range(2):
                b0 = hf * hh
                cp(wb[:, dstb + b0:dstb + b0 + qq], wf[:, srcb + b0 + qq:srcb + b0 + hh], neg=True)
                cp(wb[:, dstb + b0 + qq:dstb + b0 + hh], wf[:, srcb + b0:srcb + b0 + qq], neg=False)

        for kc in range(8):
            wf, tf = fin.next()
            wb, tb = fob.next()
            k.dma("sp", wf[:], W["w_in"][l, kc * 128:(kc + 1) * 128, :], writes=[tf])
            k.op("act", lambda en: en.activation(out=wb[:, 0:1456], in_=wf[:, 0:1456], func=AF.Copy), reads=[tf], writes=[tb])
            k.op("dve", lambda en: en.tensor_copy(out=wb[:, 1456:NIN], in_=wf[:, 1456:NIN]), reads=[tf], writes=[tb])
            partner_cols(O_KRP, O_KR, 32)
            for pos, h in enumerate(qorder):
                cp(wb[:, O_QM + pos * 64:O_QM + pos * 64 + 64], wf[:, O_GQ + h * 64:O_GQ + h * 64 + 64])
                partner_cols(O_QP + pos * 64, O_GQ + h * 64, 64)
            for h in range(2):
                partner_cols(O_KP + h * 64, O_GK + h * 64, 64)
            k.dma("pool", g.WINB[:, kc, :], wb[:], reads=[tb], writes=[g.T_WINB])
        fo = Rot([sb("wof%d" % i, [128, D]) for i in range(2)])
        fb = Rot([sb("wob%d" % i, [128, D], BF16) for i in range(2)])
        for kc in range(8):
            wf, tf = fo.next()
            wb, tb = fb.next()
            k.dma("sp", wf[:], W["w_out"][l, kc * 128:(kc + 1) * 128, :], writes=[tf])
            k.op("act" if kc % 2 else "dve", (lambda en: en.activation(out=wb[:], in_=wf[:], func=AF.Copy)) if kc % 2 else (lambda en: en.tensor_copy(out=wb[:], in_=wf[:])), reads=[tf], writes=[tb])
            k.dma("pool", g.WOUTB[:, kc, :], wb[:], reads=[tb], writes=[g.T_WOUTB])

        cT = sb("cTs", [128, 8, 3])
        scT = sb("scT", [128, 8, 3])
        t_c = Tok()
        k.dma("sp", cT[:], g.cT, writes=[t_c])
        k.op("act", lambda en: en.activation(out=scT[:], in_=cT[:], func=AF.Silu), reads=[t_c], writes=[t_c])
        mrow = sb("mrow", [3, 3 * D])
        brow = sb("brow", [3, 3 * D])
        gpre = sb("gpre", [3, D])
        gpost = sb("gpost", [3, D])
        t_m = Tok()
        t_b = Tok()
        k.dma("sp", brow[:], W["b_mod"][l:l + 1, :].broadcast_to([3, 3 * D]), writes=[t_b])
        k.dma("sp", gpre[:], W["g_pre"][l:l + 1, :].broadcast_to([3, D]), writes=[t_b])
        k.dma("sp", gpost[:], W["g_post"][l:l + 1, :].broadcast_to([3, D]), writes=[t_b])
        wm = Rot([sb("wm%d" % i, [128, 8, 512]) for i in range(2)])
        pm = Rot([ps("pm%d" % i, [3, 512]) for i in range(2)], excl=True)
        for cc in range(6):
            wt, tw = wm.next()
            pt, tp = pm.next()
            k.dma("sp", wt[:], W["w_mod"][l, :, cc * 512:(cc + 1) * 512].rearrange("(kc p) n -> p kc n", p=128), writes=[tw])
            for kc in range(8):
                k.op("pe", lambda en: en.matmul(pt[:], lhsT=scT[:, kc, :], rhs=wt[:, kc, :], start=(kc == 0), stop=(kc == 7)), reads=[t_c, tw], writes=[tp])
            k.op("dve", lambda en: en.tensor_tensor(out=mrow[:, cc * 512:(cc + 1) * 512], in0=pt[:], in1=brow[:, cc * 512:(cc + 1) * 512], op=ALU.add), reads=[tp, t_b], writes=[t_m])
        orow = sb("orow", [3, 3 * D])
        t_o = Tok()
        k.op("dve", lambda en: en.scalar_tensor_tensor(out=orow[:, 0:D], in0=mrow[:, D:2 * D], scalar=1.0, in1=gpre[:], op0=ALU.add, op1=ALU.mult), reads=[t_m, t_b], writes=[t_o])
        k.op("dve", lambda en: en.tensor_copy(out=orow[:, D:2 * D], in_=mrow[:, 0:D]), reads=[t_m, t_o], writes=[t_o])
        k.op("dve", lambda en: en.tensor_tensor(out=orow[:, 2 * D:3 * D], in0=mrow[:, 2 * D:3 * D], in1=gpost[:], op=ALU.mult), reads=[t_m, t_b, t_o], writes=[t_o])
        k.dma("pool", g.MODROWS, orow[:], reads=[t_o], writes=[g.T_MOD])
        k.barrier()

    if not hasattr(g, "lw"):
        lw = G()
        es = g.es
        sbp = lambda n, s, d=F32: es.enter_context(nc.sbuf_tensor(_nm(n), s, d))
        lw.wuq_a = sbp("wuq_a", [128, 384], BF16)
        lw.wuq_b = sbp("wuq_b", [64, 384], BF16)
        lw.wuqp_a = sbp("wuqp_a", [128, 384], BF16)
        lw.wuqp_b = sbp("wuqp_b", [64, 384], BF16)
        lw.wuk = sbp("wuk", [128, 256], BF16)
        lw.wuv = sbp("wuv", [128, 256], BF16)
        lw.gq = sbp("gqc", [128, 4])
        lw.A2 = sbp("ssmA2", [128, NDBL, 3, 16])
        lw.tok = Tok()
        g.lw = lw
    lw = g.lw
    with ExitStack() as es:
        sb = lambda n, s, d=F32: es.enter_context(nc.sbuf_tensor(_nm(n), s, d))
        uqa = sb("uqa", [128, 384])
        uqb = sb("uqb", [64, 384])
        ukv = sb("ukv", [128, 512])
        gcq = sb("gcq", [128, 2])
        gckv = sb("gckv", [128, 1])
        tl = Tok()
        k.dma("sp", uqa[:], W["mla_w_uq"][l, 0:128, :], writes=[tl])
        k.dma("sp", uqb[:], W["mla_w_uq"][l, 128:192, :], writes=[tl])
        k.dma("sp", ukv[:], W["mla_w_ukv"][l], writes=[tl])
        k.dma("sp", gcq[:, 0:1], W["mla_g_cq"][l, 0:128].rearrange("(p o) -> p o", o=1), writes=[tl])
        k.dma("sp", gcq[0:64, 1:2], W["mla_g_cq"][l, 128:192].rearrange("(p o) -> p o", o=1), writes=[tl])
        k.dma("sp", gckv[:], W["mla_g_ckv"][l].rearrange("(p o) -> p o", o=1), writes=[tl])
        k.dma("sp", lw.gq[:], W["gq_cols"][l], writes=[lw.tok])
        k.op("dve", lambda en: en.tensor_scalar(out=uqa[:], in0=uqa[:], scalar1=gcq[:, 0:1], scalar2=None, op0=ALU.mult), reads=[tl], writes=[tl])
        k.op("dve", lambda en: en.tensor_scalar(out=uqb[:], in0=uqb[:], scalar1=gcq[0:64, 1:2], scalar2=None, op0=ALU.mult), reads=[tl], writes=[tl])
        k.op("dve", lambda en: en.tensor_scalar(out=ukv[:], in0=ukv[:], scalar1=gckv[:, 0:1], scalar2=None, op0=ALU.mult), reads=[tl], writes=[tl])
        k.op("dve", lambda en: en.tensor_copy(out=lw.wuq_a[:], in_=uqa[:]), reads=[tl], writes=[lw.tok])
        k.op("dve", lambda en: en.tensor_copy(out=lw.wuq_b[:], in_=uqb[:]), reads=[tl, lw.tok], writes=[lw.tok])
        k.op("dve", lambda en: en.memset(lw.wuqp_a[:], 0.0), reads=[lw.tok], writes=[lw.tok])
        k.op("dve", lambda en: en.memset(lw.wuqp_b[:], 0.0), reads=[lw.tok], writes=[lw.tok])
        for h in range(4):
            for hf in range(2):
                b0 = h * 96 + 64 + hf * 16
                for (dst, src, tile_src) in ((lw.wuqp_a, uqa, 128), (lw.wuqp_b, uqb, 64)):
                    k.op("dve", lambda en: en.tensor_scalar(out=dst[:, b0:b0 + 8], in0=src[:, b0 + 8:b0 + 16], scalar1=-1.0, scalar2=None, op0=ALU.mult), reads=[tl, lw.tok], writes=[lw.tok])
                    k.op("dve", lambda en: en.tensor_copy(out=dst[:, b0 + 8:b0 + 16], in_=src[:, b0:b0 + 8]), reads=[tl, lw.tok], writes=[lw.tok])
            k.op("dve", lambda en: en.tensor_copy(out=lw.wuk[:, h * 64:(h + 1) * 64], in_=ukv[:, h * 128:h * 128 + 64]), reads=[tl, lw.tok], writes=[lw.tok])
            k.op("dve", lambda en: en.tensor_copy(out=lw.wuv[:, h * 64:(h + 1) * 64], in_=ukv[:, h * 128 + 64:h * 128 + 128]), reads=[tl, lw.tok], writes=[lw.tok])
        k.barrier()


def phase_P1(g, l, s, src):
    nc, k = g.nc, g.k
    with ExitStack() as es:
        sb = lambda n, s_, d=F32: es.enter_context(nc.sbuf_tensor(_nm(n), s_, d))
        ps = lambda n, s_, d=F32: es.enter_context(nc.psum_tensor(_nm(n), s_, d))
        mods = {}
        t_mod = Tok()
        for v in (s, 2):
            mods[v] = sb("mod%d" % v, [128, 2 * D])
            k.dma("sp", mods[v][:], g.MODROWS[v:v + 1, 0:2 * D].broadcast_to([128, 2 * D]), reads=[g.T_MOD], writes=[t_mod])
        neghalf = sb("neghalf", [128, 1])
        k.op("pool", lambda en: en.memset(neghalf[:], -0.5), writes=[t_mod])
        xr = Rot([sb("x%d" % i, [128, D]) for i in range(3)])
        junk = sb("junk", [128, D], BF16)
        t_junk = Tok()
        hb_r = Rot([sb("hb%d" % i, [128, D], BF16) for i in range(2)])
        tmp_r = Rot([sb("tmp%d" % i, [128, D]) for i in range(2)])
        st_r = Rot([sb("st%d" % i, [128, 4]) for i in range(3)])
        tp_r = Rot([ps("tp%d" % i, [128, 8, 128], BF16) for i in range(2)], excl=True)
        hs_r = Rot([sb("hs%d" % i, [128, 8, 512], BF16) for i in range(2)])
        hs, t_hs = None, None
        for ti in range(NT):
            if ti == 0 or (ti >= 2 and (ti - 2) % 4 == 0):
                hs, t_hs = hs_r.next()
            off = (ti * 128) if ti < 2 else (((ti - 2) % 4) * 128)
            xt, t_x = xr.next()
            k.dma("sp", xt[:], src[s, ti * 128:(ti + 1) * 128, :], reads=[g.T_XS[s]], writes=[t_x])
            st, t_st = st_r.next()
            k.op("dve", lambda en: en.scalar_tensor_tensor(out=junk[:], in0=xt[:], scalar=1.0, in1=xt[:], op0=ALU.mult, op1=ALU.mult, accum_out=st[:, 0:1]), reads=[t_x], writes=[t_junk, t_st])
            k.op("dve", lambda en: en.tensor_scalar(out=st[:, 1:2], in0=st[:, 0:1], scalar1=1.0 / D, scalar2=EPS, op0=ALU.mult, op1=ALU.add), reads=[t_st], writes=[t_st])
            k.op("pool", lambda en: en.tensor_tensor(out=st[:, 2:3], in0=st[:, 1:2], in1=neghalf[:], op=ALU.pow), reads=[t_st, t_mod], writes=[t_st])
            md = mods[2] if ti < 2 else mods[s]
            tmp, t_tmp = tmp_r.next()
            hb, t_hb = hb_r.next()
            k.op("dve", lambda en: en.scalar_tensor_tensor(out=tmp[:], in0=xt[:], scalar=st[:, 2:3], in1=md[:, 0:D], op0=ALU.mult, op1=ALU.mult), reads=[t_x, t_st, t_mod], writes=[t_tmp])
            k.op("pool", lambda en: en.tensor_tensor(out=hb[:], in0=tmp[:], in1=md[:, D:2 * D], op=ALU.add), reads=[t_tmp, t_mod], writes=[t_hb])
            tp, t_tp = tp_r.next()
            for kc in range(8):
                k.op("pe", lambda en: en.transpose(tp[:, kc, :], hb[:, kc * 128:(kc + 1) * 128], g.ident[:]), reads=[t_hb, g.T_ident], writes=[t_tp])
            k.op("act", lambda en: en.activation(out=hs[:, :, off:off + 128], in_=tp[:], func=AF.Copy), reads=[t_tp], writes=[t_hs])
            if ti == 1:
                k.dma("pool", g.HT[:, :, 0:256], hs[:, :, 0:256], reads=[t_hs], writes=[g.T_HT])
            elif ti >= 2 and (ti - 2) % 4 == 3:
                t0 = (ti - 3) * 128
                k.dma("pool", g.HT[:, :, t0:t0 + 512], hs[:, :, 0:512], reads=[t_hs], writes=[g.T_HT])
        k.barrier()


def phase_attn(g, l, s, need_ctx):
    nc, k, lw = g.nc, g.k, g.lw
    with ExitStack() as es:
        sb = lambda n, s_, d=F32: es.enter_context(nc.sbuf_tensor(_nm(n), s_, d))
        ps = lambda n, s_, d=F32: es.enter_context(nc.psum_tensor(_nm(n), s_, d))
        KTm = [sb("KTm%d" % h, [96, T], BF16) for h in range(4)]
        VM = sb("VM", [128, NT, 4 * 65], BF16)
        KTg = sb("KTg", [128, T], BF16)
        VG = sb("VG", [128, NT, 2 * 65], BF16)
        t_K = Tok()
        k.op("pool", lambda en: en.memset(VM[:], 1.0), writes=[t_K])
        k.op("pool", lambda en: en.memset(VG[:], 1.0), writes=[t_K])
        t_w = Tok()
        epsc = sb("epsc", [128, 1])
        k.op("pool", lambda en: en.memset(epsc[:], EPS), writes=[t_w])

        hT_r = Rot([sb("hT%d" % i, [128, 8, 512], BF16) for i in range(2)])
        rope_r = Rot([sb("rp%d" % i, [128, 4, 512]) for i in range(2)])
        esA = ExitStack()
        sbA = lambda n, s_, d=F32: esA.enter_context(nc.sbuf_tensor(_nm(n), s_, d))
        wkv = sbA("wkv", [128, 8, 576], BF16)
        k.dma("sp", wkv[:, :, 0:160], g.WINB[:, :, O_CKV:O_CKV + 160], reads=[g.T_WINB], writes=[t_w])
        k.dma("sp", wkv[:, :, 160:192], g.WINB[:, :, O_KRP:O_KRP + 32], reads=[g.T_WINB], writes=[t_w])
        k.dma("sp", wkv[:, :, 192:448], g.WINB[:, :, O_GK:O_GK + 256], reads=[g.T_WINB], writes=[t_w])
        k.dma("sp", wkv[:, :, 448:576], g.WINB[:, :, O_KP:O_KP + 128], reads=[g.T_WINB], writes=[t_w])
        PS = [ps("ps%d" % i, [128, 512]) for i in range(8)]
        TPS = [Tok(True) for _ in range(8)]

        blocks = [(0, 256)] + [(256 + 512 * b, 512) for b in range(8)]

        def load_block(t0, nb):
            hT, t_h = hT_r.next()
            k.dma("sp", hT[:, :, 0:nb], g.HT[:, :, t0:t0 + nb], reads=[g.T_HT], writes=[t_h])
            rp, t_rp = rope_r.next()
            k.dma("sp", rp[:, 0:2, 0:nb], g.K["k_ropeM"][:, :, t0:t0 + nb].rearrange("a p n -> p a n"), writes=[t_rp])
            k.dma("sp", rp[:, 2:4, 0:nb], g.K["k_ropeG"][:, :, t0:t0 + nb].rearrange("a p n -> p a n"), writes=[t_rp])
            return hT, t_h, rp, t_rp

        def proj(pi, M, wt, c0, hT, t_h, nb, pbase=0):
            for kc in range(8):
                k.op("pe", lambda en: en.matmul(PS[pi][pbase:pbase + M, 0:nb], lhsT=wt[:, kc, c0:c0 + M], rhs=hT[:, kc, 0:nb], start=(kc == 0), stop=(kc == 7)), reads=[t_w, t_h], writes=[TPS[pi]])

        def rstd_from_ms(out_ap, ms_ap, reads, wtok, tmp_ap):
            k.op("act", lambda en: en.activation(out=tmp_ap, in_=ms_ap, func=AF.Ln, bias=epsc[0:tmp_ap.shape[0], 0:1]), reads=reads + [t_w], writes=[wtok])
            k.op("act", lambda en: en.activation(out=out_ap, in_=tmp_ap, func=AF.Exp, scale=-0.5), reads=[wtok], writes=[wtok])

        wk_r = Rot([sbA("wk%d" % i, [128, 6, 512]) for i in range(1)])
        wkb_r = Rot([sbA("wkb%d" % i, [128, 3, 512], BF16) for i in range(2)])
        for (t0, nb) in blocks:
            hT, t_h, rp, t_rp = load_block(t0, nb)
            wk, t_wk = wk_r.next()
            wkb, t_wkb = wkb_r.next()
            proj(0, 128, wkv, 0, hT, t_h, nb)
            k.op("act", lambda en: en.activation(out=wkb[:, 0, 0:nb], in_=PS[0][:, 0:nb], func=AF.Square), reads=[TPS[0]], writes=[t_wkb])
            k.op("dve", lambda en: en.tensor_copy(out=wk[:, 0, 0:nb], in_=PS[0][:, 0:nb]), reads=[TPS[0]], writes=[t_wk])
            k.op("pe", lambda en: en.matmul(PS[1][:, 0:nb], lhsT=g.ones128[:], rhs=wkb[:, 0, 0:nb], start=True, stop=True), reads=[t_wkb, g.T_const], writes=[TPS[1]])
            rstd_from_ms(wk[:, 1, 0:nb], PS[1][:, 0:nb], [TPS[1]], t_wk, wk[:, 1, 0:nb])
            k.op("dve", lambda en: en.tensor_tensor(out=wkb[:, 1, 0:nb], in0=wk[:, 0, 0:nb], in1=wk[:, 1, 0:nb], op=ALU.mult), reads=[t_wk], writes=[t_wkb])
            for h in range(4):
                pi = 2 + (h % 2)
                k.op("pe", lambda en: en.matmul(PS[pi][0:64, 0:nb], lhsT=lw.wuk[:, h * 64:(h + 1) * 64], rhs=wkb[:, 1, 0:nb], start=True, stop=True), reads=[t_wkb, lw.tok], writes=[TPS[pi]])
                k.op("act" if h % 2 else "dve", (lambda en: en.activation(out=KTm[h][0:64, t0:t0 + nb], in_=PS[pi][0:64, 0:nb], func=AF.Copy)) if h % 2 else (lambda en: en.tensor_copy(out=KTm[h][0:64, t0:t0 + nb], in_=PS[pi][0:64, 0:nb])), reads=[TPS[pi]], writes=[t_K])
            for j in range(nb // 128):
                ti = t0 // 128 + j
                k.op("pe", lambda en: en.matmul(PS[4][:, 0:256], lhsT=wkb[:, 1, j * 128:(j + 1) * 128], rhs=lw.wuv[:], start=True, stop=True), reads=[t_wkb, lw.tok], writes=[TPS[4]])
                k.op("dve", lambda en: en.tensor_copy(out=VM[:, ti, :].rearrange("p (h d) -> p h d", d=65)[:, :, 0:64], in_=PS[4][:, 0:256].rearrange("p (h d) -> p h d", d=64)), reads=[TPS[4]], writes=[t_K])
            proj(5, 32, wkv, 128, hT, t_h, nb, pbase=64)
            proj(6, 32, wkv, 160, hT, t_h, nb, pbase=64)
            k.op("dve", lambda en: en.tensor_tensor(out=wk[64:96, 2, 0:nb], in0=PS[5][64:96, 0:nb], in1=rp[64:96, 0, 0:nb], op=ALU.mult), reads=[TPS[5], t_rp], writes=[t_wk])
            k.op("dve", lambda en: en.tensor_tensor(out=wk[64:96, 3, 0:nb], in0=PS[6][64:96, 0:nb], in1=rp[64:96, 1, 0:nb], op=ALU.mult), reads=[TPS[6], t_rp], writes=[t_wk])
            for h in range(4):
                k.op("pool" if h % 2 else "dve", lambda en: en.tensor_tensor(out=KTm[h][64:96, t0:t0 + nb], in0=wk[64:96, 2, 0:nb], in1=wk[64:96, 3, 0:nb], op=ALU.add), reads=[t_wk], writes=[t_K])
            proj(7, 128, wkv, 192, hT, t_h, nb)
            proj(0, 128, wkv, 448, hT, t_h, nb)
            k.op("act", lambda en: en.activation(out=wkb[:, 2, 0:nb], in_=PS[7][:, 0:nb], func=AF.Square), reads=[TPS[7]], writes=[t_wkb])
            k.op("pe", lambda en: en.matmul(PS[1][:, 0:nb], lhsT=g.blk64[:], rhs=wkb[:, 2, 0:nb], start=True, stop=True), reads=[t_wkb, g.T_const], writes=[TPS[1]])
            rstd_from_ms(wk[:, 4, 0:nb], PS[1][:, 0:nb], [TPS[1]], t_wk, wk[:, 4, 0:nb])
            k.op("dve", lambda en: en.scalar_tensor_tensor(out=wk[:, 0, 0:nb], in0=PS[7][:, 0:nb], scalar=lw.gq[:, 2:3], in1=rp[:, 2, 0:nb], op0=ALU.mult, op1=ALU.mult), reads=[TPS[7], t_rp, lw.tok, t_wk], writes=[t_wk])
            k.op("dve", lambda en: en.scalar_tensor_tensor(out=wk[:, 5, 0:nb], in0=PS[0][:, 0:nb], scalar=lw.gq[:, 3:4], in1=rp[:, 3, 0:nb], op0=ALU.mult, op1=ALU.mult), reads=[TPS[0], t_rp, lw.tok, t_wk], writes=[t_wk])
            k.op("pool", lambda en: en.tensor_tensor(out=wk[:, 0, 0:nb], in0=wk[:, 0, 0:nb], in1=wk[:, 5, 0:nb], op=ALU.add), reads=[t_wk], writes=[t_wk])
            k.op("dve", lambda en: en.tensor_tensor(out=KTg[:, t0:t0 + nb], in0=wk[:, 0, 0:nb], in1=wk[:, 4, 0:nb], op=ALU.mult), reads=[t_wk], writes=[t_K])
            for j in range(nb // 128):
                ti = t0 // 128 + j
                for kc in range(8):
                    k.op("pe", lambda en: en.matmul(PS[4][:, 0:128], lhsT=hT[:, kc, j * 128:(j + 1) * 128], rhs=wkv[:, kc, 320:448], start=(kc == 0), stop=(kc == 7)), reads=[t_h, t_w], writes=[TPS[4]])
                k.op("act", lambda en: en.activation(out=VG[:, ti, :].rearrange("p (h d) -> p h d", d=65)[:, :, 0:64], in_=PS[4][:, 0:128].rearrange("p (h d) -> p h d", d=64), func=AF.Copy), reads=[TPS[4]], writes=[t_K])
        if "KTm0" in g.dbg:
            k.dma("pool", g.dbg["KTm0"], KTm[0][:], reads=[t_K])
            k.dma("pool", g.dbg["KTg"], KTg[:], reads=[t_K])
            k.dma("pool", g.dbg["VM"], VM[:], reads=[t_K])
            k.dma("pool", g.dbg["VG"], VG[:], reads=[t_K])

        k.barrier()
        esA.close()
        wq = sb("wq", [128, 8, 192 + 256 + 256 + 256 + 256], BF16)
        k.dma("sp", wq[:, :, 0:192], g.WINB[:, :, O_CQ:O_CQ + 192], reads=[g.T_WINB], writes=[t_w])
        k.dma("sp", wq[:, :, 192:448], g.WINB[:, :, O_GM:O_GM + 256], reads=[g.T_WINB], writes=[t_w])
        k.dma("sp", wq[:, :, 448:960], g.WINB[:, :, O_QM:O_QM + 512], reads=[g.T_WINB], writes=[t_w])
        k.dma("sp", wq[:, :, 960:1216], g.WINB[:, :, O_GG:O_GG + 256], reads=[g.T_WINB], writes=[t_w])
        qm_r = Rot([[sb("qm%d_%d" % (i, h), [96, 512], BF16) for h in range(4)] for i in range(2)])
        qg_r = Rot([[sb("qg%d_%d" % (i, j), [128, 512], BF16) for j in range(2)] for i in range(2)])
        gate_r = Rot([sb("gate%d" % i, [64, 8, 512], BF16) for i in range(1)])
        cq_r = Rot([sb("cq%d" % i, [128, 4, 512]) for i in range(1)])
        cqb_r = Rot([sb("cqb%d" % i, [128, 4, 512], BF16) for i in range(1)])
        P_r = Rot([sb("P%d" % i, [128, 2, 512], BF16) for i in range(3)])
        osb_r = Rot([sb("osb%d" % i, [65, 512]) for i in range(2)])
        res_r = Rot([sb("res%d" % i, [64, 512], BF16) for i in range(3)])
        S_bufs = [(0, 1), (2, 3)]
        O_bufs = [4, 5]
        for (t0, nb) in blocks:
            if t0 == 0 and not need_ctx:
                continue
            hT, t_h, rp, t_rp = load_block(t0, nb)
            kts = list(range(2)) if t0 == 0 else list(range(NT))
            qm, t_qm = qm_r.next()
            qg, t_qg = qg_r.next()
            gate, t_gate = gate_r.next()
            cq, t_cq = cq_r.next()
            cqb, t_cqb = cqb_r.next()
            for hh in range(8):
                c0 = (192 + hh * 64) if hh < 4 else (960 + (hh - 4) * 64)
                pi = 6 + hh % 2
                proj(pi, 64, wq, c0, hT, t_h, nb)
                k.op("act", lambda en: en.activation(out=gate[:, hh, 0:nb], in_=PS[pi][0:64, 0:nb], func=AF.Silu), reads=[TPS[pi]], writes=[t_gate])
            proj(6, 128, wq, 0, hT, t_h, nb)
            proj(7, 64, wq, 128, hT, t_h, nb)
            k.op("act", lambda en: en.activation(out=cqb[:, 0, 0:nb], in_=PS[6][:, 0:nb], func=AF.Square), reads=[TPS[6]], writes=[t_cqb])
            k.op("act", lambda en: en.activation(out=cqb[0:64, 1, 0:nb], in_=PS[7][0:64, 0:nb], func=AF.Square), reads=[TPS[7]], writes=[t_cqb])
            k.op("dve", lambda en: en.tensor_copy(out=cq[:, 0, 0:nb], in_=PS[6][:, 0:nb]), reads=[TPS[6]], writes=[t_cq])
            k.op("dve", lambda en: en.tensor_copy(out=cq[0:64, 1, 0:nb], in_=PS[7][0:64, 0:nb]), reads=[TPS[7]], writes=[t_cq])
            k.op("pe", lambda en: en.matmul(PS[6][:, 0:nb], lhsT=g.ones192[:], rhs=cqb[:, 0, 0:nb], start=True, stop=False), reads=[t_cqb, g.T_const, t_cq], writes=[TPS[6]])
            k.op("pe", lambda en: en.matmul(PS[6][:, 0:nb], lhsT=g.ones192[0:64, :], rhs=cqb[0:64, 1, 0:nb], start=False, stop=True), reads=[t_cqb, g.T_const], writes=[TPS[6]])
            rstd_from_ms(cq[:, 2, 0:nb], PS[6][:, 0:nb], [TPS[6]], t_cq, cq[:, 2, 0:nb])
            k.op("dve", lambda en: en.tensor_tensor(out=cqb[:, 2, 0:nb], in0=cq[:, 0, 0:nb], in1=cq[:, 2, 0:nb], op=ALU.mult), reads=[t_cq], writes=[t_cqb])
            k.op("dve", lambda en: en.tensor_tensor(out=cqb[0:64, 3, 0:nb], in0=cq[0:64, 1, 0:nb], in1=cq[0:64, 2, 0:nb], op=ALU.mult), reads=[t_cq], writes=[t_cqb])
            for h in range(4):
                for (pi, wa, wb_) in ((6, lw.wuq_a, lw.wuq_b), (7, lw.wuqp_a, lw.wuqp_b)):
                    k.op("pe", lambda en: en.matmul(PS[pi][0:96, 0:nb], lhsT=wa[:, h * 96:(h + 1) * 96], rhs=cqb[:, 2, 0:nb], start=True, stop=False), reads=[t_cqb, lw.tok], writes=[TPS[pi]])
                    k.op("pe", lambda en: en.matmul(PS[pi][0:96, 0:nb], lhsT=wb_[:, h * 96:(h + 1) * 96], rhs=cqb[0:64, 3, 0:nb], start=False, stop=True), reads=[t_cqb, lw.tok], writes=[TPS[pi]])
                k.op("dve", lambda en: en.tensor_tensor(out=cq[0:96, 3, 0:nb], in0=PS[6][0:96, 0:nb], in1=rp[0:96, 0, 0:nb], op=ALU.mult), reads=[TPS[6], t_rp, t_cq], writes=[t_cq])
                k.op("dve", lambda en: en.tensor_tensor(out=cq[0:96, 1, 0:nb], in0=PS[7][0:96, 0:nb], in1=rp[0:96, 1, 0:nb], op=ALU.mult), reads=[TPS[7], t_rp, t_cq], writes=[t_cq])
                k.op("pool", lambda en: en.tensor_tensor(out=qm[h][:, 0:nb], in0=cq[0:96, 3, 0:nb], in1=cq[0:96, 1, 0:nb], op=ALU.add), reads=[t_cq], writes=[t_qm])
            for j in range(2):
                proj(6, 128, wq, 448 + j * 128, hT, t_h, nb)
                proj(7, 128, wq, 704 + j * 128, hT, t_h, nb)
                k.op("act", lambda en: en.activation(out=cqb[:, 0, 0:nb], in_=PS[6][:, 0:nb], func=AF.Square), reads=[TPS[6], t_cqb], writes=[t_cqb])
                k.op("dve", lambda en: en.scalar_tensor_tensor(out=cq[:, 0, 0:nb], in0=PS[6][:, 0:nb], scalar=lw.gq[:, 0:1], in1=rp[:, 2, 0:nb], op0=ALU.mult, op1=ALU.mult), reads=[TPS[6], t_rp, lw.tok, t_cq], writes=[t_cq])
                k.op("dve", lambda en: en.scalar_tensor_tensor(out=cq[:, 1, 0:nb], in0=PS[7][:, 0:nb], scalar=lw.gq[:, 1:2], in1=rp[:, 3, 0:nb], op0=ALU.mult, op1=ALU.mult), reads=[TPS[7], t_rp, lw.tok, t_cq], writes=[t_cq])
                k.op("pe", lambda en: en.matmul(PS[6][:, 0:nb], lhsT=g.blk64[:], rhs=cqb[:, 0, 0:nb], start=True, stop=True), reads=[t_cqb, g.T_const, t_cq], writes=[TPS[6]])
                rstd_from_ms(cq[:, 2, 0:nb], PS[6][:, 0:nb], [TPS[6]], t_cq, cq[:, 2, 0:nb])
                k.op("pool", lambda en: en.tensor_tensor(out=cq[:, 0, 0:nb], in0=cq[:, 0, 0:nb], in1=cq[:, 1, 0:nb], op=ALU.add), reads=[t_cq], writes=[t_cq])
                k.op("dve", lambda en: en.tensor_tensor(out=qg[j][:, 0:nb], in0=cq[:, 0, 0:nb], in1=cq[:, 2, 0:nb], op=ALU.mult), reads=[t_cq], writes=[t_qg])
            if "qm0" in g.dbg and t0 == 256:
                k.dma("pool", g.dbg["qm0"], qm[0][:], reads=[t_qm])
                k.dma("pool", g.dbg["qg0"], qg[0][:], reads=[t_qg])
            for hh in range(8):
                if hh < 4:
                    dk, scale = 96, 96 ** -0.5
                    Kt = KTm[hh]
                    kb0 = 0
                    Qt = qm[hh]
                    qb0 = 0
                    Vt, vc0 = VM, hh * 65
                    tq = t_qm
                else:
                    hq = hh - 4
                    kv = hq // 2
                    dk, scale = 64, 64 ** -0.5
                    Kt, kb0 = KTg, kv * 64
                    Qt, qb0 = qg[hq % 2], kv * 64
                    Vt, vc0 = VG, kv * 65
                    tq = t_qg
                oi = O_bufs[hh % 2]
                pairs = [kts[i:i + 2] for i in range(0, len(kts), 2)]

                def emit_S(pidx):
                    sb_ = S_bufs[pidx % 2]
                    for j, kt in enumerate(pairs[pidx]):
                        k.op("pe", lambda en: en.matmul(PS[sb_[j]][:, 0:nb], lhsT=Kt[kb0:kb0 + dk, kt * 128:(kt + 1) * 128], rhs=Qt[qb0:qb0 + dk, 0:nb], start=True, stop=True), reads=[t_K, tq], writes=[TPS[sb_[j]]])

                emit_S(0)
                for pidx in range(len(pairs)):
                    if pidx + 1 < len(pairs):
                        emit_S(pidx + 1)
                    sb_ = S_bufs[pidx % 2]
                    Pt, t_P = P_r.next()
                    for j, kt in enumerate(pairs[pidx]):
                        k.op("act", lambda en: en.activation(out=Pt[:, j, 0:nb], in_=PS[sb_[j]][:, 0:nb], func=AF.Exp, scale=scale), reads=[TPS[sb_[j]]], writes=[t_P])
                    for j, kt in enumerate(pairs[pidx]):
                        first = (pidx == 0 and j == 0)
                        lastm = (pidx == len(pairs) - 1 and j == len(pairs[pidx]) - 1)
                        k.op("pe", lambda en: en.matmul(PS[oi][0:65, 0:nb], lhsT=Vt[:, kt, vc0:vc0 + 65], rhs=Pt[:, j, 0:nb], start=first, stop=lastm), reads=[t_P, t_K], writes=[TPS[oi]])
                osb, t_osb = osb_r.next()
                k.op("dve", lambda en: en.tensor_copy(out=osb[:, 0:nb], in_=PS[oi][0:65, 0:nb]), reads=[TPS[oi]], writes=[t_osb])
                k.op("dve", lambda en: en.reciprocal(out=osb[64:65, 0:nb], in_=osb[64:65, 0:nb]), reads=[t_osb], writes=[t_osb])
                bi = 6 + hh % 2
                k.op("pe", lambda en: en.matmul(PS[bi][0:64, 0:nb], lhsT=g.onesf[64:65, 0:64], rhs=osb[64:65, 0:nb], start=True, stop=True), reads=[t_osb, g.T_const], writes=[TPS[bi]])
                k.op("dve", lambda en: en.tensor_tensor(out=osb[0:64, 0:nb], in0=osb[0:64, 0:nb], in1=PS[bi][0:64, 0:nb], op=ALU.mult), reads=[t_osb, TPS[bi]], writes=[t_osb])
                res, t_res = res_r.next()
                k.op("pool", lambda en: en.tensor_tensor(out=res[:, 0:nb], in0=osb[0:64, 0:nb], in1=gate[:, hh, 0:nb], op=ALU.mult), reads=[t_osb, t_gate], writes=[t_res])
                kc = hh // 2
                p0 = (hh % 2) * 64
                k.dma("pool", g.CATT[p0:p0 + 64, kc, t0:t0 + nb], res[:, 0:nb], reads=[t_res], writes=[g.T_CATT])
        k.barrier()


def phase_P6(g, l, s, src, dst, need_ctx, lat_only_out):
    nc, k = g.nc, g.k
    with ExitStack() as es:
        sb = lambda n, s_, d=F32: es.enter_context(nc.sbuf_tensor(_nm(n), s_, d))
        ps = lambda n, s_, d=F32: es.enter_context(nc.psum_tensor(_nm(n), s_, d))
        wo = sb("wo", [128, 8, D], BF16)
        t_w = Tok()
        k.dma("sp", wo[:], g.WOUTB, reads=[g.T_WOUTB], writes=[t_w])
        Gb = {}
        for v in ((s, 2) if need_ctx else (s,)):
            Gb[v] = sb("Gb%d" % v, [128, D])
            k.dma("sp", Gb[v][:], g.MODROWS[v:v + 1, 2 * D:3 * D].broadcast_to([128, D]), reads=[g.T_MOD], writes=[t_w])
        neghalf = sb("neghalf6", [128, 1])
        k.op("pool", lambda en: en.memset(neghalf[:], -0.5), writes=[t_w])
        cat_r = Rot([sb("cat%d" % i, [128, 8, 512], BF16) for i in range(2)])
        x_r = Rot([sb("x6_%d" % i, [128, D]) for i in range(3)])
        o_r = Rot([sb("o6_%d" % i, [128, D]) for i in range(3)])
        st_r = Rot([sb("st6_%d" % i, [128, 4]) for i in range(3)])
        junk = sb("junk6", [128, D], BF16)
        t_junk = Tok()
        po_r = Rot([ps("po%d" % i, [128, D]) for i in range(3)], excl=True)
        blocks = ([(0, 256)] if need_ctx else []) + [(256 + 512 * b, 512) for b in range(8)]
        for (t0, nb) in blocks:
            cat, t_cat = cat_r.next()
            k.dma("sp", cat[:, :, 0:nb], g.CATT[:, :, t0:t0 + nb], reads=[g.T_CATT], writes=[t_cat])
            for j in range(nb // 128):
                tok0 = t0 + j * 128
                po, t_po = po_r.next()
                for hf in range(2):
                    for kc in range(8):
                        k.op("pe", lambda en: en.matmul(po[:, hf * 512:(hf + 1) * 512], lhsT=cat[:, kc, j * 128:(j + 1) * 128], rhs=wo[:, kc, hf * 512:(hf + 1) * 512], start=(kc == 0), stop=(kc == 7)), reads=[t_cat, t_w], writes=[t_po])
                xt, t_x = x_r.next()
                k.dma("sp", xt[:], src[s, tok0:tok0 + 128, :], reads=[g.T_XS[s]], writes=[t_x])
                st, t_st = st_r.next()
                k.op("act", lambda en: en.activation(out=junk[:], in_=po[:], func=AF.Square, accum_out=st[:, 0:1]), reads=[t_po], writes=[t_junk, t_st])
                k.op("dve", lambda en: en.tensor_scalar(out=st[:, 1:2], in0=st[:, 0:1], scalar1=1.0 / D, scalar2=EPS, op0=ALU.mult, op1=ALU.add), reads=[t_st], writes=[t_st])
                k.op("pool", lambda en: en.tensor_tensor(out=st[:, 2:3], in0=st[:, 1:2], in1=neghalf[:], op=ALU.pow), reads=[t_st, t_w], writes=[t_st])
                ot, t_o = o_r.next()
                gb = Gb[2] if t0 == 0 else Gb[s]
                k.op("dve", lambda en: en.scalar_tensor_tensor(out=ot[:], in0=po[:], scalar=st[:, 2:3], in1=gb[:], op0=ALU.mult, op1=ALU.mult), reads=[t_po, t_st, t_w], writes=[t_o])
                k.op("pool", lambda en: en.tensor_tensor(out=ot[:], in0=ot[:], in1=xt[:], op=ALU.add), reads=[t_o, t_x], writes=[t_o])
                if lat_only_out:
                    if t0 == 0:
                        continue
                    k.dma("pool", dst[s, tok0 - C:tok0 - C + 128, :], ot[:], reads=[t_o], writes=[g.T_Y])
                else:
                    wt = g.T_XS[s] if dst is g.XS else g.T_Y
                    k.dma("pool", dst[s, tok0:tok0 + 128, :], ot[:], reads=[t_o], writes=[wt])
        k.barrier()


_CACHE = {}


def _weights_host(inputs):
    w = {}
    for n in ("w_mod", "b_mod", "g_pre", "g_post", "w_in", "w_out", "mla_g_cq", "mla_w_uq", "mla_g_ckv", "mla_w_ukv",
              "hy_conv_w", "hy_conv_b", "hy_f_w1", "hy_f_b1", "hy_f_freq1", "hy_f_w2", "hy_f_b2", "hy_f_freq2", "hy_f_w3", "hy_bias"):
        w[n] = np.ascontiguousarray(inputs[n], dtype=np.float32)
    p64, _ = _partner_index(64)
    gq = np.asarray(inputs["gqa_g_q"], np.float32)
    gk = np.asarray(inputs["gqa_g_k"], np.float32)
    cols = np.zeros((DEPTH, 128, 4), np.float32)
    for l in range(DEPTH):
        cols[l, :, 0] = np.concatenate([gq[l], gq[l]])
        cols[l, :, 1] = np.concatenate([gq[l][p64], gq[l][p64]])
        cols[l, :, 2] = np.concatenate([gk[l], gk[l]])
        cols[l, :, 3] = np.concatenate([gk[l][p64], gk[l][p64]])
    w["gq_cols"] = cols
    w.update(ssm_host_layouts(inputs))
    return w


def make_in_maps(inputs, xs_per_core):
    w = _weights_host(inputs)
    cst = host_constants()
    c = np.asarray(inputs["c"], np.float32)
    c_ctx = np.asarray(inputs["c_ctx"], np.float32)
    maps = []
    for i in range(NCORES):
        cv = np.stack([c[2 * i], c[2 * i + 1], c_ctx], -1)
        cT = np.ascontiguousarray(cv.reshape(8, 128, 3).transpose(1, 0, 2))
        m = {"xs": xs_per_core[i], "cT": cT}
        m.update(w)
        m.update(cst)
        maps.append(m)
    return maps


def kernel(**inputs):
    x = np.asarray(inputs["x"], np.float32)
    ctx = np.asarray(inputs["ctx"], np.float32)
    xs = [np.ascontiguousarray(np.concatenate([ctx[2 * i:2 * i + 2], x[2 * i:2 * i + 2]], axis=1)) for i in range(NCORES)]
    key = "fused"
    if key not in _CACHE:
        _CACHE[key] = build_program(list(range(DEPTH)), True)[0]
    nc = _CACHE[key]
    res = run_bass_kernel_spmd(nc, make_in_maps(inputs, xs), core_ids=list(range(NCORES)))
    out = np.concatenate([r["y"] for r in res.results], axis=0)
    return out.astype(np.float32)


HY_BANDS = 16
HY_DECAY = (math.log(1e-2) / 1.5, math.log(1e-2) / 0.3)


class HyCfg:
    def __init__(self, name, n, ni):
        self.name = name
        self.n = n
        self.ni = ni
        self.N = 2 * n
        self.cpg = 128 // ni
        self.ng = 256 // self.cpg
        self.ncol = 256 * ni


HY_LAT = HyCfg("L", 4096, 32)
HY_CTX = HyCfg("C", 256, 2)


def _hy_features(n, pos):
    f32 = np.float32
    t = np.linspace(0.0, 1.0, n, dtype=f32)
    omega = (f32(2.0 * math.pi) * np.arange(n, dtype=f32) / f32(n)).astype(f32)
    bands = np.linspace(1e-4, HY_BANDS - 1, HY_BANDS, dtype=f32)
    tt = t[pos]
    om = omega[pos]
    ang = (bands[:, None] * om[None, :]).astype(f32)
    z = np.concatenate([tt[None, :], np.cos(ang), -np.sin(ang)], axis=0).astype(f32)
    return z, tt


def hyena_constants():
    cst = {}
    f64 = np.float64
    j = np.arange(128)[:, None]
    b = np.arange(256)[None, :]
    ang = 2 * np.pi * j * b / 256.0
    F1 = np.concatenate([np.cos(ang), -np.sin(ang)], 1)
    sgn = np.where(b % 2 == 0, 1.0, -1.0)
    F1b = np.concatenate([np.cos(ang) * sgn, -np.sin(ang) * sgn], 1)
    cst["hk_F1"] = np.stack([F1, F1b], 0).astype(np.float32)
    for cfg in (HY_LAT, HY_CTX):
        p = np.arange(128)
        i_of_p = (p // cfg.cpg)[:, None]
        ang = 2 * np.pi * i_of_p * b / cfg.N
        TW = np.stack([np.concatenate([np.cos(ang), np.cos(ang)], 1), np.concatenate([-np.sin(ang), -np.sin(ang)], 1)], 0)
        cst["hk_TW" + cfg.name] = TW.astype(np.float32)
        ii = (p // cfg.cpg)[:, None]
        ci = (p % cfg.cpg)[:, None]
        aa = (p // cfg.cpg)[None, :]
        ca = (p % cfg.cpg)[None, :]
        dl = (ci == ca).astype(f64)
        ang = 2 * np.pi * ii * aa / cfg.ni
        Gr = dl * np.cos(ang)
        Gi = -dl * np.sin(ang)
        cst["hk_G" + cfg.name] = np.stack([Gr, Gi, -Gi], 0).astype(np.float32)
        ang = 2 * np.pi * aa.T * ii.T / cfg.ni
        dl2 = (ci.T == ca.T)
        angm = 2 * np.pi * (p // cfg.cpg)[:, None] * (p // cfg.cpg)[None, :] / cfg.ni
        dlm = ((p % cfg.cpg)[:, None] == (p % cfg.cpg)[None, :]).astype(f64)
        GIr = dlm * np.cos(angm)
        GIi = dlm * np.sin(angm)
        cst["hk_GI" + cfg.name] = np.stack([np.concatenate([GIr, GIi], 1), np.concatenate([-GIi, GIr], 1)], 0).astype(np.float32)
        TWI = np.zeros((2, 2, 128, 256), f64)
        for bc in range(2):
            bb = (bc * 128 + np.arange(128))[:, None]
            ang = 2 * np.pi * bb * (p // cfg.cpg)[None, :] / cfg.N
            TWI[bc, 0] = np.concatenate([np.cos(ang), np.cos(ang)], 1)
            TWI[bc, 1] = np.concatenate([np.sin(ang), np.sin(ang)], 1)
        cst["hk_TWI" + cfg.name] = TWI.astype(np.float32)
        FI = np.zeros((2, 2, 128, 128), f64)
        for bc in range(2):
            bb = (bc * 128 + np.arange(128))[:, None]
            ang = 2 * np.pi * bb * np.arange(128)[None, :] / 256.0
            FI[bc, 0] = np.cos(ang) / cfg.N
            FI[bc, 1] = -np.sin(ang) / cfg.N
        cst["hk_FI" + cfg.name] = FI.astype(np.float32)
        n = cfg.n
        u = np.arange(n)
        posr = np.where(u == 0, 0, n - u)
        zf, tf_ = _hy_features(n, u)
        zb, tb_ = _hy_features(n, posr)
        cst["hk_Z" + cfg.name] = np.stack([zf, zb], 0)
        deltas = np.abs(np.linspace(HY_DECAY[0], HY_DECAY[1], 256, dtype=np.float32))
        decf = np.exp(-tf_[:, None] * deltas[None, :]).astype(np.float32)
        decb = np.exp(-tb_[:, None] * deltas[None, :]).astype(np.float32)
        cst["hk_DEC" + cfg.name] = np.stack([decf.reshape(128, cfg.ni, 256), decb.reshape(128, cfg.ni, 256)], 0)
        msk = (p[:, None] % cfg.cpg == np.arange(cfg.cpg)[None, :]).astype(np.float32)
        cst["hk_MSK" + cfg.name] = msk
    return cst


def _hy_twiddle(k, A_ps, t_A, TW, t_TW, m1, m2, t_m, outr, outi, t_out, n2, sub_first=True):
    k.op("dve", lambda en: en.tensor_tensor(out=m1[:, 0:2 * n2], in0=A_ps, in1=TW[:, 0, 0:2 * n2], op=ALU.mult), reads=[t_A, t_TW], writes=[t_m])
    k.op("dve", lambda en: en.tensor_tensor(out=m2[:, 0:2 * n2], in0=A_ps, in1=TW[:, 1, 0:2 * n2], op=ALU.mult), reads=[t_A, t_TW, t_m], writes=[t_m])
    k.op("pool", lambda en: en.tensor_tensor(out=outr, in0=m1[:, 0:n2], in1=m2[:, n2:2 * n2], op=ALU.subtract), reads=[t_m], writes=[t_out])
    k.op("pool", lambda en: en.tensor_tensor(out=outi, in0=m2[:, 0:n2], in1=m1[:, n2:2 * n2], op=ALU.add), reads=[t_m, t_out], writes=[t_out])


def phase_hy_filter(g, l, cfg):
    nc, k, W, K = g.nc, g.k, g.W, g.K
    n, ni, ng, cpg, ncol = cfg.n, cfg.ni, cfg.ng, cfg.cpg, cfg.ncol
    KH = g.KHAT[cfg.name]
    with ExitStack() as es:
        sb = lambda nm, s_, d=F32: es.enter_context(nc.sbuf_tensor(_nm(nm), s_, d))
        ps = lambda nm, s_, d=F32: es.enter_context(nc.psum_tensor(_nm(nm), s_, d))
        t_w = Tok()
        w1 = sb("hw1", [33, 64]); w2 = sb("hw2", [64, 64]); w3 = sb("hw3", [64, 1024])
        cols = sb("hcols", [64, 8])
        k.dma("sp", w1[:], W["hy_f_w1"][l], writes=[t_w])
        k.dma("sp", w2[:], W["hy_f_w2"][l], writes=[t_w])
        k.dma("sp", w3[:], W["hy_f_w3"][l], writes=[t_w])
        for ci, nm in enumerate(("hy_f_b1", "hy_f_freq1", "hy_f_b2", "hy_f_freq2")):
            k.dma("sp", cols[:, ci:ci + 1], W[nm][l].rearrange("(p o) -> p o", o=1), writes=[t_w])
        k.op("dve", lambda en: en.tensor_tensor(out=cols[:, 4:5], in0=cols[:, 0:1], in1=cols[:, 1:2], op=ALU.mult), reads=[t_w], writes=[t_w])
        k.op("dve", lambda en: en.tensor_tensor(out=cols[:, 5:6], in0=cols[:, 2:3], in1=cols[:, 3:4], op=ALU.mult), reads=[t_w], writes=[t_w])
        F1 = sb("hF1", [128, 2, 512]); TW = sb("hTW", [128, 2, 512]); G3 = sb("hG", [128, 3, 128]); MSK = sb("hmsk", [128, cpg])
        t_c = Tok()
        k.dma("sp", F1[:], K["hk_F1"].rearrange("a p n -> p a n"), writes=[t_c])
        k.dma("sp", TW[:], K["hk_TW" + cfg.name].rearrange("a p n -> p a n"), writes=[t_c])
        k.dma("sp", G3[:], K["hk_G" + cfg.name].rearrange("a p n -> p a n"), writes=[t_c])
        k.dma("sp", MSK[:], K["hk_MSK" + cfg.name], writes=[t_c])
        epsc = sb("hyeps", [128, 1])
        k.op("pool", lambda en: en.memset(epsc[:], EPS), writes=[t_c])
        h2T = [sb("h2T%d" % d_, [64, n]) for d_ in range(2)]
        t_h2 = Tok()
        PSm = [ps("hps%d" % i, [128, 512]) for i in range(4)]
        TP = [Tok(True) for _ in range(4)]
        PI = math.pi

        def sin_layer(out_ap, ps_ap, t_ps, fr_col, fb_col, tmp, msk_, t_tmp, wtok):
            k.op("dve", lambda en: en.tensor_scalar(out=tmp, in0=ps_ap, scalar1=fr_col, scalar2=fb_col, op0=ALU.mult, op1=ALU.add), reads=[t_ps, t_w], writes=[t_tmp])
            k.op("dve", lambda en: en.tensor_scalar(out=msk_, in0=tmp, scalar1=PI, scalar2=-2 * PI, op0=ALU.is_gt, op1=ALU.mult), reads=[t_tmp], writes=[t_tmp])
            k.op("dve", lambda en: en.tensor_tensor(out=tmp, in0=tmp, in1=msk_, op=ALU.add), reads=[t_tmp], writes=[t_tmp])
            k.op("dve", lambda en: en.tensor_scalar(out=msk_, in0=tmp, scalar1=-PI, scalar2=2 * PI, op0=ALU.is_lt, op1=ALU.mult), reads=[t_tmp], writes=[t_tmp])
            k.op("dve", lambda en: en.tensor_tensor(out=tmp, in0=tmp, in1=msk_, op=ALU.add), reads=[t_tmp], writes=[t_tmp])
            k.op("act", lambda en: en.activation(out=out_ap, in_=tmp, func=AF.Sin), reads=[t_tmp], writes=[wtok])

        with ExitStack() as es2:
            sb2 = lambda nm, s_, d=F32: es2.enter_context(nc.sbuf_tensor(_nm(nm), s_, d))
            Zt = sb2("hZ", [33, n])
            t_z = Tok()
            h1 = sb2("hh1", [64, 512]); tmp = sb2("htmp", [64, 512]); msk_ = sb2("hmk", [64, 512])
            t_h1 = Tok(); t_tmp = Tok()
            bw = min(512, n)
            for d_ in range(2):
                k.dma("sp", Zt[:], K["hk_Z" + cfg.name][d_], reads=[], writes=[t_z])
                for b0 in range(0, n, bw):
                    k.op("pe", lambda en: en.matmul(PSm[0][0:64, 0:bw], lhsT=w1[:], rhs=Zt[:, b0:b0 + bw], start=True, stop=True), reads=[t_w, t_z], writes=[TP[0]])
                    sin_layer(h1[:, 0:bw], PSm[0][0:64, 0:bw], TP[0], cols[:, 1:2], cols[:, 4:5], tmp[:, 0:bw], msk_[:, 0:bw], t_tmp, t_h1)
                    k.op("pe", lambda en: en.matmul(PSm[1][0:64, 0:bw], lhsT=w2[:], rhs=h1[:, 0:bw], start=True, stop=True), reads=[t_w, t_h1], writes=[TP[1]])
                    sin_layer(h2T[d_][:, b0:b0 + bw], PSm[1][0:64, 0:bw], TP[1], cols[:, 3:4], cols[:, 5:6], tmp[:, 0:bw], msk_[:, 0:bw], t_tmp, t_h2)
            k.barrier()
        UF = [sb("hUF%d" % d_, [128, ncol]) for d_ in range(2)]
        t_uf = Tok()
        DEC = sb("hDEC", [128, ni, 256])
        t_dec = Tok()
        HSQ = sb("hHSQ", [128, 256]); sq = sb("hsq", [128, 256]); hh = sb("hhh", [128, 256])
        t_hsq = Tok(); t_hh = Tok(); t_sq = Tok()
        RS = sb("hRS", [128, 256]); SC = sb("hSC", [128, ng]); rtmp = sb("hrtmp", [128, 256])
        t_rs = Tok()
        m1 = sb("hm1", [128, 512]); m2 = sb("hm2", [128, 512])
        P1 = sb("hP1", [128, 2, 256])
        t_m = Tok(); t_p1 = Tok()
        ko_r = Rot([sb("hko%d" % i, [128, 512]) for i in range(2)])
        for o in range(2):
            k.op("pool", lambda en: en.memset(HSQ[:], 0.0), reads=[t_rs], writes=[t_hsq])
            for d_ in range(2):
                k.dma("sp", DEC[:], K["hk_DEC" + cfg.name][d_], writes=[t_dec])
                c0 = o * 512 + d_ * 256
                for i in range(ni):
                    pi_ = i % 2
                    k.op("pe", lambda en: en.matmul(PSm[pi_][:, 0:256], lhsT=h2T[d_][:, i:n:ni], rhs=w3[:, c0:c0 + 256], start=True, stop=True), reads=[t_h2, t_w], writes=[TP[pi_]])
                    k.op("dve", lambda en: en.tensor_tensor(out=hh[:], in0=PSm[pi_][:, 0:256], in1=DEC[:, i, :], op=ALU.mult), reads=[TP[pi_], t_dec], writes=[t_hh])
                    k.op("act", lambda en: en.activation(out=UF[d_][:].rearrange("p (g i c) -> p g i c", i=ni, c=cpg)[:, :, i, :], in_=hh[:].rearrange("p (g c) -> p g c", c=cpg), func=AF.Copy), reads=[t_hh], writes=[t_uf])
                    k.op("act", lambda en: en.activation(out=sq[:], in_=hh[:], func=AF.Square), reads=[t_hh], writes=[t_sq])
                    k.op("pool", lambda en: en.tensor_tensor(out=HSQ[:], in0=HSQ[:], in1=sq[:], op=ALU.add), reads=[t_sq, t_hsq], writes=[t_hsq])
            k.op("pe", lambda en: en.matmul(PSm[2][:, 0:256], lhsT=g.onesf[:], rhs=HSQ[:], start=True, stop=True), reads=[t_hsq, g.T_const], writes=[TP[2]])
            k.op("act", lambda en: en.activation(out=rtmp[:], in_=PSm[2][:, 0:256], func=AF.Ln, bias=epsc[:, 0:1]), reads=[TP[2], t_c], writes=[t_rs])
            k.op("act", lambda en: en.activation(out=RS[:], in_=rtmp[:], func=AF.Exp, scale=-0.5), reads=[t_rs], writes=[t_rs])
            k.op("dve", lambda en: en.tensor_tensor(out=rtmp[:].rearrange("p (g c) -> p g c", c=cpg), in0=RS[:].rearrange("p (g c) -> p g c", c=cpg), in1=MSK[:].unsqueeze(1).broadcast_to([128, ng, cpg]), op=ALU.mult), reads=[t_rs, t_c], writes=[t_rs])
            k.op("dve", lambda en: en.tensor_reduce(out=SC[:], in_=rtmp[:].rearrange("p (g c) -> p g c", c=cpg), axis=AX.X, op=ALU.add), reads=[t_rs], writes=[t_rs])
            k.op("dve", lambda en: en.memset(UF[1][0:1, :].rearrange("p (g i c) -> p g i c", i=ni, c=cpg)[:, :, 0, :], 0.0), reads=[t_uf], writes=[t_uf])
            for grp in range(ng):
                k.op("pe", lambda en: en.matmul(PSm[0][:], lhsT=UF[0][:, grp * 128:(grp + 1) * 128], rhs=F1[:, 0, :], start=True, stop=False), reads=[t_uf, t_c], writes=[TP[0]])
                k.op("pe", lambda en: en.matmul(PSm[0][:], lhsT=UF[1][:, grp * 128:(grp + 1) * 128], rhs=F1[:, 1, :], start=False, stop=True), reads=[t_uf, t_c], writes=[TP[0]])
                _hy_twiddle(k, PSm[0][:], TP[0], TW, t_c, m1, m2, t_m, P1[:, 0, :], P1[:, 1, :], t_p1, 256)
                k.op("pe", lambda en: en.matmul(PSm[1][:, 0:256], lhsT=G3[:, 0, :], rhs=P1[:, 0, :], start=True, stop=False), reads=[t_p1, t_c], writes=[TP[1]])
                k.op("pe", lambda en: en.matmul(PSm[1][:, 0:256], lhsT=G3[:, 2, :], rhs=P1[:, 1, :], start=False, stop=True), reads=[t_p1, t_c], writes=[TP[1]])
                k.op("pe", lambda en: en.matmul(PSm[1][:, 256:512], lhsT=G3[:, 0, :], rhs=P1[:, 1, :], start=False, stop=False), reads=[t_p1, t_c], writes=[TP[1]])
                k.op("pe", lambda en: en.matmul(PSm[1][:, 256:512], lhsT=G3[:, 1, :], rhs=P1[:, 0, :], start=False, stop=True), reads=[t_p1, t_c], writes=[TP[1]])
                ko, t_ko = ko_r.next()
                k.op("dve", lambda en: en.tensor_scalar(out=ko[:], in0=PSm[1][:], scalar1=SC[:, grp:grp + 1], scalar2=None, op0=ALU.mult), reads=[TP[1], t_rs], writes=[t_ko])
                k.dma("pool", KH[o, grp], ko[:], reads=[t_ko], writes=[g.T_KHAT])
        k.barrier()


def phase_hyena(g, l, s, cfg, tok0):
    nc, k, W, K = g.nc, g.k, g.W, g.K
    n, ni, ng, cpg, ncol = cfg.n, cfg.ni, cfg.ng, cfg.cpg, cfg.ncol
    KH = g.KHAT[cfg.name]
    with ExitStack() as es:
        sb = lambda nm, s_, d=F32: es.enter_context(nc.sbuf_tensor(_nm(nm), s_, d))
        ps = lambda nm, s_, d=F32: es.enter_context(nc.psum_tensor(_nm(nm), s_, d))
        PSm = [ps("yps%d" % i, [128, 512]) for i in range(6)]
        TP = [Tok(True) for _ in range(6)]
        PT = [ps("ypt%d" % i, [128, 8, 128], BF16) for i in range(2)]
        TPT = [Tok(True) for _ in range(2)]
        t_c = Tok()
        cf = sb("ycf", [128, 1024])
        F1b = sb("yF1", [128, 512], BF16); G3 = sb("yG", [128, 3, 128], BF16); GI = sb("yGI", [128, 2, 256], BF16); FI = sb("yFI", [128, 2, 2, 128], BF16)
        TW = sb("yTW", [128, 2, 512]); TWI = sb("yTWI", [128, 2, 2, 256])
        k.dma("sp", cf[:, 0:512], K["hk_F1"][0], writes=[t_c])
        k.op("dve", lambda en: en.tensor_copy(out=F1b[:], in_=cf[:, 0:512]), reads=[t_c], writes=[t_c])
        k.dma("sp", cf[:, 0:384].rearrange("p (a n) -> p a n", a=3), K["hk_G" + cfg.name].rearrange("a p n -> p a n"), reads=[t_c], writes=[t_c])
        k.op("dve", lambda en: en.tensor_copy(out=G3[:], in_=cf[:, 0:384].rearrange("p (a n) -> p a n", a=3)), reads=[t_c], writes=[t_c])
        k.dma("sp", cf[:, 0:512].rearrange("p (a n) -> p a n", a=2), K["hk_GI" + cfg.name].rearrange("a p n -> p a n"), reads=[t_c], writes=[t_c])
        k.op("dve", lambda en: en.tensor_copy(out=GI[:], in_=cf[:, 0:512].rearrange("p (a n) -> p a n", a=2)), reads=[t_c], writes=[t_c])
        k.dma("sp", cf[:, 0:512].rearrange("p (b a n) -> p b a n", b=2, a=2), K["hk_FI" + cfg.name].rearrange("b a p n -> p b a n"), reads=[t_c], writes=[t_c])
        k.op("dve", lambda en: en.tensor_copy(out=FI[:], in_=cf[:, 0:512].rearrange("p (b a n) -> p b a n", b=2, a=2)), reads=[t_c], writes=[t_c])
        k.dma("sp", TW[:], K["hk_TW" + cfg.name].rearrange("a p n -> p a n"), writes=[t_c])
        k.dma("sp", TWI[:], K["hk_TWI" + cfg.name].rearrange("b a p n -> p b a n"), writes=[t_c])
        cw = sb("ycw", [128, 6, 3]); cb = sb("ycb", [128, 6]); BR = sb("yBR", [128, 2, 256])
        for kk in range(3):
            k.dma("sp", cw[:, :, kk:kk + 1], W["hy_conv_w"][l, kk, :].rearrange("(c p o) -> p c o", p=128, o=1), writes=[t_c], allow_slow_non_contiguous=True)
        k.dma("sp", cb[:].unsqueeze(2), W["hy_conv_b"][l].rearrange("(c p o) -> p c o", p=128, o=1), writes=[t_c], allow_slow_non_contiguous=True)
        k.dma("sp", BR[:], W["hy_bias"][l:l + 1].broadcast_to([128, 2, 256]), writes=[t_c])
        U = {nm: sb("yU" + nm, [128, ncol], BF16) for nm in ("gt", "x2", "v", "x1")}
        t_U = {nm: Tok() for nm in ("gt", "x2", "v", "x1", "z1")}
        names = ["v", "v", "x1", "x1", "x2", "x2", "gt", "gt"]
        with ExitStack() as es1:
            sb1 = lambda nm, s_, d=F32: es1.enter_context(nc.sbuf_tensor(_nm(nm), s_, d))
            hT = sb1("yhT", [128, 8, n], BF16)
            whb_r = Rot([sb1("ywhb%d" % i, [128, 8, 128], BF16) for i in range(2)])
            t_h = Tok()
            k.dma("sp", hT[:], g.HT[:, :, tok0:tok0 + n], reads=[g.T_HT], writes=[t_h])
            PJ = sb1("yPJ", [128, n]); CV = sb1("yCV", [128, n]); CVb = sb1("yCVb", [128, n], BF16)
            t_pj = Tok(); t_cv = Tok(); t_cvb = Tok()
            bw = min(512, n)
            for c8 in range(8):
                whb, t_whb = whb_r.next()
                k.dma("sp", whb[:], g.WINB[:, :, O_HY + c8 * 128:O_HY + (c8 + 1) * 128], reads=[g.T_WINB], writes=[t_whb])
                for bi, b0 in enumerate(range(0, n, bw)):
                    pi_ = bi % 2
                    for kc in range(8):
                        k.op("pe", lambda en: en.matmul(PSm[pi_][:, 0:bw], lhsT=whb[:, kc, :], rhs=hT[:, kc, b0:b0 + bw], start=(kc == 0), stop=(kc == 7)), reads=[t_h, t_whb], writes=[TP[pi_]])
                    if c8 < 6:
                        k.op("act", lambda en: en.activation(out=PJ[:, b0:b0 + bw], in_=PSm[pi_][:, 0:bw], func=AF.Copy), reads=[TP[pi_]], writes=[t_pj])
                    else:
                        k.op("act", lambda en: en.activation(out=CVb[:, b0:b0 + bw], in_=PSm[pi_][:, 0:bw], func=AF.Silu), reads=[TP[pi_]], writes=[t_cvb])
                if c8 < 6:
                    k.op("dve", lambda en: en.tensor_scalar(out=CV[:], in0=PJ[:], scalar1=cw[:, c8, 1:2], scalar2=cb[:, c8:c8 + 1], op0=ALU.mult, op1=ALU.add), reads=[t_pj, t_c], writes=[t_cv])
                    k.op("dve", lambda en: en.scalar_tensor_tensor(out=CV[:, 1:n], in0=PJ[:, 0:n - 1], scalar=cw[:, c8, 0:1], in1=CV[:, 1:n], op0=ALU.mult, op1=ALU.add), reads=[t_pj, t_c, t_cv], writes=[t_cv])
                    k.op("dve", lambda en: en.scalar_tensor_tensor(out=CVb[:, 0:n - 1], in0=PJ[:, 1:n], scalar=cw[:, c8, 2:3], in1=CV[:, 0:n - 1], op0=ALU.mult, op1=ALU.add), reads=[t_pj, t_c, t_cv], writes=[t_cvb])
                    k.op("dve", lambda en: en.tensor_copy(out=CVb[:, n - 1:n], in_=CV[:, n - 1:n]), reads=[t_cv, t_cvb], writes=[t_cvb])
                nm = names[c8]
                g0 = (c8 % 2) * (128 // cpg)
                Uv = U[nm][:].rearrange("p (g i c) -> p g i c", i=ni, c=cpg)
                nb4 = min(4, ni)
                for i0 in range(0, ni, nb4):
                    pt = (i0 // nb4) % 2
                    for ii in range(nb4):
                        k.op("pe", lambda en: en.transpose(PT[pt][:, ii, :], CVb[:, i0 + ii:n:ni], g.ident[:]), reads=[t_cvb, g.T_ident], writes=[TPT[pt]])
                    k.op("act" if (i0 // nb4) % 2 else "dve",
                         (lambda en: en.activation(out=Uv[:, g0:g0 + 128 // cpg, i0:i0 + nb4, :].rearrange("p g i c -> p i g c"), in_=PT[pt][:, 0:nb4, :].rearrange("p i (g c) -> p i g c", c=cpg), func=AF.Copy)) if (i0 // nb4) % 2 else
                         (lambda en: en.tensor_copy(out=Uv[:, g0:g0 + 128 // cpg, i0:i0 + nb4, :].rearrange("p g i c -> p i g c"), in_=PT[pt][:, 0:nb4, :].rearrange("p i (g c) -> p i g c", c=cpg))),
                         reads=[TPT[pt]], writes=[t_U[nm]])
            k.barrier()
        with ExitStack() as es2:
            sb2 = lambda nm, s_, d=F32: es2.enter_context(nc.sbuf_tensor(_nm(nm), s_, d))
            U["z1"] = sb2("yUz1", [128, ncol], BF16)
            BW = [[sb2("yBW%d%d" % (bc, ri), [128, ncol], BF16) for ri in range(2)] for bc in range(2)]
            t_bw = Tok()
            m1 = sb2("ym1", [128, 512]); m2 = sb2("ym2", [128, 512]); t_m = Tok()
            P1 = sb2("yP1", [128, 2, 256], BF16); t_p1 = Tok()
            Y = sb2("yY", [128, 2, 256], BF16); t_y = Tok()
            kh_r = Rot([sb2("ykh%d" % i, [128, 512]) for i in range(2)])
            e1 = sb2("ye1", [128, 512]); e2 = sb2("ye2", [128, 512]); e3 = sb2("ye3", [128, 512]); t_e = Tok()
            gpt = 512 // (ni * cpg)

            def conv(src_nm, o, epilogue):
                Uin = U[src_nm]
                for grp in range(ng):
                    kh, t_kh = kh_r.next()
                    k.dma("sp", kh[:], KH[o, grp], reads=[g.T_KHAT], writes=[t_kh])
                    k.op("pe", lambda en: en.matmul(PSm[0][:], lhsT=Uin[:, grp * 128:(grp + 1) * 128], rhs=F1b[:], start=True, stop=True), reads=[t_U[src_nm], t_c], writes=[TP[0]])
                    _hy_twiddle(k, PSm[0][:], TP[0], TW, t_c, m1, m2, t_m, P1[:, 0, :], P1[:, 1, :], t_p1, 256)
                    k.op("pe", lambda en: en.matmul(PSm[1][:, 0:256], lhsT=G3[:, 0, :], rhs=P1[:, 0, :], start=True, stop=False), reads=[t_p1, t_c], writes=[TP[1]])
                    k.op("pe", lambda en: en.matmul(PSm[1][:, 0:256], lhsT=G3[:, 2, :], rhs=P1[:, 1, :], start=False, stop=True), reads=[t_p1, t_c], writes=[TP[1]])
                    k.op("pe", lambda en: en.matmul(PSm[1][:, 256:512], lhsT=G3[:, 0, :], rhs=P1[:, 1, :], start=False, stop=False), reads=[t_p1, t_c], writes=[TP[1]])
                    k.op("pe", lambda en: en.matmul(PSm[1][:, 256:512], lhsT=G3[:, 1, :], rhs=P1[:, 0, :], start=False, stop=True), reads=[t_p1, t_c], writes=[TP[1]])
                    Xv = PSm[1][:].rearrange("p (a n) -> p a n", a=2)
                    k.op("dve", lambda en: en.tensor_tensor(out=m1[:].rearrange("p (a n) -> p a n", a=2), in0=Xv, in1=kh[:, 0:256].unsqueeze(1).broadcast_to([128, 2, 256]), op=ALU.mult), reads=[TP[1], t_kh, t_m], writes=[t_m])
                    k.op("dve", lambda en: en.tensor_tensor(out=m2[:].rearrange("p (a n) -> p a n", a=2), in0=Xv, in1=kh[:, 256:512].unsqueeze(1).broadcast_to([128, 2, 256]), op=ALU.mult), reads=[TP[1], t_kh, t_m], writes=[t_m])
                    k.op("pool", lambda en: en.tensor_tensor(out=Y[:, 0, :], in0=m1[:, 0:256], in1=m2[:, 256:512], op=ALU.subtract), reads=[t_m], writes=[t_y])
                    k.op("pool", lambda en: en.tensor_tensor(out=Y[:, 1, :], in0=m2[:, 0:256], in1=m1[:, 256:512], op=ALU.add), reads=[t_m, t_y], writes=[t_y])
                    for bc in range(2):
                        pi_ = 2 + bc
                        k.op("pe", lambda en: en.matmul(PSm[pi_][:, 0:256], lhsT=Y[:, 0, bc * 128:(bc + 1) * 128], rhs=GI[:, 0, :], start=True, stop=False), reads=[t_y, t_c], writes=[TP[pi_]])
                        k.op("pe", lambda en: en.matmul(PSm[pi_][:, 0:256], lhsT=Y[:, 1, bc * 128:(bc + 1) * 128], rhs=GI[:, 1, :], start=False, stop=True), reads=[t_y, t_c], writes=[TP[pi_]])
                        k.op("dve", lambda en: en.tensor_tensor(out=m1[:, 0:256], in0=PSm[pi_][:, 0:256], in1=TWI[:, bc, 0, :], op=ALU.mult), reads=[TP[pi_], t_c, t_m], writes=[t_m])
                        k.op("dve", lambda en: en.tensor_tensor(out=m2[:, 0:256], in0=PSm[pi_][:, 0:256], in1=TWI[:, bc, 1, :], op=ALU.mult), reads=[TP[pi_], t_c, t_m], writes=[t_m])
                        k.op("pool", lambda en: en.tensor_tensor(out=BW[bc][0][:, grp * 128:(grp + 1) * 128], in0=m1[:, 0:128], in1=m2[:, 128:256], op=ALU.subtract), reads=[t_m], writes=[t_bw])
                        k.op("pool", lambda en: en.tensor_tensor(out=BW[bc][1][:, grp * 128:(grp + 1) * 128], in0=m2[:, 0:128], in1=m1[:, 128:256], op=ALU.add), reads=[t_m, t_bw], writes=[t_bw])
                for ct in range(ncol // 512):
                    pi_ = 4 + ct % 2
                    cs = slice(ct * 512, (ct + 1) * 512)
                    idx = 0
                    for bc in range(2):
                        for ri in range(2):
                            k.op("pe", lambda en: en.matmul(PSm[pi_][:], lhsT=FI[:, bc, ri, :], rhs=BW[bc][ri][:, cs], start=(idx == 0), stop=(idx == 3)), reads=[t_bw, t_c], writes=[TP[pi_]])
                            idx += 1
                    epilogue(ct, cs, PSm[pi_], TP[pi_], o)

            def v4(ap):
                return ap.rearrange("p (g i c) -> p g i c", i=ni, c=cpg)

            def epi1(ct, cs, ps_, tps, o):
                bview = BR[:, o, ct * gpt * cpg:(ct + 1) * gpt * cpg].rearrange("p (g c) -> p g c", c=cpg).unsqueeze(2).broadcast_to([128, gpt, ni, cpg])
                k.op("pool", lambda en: en.tensor_tensor(out=v4(e1[:]), in0=v4(U["v"][:, cs]), in1=bview, op=ALU.mult), reads=[t_U["v"], t_c, t_e], writes=[t_e])
                k.op("dve", lambda en: en.tensor_tensor(out=e2[:], in0=ps_[:], in1=e1[:], op=ALU.add), reads=[tps, t_e], writes=[t_e])
                k.op("pool", lambda en: en.tensor_tensor(out=U["z1"][:, cs], in0=e2[:], in1=U["x1"][:, cs], op=ALU.mult), reads=[t_e, t_U["x1"]], writes=[t_U["z1"]])

            ZN = U["v"]

            def epi2(ct, cs, ps_, tps, o):
                bview = BR[:, o, ct * gpt * cpg:(ct + 1) * gpt * cpg].rearrange("p (g c) -> p g c", c=cpg).unsqueeze(2).broadcast_to([128, gpt, ni, cpg])
                k.op("pool", lambda en: en.tensor_tensor(out=v4(e1[:]), in0=v4(U["z1"][:, cs]), in1=bview, op=ALU.mult), reads=[t_U["z1"], t_c, t_e], writes=[t_e])
                k.op("dve", lambda en: en.tensor_tensor(out=e2[:], in0=ps_[:], in1=e1[:], op=ALU.add), reads=[tps, t_e], writes=[t_e])
                k.op("pool", lambda en: en.tensor_tensor(out=e3[:], in0=e2[:], in1=U["x2"][:, cs], op=ALU.mult), reads=[t_e, t_U["x2"]], writes=[t_e])
                outv = ZN[:].rearrange("p (i g c) -> p g i c", g=ng, c=cpg)[:, ct * gpt:(ct + 1) * gpt, :, :]
                k.op("dve", lambda en: en.tensor_tensor(out=outv, in0=v4(e3[:]), in1=v4(U["gt"][:, cs]), op=ALU.mult), reads=[t_e, t_U["gt"]], writes=[t_U["v"]])

            conv("v", 0, epi1)
            conv("z1", 1, epi2)
            ZT = U["x1"][:].rearrange("p (h t) -> p h t", h=2)
            cnt = 0
            for ch in range(2):
                nb4 = min(4, ni)
                for i0 in range(0, ni, nb4):
                    pt = cnt % 2
                    cnt += 1
                    for ii in range(nb4):
                        i = i0 + ii
                        k.op("pe", lambda en: en.transpose(PT[pt][:, ii, :], ZN[:, i * 256 + ch * 128:i * 256 + ch * 128 + 128], g.ident[:]), reads=[t_U["v"], g.T_ident], writes=[TPT[pt]])
                    outv = ZT[:, ch, :].rearrange("p (j i) -> p i j", i=ni)[:, i0:i0 + nb4, :]
                    k.op("act" if cnt % 2 else "dve",
                         (lambda en: en.activation(out=outv, in_=PT[pt][:, 0:nb4, :], func=AF.Copy)) if cnt % 2 else (lambda en: en.tensor_copy(out=outv, in_=PT[pt][:, 0:nb4, :])),
                         reads=[TPT[pt], t_U["x1"]], writes=[t_U["x1"]])
            k.dma("pool", g.CATT[:, 6:8, tok0:tok0 + n], ZT, reads=[t_U["x1"]], writes=[g.T_CATT])
            k.barrier()


TC = 16
NCH = T // TC
NDBL = 9


def ssm_host_layouts(inputs):
    f = lambda n: np.asarray(inputs[n], np.float32)
    lr, li, ls = f("ssm_lambda_re"), f("ssm_lambda_im"), f("ssm_log_step")
    br, bi, cr, ci = f("ssm_b_re"), f("ssm_b_im"), f("ssm_c_re"), f("ssm_c_im")
    lam = np.zeros((DEPTH, 128, 2, 16), np.float32)
    lsb = np.zeros((DEPTH, 128, 16), np.float32)
    Bp = np.zeros((DEPTH, 128, 2, 16, 64), np.float32)
    Cp = np.zeros((DEPTH, 128, 2, 16, 64), np.float32)
    for d in range(2):
        for q in range(8):
            for e in range(2):
                grp = 2 * q + e
                rows = slice(e * 64, (e + 1) * 64)
                dq = d * 8 + q
                lam[:, rows, 0, dq] = lr[:, d, grp, :]
                lam[:, rows, 1, dq] = li[:, d, grp, :]
                lsb[:, rows, dq] = ls[:, d, grp][:, None]
                pp = q % 2
                c0 = pp * 32 + e * 16
                Bp[:, rows, 0, dq, c0:c0 + 16] = br[:, d, grp, :, :]
                Bp[:, rows, 1, dq, c0:c0 + 16] = bi[:, d, grp, :, :]
                Cp[:, rows, 0, dq, c0:c0 + 16] = cr[:, d, grp, :, :].transpose(0, 2, 1)
                Cp[:, rows, 1, dq, c0:c0 + 16] = ci[:, d, grp, :, :].transpose(0, 2, 1)
    out = {"ssm_lam": lam, "ssm_ls": lsb, "ssm_Bp": Bp, "ssm_Cp": Cp}
    dd = f("ssm_d")
    gb = f("ssm_glu_b")
    out["ssm_cols"] = np.ascontiguousarray(np.stack([dd.reshape(DEPTH, 2, 128).transpose(0, 2, 1), gb.reshape(DEPTH, 2, 128).transpose(0, 2, 1)], -1))
    out["ssm_glu_w"] = f("ssm_glu_w")
    return out


def phase_ssm_weights(g, l):
    nc, k, W = g.nc, g.k, g.W
    PI = math.pi
    with ExitStack() as es:
        sb = lambda nm, s_, d=F32: es.enter_context(nc.sbuf_tensor(_nm(nm), s_, d))
        ps = lambda nm, s_, d=F32: es.enter_context(nc.psum_tensor(_nm(nm), s_, d))
        t = Tok()
        lam = sb("slam", [128, 2, 16]); ls = sb("sls", [128, 16])
        Bp = sb("sBp", [128, 2, 16, 64]); Cp = sb("sCp", [128, 2, 16, 64])
        k.dma("sp", lam[:], W["ssm_lam"][l], writes=[t])
        k.dma("sp", ls[:], W["ssm_ls"][l], writes=[t])
        k.dma("sp", Bp[:], W["ssm_Bp"][l], writes=[t])
        k.dma("sp", Cp[:], W["ssm_Cp"][l], writes=[t])
        sc = sb("ssc", [128, 24, 16])
        R = lambda i: sc[:, i, :]
        STEP, RHO, TH, MK, SIN, COS, AR, AI, DEN, NR, NI_, CRc, CIc, T1, T2 = range(15)

        def dv(fn, eng="dve"):
            k.op(eng, fn, reads=[t], writes=[t])

        dv(lambda en: en.activation(out=R(STEP), in_=ls[:], func=AF.Exp), "act")
        dv(lambda en: en.tensor_tensor(out=R(T1), in0=lam[:, 0, :], in1=R(STEP), op=ALU.mult))
        dv(lambda en: en.activation(out=R(RHO), in_=R(T1), func=AF.Exp), "act")
        dv(lambda en: en.tensor_tensor(out=R(TH), in0=lam[:, 1, :], in1=R(STEP), op=ALU.mult))
        for thr in (PI, 3 * PI, 5 * PI, 7 * PI):
            dv(lambda en: en.tensor_scalar(out=R(MK), in0=R(TH), scalar1=thr, scalar2=-2 * PI, op0=ALU.is_gt, op1=ALU.mult))
            if thr == PI:
                dv(lambda en: en.tensor_copy(out=R(T1), in_=R(MK)))
            else:
                dv(lambda en: en.tensor_tensor(out=R(T1), in0=R(T1), in1=R(MK), op=ALU.add))
        dv(lambda en: en.tensor_tensor(out=R(TH), in0=R(TH), in1=R(T1), op=ALU.add))
        dv(lambda en: en.activation(out=R(SIN), in_=R(TH), func=AF.Sin), "act")
        dv(lambda en: en.tensor_scalar(out=R(T2), in0=R(TH), scalar1=-1.0, scalar2=None, op0=ALU.mult))
        dv(lambda en: en.tensor_tensor(out=R(T1), in0=R(TH), in1=R(T2), op=ALU.max))
        dv(lambda en: en.tensor_scalar(out=R(T1), in0=R(T1), scalar1=-1.0, scalar2=PI / 2, op0=ALU.mult, op1=ALU.add))
        dv(lambda en: en.activation(out=R(COS), in_=R(T1), func=AF.Sin), "act")
        dv(lambda en: en.tensor_tensor(out=R(AR), in0=R(RHO), in1=R(COS), op=ALU.mult))
        dv(lambda en: en.tensor_tensor(out=R(AI), in0=R(RHO), in1=R(SIN), op=ALU.mult))
        dv(lambda en: en.tensor_tensor(out=R(DEN), in0=lam[:, 0, :], in1=lam[:, 0, :], op=ALU.mult))
        dv(lambda en: en.tensor_tensor(out=R(T1), in0=lam[:, 1, :], in1=lam[:, 1, :], op=ALU.mult))
        dv(lambda en: en.tensor_tensor(out=R(DEN), in0=R(DEN), in1=R(T1), op=ALU.add))
        dv(lambda en: en.reciprocal(out=R(DEN), in_=R(DEN)))
        dv(lambda en: en.tensor_scalar(out=R(T2), in0=R(AR), scalar1=-1.0, scalar2=None, op0=ALU.add))
        dv(lambda en: en.tensor_tensor(out=R(NR), in0=R(T2), in1=lam[:, 0, :], op=ALU.mult))
        dv(lambda en: en.tensor_tensor(out=R(T1), in0=R(AI), in1=lam[:, 1, :], op=ALU.mult))
        dv(lambda en: en.tensor_tensor(out=R(NR), in0=R(NR), in1=R(T1), op=ALU.add))
        dv(lambda en: en.tensor_tensor(out=R(NI_), in0=R(AI), in1=lam[:, 0, :], op=ALU.mult))
        dv(lambda en: en.tensor_tensor(out=R(T1), in0=R(T2), in1=lam[:, 1, :], op=ALU.mult))
        dv(lambda en: en.tensor_tensor(out=R(NI_), in0=R(NI_), in1=R(T1), op=ALU.subtract))
        dv(lambda en: en.tensor_tensor(out=R(CRc), in0=R(NR), in1=R(DEN), op=ALU.mult))
        dv(lambda en: en.tensor_tensor(out=R(CIc), in0=R(NI_), in1=R(DEN), op=ALU.mult))
        PW = sb("sPW", [128, 2, 17, 16])
        dv(lambda en: en.memset(PW[:, 0, 0, :], 1.0))
        dv(lambda en: en.memset(PW[:, 1, 0, :], 0.0))
        for n_ in range(16):
            dv(lambda en: en.tensor_tensor(out=R(T1), in0=PW[:, 0, n_, :], in1=R(AR), op=ALU.mult))
            dv(lambda en: en.tensor_tensor(out=R(T2), in0=PW[:, 1, n_, :], in1=R(AI), op=ALU.mult))
            dv(lambda en: en.tensor_tensor(out=PW[:, 0, n_ + 1, :], in0=R(T1), in1=R(T2), op=ALU.subtract))
            dv(lambda en: en.tensor_tensor(out=R(T1), in0=PW[:, 0, n_, :], in1=R(AI), op=ALU.mult))
            dv(lambda en: en.tensor_tensor(out=R(T2), in0=PW[:, 1, n_, :], in1=R(AR), op=ALU.mult))
            dv(lambda en: en.tensor_tensor(out=PW[:, 1, n_ + 1, :], in0=R(T1), in1=R(T2), op=ALU.add))
        A2 = g.lw.A2
        dv(lambda en: en.tensor_copy(out=A2[:, 0, 0, :], in_=PW[:, 0, 16, :]))
        dv(lambda en: en.tensor_copy(out=A2[:, 0, 1, :], in_=PW[:, 1, 16, :]))
        for kk in range(NDBL):
            if kk > 0:
                dv(lambda en: en.tensor_tensor(out=R(T1), in0=A2[:, kk - 1, 0, :], in1=A2[:, kk - 1, 0, :], op=ALU.mult))
                dv(lambda en: en.tensor_tensor(out=R(T2), in0=A2[:, kk - 1, 1, :], in1=A2[:, kk - 1, 1, :], op=ALU.mult))
                dv(lambda en: en.tensor_tensor(out=A2[:, kk, 0, :], in0=R(T1), in1=R(T2), op=ALU.subtract))
                dv(lambda en: en.tensor_tensor(out=R(T1), in0=A2[:, kk - 1, 0, :], in1=A2[:, kk - 1, 1, :], op=ALU.mult))
                dv(lambda en: en.tensor_scalar(out=A2[:, kk, 1, :], in0=R(T1), scalar1=2.0, scalar2=None, op0=ALU.mult))
            dv(lambda en: en.tensor_scalar(out=A2[:, kk, 2, :], in0=A2[:, kk, 1, :], scalar1=-1.0, scalar2=None, op0=ALU.mult))
        bc3 = lambda ap: ap.unsqueeze(2).broadcast_to([128, 16, 64])
        Bb = sb("sBb", [128, 2, 16, 64])
        big = [sb("sbig%d" % i, [128, 16, 64]) for i in range(4)]
        dv(lambda en: en.tensor_tensor(out=big[0][:], in0=Bp[:, 0], in1=bc3(R(CRc)), op=ALU.mult))
        dv(lambda en: en.tensor_tensor(out=big[1][:], in0=Bp[:, 1], in1=bc3(R(CIc)), op=ALU.mult))
        dv(lambda en: en.tensor_tensor(out=Bb[:, 0], in0=big[0][:], in1=big[1][:], op=ALU.subtract))
        dv(lambda en: en.tensor_tensor(out=big[0][:], in0=Bp[:, 1], in1=bc3(R(CRc)), op=ALU.mult))
        dv(lambda en: en.tensor_tensor(out=big[1][:], in0=Bp[:, 0], in1=bc3(R(CIc)), op=ALU.mult))
        dv(lambda en: en.tensor_tensor(out=Bb[:, 1], in0=big[0][:], in1=big[1][:], op=ALU.add))
        Bbb = sb("sBbb", [128, 2, 16, 64], BF16)
        dv(lambda en: en.tensor_copy(out=Bbb[:], in_=Bb[:]))
        CR = sb("sCR", [128, 2, 16, 17, 64], BF16)
        engs = ("dve", "pool")
        for n_ in range(17):
            e1 = engs[n_ % 2]
            dv(lambda en: en.tensor_tensor(out=big[0][:], in0=Cp[:, 0], in1=bc3(PW[:, 0, n_, :]), op=ALU.mult), e1)
            dv(lambda en: en.tensor_tensor(out=big[1][:], in0=Cp[:, 1], in1=bc3(PW[:, 1, n_, :]), op=ALU.mult), e1)
            dv(lambda en: en.tensor_tensor(out=CR[:, 0, :, n_, :], in0=big[0][:], in1=big[1][:], op=ALU.subtract), e1)
            dv(lambda en: en.tensor_tensor(out=big[2][:], in0=Cp[:, 0], in1=bc3(PW[:, 1, n_, :]), op=ALU.mult), e1)
            dv(lambda en: en.tensor_tensor(out=big[3][:], in0=Cp[:, 1], in1=bc3(PW[:, 0, n_, :]), op=ALU.mult), e1)
            dv(lambda en: en.tensor_tensor(out=big[2][:], in0=big[2][:], in1=big[3][:], op=ALU.add), e1)
            dv(lambda en: en.tensor_scalar(out=CR[:, 1, :, n_, :], in0=big[2][:], scalar1=-1.0, scalar2=None, op0=ALU.mult), e1)
        k.dma("pool", g.SSM_L3, CR[:], reads=[t], writes=[g.T_SSMW])
        L1W = sb("sL1W", [128, 2, 16, 2, 128], BF16)
        dv(lambda en: en.memset(L1W[:], 0.0), "pool")
        PS_ = [ps("sps%d" % i, [128, 512]) for i in range(2)]
        TPS_ = [Tok(True) for _ in range(2)]
        for d in range(2):
            for Q in range(4):
                hc, Ql = Q // 2, Q % 2
                for half in range(2):
                    first = True
                    for pp in range(2):
                        dq = d * 8 + 2 * Q + pp
                        for ri in range(2):
                            rhs = CR[:, ri, dq, half * 8:(half + 1) * 8, :].rearrange("p n c -> p (n c)")
                            k.op("pe", lambda en: en.matmul(PS_[half][Ql * 64:(Ql + 1) * 64, :], lhsT=Bbb[:, ri, dq, :], rhs=rhs, start=first, stop=(pp == 1 and ri == 1)), reads=[t], writes=[TPS_[half]])
                            first = False
                    k.op("act" if half else "dve",
                         (lambda en: en.activation(out=L1W[Ql * 64:(Ql + 1) * 64, d, half * 8:(half + 1) * 8, hc, Ql * 64:(Ql + 1) * 64], in_=PS_[half][Ql * 64:(Ql + 1) * 64, :].rearrange("p (n c) -> p n c", c=64), func=AF.Copy)) if half else
                         (lambda en: en.tensor_copy(out=L1W[Ql * 64:(Ql + 1) * 64, d, half * 8:(half + 1) * 8, hc, Ql * 64:(Ql + 1) * 64], in_=PS_[half][Ql * 64:(Ql + 1) * 64, :].rearrange("p (n c) -> p n c", c=64))),
                         reads=[TPS_[half], t], writes=[t])
        k.dma("pool", g.SSM_L1, L1W[:], reads=[t], writes=[g.T_SSMW])
        ZZ = sb("sZZ", [128, 2, 16, 64], BF16)
        PT = [ps("spt%d" % i, [128, 8, 128], BF16) for i in range(2)]
        TPT = [Tok(True) for _ in range(2)]
        sg_r = Rot([sb("ssg%d" % i, [128, 2, 2, 2, 128], BF16) for i in range(2)])
        for n_ in range(16):
            dv(lambda en: en.tensor_tensor(out=big[0][:], in0=Bb[:, 0], in1=bc3(PW[:, 0, n_, :]), op=ALU.mult))
            dv(lambda en: en.tensor_tensor(out=big[1][:], in0=Bb[:, 1], in1=bc3(PW[:, 1, n_, :]), op=ALU.mult))
            dv(lambda en: en.tensor_tensor(out=ZZ[:, 0], in0=big[0][:], in1=big[1][:], op=ALU.subtract))
            dv(lambda en: en.tensor_tensor(out=big[2][:], in0=Bb[:, 0], in1=bc3(PW[:, 1, n_, :]), op=ALU.mult), "pool")
            dv(lambda en: en.tensor_tensor(out=big[3][:], in0=Bb[:, 1], in1=bc3(PW[:, 0, n_, :]), op=ALU.mult), "pool")
            dv(lambda en: en.tensor_tensor(out=ZZ[:, 1], in0=big[2][:], in1=big[3][:], op=ALU.add), "pool")
            for d in range(2):
                sg, t_sg = sg_r.next()
                for hc in range(2):
                    pt = hc
                    for ri in range(2):
                        for Ql in range(2):
                            for pp in range(2):
                                dq = d * 8 + 2 * (hc * 2 + Ql) + pp
                                k.op("pe", lambda en: en.transpose(PT[pt][Ql * 64:(Ql + 1) * 64, ri * 2 + pp, :], ZZ[:, ri, dq, :], g.ident[:]), reads=[t, g.T_ident], writes=[TPT[pt]])
                    k.op("act" if hc else "dve",
                         (lambda en: en.activation(out=sg[:, hc].rearrange("p r q n -> p (r q) n"), in_=PT[pt][:, 0:4, :], func=AF.Copy)) if hc else
                         (lambda en: en.tensor_copy(out=sg[:, hc].rearrange("p r q n -> p (r q) n"), in_=PT[pt][:, 0:4, :])),
                         reads=[TPT[pt]], writes=[t_sg])
                k.dma("pool", g.SSM_SG[:, d, n_], sg[:], reads=[t_sg], writes=[g.T_SSMW])
        k.barrier()


def phase_ssm(g, l, s, need_ctx):
    nc, k, W, lw = g.nc, g.k, g.W, g.lw
    A2 = lw.A2
    with ExitStack() as es:
        sb = lambda nm, s_, d=F32: es.enter_context(nc.sbuf_tensor(_nm(nm), s_, d))
        ps = lambda nm, s_, d=F32: es.enter_context(nc.psum_tensor(_nm(nm), s_, d))
        PSm = [ps("mps%d" % i, [128, 512]) for i in range(8)]
        TP = [Tok(True) for _ in range(8)]
        ujm = sb("mujm", [128, 2, TC, NCH], BF16)
        gjm = sb("mgjm", [128, 2, TC, NCH], BF16)
        Sb = [[sb("mSb%d%d" % (d, ri), [128, 8, NCH], BF16) for ri in range(2)] for d in range(2)]
        t_u = Tok(); t_g = Tok(); t_sb = Tok()
        cols = sb("mcols", [128, 2, 2])
        GW = sb("mGW", [128, 2, 256], BF16)
        t_c = Tok()
        k.dma("sp", cols[:], W["ssm_cols"][l], writes=[t_c])
        with ExitStack() as es1:
            sb1 = lambda nm, s_, d=F32: es1.enter_context(nc.sbuf_tensor(_nm(nm), s_, d))
            wss = sb1("mwss", [128, 8, 512], BF16)
            gwf = sb1("mgwf", [128, 2, 256])
            t_w = Tok()
            k.dma("sp", wss[:], g.WINB[:, :, O_SU:O_SU + 512], reads=[g.T_WINB], writes=[t_w])
            k.dma("sp", gwf[:], W["ssm_glu_w"][l].rearrange("(kc p) n -> p kc n", p=128), writes=[t_w])
            k.op("dve", lambda en: en.tensor_copy(out=GW[:], in_=gwf[:]), reads=[t_w], writes=[t_c])
            hT_r = Rot([sb1("mhT%d" % i, [128, 8, 512], BF16) for i in range(2)])
            blocks = [(0, 256)] + [(256 + 512 * b, 512) for b in range(8)]
            cnt = 0
            for (t0, nb) in blocks:
                hT, t_h = hT_r.next()
                k.dma("sp", hT[:, :, 0:nb], g.HT[:, :, t0:t0 + nb], reads=[g.T_HT], writes=[t_h])
                c0, ncb = t0 // TC, nb // TC
                for cc in range(4):
                    pi_ = cnt % 4
                    cnt += 1
                    for kc in range(8):
                        k.op("pe", lambda en: en.matmul(PSm[pi_][:, 0:nb], lhsT=wss[:, kc, cc * 128:(cc + 1) * 128], rhs=hT[:, kc, 0:nb], start=(kc == 0), stop=(kc == 7)), reads=[t_w, t_h], writes=[TP[pi_]])
                    src = PSm[pi_][:, 0:nb].rearrange("p (c j) -> p j c", j=TC)
                    if cc < 2:
                        k.op("dve", lambda en: en.tensor_copy(out=ujm[:, cc, :, c0:c0 + ncb], in_=src), reads=[TP[pi_]], writes=[t_u])
                    else:
                        k.op("act", lambda en: en.activation(out=gjm[:, cc - 2, :, c0:c0 + ncb], in_=src, func=AF.Silu), reads=[TP[pi_]], writes=[t_g])
            k.barrier()
        with ExitStack() as es2:
            sb2 = lambda nm, s_, d=F32: es2.enter_context(nc.sbuf_tensor(_nm(nm), s_, d))
            SG = sb2("mSG", [128, 2, 16, 2, 2, 2, 128], BF16)
            t_sg = Tok()
            k.dma("sp", SG[:, 0], g.SSM_SG[:, 0], reads=[g.T_SSMW], writes=[t_sg])
            k.dma("sp", SG[:, 1], g.SSM_SG[:, 1], reads=[g.T_SSMW], writes=[t_sg])
            S = [[sb2("mS%d%d" % (d, ri), [128, 8, NCH]) for ri in range(2)] for d in range(2)]
            t_S = [[Tok() for q in range(8)] for d in range(2)]
            Tt = [[sb2("mT%d%d" % (i, ri), [128, NCH]) for ri in range(2)] for i in range(2)]
            t_T = [Tok(), Tok()]
            cnt = 0
            for d in range(2):
                for q in range(8):
                    Q, pp = q // 2, q % 2
                    hc, Ql = Q // 2, Q % 2
                    rows = slice(Ql * 64, (Ql + 1) * 64)
                    for ri in range(2):
                        pi_ = cnt % 4
                        cnt += 1
                        for i in range(TC):
                            n_ = (TC - 1 - i) if d == 0 else i
                            k.op("pe", lambda en: en.matmul(PSm[pi_][:, 0:NCH], lhsT=SG[rows, d, n_, hc, ri, pp, :], rhs=ujm[rows, hc, i, :], start=(i == 0), stop=(i == TC - 1)), reads=[t_sg, t_u], writes=[TP[pi_]])
                        if d == 0:
                            k.op("act" if ri else "dve", (lambda en: en.activation(out=S[d][ri][:, q, :], in_=PSm[pi_][:, 0:NCH], func=AF.Copy)) if ri else (lambda en: en.tensor_copy(out=S[d][ri][:, q, :], in_=PSm[pi_][:, 0:NCH])), reads=[TP[pi_]], writes=[t_S[d][q]])
                        else:
                            k.op("dve", lambda en: en.tensor_copy(out=S[d][ri][:, q, 0:256], in_=PSm[pi_][:, 16:NCH]), reads=[TP[pi_]], writes=[t_S[d][q]])
                            k.op("act", lambda en: en.activation(out=S[d][ri][:, q, 256:NCH], in_=PSm[pi_][:, 0:16], func=AF.Copy), reads=[TP[pi_]], writes=[t_S[d][q]])
            for kk in range(NDBL):
                sh = 1 << kk
                w_ = NCH - sh
                it = 0
                for d in range(2):
                    dst = slice(sh, NCH) if d == 0 else slice(0, w_)
                    srcs = slice(0, w_) if d == 0 else slice(sh, NCH)
                    for q in range(8):
                        dq = d * 8 + q
                        Ar, Ai, nAi = A2[:, kk, 0, dq:dq + 1], A2[:, kk, 1, dq:dq + 1], A2[:, kk, 2, dq:dq + 1]
                        Tr, Ti = Tt[it % 2]
                        tT = t_T[it % 2]
                        it += 1
                        Sr, Si = S[d][0], S[d][1]
                        ts = t_S[d][q]
                        k.op("pool", lambda en: en.tensor_scalar(out=Tr[:, 0:w_], in0=Sr[:, q, srcs], scalar1=Ar, scalar2=None, op0=ALU.mult), reads=[ts, lw.tok], writes=[tT])
                        k.op("dve", lambda en: en.scalar_tensor_tensor(out=Tr[:, 0:w_], in0=Si[:, q, srcs], scalar=nAi, in1=Tr[:, 0:w_], op0=ALU.mult, op1=ALU.add), reads=[ts, tT, lw.tok], writes=[tT])
                        k.op("pool", lambda en: en.tensor_scalar(out=Ti[:, 0:w_], in0=Si[:, q, srcs], scalar1=Ar, scalar2=None, op0=ALU.mult), reads=[ts, lw.tok, tT], writes=[tT])
                        k.op("dve", lambda en: en.scalar_tensor_tensor(out=Ti[:, 0:w_], in0=Sr[:, q, srcs], scalar=Ai, in1=Ti[:, 0:w_], op0=ALU.mult, op1=ALU.add), reads=[ts, tT, lw.tok], writes=[tT])
                        k.op("pool", lambda en: en.tensor_tensor(out=Sr[:, q, dst], in0=Sr[:, q, dst], in1=Tr[:, 0:w_], op=ALU.add), reads=[tT, ts], writes=[ts])
                        k.op("pool", lambda en: en.tensor_tensor(out=Si[:, q, dst], in0=Si[:, q, dst], in1=Ti[:, 0:w_], op=ALU.add), reads=[tT, ts], writes=[ts])
            for d in range(2):
                for ri in range(2):
                    k.op("act" if ri else "dve", (lambda en: en.activation(out=Sb[d][ri][:], in_=S[d][ri][:], func=AF.Copy)) if ri else (lambda en: en.tensor_copy(out=Sb[d][ri][:], in_=S[d][ri][:])), reads=[t_S[d][q] for q in range(8)], writes=[t_sb])
            k.barrier()
        with ExitStack() as es3:
            sb3 = lambda nm, s_, d=F32: es3.enter_context(nc.sbuf_tensor(_nm(nm), s_, d))
            L3 = sb3("mL3", [128, 2, 16, 17, 64], BF16)
            L1W = sb3("mL1W", [128, 2, 16, 2, 128], BF16)
            t_l = Tok()
            k.dma("sp", L3[:, 0], g.SSM_L3[:, 0], reads=[g.T_SSMW], writes=[t_l])
            k.dma("sp", L3[:, 1], g.SSM_L3[:, 1], reads=[g.T_SSMW], writes=[t_l])
            k.dma("sp", L1W[:], g.SSM_L1, reads=[g.T_SSMW], writes=[t_l])
            Yjm = sb3("mYjm", [128, 2, TC, NCH])
            t_y = Tok()
            cnt = 0
            for j in range(TC):
                for hc in range(2):
                    pi_ = cnt % 4
                    cnt += 1
                    yp = PSm[pi_]
                    first = True
                    for tau in range(j + 1):
                        k.op("pe", lambda en: en.matmul(yp[:, 0:NCH], lhsT=L1W[:, 0, tau, hc, :], rhs=ujm[:, hc, j - tau, :], start=first, stop=False), reads=[t_l, t_u], writes=[TP[pi_]])
                        first = False
                    for tau in range(TC - j):
                        k.op("pe", lambda en: en.matmul(yp[:, 0:NCH], lhsT=L1W[:, 1, tau, hc, :], rhs=ujm[:, hc, j + tau, :], start=False, stop=False), reads=[t_l, t_u], writes=[TP[pi_]])
                    for Ql in range(2):
                        rows = slice(Ql * 64, (Ql + 1) * 64)
                        for pp in range(2):
                            q = 2 * (2 * hc + Ql) + pp
                            for ri in range(2):
                                k.op("pe", lambda en: en.matmul(yp[rows, 1:NCH], lhsT=L3[:, ri, q, j + 1, :], rhs=Sb[0][ri][:, q, 0:NCH - 1], start=False, stop=False), reads=[t_l, t_sb], writes=[TP[pi_]])
                                k.op("pe", lambda en: en.matmul(yp[rows, 16:NCH], lhsT=L3[:, ri, 8 + q, TC - j, :], rhs=Sb[1][ri][:, q, 1:257], start=False, stop=False), reads=[t_l, t_sb], writes=[TP[pi_]])
                                lastm = (Ql == 1 and pp == 1 and ri == 1)
                                k.op("pe", lambda en: en.matmul(yp[rows, 0:15], lhsT=L3[:, ri, 8 + q, TC - j, :], rhs=Sb[1][ri][:, q, 257:NCH], start=False, stop=lastm), reads=[t_l, t_sb], writes=[TP[pi_]])
                    k.op("dve", lambda en: en.scalar_tensor_tensor(out=Yjm[:, hc, j, :], in0=ujm[:, hc, j, :], scalar=cols[:, hc, 0:1], in1=yp[:, 0:NCH], op0=ALU.mult, op1=ALU.add), reads=[TP[pi_], t_u, t_c], writes=[t_y])
            k.barrier()
            FL = TC * NCH
            Yf = [Yjm[:, hc].rearrange("p j c -> p (j c)") for hc in range(2)]
            Gf = [gjm[:, hc].rearrange("p j c -> p (j c)") for hc in range(2)]
            Zb = ujm
            Zf = [Zb[:, hc].rearrange("p j c -> p (j c)") for hc in range(2)]
            w1 = sb3("mw1", [128, 512]); w2 = sb3("mw2", [128, 512]); w3 = sb3("mw3", [128, 2, 512])
            t_w1 = Tok(); t_z = Tok(); t_w3 = Tok()
            CG = 1.5957691216057308
            pieces = [(c0, min(512, FL - c0)) for c0 in range(0, FL, 512)]
            for (c0, w_) in pieces:
                cs = slice(c0, c0 + w_)
                for hc in range(2):
                    k.op("pool", lambda en: en.tensor_tensor(out=w1[:, 0:w_], in0=Yf[hc][:, cs], in1=Yf[hc][:, cs], op=ALU.mult), reads=[t_y, t_w1], writes=[t_w1])
                    k.op("dve", lambda en: en.tensor_scalar(out=w1[:, 0:w_], in0=w1[:, 0:w_], scalar1=0.044715, scalar2=1.0, op0=ALU.mult, op1=ALU.add), reads=[t_w1], writes=[t_w1])
                    k.op("pool", lambda en: en.tensor_tensor(out=w1[:, 0:w_], in0=w1[:, 0:w_], in1=Yf[hc][:, cs], op=ALU.mult), reads=[t_w1, t_y], writes=[t_w1])
                    k.op("act", lambda en: en.activation(out=w2[:, 0:w_], in_=w1[:, 0:w_], func=AF.Sigmoid, scale=CG), reads=[t_w1], writes=[t_w1])
                    k.op("dve", lambda en: en.tensor_tensor(out=w3[:, hc, 0:w_], in0=w2[:, 0:w_], in1=Yf[hc][:, cs], op=ALU.mult), reads=[t_w1, t_y, t_w3], writes=[t_w3])
                    k.op("pool", lambda en: en.tensor_copy(out=Zf[hc][:, cs], in_=w3[:, hc, 0:w_]), reads=[t_w3, t_u], writes=[t_z])
                for oc in range(2):
                    pi_ = 4 + oc
                    for kc in range(2):
                        k.op("pe", lambda en: en.matmul(PSm[pi_][:, 0:w_], lhsT=GW[:, kc, oc * 128:(oc + 1) * 128], rhs=Zf[kc][:, cs], start=(kc == 0), stop=(kc == 1)), reads=[t_z, t_c], writes=[TP[pi_]])
                    k.op("act", lambda en: en.activation(out=w2[:, 0:w_], in_=PSm[pi_][:, 0:w_], func=AF.Sigmoid, bias=cols[:, oc, 1:2]), reads=[TP[pi_], t_c, t_w1], writes=[t_w1])
                    k.op("dve", lambda en: en.tensor_tensor(out=w2[:, 0:w_], in0=w2[:, 0:w_], in1=w3[:, oc, 0:w_], op=ALU.mult), reads=[t_w1, t_w3], writes=[t_w1])
                    k.op("pool", lambda en: en.tensor_tensor(out=Gf[oc][:, cs], in0=w2[:, 0:w_], in1=Gf[oc][:, cs], op=ALU.mult), reads=[t_w1, t_g], writes=[t_g])
            on_t = sb3("mON", [128, 2, T], BF16)
            t_on = Tok()
            for hc in range(2):
                k.op("dve" if hc else "pool", lambda en: en.tensor_copy(out=on_t[:, hc, :].rearrange("p (c j) -> p j c", j=TC), in_=gjm[:, hc]), reads=[t_g], writes=[t_on])
            if need_ctx:
                k.dma("pool", g.CATT[:, 4:6, :], on_t[:], reads=[t_on], writes=[g.T_CATT])
            else:
                k.dma("pool", g.CATT[:, 4:6, C:T], on_t[:, :, C:T], reads=[t_on], writes=[g.T_CATT])
            k.barrier()
```

```python
import math
import numpy as np
from contextlib import ExitStack
import concourse.bass as bass
import concourse.mybir as mybir
from concourse.bass_utils import run_bass_kernel_spmd

F32 = mybir.dt.float32
BF16 = mybir.dt.bfloat16
AF = mybir.ActivationFunctionType
ALU = mybir.AluOpType
AX = mybir.AxisListType

D = 1024
L = 4096
C = 256
T = L + C
NT = T // 128
DEPTH = 4
EPS = 1e-6
NCORES = 8

O_CQ, O_CKV, O_KR, O_GM = 0, 192, 320, 352
O1 = 608
O_GQ, O_GK, O_GV, O_GG = O1, O1 + 256, O1 + 384, O1 + 512
O2 = O1 + 768
O_SU, O_SG = O2, O2 + 256
O3 = O2 + 512
O_HY, O_HG = O3, O3 + 768
NIN = 2912
O_KRP = NIN
O_QM = O_KRP + 32
O_QP = O_QM + 256
O_KP = O_QP + 256
NCB = O_KP + 128

EPOCH = 30000
NDMASLOT = 8


class Tok:
    __slots__ = ("w", "r", "excl")

    def __init__(self, excl=False):
        self.w = []
        self.r = []
        self.excl = excl


class KB:
    def __init__(self, nc, es):
        self.nc = nc
        self.es = es
        self.eng = {"pe": nc.tensor, "act": nc.scalar, "dve": nc.vector, "pool": nc.gpsimd, "sp": nc.sync}
        self.cnt = {e: 0 for e in self.eng}
        self.epoch = {e: 0 for e in self.eng}
        self.sems = {}
        self.seen = {e: {} for e in self.eng}
        self.dma_slots = {}
        self.dma_rr = {e: 0 for e in self.eng}
        self.ninst = 0

    def _sem(self, key):
        if key not in self.sems:
            self.sems[key] = self.es.enter_context(self.nc.semaphore("s_%s_%s" % key))
        return self.sems[key]

    def _wait(self, e, ev):
        key, val = ev
        if self.seen[e].get(key, 0) >= val:
            return
        self.eng[e].wait_ge(self._sem(key), val)
        self.seen[e][key] = val

    def _deps(self, e, reads, writes):
        best = {}

        def add(k_, v):
            if best.get(k_, 0) < v:
                best[k_] = v
        for t in reads:
            for k_, v in t.w:
                add(k_, v)
            if t.excl:
                for k_, v in t.r:
                    if k_[0] != e:
                        add(k_, v)
        for t in writes:
            for k_, v in t.w:
                if k_[0] != e:
                    add(k_, v)
            for k_, v in t.r:
                if k_[0] != e:
                    add(k_, v)
        for k_, v in best.items():
            if e == "pe" and k_[0] == "pe":
                continue
            self._wait(e, (k_, v))

    def _record(self, ev, reads, writes):
        for t in reads:
            t.r.append(ev)
            if len(t.r) > 16:
                best = {}
                for k_, v in t.r:
                    if best.get(k_, 0) < v:
                        best[k_] = v
                t.r = list(best.items())
        for t in writes:
            t.w = [ev]
            t.r = []

    def op(self, e, fn, reads=(), writes=()):
        self._deps(e, reads, writes)
        if self.cnt[e] >= EPOCH:
            self.epoch[e] += 1
            self.cnt[e] = 0
        key = (e, self.epoch[e])
        ins = fn(self.eng[e])
        self.cnt[e] += 1
        ins.then_inc(self._sem(key), 1)
        ev = (key, self.cnt[e])
        self._record(ev, reads, writes)
        self.ninst += 1
        return ev

    def dma(self, e, out, in_, reads=(), writes=(), **kw):
        self._deps(e, reads, writes)
        if e not in self.dma_slots:
            self.dma_slots[e] = [[("d" + e, i), 0] for i in range(NDMASLOT)]
        i = self.dma_rr[e]
        self.dma_rr[e] = (i + 1) % NDMASLOT
        slot = self.dma_slots[e][i]
        key = slot[0]
        if slot[1] > 0:
            self._wait(e, (key, 16 * slot[1]))
        slot[1] += 1
        ins = self.eng[e].dma_start(out=out, in_=in_, **kw)
        ins.then_inc(self._sem(key), 16)
        ev = (key, 16 * slot[1])
        self._record(ev, reads, writes)
        self.ninst += 1
        return ev

    def all_events(self):
        evs = []
        for e in self.eng:
            for ep in range(self.epoch[e] + 1):
                v = self.cnt[e] if ep == self.epoch[e] else EPOCH
                if v > 0:
                    evs.append(((e, ep), v))
        for e, slots in self.dma_slots.items():
            for key, uses in slots:
                if uses:
                    evs.append((key, 16 * uses))
        return evs

    def barrier(self):
        evs = self.all_events()
        for e in ("pe", "act", "dve", "pool", "sp"):
            for ev in evs:
                if ev[0][0] == e:
                    continue
                self._wait(e, ev)

    def drain(self, e="sp"):
        for ev in self.all_events():
            self._wait(e, ev)


_NMC = [0]


def _nm(n):
    _NMC[0] += 1
    return "%s_%d" % (n, _NMC[0])


class Rot:
    def __init__(self, tiles, excl=False):
        self.tiles = [(t, Tok(excl)) for t in tiles]
        self.i = 0

    def next(self):
        r = self.tiles[self.i]
        self.i = (self.i + 1) % len(self.tiles)
        return r


def _rope_table(d):
    hh = d // 2
    qq = hh // 2
    inv = (np.float32(10000.0) ** (-np.arange(0, hh, 2, dtype=np.float32) / np.float32(hh))).astype(np.float32)
    t = np.arange(L)
    row = (t // 64).astype(np.float32)
    col = (t % 64).astype(np.float32)
    cos = np.ones((d, T), np.float32)
    sin = np.zeros((d, T), np.float32)
    for i in range(d):
        hf, within = divmod(i, hh)
        fi = within % qq
        pos = row if hf == 0 else col
        ang = (pos * inv[fi]).astype(np.float32)
        cos[i, C:] = np.cos(ang).astype(np.float32)
        sin[i, C:] = np.sin(ang).astype(np.float32)
    return cos, sin


def _partner_index(d):
    hh = d // 2
    qq = hh // 2
    idx = np.zeros(d, np.int64)
    sg = np.zeros(d, np.float32)
    for i in range(d):
        hf, within = divmod(i, hh)
        if within < qq:
            idx[i] = i + qq
            sg[i] = -1.0
        else:
            idx[i] = i - qq
            sg[i] = 1.0
    return idx, sg


def host_constants():
    cst = {}
    cst["k_ident"] = np.eye(128, dtype=np.float32)
    c32, s32 = _rope_table(32)
    c64, s64 = _rope_table(64)
    mc = np.ones((128, T), np.float32)
    ms = np.zeros((128, T), np.float32)
    mc[64:96] = c32
    ms[64:96] = s32
    cst["k_ropeM"] = np.stack([mc, ms], 0)
    cst["k_ropeG"] = np.stack([np.concatenate([c64, c64], 0), np.concatenate([s64, s64], 0)], 0)
    cst.update(hyena_constants())
    return cst


class G:
    pass


def build_program(layers, final_lat_only, dbg=None):
    nc = bass.Bass("TRN2", target_bir_lowering=False)
    g = G()
    g.nc = nc
    g.dbg = dbg or {}

    def din(name, shape, dt=F32):
        return nc.dram_tensor(name, list(shape), dt, kind="ExternalInput").ap()

    def dscr(name, shape, dt=F32):
        return nc.dram_tensor(name, list(shape), dt).ap()

    g.xs = din("xs", [2, T, D])
    g.cT = din("cT", [128, 8, 3])
    W = {}
    W["w_mod"] = din("w_mod", [DEPTH, D, 3 * D])
    W["b_mod"] = din("b_mod", [DEPTH, 3 * D])
    W["g_pre"] = din("g_pre", [DEPTH, D])
    W["g_post"] = din("g_post", [DEPTH, D])
    W["w_in"] = din("w_in", [DEPTH, D, NIN])
    W["w_out"] = din("w_out", [DEPTH, D, D])
    W["mla_g_cq"] = din("mla_g_cq", [DEPTH, 192])
    W["mla_w_uq"] = din("mla_w_uq", [DEPTH, 192, 384])
    W["mla_g_ckv"] = din("mla_g_ckv", [DEPTH, 128])
    W["mla_w_ukv"] = din("mla_w_ukv", [DEPTH, 128, 512])
    W["gq_cols"] = din("gq_cols", [DEPTH, 128, 4])
    for nm, shp in (("hy_conv_w", [DEPTH, 3, 768]), ("hy_conv_b", [DEPTH, 768]), ("hy_f_w1", [DEPTH, 33, 64]), ("hy_f_b1", [DEPTH, 64]),
                    ("hy_f_freq1", [DEPTH, 64]), ("hy_f_w2", [DEPTH, 64, 64]), ("hy_f_b2", [DEPTH, 64]), ("hy_f_freq2", [DEPTH, 64]),
                    ("hy_f_w3", [DEPTH, 64, 1024]), ("hy_bias", [DEPTH, 2, 256])):
        W[nm] = din(nm, shp)
    for nm, shp in (("ssm_lam", [DEPTH, 128, 2, 16]), ("ssm_ls", [DEPTH, 128, 16]), ("ssm_Bp", [DEPTH, 128, 2, 16, 64]), ("ssm_Cp", [DEPTH, 128, 2, 16, 64]),
                    ("ssm_cols", [DEPTH, 128, 2, 2]), ("ssm_glu_w", [DEPTH, 256, 256])):
        W[nm] = din(nm, shp)
    g.W = W
    K = {}
    K["k_ident"] = din("k_ident", [128, 128])
    K["k_ropeM"] = din("k_ropeM", [2, 128, T])
    K["k_ropeG"] = din("k_ropeG", [2, 128, T])
    for nm, arr in hyena_constants().items():
        K[nm] = din(nm, list(arr.shape))
    g.K = K
    if final_lat_only:
        g.y = nc.dram_tensor("y", [2, L, D], F32, kind="ExternalOutput").ap()
    else:
        g.y = nc.dram_tensor("y", [2, T, D], F32, kind="ExternalOutput").ap()
    for name, (shape, dt) in g.dbg.items():
        g.dbg[name] = nc.dram_tensor(name, list(shape), dt, kind="ExternalOutput").ap()

    g.XS = dscr("XS", [2, T, D]) if len(layers) > 1 else None
    g.WINB = dscr("WINB", [128, 8, NCB], BF16)
    g.WOUTB = dscr("WOUTB", [128, 8, D], BF16)
    g.MODROWS = dscr("MODROWS", [3, 3 * D])
    g.HT = dscr("HT", [128, 8, T], BF16)
    g.CATT = dscr("CATT", [128, 8, T], BF16)
    g.KHAT = {"L": dscr("KHATL", [2, HY_LAT.ng, 128, 512]), "C": dscr("KHATC", [2, HY_CTX.ng, 128, 512])}
    g.T_KHAT = Tok()
    g.SSM_L3 = dscr("SSM_L3", [128, 2, 16, 17, 64], BF16)
    g.SSM_L1 = dscr("SSM_L1", [128, 2, 16, 2, 128], BF16)
    g.SSM_SG = dscr("SSM_SG", [128, 2, 16, 2, 2, 2, 128], BF16)
    g.T_SSMW = Tok()
    g.T_XS = [Tok(), Tok()]
    g.T_WINB = Tok()
    g.T_WOUTB = Tok()
    g.T_MOD = Tok()
    g.T_HT = Tok()
    g.T_CATT = Tok()
    g.T_Y = Tok()

    with ExitStack() as es:
        k = KB(nc, es)
        g.k = k
        g.es = es
        g.ident = es.enter_context(nc.sbuf_tensor("ident", [128, 128], BF16))
        g.T_ident = Tok()
        g.onesf = es.enter_context(nc.sbuf_tensor("onesf", [128, 128], F32))
        g.ones128 = es.enter_context(nc.sbuf_tensor("ones128", [128, 128], BF16))
        g.ones192 = es.enter_context(nc.sbuf_tensor("ones192", [128, 128], BF16))
        g.blk64 = es.enter_context(nc.sbuf_tensor("blk64", [128, 128], BF16))
        g.T_const = Tok()
        with ExitStack() as es2:
            tmp = es2.enter_context(nc.sbuf_tensor("idtmp", [128, 128], F32))
            tt = Tok()
            k.dma("sp", tmp[:], K["k_ident"], writes=[tt])
            k.op("dve", lambda e: e.tensor_copy(out=g.ident[:], in_=tmp[:]), reads=[tt], writes=[g.T_ident])
            k.op("dve", lambda e: e.memset(g.onesf[:], 1.0), writes=[g.T_const])
            k.op("dve", lambda e: e.memset(g.ones128[:], 1.0 / 128), writes=[g.T_const])
            k.op("dve", lambda e: e.memset(g.ones192[:], 1.0 / 192), writes=[g.T_const])
            k.op("dve", lambda e: e.memset(g.blk64[:], 0.0), writes=[g.T_const])
            k.op("dve", lambda e: e.memset(g.blk64[0:64, 0:64], 1.0 / 64), reads=[g.T_const], writes=[g.T_const])
            k.op("dve", lambda e: e.memset(g.blk64[64:128, 64:128], 1.0 / 64), reads=[g.T_const], writes=[g.T_const])
            k.barrier()

        for li, l in enumerate(layers):
            need_ctx = l < DEPTH - 1
            src = g.xs if li == 0 else g.XS
            last = li == len(layers) - 1
            dst = g.y if last else g.XS
            phase_weights(g, l)
            k.barrier()
            phase_ssm_weights(g, l)
            phase_hy_filter(g, l, HY_LAT)
            if need_ctx:
                phase_hy_filter(g, l, HY_CTX)
            for s in range(2):
                phase_P1(g, l, s, src)
                k.barrier()
                phase_hyena(g, l, s, HY_LAT, C)
                if need_ctx:
                    phase_hyena(g, l, s, HY_CTX, 0)
                phase_ssm(g, l, s, need_ctx)
                phase_attn(g, l, s, need_ctx)
                k.barrier()
                phase_P6(g, l, s, src, dst, need_ctx, last and final_lat_only)
                k.barrier()
        k.drain("sp")
    return nc, g


def phase_weights(g, l):
    nc, k, W = g.nc, g.k, g.W
    with ExitStack() as es:
        sb = lambda n, s, d=F32: es.enter_context(nc.sbuf_tensor(_nm(n), s, d))
        ps = lambda n, s, d=F32: es.enter_context(nc.psum_tensor(_nm(n), s, d))
        p32, s32 = _partner_index(32)
        p64, s64 = _partner_index(64)
        fin = Rot([sb("wf%d" % i, [128, NIN]) for i in range(2)])
        fob = Rot([sb("wb%d" % i, [128, NCB], BF16) for i in range(2)])
        qorder = (0, 2, 1, 3)
        engs = ["act", "pool", "dve"]
        ei = 0

        def cp(dst, src, neg=False):
            nonlocal ei
            e = engs[ei % 3]
            ei += 1
            if e == "act":
                k.op("act", lambda en: en.activation(out=dst, in_=src, func=AF.Copy, scale=(-1.0 if neg else 1.0)), reads=[tf], writes=[tb])
            else:
                k.op(e, lambda en: en.tensor_scalar(out=dst, in0=src, scalar1=(-1.0 if neg else 1.0), scalar2=None, op0=ALU.mult), reads=[tf], writes=[tb])

        def partner_cols(dstb, srcb, d):
            hh, qq = d // 2, d // 4
            for hf in range(2):
                b0 = hf * hh
                cp(wb[:, dstb + b0:dstb + b0 + qq], wf[:, srcb + b0 + qq:srcb + b0 + hh], neg=True)
                cp(wb[:, dstb + b0 + qq:dstb + b0 + hh], wf[:, srcb + b0:srcb + b0 + qq], neg=False)

        for kc in range(8):
            wf, tf = fin.next()
            wb, tb = fob.next()
            k.dma("sp", wf[:], W["w_in"][l, kc * 128:(kc + 1) * 128, :], writes=[tf])
            k.op("act", lambda en: en.activation(out=wb[:, 0:1456], in_=wf[:, 0:1456], func=AF.Copy), reads=[tf], writes=[tb])
            k.op("dve", lambda en: en.tensor_copy(out=wb[:, 1456:NIN], in_=wf[:, 1456:NIN]), reads=[tf], writes=[tb])
            partner_cols(O_KRP, O_KR, 32)
            for pos, h in enumerate(qorder):
                cp(wb[:, O_QM + pos * 64:O_QM + pos * 64 + 64], wf[:, O_GQ + h * 64:O_GQ + h * 64 + 64])
                partner_cols(O_QP + pos * 64, O_GQ + h * 64, 64)
            for h in range(2):
                partner_cols(O_KP + h * 64, O_GK + h * 64, 64)
            k.dma("pool", g.WINB[:, kc, :], wb[:], reads=[tb], writes=[g.T_WINB])
        fo = Rot([sb("wof%d" % i, [128, D]) for i in range(2)])
        fb = Rot([sb("wob%d" % i, [128, D], BF16) for i in range(2)])
        for kc in range(8):
            wf, tf = fo.next()
            wb, tb = fb.next()
            k.dma("sp", wf[:], W["w_out"][l, kc * 128:(kc + 1) * 128, :], writes=[tf])
            k.op("act" if kc % 2 else "dve", (lambda en: en.activation(out=wb[:], in_=wf[:], func=AF.Copy)) if kc % 2 else (lambda en: en.tensor_copy(out=wb[:], in_=wf[:])), reads=[tf], writes=[tb])
            k.dma("pool", g.WOUTB[:, kc, :], wb[:], reads=[tb], writes=[g.T_WOUTB])

        cT = sb("cTs", [128, 8, 3])
        scT = sb("scT", [128, 8, 3])
        t_c = Tok()
        k.dma("sp", cT[:], g.cT, writes=[t_c])
        k.op("act", lambda en: en.activation(out=scT[:], in_=cT[:], func=AF.Silu), reads=[t_c], writes=[t_c])
        mrow = sb("mrow", [3, 3 * D])
        brow = sb("brow", [3, 3 * D])
        gpre = sb("gpre", [3, D])
        gpost = sb("gpost", [3, D])
        t_m = Tok()
        t_b = Tok()
        k.dma("sp", brow[:], W["b_mod"][l:l + 1, :].broadcast_to([3, 3 * D]), writes=[t_b])
        k.dma("sp", gpre[:], W["g_pre"][l:l + 1, :].broadcast_to([3, D]), writes=[t_b])
        k.dma("sp", gpost[:], W["g_post"][l:l + 1, :].broadcast_to([3, D]), writes=[t_b])
        wm = Rot([sb("wm%d" % i, [128, 8, 512]) for i in range(2)])
        pm = Rot([ps("pm%d" % i, [3, 512]) for i in range(2)], excl=True)
        for cc in range(6):
            wt, tw = wm.next()
            pt, tp = pm.next()
            k.dma("sp", wt[:], W["w_mod"][l, :, cc * 512:(cc + 1) * 512].rearrange("(kc p) n -> p kc n", p=128), writes=[tw])
            for kc in range(8):
                k.op("pe", lambda en: en.matmul(pt[:], lhsT=scT[:, kc, :], rhs=wt[:, kc, :], start=(kc == 0), stop=(kc == 7)), reads=[t_c, tw], writes=[tp])
            k.op("dve", lambda en: en.tensor_tensor(out=mrow[:, cc * 512:(cc + 1) * 512], in0=pt[:], in1=brow[:, cc * 512:(cc + 1) * 512], op=ALU.add), reads=[tp, t_b], writes=[t_m])
        orow = sb("orow", [3, 3 * D])
        t_o = Tok()
        k.op("dve", lambda en: en.scalar_tensor_tensor(out=orow[:, 0:D], in0=mrow[:, D:2 * D], scalar=1.0, in1=gpre[:], op0=ALU.add, op1=ALU.mult), reads=[t_m, t_b], writes=[t_o])
        k.op("dve", lambda en: en.tensor_copy(out=orow[:, D:2 * D], in_=mrow[:, 0:D]), reads=[t_m, t_o], writes=[t_o])
        k.op("dve", lambda en: en.tensor_tensor(out=orow[:, 2 * D:3 * D], in0=mrow[:, 2 * D:3 * D], in1=gpost[:], op=ALU.mult), reads=[t_m, t_b, t_o], writes=[t_o])
        k.dma("pool", g.MODROWS, orow[:], reads=[t_o], writes=[g.T_MOD])
        k.barrier()

    if not hasattr(g, "lw"):
        lw = G()
        es = g.es
        sbp = lambda n, s, d=F32: es.enter_context(nc.sbuf_tensor(_nm(n), s, d))
        lw.wuq_a = sbp("wuq_a", [128, 384], BF16)
        lw.wuq_b = sbp("wuq_b", [64, 384], BF16)
        lw.wuqp_a = sbp("wuqp_a", [128, 384], BF16)
        lw.wuqp_b = sbp("wuqp_b", [64, 384], BF16)
        lw.wuk = sbp("wuk", [128, 256], BF16)
        lw.wuv = sbp("wuv", [128, 256], BF16)
        lw.gq = sbp("gqc", [128, 4])
        lw.A2 = sbp("ssmA2", [128, NDBL, 3, 16])
        lw.tok = Tok()
        g.lw = lw
    lw = g.lw
    with ExitStack() as es:
        sb = lambda n, s, d=F32: es.enter_context(nc.sbuf_tensor(_nm(n), s, d))
        uqa = sb("uqa", [128, 384])
        uqb = sb("uqb", [64, 384])
        ukv = sb("ukv", [128, 512])
        gcq = sb("gcq", [128, 2])
        gckv = sb("gckv", [128, 1])
        tl = Tok()
        k.dma("sp", uqa[:], W["mla_w_uq"][l, 0:128, :], writes=[tl])
        k.dma("sp", uqb[:], W["mla_w_uq"][l, 128:192, :], writes=[tl])
        k.dma("sp", ukv[:], W["mla_w_ukv"][l], writes=[tl])
        k.dma("sp", gcq[:, 0:1], W["mla_g_cq"][l, 0:128].rearrange("(p o) -> p o", o=1), writes=[tl])
        k.dma("sp", gcq[0:64, 1:2], W["mla_g_cq"][l, 128:192].rearrange("(p o) -> p o", o=1), writes=[tl])
        k.dma("sp", gckv[:], W["mla_g_ckv"][l].rearrange("(p o) -> p o", o=1), writes=[tl])
        k.dma("sp", lw.gq[:], W["gq_cols"][l], writes=[lw.tok])
        k.op("dve", lambda en: en.tensor_scalar(out=uqa[:], in0=uqa[:], scalar1=gcq[:, 0:1], scalar2=None, op0=ALU.mult), reads=[tl], writes=[tl])
        k.op("dve", lambda en: en.tensor_scalar(out=uqb[:], in0=uqb[:], scalar1=gcq[0:64, 1:2], scalar2=None, op0=ALU.mult), reads=[tl], writes=[tl])
        k.op("dve", lambda en: en.tensor_scalar(out=ukv[:], in0=ukv[:], scalar1=gckv[:, 0:1], scalar2=None, op0=ALU.mult), reads=[tl], writes=[tl])
        k.op("dve", lambda en: en.tensor_copy(out=lw.wuq_a[:], in_=uqa[:]), reads=[tl], writes=[lw.tok])
        k.op("dve", lambda en: en.tensor_copy(out=lw.wuq_b[:], in_=uqb[:]), reads=[tl, lw.tok], writes=[lw.tok])
        k.op("dve", lambda en: en.memset(lw.wuqp_a[:], 0.0), reads=[lw.tok], writes=[lw.tok])
        k.op("dve", lambda en: en.memset(lw.wuqp_b[:], 0.0), reads=[lw.tok], writes=[lw.tok])
        for h in range(4):
            for hf in range(2):
                b0 = h * 96 + 64 + hf * 16
                for (dst, src, tile_src) in ((lw.wuqp_a, uqa, 128), (lw.wuqp_b, uqb, 64)):
                    k.op("dve", lambda en: en.tensor_scalar(out=dst[:, b0:b0 + 8], in0=src[:, b0 + 8:b0 + 16], scalar1=-1.0, scalar2=None, op0=ALU.mult), reads=[tl, lw.tok], writes=[lw.tok])
                    k.op("dve", lambda en: en.tensor_copy(out=dst[:, b0 + 8:b0 + 16], in_=src[:, b0:b0 + 8]), reads=[tl, lw.tok], writes=[lw.tok])
            k.op("dve", lambda en: en.tensor_copy(out=lw.wuk[:, h * 64:(h + 1) * 64], in_=ukv[:, h * 128:h * 128 + 64]), reads=[tl, lw.tok], writes=[lw.tok])
            k.op("dve", lambda en: en.tensor_copy(out=lw.wuv[:, h * 64:(h + 1) * 64], in_=ukv[:, h * 128 + 64:h * 128 + 128]), reads=[tl, lw.tok], writes=[lw.tok])
        k.barrier()


def phase_P1(g, l, s, src):
    nc, k = g.nc, g.k
    with ExitStack() as es:
        sb = lambda n, s_, d=F32: es.enter_context(nc.sbuf_tensor(_nm(n), s_, d))
        ps = lambda n, s_, d=F32: es.enter_context(nc.psum_tensor(_nm(n), s_, d))
        mods = {}
        t_mod = Tok()
        for v in (s, 2):
            mods[v] = sb("mod%d" % v, [128, 2 * D])
            k.dma("sp", mods[v][:], g.MODROWS[v:v + 1, 0:2 * D].broadcast_to([128, 2 * D]), reads=[g.T_MOD], writes=[t_mod])
        neghalf = sb("neghalf", [128, 1])
        k.op("pool", lambda en: en.memset(neghalf[:], -0.5), writes=[t_mod])
        xr = Rot([sb("x%d" % i, [128, D]) for i in range(3)])
        junk = sb("junk", [128, D], BF16)
        t_junk = Tok()
        hb_r = Rot([sb("hb%d" % i, [128, D], BF16) for i in range(2)])
        tmp_r = Rot([sb("tmp%d" % i, [128, D]) for i in range(2)])
        st_r = Rot([sb("st%d" % i, [128, 4]) for i in range(3)])
        tp_r = Rot([ps("tp%d" % i, [128, 8, 128], BF16) for i in range(2)], excl=True)
        hs_r = Rot([sb("hs%d" % i, [128, 8, 512], BF16) for i in range(2)])
        hs, t_hs = None, None
        for ti in range(NT):
            if ti == 0 or (ti >= 2 and (ti - 2) % 4 == 0):
                hs, t_hs = hs_r.next()
            off = (ti * 128) if ti < 2 else (((ti - 2) % 4) * 128)
            xt, t_x = xr.next()
            k.dma("sp", xt[:], src[s, ti * 128:(ti + 1) * 128, :], reads=[g.T_XS[s]], writes=[t_x])
            st, t_st = st_r.next()
            k.op("dve", lambda en: en.scalar_tensor_tensor(out=junk[:], in0=xt[:], scalar=1.0, in1=xt[:], op0=ALU.mult, op1=ALU.mult, accum_out=st[:, 0:1]), reads=[t_x], writes=[t_junk, t_st])
            k.op("dve", lambda en: en.tensor_scalar(out=st[:, 1:2], in0=st[:, 0:1], scalar1=1.0 / D, scalar2=EPS, op0=ALU.mult, op1=ALU.add), reads=[t_st], writes=[t_st])
            k.op("pool", lambda en: en.tensor_tensor(out=st[:, 2:3], in0=st[:, 1:2], in1=neghalf[:], op=ALU.pow), reads=[t_st, t_mod], writes=[t_st])
            md = mods[2] if ti < 2 else mods[s]
            tmp, t_tmp = tmp_r.next()
            hb, t_hb = hb_r.next()
            k.op("dve", lambda en: en.scalar_tensor_tensor(out=tmp[:], in0=xt[:], scalar=st[:, 2:3], in1=md[:, 0:D], op0=ALU.mult, op1=ALU.mult), reads=[t_x, t_st, t_mod], writes=[t_tmp])
            k.op("pool", lambda en: en.tensor_tensor(out=hb[:], in0=tmp[:], in1=md[:, D:2 * D], op=ALU.add), reads=[t_tmp, t_mod], writes=[t_hb])
            tp, t_tp = tp_r.next()
            for kc in range(8):
                k.op("pe", lambda en: en.transpose(tp[:, kc, :], hb[:, kc * 128:(kc + 1) * 128], g.ident[:]), reads=[t_hb, g.T_ident], writes=[t_tp])
            k.op("act", lambda en: en.activation(out=hs[:, :, off:off + 128], in_=tp[:], func=AF.Copy), reads=[t_tp], writes=[t_hs])
            if ti == 1:
                k.dma("pool", g.HT[:, :, 0:256], hs[:, :, 0:256], reads=[t_hs], writes=[g.T_HT])
            elif ti >= 2 and (ti - 2) % 4 == 3:
                t0 = (ti - 3) * 128
                k.dma("pool", g.HT[:, :, t0:t0 + 512], hs[:, :, 0:512], reads=[t_hs], writes=[g.T_HT])
        k.barrier()


def phase_attn(g, l, s, need_ctx):
    nc, k, lw = g.nc, g.k, g.lw
    with ExitStack() as es:
        sb = lambda n, s_, d=F32: es.enter_context(nc.sbuf_tensor(_nm(n), s_, d))
        ps = lambda n, s_, d=F32: es.enter_context(nc.psum_tensor(_nm(n), s_, d))
        KTm = [sb("KTm%d" % h, [96, T], BF16) for h in range(4)]
        VM = sb("VM", [128, NT, 4 * 65], BF16)
        KTg = sb("KTg", [128, T], BF16)
        VG = sb("VG", [128, NT, 2 * 65], BF16)
        t_K = Tok()
        k.op("pool", lambda en: en.memset(VM[:], 1.0), writes=[t_K])
        k.op("pool", lambda en: en.memset(VG[:], 1.0), writes=[t_K])
        t_w = Tok()
        epsc = sb("epsc", [128, 1])
        k.op("pool", lambda en: en.memset(epsc[:], EPS), writes=[t_w])

        hT_r = Rot([sb("hT%d" % i, [128, 8, 512], BF16) for i in range(2)])
        rope_r = Rot([sb("rp%d" % i, [128, 4, 512]) for i in range(2)])
        esA = ExitStack()
        sbA = lambda n, s_, d=F32: esA.enter_context(nc.sbuf_tensor(_nm(n), s_, d))
        wkv = sbA("wkv", [128, 8, 576], BF16)
        k.dma("sp", wkv[:, :, 0:160], g.WINB[:, :, O_CKV:O_CKV + 160], reads=[g.T_WINB], writes=[t_w])
        k.dma("sp", wkv[:, :, 160:192], g.WINB[:, :, O_KRP:O_KRP + 32], reads=[g.T_WINB], writes=[t_w])
        k.dma("sp", wkv[:, :, 192:448], g.WINB[:, :, O_GK:O_GK + 256], reads=[g.T_WINB], writes=[t_w])
        k.dma("sp", wkv[:, :, 448:576], g.WINB[:, :, O_KP:O_KP + 128], reads=[g.T_WINB], writes=[t_w])
        SB2 = [ps("psS%d" % i, [128, 2, 512]) for i in range(2)]
        TSB = [Tok(True) for _ in range(2)]
        PS = [None, None, None, None] + [ps("ps%d" % i, [128, 512]) for i in range(4, 8)]
        PS[0], PS[1], PS[2], PS[3] = SB2[0][:, 0, :], SB2[0][:, 1, :], SB2[1][:, 0, :], SB2[1][:, 1, :]
        TPS = [TSB[0], TSB[0], TSB[1], TSB[1]] + [Tok(True) for _ in range(4)]

        blocks = [(0, 256)] + [(256 + 512 * b, 512) for b in range(8)]

        def load_block(t0, nb):
            hT, t_h = hT_r.next()
            k.dma("sp", hT[:, :, 0:nb], g.HT[:, :, t0:t0 + nb], reads=[g.T_HT], writes=[t_h])
            rp, t_rp = rope_r.next()
            k.dma("sp", rp[:, 0:2, 0:nb], g.K["k_ropeM"][:, :, t0:t0 + nb].rearrange("a p n -> p a n"), writes=[t_rp])
            k.dma("sp", rp[:, 2:4, 0:nb], g.K["k_ropeG"][:, :, t0:t0 + nb].rearrange("a p n -> p a n"), writes=[t_rp])
            return hT, t_h, rp, t_rp

        def proj(pi, M, wt, c0, hT, t_h, nb, pbase=0):
            for kc in range(8):
                k.op("pe", lambda en: en.matmul(PS[pi][pbase:pbase + M, 0:nb], lhsT=wt[:, kc, c0:c0 + M], rhs=hT[:, kc, 0:nb], start=(kc == 0), stop=(kc == 7)), reads=[t_w, t_h], writes=[TPS[pi]])

        def rstd_from_ms(out_ap, ms_ap, reads, wtok, tmp_ap):
            k.op("act", lambda en: en.activation(out=tmp_ap, in_=ms_ap, func=AF.Ln, bias=epsc[0:tmp_ap.shape[0], 0:1]), reads=reads + [t_w], writes=[wtok])
            k.op("act", lambda en: en.activation(out=out_ap, in_=tmp_ap, func=AF.Exp, scale=-0.5), reads=[wtok], writes=[wtok])

        wk_r = Rot([sbA("wk%d" % i, [128, 6, 512]) for i in range(1)])
        wkb_r = Rot([sbA("wkb%d" % i, [128, 3, 512], BF16) for i in range(2)])
        for (t0, nb) in blocks:
            hT, t_h, rp, t_rp = load_block(t0, nb)
            wk, t_wk = wk_r.next()
            wkb, t_wkb = wkb_r.next()
            proj(0, 128, wkv, 0, hT, t_h, nb)
            k.op("act", lambda en: en.activation(out=wkb[:, 0, 0:nb], in_=PS[0][:, 0:nb], func=AF.Square), reads=[TPS[0]], writes=[t_wkb])
            k.op("dve", lambda en: en.tensor_copy(out=wk[:, 0, 0:nb], in_=PS[0][:, 0:nb]), reads=[TPS[0]], writes=[t_wk])
            k.op("pe", lambda en: en.matmul(PS[1][:, 0:nb], lhsT=g.ones128[:], rhs=wkb[:, 0, 0:nb], start=True, stop=True), reads=[t_wkb, g.T_const], writes=[TPS[1]])
            rstd_from_ms(wk[:, 1, 0:nb], PS[1][:, 0:nb], [TPS[1]], t_wk, wk[:, 1, 0:nb])
            k.op("dve", lambda en: en.tensor_tensor(out=wkb[:, 1, 0:nb], in0=wk[:, 0, 0:nb], in1=wk[:, 1, 0:nb], op=ALU.mult), reads=[t_wk], writes=[t_wkb])
            for h in range(4):
                pi = 2 + (h % 2)
                k.op("pe", lambda en: en.matmul(PS[pi][0:64, 0:nb], lhsT=lw.wuk[:, h * 64:(h + 1) * 64], rhs=wkb[:, 1, 0:nb], start=True, stop=True), reads=[t_wkb, lw.tok], writes=[TPS[pi]])
                k.op("act" if h % 2 else "dve", (lambda en: en.activation(out=KTm[h][0:64, t0:t0 + nb], in_=PS[pi][0:64, 0:nb], func=AF.Copy)) if h % 2 else (lambda en: en.tensor_copy(out=KTm[h][0:64, t0:t0 + nb], in_=PS[pi][0:64, 0:nb])), reads=[TPS[pi]], writes=[t_K])
            for j in range(nb // 128):
                ti = t0 // 128 + j
                k.op("pe", lambda en: en.matmul(PS[4][:, 0:256], lhsT=wkb[:, 1, j * 128:(j + 1) * 128], rhs=lw.wuv[:], start=True, stop=True), reads=[t_wkb, lw.tok], writes=[TPS[4]])
                k.op("dve", lambda en: en.tensor_copy(out=VM[:, ti, :].rearrange("p (h d) -> p h d", d=65)[:, :, 0:64], in_=PS[4][:, 0:256].rearrange("p (h d) -> p h d", d=64)), reads=[TPS[4]], writes=[t_K])
            proj(5, 32, wkv, 128, hT, t_h, nb, pbase=64)
            proj(6, 32, wkv, 160, hT, t_h, nb, pbase=64)
            k.op("dve", lambda en: en.tensor_tensor(out=wk[64:96, 2, 0:nb], in0=PS[5][64:96, 0:nb], in1=rp[64:96, 0, 0:nb], op=ALU.mult), reads=[TPS[5], t_rp], writes=[t_wk])
            k.op("dve", lambda en: en.tensor_tensor(out=wk[64:96, 3, 0:nb], in0=PS[6][64:96, 0:nb], in1=rp[64:96, 1, 0:nb], op=ALU.mult), reads=[TPS[6], t_rp], writes=[t_wk])
            for h in range(4):
                k.op("pool" if h % 2 else "dve", lambda en: en.tensor_tensor(out=KTm[h][64:96, t0:t0 + nb], in0=wk[64:96, 2, 0:nb], in1=wk[64:96, 3, 0:nb], op=ALU.add), reads=[t_wk], writes=[t_K])
            proj(7, 128, wkv, 192, hT, t_h, nb)
            proj(0, 128, wkv, 448, hT, t_h, nb)
            k.op("act", lambda en: en.activation(out=wkb[:, 2, 0:nb], in_=PS[7][:, 0:nb], func=AF.Square), reads=[TPS[7]], writes=[t_wkb])
            k.op("pe", lambda en: en.matmul(PS[1][:, 0:nb], lhsT=g.blk64[:], rhs=wkb[:, 2, 0:nb], start=True, stop=True), reads=[t_wkb, g.T_const], writes=[TPS[1]])
            rstd_from_ms(wk[:, 4, 0:nb], PS[1][:, 0:nb], [TPS[1]], t_wk, wk[:, 4, 0:nb])
            k.op("dve", lambda en: en.scalar_tensor_tensor(out=wk[:, 0, 0:nb], in0=PS[7][:, 0:nb], scalar=lw.gq[:, 2:3], in1=rp[:, 2, 0:nb], op0=ALU.mult, op1=ALU.mult), reads=[TPS[7], t_rp, lw.tok, t_wk], writes=[t_wk])
            k.op("dve", lambda en: en.scalar_tensor_tensor(out=wk[:, 5, 0:nb], in0=PS[0][:, 0:nb], scalar=lw.gq[:, 3:4], in1=rp[:, 3, 0:nb], op0=ALU.mult, op1=ALU.mult), reads=[TPS[0], t_rp, lw.tok, t_wk], writes=[t_wk])
            k.op("pool", lambda en: en.tensor_tensor(out=wk[:, 0, 0:nb], in0=wk[:, 0, 0:nb], in1=wk[:, 5, 0:nb], op=ALU.add), reads=[t_wk], writes=[t_wk])
            k.op("dve", lambda en: en.tensor_tensor(out=KTg[:, t0:t0 + nb], in0=wk[:, 0, 0:nb], in1=wk[:, 4, 0:nb], op=ALU.mult), reads=[t_wk], writes=[t_K])
            for j in range(nb // 128):
                ti = t0 // 128 + j
                for kc in range(8):
                    k.op("pe", lambda en: en.matmul(PS[4][:, 0:128], lhsT=hT[:, kc, j * 128:(j + 1) * 128], rhs=wkv[:, kc, 320:448], start=(kc == 0), stop=(kc == 7)), reads=[t_h, t_w], writes=[TPS[4]])
                k.op("act", lambda en: en.activation(out=VG[:, ti, :].rearrange("p (h d) -> p h d", d=65)[:, :, 0:64], in_=PS[4][:, 0:128].rearrange("p (h d) -> p h d", d=64), func=AF.Copy), reads=[TPS[4]], writes=[t_K])
        if "KTm0" in g.dbg:
            k.dma("pool", g.dbg["KTm0"], KTm[0][:], reads=[t_K])
            k.dma("pool", g.dbg["KTg"], KTg[:], reads=[t_K])
            k.dma("pool", g.dbg["VM"], VM[:], reads=[t_K])
            k.dma("pool", g.dbg["VG"], VG[:], reads=[t_K])

        k.barrier()
        esA.close()
        wq = sb("wq", [128, 8, 192 + 256 + 256 + 256 + 256], BF16)
        k.dma("sp", wq[:, :, 0:192], g.WINB[:, :, O_CQ:O_CQ + 192], reads=[g.T_WINB], writes=[t_w])
        k.dma("sp", wq[:, :, 192:448], g.WINB[:, :, O_GM:O_GM + 256], reads=[g.T_WINB], writes=[t_w])
        k.dma("sp", wq[:, :, 448:960], g.WINB[:, :, O_QM:O_QM + 512], reads=[g.T_WINB], writes=[t_w])
        k.dma("sp", wq[:, :, 960:1216], g.WINB[:, :, O_GG:O_GG + 256], reads=[g.T_WINB], writes=[t_w])
        qm_r = Rot([[sb("qm%d_%d" % (i, h), [96, 512], BF16) for h in range(4)] for i in range(2)])
        qg_r = Rot([[sb("qg%d_%d" % (i, j), [128, 512], BF16) for j in range(2)] for i in range(2)])
        gate_r = Rot([sb("gate%d" % i, [64, 8, 512], BF16) for i in range(1)])
        cq_r = Rot([sb("cq%d" % i, [128, 4, 512]) for i in range(1)])
        cqb_r = Rot([sb("cqb%d" % i, [128, 4, 512], BF16) for i in range(1)])
        P_r = Rot([sb("P%d" % i, [128, 2, 512], BF16) for i in range(4)])
        osb_r = Rot([sb("osb%d" % i, [65, 512]) for i in range(2)])
        res_r = Rot([sb("res%d" % i, [64, 512], BF16) for i in range(3)])
        S_bufs = [(0, 1), (2, 3)]
        O_bufs = [4, 5]
        for (t0, nb) in blocks:
            if t0 == 0 and not need_ctx:
                continue
            hT, t_h, rp, t_rp = load_block(t0, nb)
            kts = list(range(2)) if t0 == 0 else list(range(NT))
            qm, t_qm = qm_r.next()
            qg, t_qg = qg_r.next()
            gate, t_gate = gate_r.next()
            cq, t_cq = cq_r.next()
            cqb, t_cqb = cqb_r.next()
            for hh in range(8):
                c0 = (192 + hh * 64) if hh < 4 else (960 + (hh - 4) * 64)
                pi = 6 + hh % 2
                proj(pi, 64, wq, c0, hT, t_h, nb)
                k.op("act", lambda en: en.activation(out=gate[:, hh, 0:nb], in_=PS[pi][0:64, 0:nb], func=AF.Silu), reads=[TPS[pi]], writes=[t_gate])
            proj(6, 128, wq, 0, hT, t_h, nb)
            proj(7, 64, wq, 128, hT, t_h, nb)
            k.op("act", lambda en: en.activation(out=cqb[:, 0, 0:nb], in_=PS[6][:, 0:nb], func=AF.Square), reads=[TPS[6]], writes=[t_cqb])
            k.op("act", lambda en: en.activation(out=cqb[0:64, 1, 0:nb], in_=PS[7][0:64, 0:nb], func=AF.Square), reads=[TPS[7]], writes=[t_cqb])
            k.op("dve", lambda en: en.tensor_copy(out=cq[:, 0, 0:nb], in_=PS[6][:, 0:nb]), reads=[TPS[6]], writes=[t_cq])
            k.op("dve", lambda en: en.tensor_copy(out=cq[0:64, 1, 0:nb], in_=PS[7][0:64, 0:nb]), reads=[TPS[7]], writes=[t_cq])
            k.op("pe", lambda en: en.matmul(PS[6][:, 0:nb], lhsT=g.ones192[:], rhs=cqb[:, 0, 0:nb], start=True, stop=False), reads=[t_cqb, g.T_const, t_cq], writes=[TPS[6]])
            k.op("pe", lambda en: en.matmul(PS[6][:, 0:nb], lhsT=g.ones192[0:64, :], rhs=cqb[0:64, 1, 0:nb], start=False, stop=True), reads=[t_cqb, g.T_const], writes=[TPS[6]])
            rstd_from_ms(cq[:, 2, 0:nb], PS[6][:, 0:nb], [TPS[6]], t_cq, cq[:, 2, 0:nb])
            k.op("dve", lambda en: en.tensor_tensor(out=cqb[:, 2, 0:nb], in0=cq[:, 0, 0:nb], in1=cq[:, 2, 0:nb], op=ALU.mult), reads=[t_cq], writes=[t_cqb])
            k.op("dve", lambda en: en.tensor_tensor(out=cqb[0:64, 3, 0:nb], in0=cq[0:64, 1, 0:nb], in1=cq[0:64, 2, 0:nb], op=ALU.mult), reads=[t_cq], writes=[t_cqb])
            for h in range(4):
                for (pi, wa, wb_) in ((6, lw.wuq_a, lw.wuq_b), (7, lw.wuqp_a, lw.wuqp_b)):
                    k.op("pe", lambda en: en.matmul(PS[pi][0:96, 0:nb], lhsT=wa[:, h * 96:(h + 1) * 96], rhs=cqb[:, 2, 0:nb], start=True, stop=False), reads=[t_cqb, lw.tok], writes=[TPS[pi]])
                    k.op("pe", lambda en: en.matmul(PS[pi][0:96, 0:nb], lhsT=wb_[:, h * 96:(h + 1) * 96], rhs=cqb[0:64, 3, 0:nb], start=False, stop=True), reads=[t_cqb, lw.tok], writes=[TPS[pi]])
                k.op("dve", lambda en: en.tensor_tensor(out=cq[0:96, 3, 0:nb], in0=PS[6][0:96, 0:nb], in1=rp[0:96, 0, 0:nb], op=ALU.mult), reads=[TPS[6], t_rp, t_cq], writes=[t_cq])
                k.op("dve", lambda en: en.tensor_tensor(out=cq[0:96, 1, 0:nb], in0=PS[7][0:96, 0:nb], in1=rp[0:96, 1, 0:nb], op=ALU.mult), reads=[TPS[7], t_rp, t_cq], writes=[t_cq])
                k.op("pool", lambda en: en.tensor_tensor(out=qm[h][:, 0:nb], in0=cq[0:96, 3, 0:nb], in1=cq[0:96, 1, 0:nb], op=ALU.add), reads=[t_cq], writes=[t_qm])
            for j in range(2):
                proj(6, 128, wq, 448 + j * 128, hT, t_h, nb)
                proj(7, 128, wq, 704 + j * 128, hT, t_h, nb)
                k.op("act", lambda en: en.activation(out=cqb[:, 0, 0:nb], in_=PS[6][:, 0:nb], func=AF.Square), reads=[TPS[6], t_cqb], writes=[t_cqb])
                k.op("dve", lambda en: en.scalar_tensor_tensor(out=cq[:, 0, 0:nb], in0=PS[6][:, 0:nb], scalar=lw.gq[:, 0:1], in1=rp[:, 2, 0:nb], op0=ALU.mult, op1=ALU.mult), reads=[TPS[6], t_rp, lw.tok, t_cq], writes=[t_cq])
                k.op("dve", lambda en: en.scalar_tensor_tensor(out=cq[:, 1, 0:nb], in0=PS[7][:, 0:nb], scalar=lw.gq[:, 1:2], in1=rp[:, 3, 0:nb], op0=ALU.mult, op1=ALU.mult), reads=[TPS[7], t_rp, lw.tok, t_cq], writes=[t_cq])
                k.op("pe", lambda en: en.matmul(PS[6][:, 0:nb], lhsT=g.blk64[:], rhs=cqb[:, 0, 0:nb], start=True, stop=True), reads=[t_cqb, g.T_const, t_cq], writes=[TPS[6]])
                rstd_from_ms(cq[:, 2, 0:nb], PS[6][:, 0:nb], [TPS[6]], t_cq, cq[:, 2, 0:nb])
                k.op("pool", lambda en: en.tensor_tensor(out=cq[:, 0, 0:nb], in0=cq[:, 0, 0:nb], in1=cq[:, 1, 0:nb], op=ALU.add), reads=[t_cq], writes=[t_cq])
                k.op("dve", lambda en: en.tensor_tensor(out=qg[j][:, 0:nb], in0=cq[:, 0, 0:nb], in1=cq[:, 2, 0:nb], op=ALU.mult), reads=[t_cq], writes=[t_qg])
            if "qm0" in g.dbg and t0 == 256:
                k.dma("pool", g.dbg["qm0"], qm[0][:], reads=[t_qm])
                k.dma("pool", g.dbg["qg0"], qg[0][:], reads=[t_qg])
            for hh in range(8):
                if hh < 4:
                    dk, scale = 96, 96 ** -0.5
                    Kt = KTm[hh]
                    kb0 = 0
                    Qt = qm[hh]
                    qb0 = 0
                    Vt, vc0 = VM, hh * 65
                    tq = t_qm
                else:
                    hq = hh - 4
                    kv = hq // 2
                    dk, scale = 64, 64 ** -0.5
                    Kt, kb0 = KTg, kv * 64
                    Qt, qb0 = qg[hq % 2], kv * 64
                    Vt, vc0 = VG, kv * 65
                    tq = t_qg
                oi = O_bufs[hh % 2]
                pairs = [kts[i:i + 2] for i in range(0, len(kts), 2)]

                def emit_S(pidx):
                    sbi = pidx % 2
                    for j, kt in enumerate(pairs[pidx]):
                        k.op("pe", lambda en: en.matmul(SB2[sbi][:, j, 0:nb], lhsT=Kt[kb0:kb0 + dk, kt * 128:(kt + 1) * 128], rhs=Qt[qb0:qb0 + dk, 0:nb], start=True, stop=True), reads=[t_K, tq], writes=[TSB[sbi]])

                emit_S(0)
                for pidx in range(len(pairs)):
                    if pidx + 1 < len(pairs):
                        emit_S(pidx + 1)
                    sbi = pidx % 2
                    npr = len(pairs[pidx])
                    Pt, t_P = P_r.next()
                    k.op("act", lambda en: en.activation(out=Pt[:, 0:npr, 0:nb], in_=SB2[sbi][:, 0:npr, 0:nb], func=AF.Exp, scale=scale), reads=[TSB[sbi]], writes=[t_P])
                    for j, kt in enumerate(pairs[pidx]):
                        first = (pidx == 0 and j == 0)
                        lastm = (pidx == len(pairs) - 1 and j == npr - 1)
                        k.op("pe", lambda en: en.matmul(PS[oi][0:65, 0:nb], lhsT=Vt[:, kt, vc0:vc0 + 65], rhs=Pt[:, j, 0:nb], start=first, stop=lastm), reads=[t_P, t_K], writes=[TPS[oi]])
                osb, t_osb = osb_r.next()
                k.op("dve", lambda en: en.tensor_copy(out=osb[:, 0:nb], in_=PS[oi][0:65, 0:nb]), reads=[TPS[oi]], writes=[t_osb])
                k.op("dve", lambda en: en.reciprocal(out=osb[64:65, 0:nb], in_=osb[64:65, 0:nb]), reads=[t_osb], writes=[t_osb])
                bi = 6 + hh % 2
                k.op("pe", lambda en: en.matmul(PS[bi][0:64, 0:nb], lhsT=g.onesf[64:65, 0:64], rhs=osb[64:65, 0:nb], start=True, stop=True), reads=[t_osb, g.T_const], writes=[TPS[bi]])
                k.op("dve", lambda en: en.tensor_tensor(out=osb[0:64, 0:nb], in0=osb[0:64, 0:nb], in1=PS[bi][0:64, 0:nb], op=ALU.mult), reads=[t_osb, TPS[bi]], writes=[t_osb])
                res, t_res = res_r.next()
                k.op("pool", lambda en: en.tensor_tensor(out=res[:, 0:nb], in0=osb[0:64, 0:nb], in1=gate[:, hh, 0:nb], op=ALU.mult), reads=[t_osb, t_gate], writes=[t_res])
                kc = hh // 2
                p0 = (hh % 2) * 64
                k.dma("pool", g.CATT[p0:p0 + 64, kc, t0:t0 + nb], res[:, 0:nb], reads=[t_res], writes=[g.T_CATT])
        k.barrier()


def phase_P6(g, l, s, src, dst, need_ctx, lat_only_out):
    nc, k = g.nc, g.k
    with ExitStack() as es:
        sb = lambda n, s_, d=F32: es.enter_context(nc.sbuf_tensor(_nm(n), s_, d))
        ps = lambda n, s_, d=F32: es.enter_context(nc.psum_tensor(_nm(n), s_, d))
        wo = sb("wo", [128, 8, D], BF16)
        t_w = Tok()
        k.dma("sp", wo[:], g.WOUTB, reads=[g.T_WOUTB], writes=[t_w])
        Gb = {}
        for v in ((s, 2) if need_ctx else (s,)):
            Gb[v] = sb("Gb%d" % v, [128, D])
            k.dma("sp", Gb[v][:], g.MODROWS[v:v + 1, 2 * D:3 * D].broadcast_to([128, D]), reads=[g.T_MOD], writes=[t_w])
        neghalf = sb("neghalf6", [128, 1])
        k.op("pool", lambda en: en.memset(neghalf[:], -0.5), writes=[t_w])
        cat_r = Rot([sb("cat%d" % i, [128, 8, 512], BF16) for i in range(2)])
        x_r = Rot([sb("x6_%d" % i, [128, D]) for i in range(3)])
        o_r = Rot([sb("o6_%d" % i, [128, D]) for i in range(3)])
        st_r = Rot([sb("st6_%d" % i, [128, 4]) for i in range(3)])
        junk = sb("junk6", [128, D], BF16)
        t_junk = Tok()
        po_r = Rot([ps("po%d" % i, [128, D]) for i in range(3)], excl=True)
        blocks = ([(0, 256)] if need_ctx else []) + [(256 + 512 * b, 512) for b in range(8)]
        for (t0, nb) in blocks:
            cat, t_cat = cat_r.next()
            k.dma("sp", cat[:, :, 0:nb], g.CATT[:, :, t0:t0 + nb], reads=[g.T_CATT], writes=[t_cat])
            for j in range(nb // 128):
                tok0 = t0 + j * 128
                po, t_po = po_r.next()
                for hf in range(2):
                    for kc in range(8):
                        k.op("pe", lambda en: en.matmul(po[:, hf * 512:(hf + 1) * 512], lhsT=cat[:, kc, j * 128:(j + 1) * 128], rhs=wo[:, kc, hf * 512:(hf + 1) * 512], start=(kc == 0), stop=(kc == 7)), reads=[t_cat, t_w], writes=[t_po])
                xt, t_x = x_r.next()
                k.dma("sp", xt[:], src[s, tok0:tok0 + 128, :], reads=[g.T_XS[s]], writes=[t_x])
                st, t_st = st_r.next()
                k.op("act", lambda en: en.activation(out=junk[:], in_=po[:], func=AF.Square, accum_out=st[:, 0:1]), reads=[t_po], writes=[t_junk, t_st])
                k.op("dve", lambda en: en.tensor_scalar(out=st[:, 1:2], in0=st[:, 0:1], scalar1=1.0 / D, scalar2=EPS, op0=ALU.mult, op1=ALU.add), reads=[t_st], writes=[t_st])
                k.op("pool", lambda en: en.tensor_tensor(out=st[:, 2:3], in0=st[:, 1:2], in1=neghalf[:], op=ALU.pow), reads=[t_st, t_w], writes=[t_st])
                ot, t_o = o_r.next()
                gb = Gb[2] if t0 == 0 else Gb[s]
                k.op("dve", lambda en: en.scalar_tensor_tensor(out=ot[:], in0=po[:], scalar=st[:, 2:3], in1=gb[:], op0=ALU.mult, op1=ALU.mult), reads=[t_po, t_st, t_w], writes=[t_o])
                k.op("pool", lambda en: en.tensor_tensor(out=ot[:], in0=ot[:], in1=xt[:], op=ALU.add), reads=[t_o, t_x], writes=[t_o])
                if lat_only_out:
                    if t0 == 0:
                        continue
                    k.dma("pool", dst[s, tok0 - C:tok0 - C + 128, :], ot[:], reads=[t_o], writes=[g.T_Y])
                else:
                    wt = g.T_XS[s] if dst is g.XS else g.T_Y
                    k.dma("pool", dst[s, tok0:tok0 + 128, :], ot[:], reads=[t_o], writes=[wt])
        k.barrier()


_CACHE = {}


def _weights_host(inputs):
    w = {}
    for n in ("w_mod", "b_mod", "g_pre", "g_post", "w_in", "w_out", "mla_g_cq", "mla_w_uq", "mla_g_ckv", "mla_w_ukv",
              "hy_conv_w", "hy_conv_b", "hy_f_w1", "hy_f_b1", "hy_f_freq1", "hy_f_w2", "hy_f_b2", "hy_f_freq2", "hy_f_w3", "hy_bias"):
        w[n] = np.ascontiguousarray(inputs[n], dtype=np.float32)
    p64, _ = _partner_index(64)
    gq = np.asarray(inputs["gqa_g_q"], np.float32)
    gk = np.asarray(inputs["gqa_g_k"], np.float32)
    cols = np.zeros((DEPTH, 128, 4), np.float32)
    for l in range(DEPTH):
        cols[l, :, 0] = np.concatenate([gq[l], gq[l]])
        cols[l, :, 1] = np.concatenate([gq[l][p64], gq[l][p64]])
        cols[l, :, 2] = np.concatenate([gk[l], gk[l]])
        cols[l, :, 3] = np.concatenate([gk[l][p64], gk[l][p64]])
    w["gq_cols"] = cols
    w.update(ssm_host_layouts(inputs))
    return w


def make_in_maps(inputs, xs_per_core):
    w = _weights_host(inputs)
    cst = host_constants()
    c = np.asarray(inputs["c"], np.float32)
    c_ctx = np.asarray(inputs["c_ctx"], np.float32)
    maps = []
    for i in range(NCORES):
        cv = np.stack([c[2 * i], c[2 * i + 1], c_ctx], -1)
        cT = np.ascontiguousarray(cv.reshape(8, 128, 3).transpose(1, 0, 2))
        m = {"xs": xs_per_core[i], "cT": cT}
        m.update(w)
        m.update(cst)
        maps.append(m)
    return maps


def kernel(**inputs):
    x = np.asarray(inputs["x"], np.float32)
    ctx = np.asarray(inputs["ctx"], np.float32)
    xs = [np.ascontiguousarray(np.concatenate([ctx[2 * i:2 * i + 2], x[2 * i:2 * i + 2]], axis=1)) for i in range(NCORES)]
    key = "fused"
    if key not in _CACHE:
        _CACHE[key] = build_program(list(range(DEPTH)), True)[0]
    nc = _CACHE[key]
    res = run_bass_kernel_spmd(nc, make_in_maps(inputs, xs), core_ids=list(range(NCORES)))
    out = np.concatenate([r["y"] for r in res.results], axis=0)
    return out.astype(np.float32)


HY_BANDS = 16
HY_DECAY = (math.log(1e-2) / 1.5, math.log(1e-2) / 0.3)


class HyCfg:
    def __init__(self, name, n, ni):
        self.name = name
        self.n = n
        self.ni = ni
        self.N = 2 * n
        self.cpg = 128 // ni
        self.ng = 256 // self.cpg
        self.ncol = 256 * ni


HY_LAT = HyCfg("L", 4096, 32)
HY_CTX = HyCfg("C", 256, 2)


def _hy_features(n, pos):
    f32 = np.float32
    t = np.linspace(0.0, 1.0, n, dtype=f32)
    omega = (f32(2.0 * math.pi) * np.arange(n, dtype=f32) / f32(n)).astype(f32)
    bands = np.linspace(1e-4, HY_BANDS - 1, HY_BANDS, dtype=f32)
    tt = t[pos]
    om = omega[pos]
    ang = (bands[:, None] * om[None, :]).astype(f32)
    z = np.concatenate([tt[None, :], np.cos(ang), -np.sin(ang)], axis=0).astype(f32)
    return z, tt


def hyena_constants():
    cst = {}
    f64 = np.float64
    j = np.arange(128)[:, None]
    b = np.arange(256)[None, :]
    ang = 2 * np.pi * j * b / 256.0
    F1 = np.concatenate([np.cos(ang), -np.sin(ang)], 1)
    sgn = np.where(b % 2 == 0, 1.0, -1.0)
    F1b = np.concatenate([np.cos(ang) * sgn, -np.sin(ang) * sgn], 1)
    cst["hk_F1"] = np.stack([F1, F1b], 0).astype(np.float32)
    for cfg in (HY_LAT, HY_CTX):
        p = np.arange(128)
        i_of_p = (p // cfg.cpg)[:, None]
        ang = 2 * np.pi * i_of_p * b / cfg.N
        TW = np.stack([np.concatenate([np.cos(ang), np.cos(ang)], 1), np.concatenate([-np.sin(ang), -np.sin(ang)], 1)], 0)
        cst["hk_TW" + cfg.name] = TW.astype(np.float32)
        ii = (p // cfg.cpg)[:, None]
        ci = (p % cfg.cpg)[:, None]
        aa = (p // cfg.cpg)[None, :]
        ca = (p % cfg.cpg)[None, :]
        dl = (ci == ca).astype(f64)
        ang = 2 * np.pi * ii * aa / cfg.ni
        Gr = dl * np.cos(ang)
        Gi = -dl * np.sin(ang)
        cst["hk_G" + cfg.name] = np.stack([Gr, Gi, -Gi], 0).astype(np.float32)
        ang = 2 * np.pi * aa.T * ii.T / cfg.ni
        dl2 = (ci.T == ca.T)
        angm = 2 * np.pi * (p // cfg.cpg)[:, None] * (p // cfg.cpg)[None, :] / cfg.ni
        dlm = ((p % cfg.cpg)[:, None] == (p % cfg.cpg)[None, :]).astype(f64)
        GIr = dlm * np.cos(angm)
        GIi = dlm * np.sin(angm)
        cst["hk_GI" + cfg.name] = np.stack([np.concatenate([GIr, GIi], 1), np.concatenate([-GIi, GIr], 1)], 0).astype(np.float32)
        TWI = np.zeros((2, 2, 128, 256), f64)
        for bc in range(2):
            bb = (bc * 128 + np.arange(128))[:, None]
            ang = 2 * np.pi * bb * (p // cfg.cpg)[None, :] / cfg.N
            TWI[bc, 0] = np.concatenate([np.cos(ang), np.cos(ang)], 1)
            TWI[bc, 1] = np.concatenate([np.sin(ang), np.sin(ang)], 1)
        cst["hk_TWI" + cfg.name] = TWI.astype(np.float32)
        FI = np.zeros((2, 2, 128, 128), f64)
        for bc in range(2):
            bb = (bc * 128 + np.arange(128))[:, None]
            ang = 2 * np.pi * bb * np.arange(128)[None, :] / 256.0
            FI[bc, 0] = np.cos(ang) / cfg.N
            FI[bc, 1] = -np.sin(ang) / cfg.N
        cst["hk_FI" + cfg.name] = FI.astype(np.float32)
        n = cfg.n
        u = np.arange(n)
        posr = np.where(u == 0, 0, n - u)
        zf, tf_ = _hy_features(n, u)
        zb, tb_ = _hy_features(n, posr)
        cst["hk_Z" + cfg.name] = np.stack([zf, zb], 0)
        deltas = np.abs(np.linspace(HY_DECAY[0], HY_DECAY[1], 256, dtype=np.float32))
        decf = np.exp(-tf_[:, None] * deltas[None, :]).astype(np.float32)
        decb = np.exp(-tb_[:, None] * deltas[None, :]).astype(np.float32)
        cst["hk_DEC" + cfg.name] = np.stack([decf.reshape(128, cfg.ni, 256), decb.reshape(128, cfg.ni, 256)], 0)
        msk = (p[:, None] % cfg.cpg == np.arange(cfg.cpg)[None, :]).astype(np.float32)
        cst["hk_MSK" + cfg.name] = msk
    return cst


def _hy_twiddle(k, A_ps, t_A, TW, t_TW, m1, m2, t_m, outr, outi, t_out, n2, sub_first=True):
    k.op("dve", lambda en: en.tensor_tensor(out=m1[:, 0:2 * n2], in0=A_ps, in1=TW[:, 0, 0:2 * n2], op=ALU.mult), reads=[t_A, t_TW], writes=[t_m])
    k.op("dve", lambda en: en.tensor_tensor(out=m2[:, 0:2 * n2], in0=A_ps, in1=TW[:, 1, 0:2 * n2], op=ALU.mult), reads=[t_A, t_TW, t_m], writes=[t_m])
    k.op("pool", lambda en: en.tensor_tensor(out=outr, in0=m1[:, 0:n2], in1=m2[:, n2:2 * n2], op=ALU.subtract), reads=[t_m], writes=[t_out])
    k.op("pool", lambda en: en.tensor_tensor(out=outi, in0=m2[:, 0:n2], in1=m1[:, n2:2 * n2], op=ALU.add), reads=[t_m, t_out], writes=[t_out])


def phase_hy_filter(g, l, cfg):
    nc, k, W, K = g.nc, g.k, g.W, g.K
    n, ni, ng, cpg, ncol = cfg.n, cfg.ni, cfg.ng, cfg.cpg, cfg.ncol
    KH = g.KHAT[cfg.name]
    with ExitStack() as es:
        sb = lambda nm, s_, d=F32: es.enter_context(nc.sbuf_tensor(_nm(nm), s_, d))
        ps = lambda nm, s_, d=F32: es.enter_context(nc.psum_tensor(_nm(nm), s_, d))
        t_w = Tok()
        w1 = sb("hw1", [33, 64]); w2 = sb("hw2", [64, 64]); w3 = sb("hw3", [64, 1024])
        cols = sb("hcols", [64, 8])
        k.dma("sp", w1[:], W["hy_f_w1"][l], writes=[t_w])
        k.dma("sp", w2[:], W["hy_f_w2"][l], writes=[t_w])
        k.dma("sp", w3[:], W["hy_f_w3"][l], writes=[t_w])
        for ci, nm in enumerate(("hy_f_b1", "hy_f_freq1", "hy_f_b2", "hy_f_freq2")):
            k.dma("sp", cols[:, ci:ci + 1], W[nm][l].rearrange("(p o) -> p o", o=1), writes=[t_w])
        k.op("dve", lambda en: en.tensor_tensor(out=cols[:, 4:5], in0=cols[:, 0:1], in1=cols[:, 1:2], op=ALU.mult), reads=[t_w], writes=[t_w])
        k.op("dve", lambda en: en.tensor_tensor(out=cols[:, 5:6], in0=cols[:, 2:3], in1=cols[:, 3:4], op=ALU.mult), reads=[t_w], writes=[t_w])
        F1 = sb("hF1", [128, 2, 512]); TW = sb("hTW", [128, 2, 512]); G3 = sb("hG", [128, 3, 128]); MSK = sb("hmsk", [128, cpg])
        t_c = Tok()
        k.dma("sp", F1[:], K["hk_F1"].rearrange("a p n -> p a n"), writes=[t_c])
        k.dma("sp", TW[:], K["hk_TW" + cfg.name].rearrange("a p n -> p a n"), writes=[t_c])
        k.dma("sp", G3[:], K["hk_G" + cfg.name].rearrange("a p n -> p a n"), writes=[t_c])
        k.dma("sp", MSK[:], K["hk_MSK" + cfg.name], writes=[t_c])
        epsc = sb("hyeps", [128, 1])
        k.op("pool", lambda en: en.memset(epsc[:], EPS), writes=[t_c])
        h2T = [sb("h2T%d" % d_, [64, n]) for d_ in range(2)]
        t_h2 = Tok()
        PSm = [ps("hps%d" % i, [128, 512]) for i in range(4)]
        TP = [Tok(True) for _ in range(4)]
        PI = math.pi

        def sin_layer(out_ap, ps_ap, t_ps, fr_col, fb_col, tmp, msk_, t_tmp, wtok):
            k.op("dve", lambda en: en.tensor_scalar(out=tmp, in0=ps_ap, scalar1=fr_col, scalar2=fb_col, op0=ALU.mult, op1=ALU.add), reads=[t_ps, t_w], writes=[t_tmp])
            k.op("dve", lambda en: en.tensor_scalar(out=msk_, in0=tmp, scalar1=PI, scalar2=-2 * PI, op0=ALU.is_gt, op1=ALU.mult), reads=[t_tmp], writes=[t_tmp])
            k.op("dve", lambda en: en.tensor_tensor(out=tmp, in0=tmp, in1=msk_, op=ALU.add), reads=[t_tmp], writes=[t_tmp])
            k.op("dve", lambda en: en.tensor_scalar(out=msk_, in0=tmp, scalar1=-PI, scalar2=2 * PI, op0=ALU.is_lt, op1=ALU.mult), reads=[t_tmp], writes=[t_tmp])
            k.op("dve", lambda en: en.tensor_tensor(out=tmp, in0=tmp, in1=msk_, op=ALU.add), reads=[t_tmp], writes=[t_tmp])
            k.op("act", lambda en: en.activation(out=out_ap, in_=tmp, func=AF.Sin), reads=[t_tmp], writes=[wtok])

        with ExitStack() as es2:
            sb2 = lambda nm, s_, d=F32: es2.enter_context(nc.sbuf_tensor(_nm(nm), s_, d))
            Zt = sb2("hZ", [33, n])
            t_z = Tok()
            h1 = sb2("hh1", [64, 512]); tmp = sb2("htmp", [64, 512]); msk_ = sb2("hmk", [64, 512])
            t_h1 = Tok(); t_tmp = Tok()
            bw = min(512, n)
            for d_ in range(2):
                k.dma("sp", Zt[:], K["hk_Z" + cfg.name][d_], reads=[], writes=[t_z])
                for b0 in range(0, n, bw):
                    k.op("pe", lambda en: en.matmul(PSm[0][0:64, 0:bw], lhsT=w1[:], rhs=Zt[:, b0:b0 + bw], start=True, stop=True), reads=[t_w, t_z], writes=[TP[0]])
                    sin_layer(h1[:, 0:bw], PSm[0][0:64, 0:bw], TP[0], cols[:, 1:2], cols[:, 4:5], tmp[:, 0:bw], msk_[:, 0:bw], t_tmp, t_h1)
                    k.op("pe", lambda en: en.matmul(PSm[1][0:64, 0:bw], lhsT=w2[:], rhs=h1[:, 0:bw], start=True, stop=True), reads=[t_w, t_h1], writes=[TP[1]])
                    sin_layer(h2T[d_][:, b0:b0 + bw], PSm[1][0:64, 0:bw], TP[1], cols[:, 3:4], cols[:, 5:6], tmp[:, 0:bw], msk_[:, 0:bw], t_tmp, t_h2)
            k.barrier()
        UF = [sb("hUF%d" % d_, [128, ncol]) for d_ in range(2)]
        t_uf = Tok()
        DEC = sb("hDEC", [128, ni, 256])
        t_dec = Tok()
        HSQ = sb("hHSQ", [128, 256]); sq = sb("hsq", [128, 256]); hh = sb("hhh", [128, 256])
        t_hsq = Tok(); t_hh = Tok(); t_sq = Tok()
        RS = sb("hRS", [128, 256]); SC = sb("hSC", [128, ng]); rtmp = sb("hrtmp", [128, 256])
        t_rs = Tok()
        m1 = sb("hm1", [128, 512]); m2 = sb("hm2", [128, 512])
        P1 = sb("hP1", [128, 2, 256])
        t_m = Tok(); t_p1 = Tok()
        ko_r = Rot([sb("hko%d" % i, [128, 512]) for i in range(2)])
        for o in range(2):
            k.op("pool", lambda en: en.memset(HSQ[:], 0.0), reads=[t_rs], writes=[t_hsq])
            for d_ in range(2):
                k.dma("sp", DEC[:], K["hk_DEC" + cfg.name][d_], writes=[t_dec])
                c0 = o * 512 + d_ * 256
                for i in range(ni):
                    pi_ = i % 2
                    k.op("pe", lambda en: en.matmul(PSm[pi_][:, 0:256], lhsT=h2T[d_][:, i:n:ni], rhs=w3[:, c0:c0 + 256], start=True, stop=True), reads=[t_h2, t_w], writes=[TP[pi_]])
                    k.op("dve", lambda en: en.tensor_tensor(out=hh[:], in0=PSm[pi_][:, 0:256], in1=DEC[:, i, :], op=ALU.mult), reads=[TP[pi_], t_dec], writes=[t_hh])
                    k.op("act", lambda en: en.activation(out=UF[d_][:].rearrange("p (g i c) -> p g i c", i=ni, c=cpg)[:, :, i, :], in_=hh[:].rearrange("p (g c) -> p g c", c=cpg), func=AF.Copy), reads=[t_hh], writes=[t_uf])
                    k.op("act", lambda en: en.activation(out=sq[:], in_=hh[:], func=AF.Square), reads=[t_hh], writes=[t_sq])
                    k.op("pool", lambda en: en.tensor_tensor(out=HSQ[:], in0=HSQ[:], in1=sq[:], op=ALU.add), reads=[t_sq, t_hsq], writes=[t_hsq])
            k.op("pe", lambda en: en.matmul(PSm[2][:, 0:256], lhsT=g.onesf[:], rhs=HSQ[:], start=True, stop=True), reads=[t_hsq, g.T_const], writes=[TP[2]])
            k.op("act", lambda en: en.activation(out=rtmp[:], in_=PSm[2][:, 0:256], func=AF.Ln, bias=epsc[:, 0:1]), reads=[TP[2], t_c], writes=[t_rs])
            k.op("act", lambda en: en.activation(out=RS[:], in_=rtmp[:], func=AF.Exp, scale=-0.5), reads=[t_rs], writes=[t_rs])
            k.op("dve", lambda en: en.tensor_tensor(out=rtmp[:].rearrange("p (g c) -> p g c", c=cpg), in0=RS[:].rearrange("p (g c) -> p g c", c=cpg), in1=MSK[:].unsqueeze(1).broadcast_to([128, ng, cpg]), op=ALU.mult), reads=[t_rs, t_c], writes=[t_rs])
            k.op("dve", lambda en: en.tensor_reduce(out=SC[:], in_=rtmp[:].rearrange("p (g c) -> p g c", c=cpg), axis=AX.X, op=ALU.add), reads=[t_rs], writes=[t_rs])
            k.op("dve", lambda en: en.memset(UF[1][0:1, :].rearrange("p (g i c) -> p g i c", i=ni, c=cpg)[:, :, 0, :], 0.0), reads=[t_uf], writes=[t_uf])
            for grp in range(ng):
                k.op("pe", lambda en: en.matmul(PSm[0][:], lhsT=UF[0][:, grp * 128:(grp + 1) * 128], rhs=F1[:, 0, :], start=True, stop=False), reads=[t_uf, t_c], writes=[TP[0]])
                k.op("pe", lambda en: en.matmul(PSm[0][:], lhsT=UF[1][:, grp * 128:(grp + 1) * 128], rhs=F1[:, 1, :], start=False, stop=True), reads=[t_uf, t_c], writes=[TP[0]])
                _hy_twiddle(k, PSm[0][:], TP[0], TW, t_c, m1, m2, t_m, P1[:, 0, :], P1[:, 1, :], t_p1, 256)
                k.op("pe", lambda en: en.matmul(PSm[1][:, 0:256], lhsT=G3[:, 0, :], rhs=P1[:, 0, :], start=True, stop=False), reads=[t_p1, t_c], writes=[TP[1]])
                k.op("pe", lambda en: en.matmul(PSm[1][:, 0:256], lhsT=G3[:, 2, :], rhs=P1[:, 1, :], start=False, stop=True), reads=[t_p1, t_c], writes=[TP[1]])
                k.op("pe", lambda en: en.matmul(PSm[1][:, 256:512], lhsT=G3[:, 0, :], rhs=P1[:, 1, :], start=False, stop=False), reads=[t_p1, t_c], writes=[TP[1]])
                k.op("pe", lambda en: en.matmul(PSm[1][:, 256:512], lhsT=G3[:, 1, :], rhs=P1[:, 0, :], start=False, stop=True), reads=[t_p1, t_c], writes=[TP[1]])
                ko, t_ko = ko_r.next()
                k.op("dve", lambda en: en.tensor_scalar(out=ko[:], in0=PSm[1][:], scalar1=SC[:, grp:grp + 1], scalar2=None, op0=ALU.mult), reads=[TP[1], t_rs], writes=[t_ko])
                k.dma("pool", KH[o, grp], ko[:], reads=[t_ko], writes=[g.T_KHAT])
        k.barrier()


def phase_hyena(g, l, s, cfg, tok0):
    nc, k, W, K = g.nc, g.k, g.W, g.K
    n, ni, ng, cpg, ncol = cfg.n, cfg.ni, cfg.ng, cfg.cpg, cfg.ncol
    KH = g.KHAT[cfg.name]
    with ExitStack() as es:
        sb = lambda nm, s_, d=F32: es.enter_context(nc.sbuf_tensor(_nm(nm), s_, d))
        ps = lambda nm, s_, d=F32: es.enter_context(nc.psum_tensor(_nm(nm), s_, d))
        PSm = [ps("yps%d" % i, [128, 512]) for i in range(6)]
        TP = [Tok(True) for _ in range(6)]
        PT = [ps("ypt%d" % i, [128, 8, 128], BF16) for i in range(2)]
        TPT = [Tok(True) for _ in range(2)]
        t_c = Tok()
        cf = sb("ycf", [128, 1024])
        F1b = sb("yF1", [128, 512], BF16); G3 = sb("yG", [128, 3, 128], BF16); GI = sb("yGI", [128, 2, 256], BF16); FI = sb("yFI", [128, 2, 2, 128], BF16)
        TW = sb("yTW", [128, 2, 512]); TWI = sb("yTWI", [128, 2, 2, 256])
        k.dma("sp", cf[:, 0:512], K["hk_F1"][0], writes=[t_c])
        k.op("dve", lambda en: en.tensor_copy(out=F1b[:], in_=cf[:, 0:512]), reads=[t_c], writes=[t_c])
        k.dma("sp", cf[:, 0:384].rearrange("p (a n) -> p a n", a=3), K["hk_G" + cfg.name].rearrange("a p n -> p a n"), reads=[t_c], writes=[t_c])
        k.op("dve", lambda en: en.tensor_copy(out=G3[:], in_=cf[:, 0:384].rearrange("p (a n) -> p a n", a=3)), reads=[t_c], writes=[t_c])
        k.dma("sp", cf[:, 0:512].rearrange("p (a n) -> p a n", a=2), K["hk_GI" + cfg.name].rearrange("a p n -> p a n"), reads=[t_c], writes=[t_c])
        k.op("dve", lambda en: en.tensor_copy(out=GI[:], in_=cf[:, 0:512].rearrange("p (a n) -> p a n", a=2)), reads=[t_c], writes=[t_c])
        k.dma("sp", cf[:, 0:512].rearrange("p (b a n) -> p b a n", b=2, a=2), K["hk_FI" + cfg.name].rearrange("b a p n -> p b a n"), reads=[t_c], writes=[t_c])
        k.op("dve", lambda en: en.tensor_copy(out=FI[:], in_=cf[:, 0:512].rearrange("p (b a n) -> p b a n", b=2, a=2)), reads=[t_c], writes=[t_c])
        k.dma("sp", TW[:], K["hk_TW" + cfg.name].rearrange("a p n -> p a n"), writes=[t_c])
        k.dma("sp", TWI[:], K["hk_TWI" + cfg.name].rearrange("b a p n -> p b a n"), writes=[t_c])
        cw = sb("ycw", [128, 6, 3]); cb = sb("ycb", [128, 6]); BR = sb("yBR", [128, 2, 256])
        for kk in range(3):
            k.dma("sp", cw[:, :, kk:kk + 1], W["hy_conv_w"][l, kk, :].rearrange("(c p o) -> p c o", p=128, o=1), writes=[t_c], allow_slow_non_contiguous=True)
        k.dma("sp", cb[:].unsqueeze(2), W["hy_conv_b"][l].rearrange("(c p o) -> p c o", p=128, o=1), writes=[t_c], allow_slow_non_contiguous=True)
        k.dma("sp", BR[:], W["hy_bias"][l:l + 1].broadcast_to([128, 2, 256]), writes=[t_c])
        U = {nm: sb("yU" + nm, [128, ncol], BF16) for nm in ("gt", "x2", "v", "x1")}
        t_U = {nm: Tok() for nm in ("gt", "x2", "v", "x1", "z1")}
        names = ["v", "v", "x1", "x1", "x2", "x2", "gt", "gt"]
        with ExitStack() as es1:
            sb1 = lambda nm, s_, d=F32: es1.enter_context(nc.sbuf_tensor(_nm(nm), s_, d))
            hT = sb1("yhT", [128, 8, n], BF16)
            whb_r = Rot([sb1("ywhb%d" % i, [128, 8, 128], BF16) for i in range(2)])
            t_h = Tok()
            k.dma("sp", hT[:], g.HT[:, :, tok0:tok0 + n], reads=[g.T_HT], writes=[t_h])
            PJ = sb1("yPJ", [128, n]); CV = sb1("yCV", [128, n]); CVb = sb1("yCVb", [128, n], BF16)
            t_pj = Tok(); t_cv = Tok(); t_cvb = Tok()
            bw = min(512, n)
            for c8 in range(8):
                whb, t_whb = whb_r.next()
                k.dma("sp", whb[:], g.WINB[:, :, O_HY + c8 * 128:O_HY + (c8 + 1) * 128], reads=[g.T_WINB], writes=[t_whb])
                for bi, b0 in enumerate(range(0, n, bw)):
                    pi_ = bi % 2
                    for kc in range(8):
                        k.op("pe", lambda en: en.matmul(PSm[pi_][:, 0:bw], lhsT=whb[:, kc, :], rhs=hT[:, kc, b0:b0 + bw], start=(kc == 0), stop=(kc == 7)), reads=[t_h, t_whb], writes=[TP[pi_]])
                    if c8 < 6:
                        k.op("act", lambda en: en.activation(out=PJ[:, b0:b0 + bw], in_=PSm[pi_][:, 0:bw], func=AF.Copy), reads=[TP[pi_]], writes=[t_pj])
                    else:
                        k.op("act", lambda en: en.activation(out=CVb[:, b0:b0 + bw], in_=PSm[pi_][:, 0:bw], func=AF.Silu), reads=[TP[pi_]], writes=[t_cvb])
                if c8 < 6:
                    k.op("dve", lambda en: en.tensor_scalar(out=CV[:], in0=PJ[:], scalar1=cw[:, c8, 1:2], scalar2=cb[:, c8:c8 + 1], op0=ALU.mult, op1=ALU.add), reads=[t_pj, t_c], writes=[t_cv])
                    k.op("dve", lambda en: en.scalar_tensor_tensor(out=CV[:, 1:n], in0=PJ[:, 0:n - 1], scalar=cw[:, c8, 0:1], in1=CV[:, 1:n], op0=ALU.mult, op1=ALU.add), reads=[t_pj, t_c, t_cv], writes=[t_cv])
                    k.op("dve", lambda en: en.scalar_tensor_tensor(out=CVb[:, 0:n - 1], in0=PJ[:, 1:n], scalar=cw[:, c8, 2:3], in1=CV[:, 0:n - 1], op0=ALU.mult, op1=ALU.add), reads=[t_pj, t_c, t_cv], writes=[t_cvb])
                    k.op("dve", lambda en: en.tensor_copy(out=CVb[:, n - 1:n], in_=CV[:, n - 1:n]), reads=[t_cv, t_cvb], writes=[t_cvb])
                nm = names[c8]
                g0 = (c8 % 2) * (128 // cpg)
                Uv = U[nm][:].rearrange("p (g i c) -> p g i c", i=ni, c=cpg)
                nb4 = min(4, ni)
                for i0 in range(0, ni, nb4):
                    pt = (i0 // nb4) % 2
                    for ii in range(nb4):
                        k.op("pe", lambda en: en.transpose(PT[pt][:, ii, :], CVb[:, i0 + ii:n:ni], g.ident[:]), reads=[t_cvb, g.T_ident], writes=[TPT[pt]])
                    k.op("act" if (i0 // nb4) % 2 else "dve",
                         (lambda en: en.activation(out=Uv[:, g0:g0 + 128 // cpg, i0:i0 + nb4, :].rearrange("p g i c -> p i g c"), in_=PT[pt][:, 0:nb4, :].rearrange("p i (g c) -> p i g c", c=cpg), func=AF.Copy)) if (i0 // nb4) % 2 else
                         (lambda en: en.tensor_copy(out=Uv[:, g0:g0 + 128 // cpg, i0:i0 + nb4, :].rearrange("p g i c -> p i g c"), in_=PT[pt][:, 0:nb4, :].rearrange("p i (g c) -> p i g c", c=cpg))),
                         reads=[TPT[pt]], writes=[t_U[nm]])
            k.barrier()
        with ExitStack() as es2:
            sb2 = lambda nm, s_, d=F32: es2.enter_context(nc.sbuf_tensor(_nm(nm), s_, d))
            U["z1"] = sb2("yUz1", [128, ncol], BF16)
            BW = [[sb2("yBW%d%d" % (bc, ri), [128, ncol], BF16) for ri in range(2)] for bc in range(2)]
            t_bw = Tok()
            mm_r = Rot([(sb2("ym1_%d" % i, [128, 512]), sb2("ym2_%d" % i, [128, 512])) for i in range(4)])
            P1_r = Rot([sb2("yP1_%d" % i, [128, 2, 256], BF16) for i in range(2)])
            Y_r = Rot([sb2("yY_%d" % i, [128, 2, 256], BF16) for i in range(2)])
            kh_r = Rot([sb2("ykh%d" % i, [128, 512]) for i in range(2)])
            e1 = sb2("ye1", [128, 512]); e2 = sb2("ye2", [128, 512]); e3 = sb2("ye3", [128, 512]); t_e = Tok()
            gpt = 512 // (ni * cpg)

            def conv(src_nm, o, epilogue):
                Uin = U[src_nm]
                for grp in range(ng):
                    kh, t_kh = kh_r.next()
                    k.dma("sp", kh[:], KH[o, grp], reads=[g.T_KHAT], writes=[t_kh])
                    s1 = grp % 2
                    sx = 2 + grp % 2
                    (m1, m2), t_m = mm_r.next()
                    P1, t_p1 = P1_r.next()
                    Y, t_y = Y_r.next()
                    k.op("pe", lambda en: en.matmul(PSm[s1][:], lhsT=Uin[:, grp * 128:(grp + 1) * 128], rhs=F1b[:], start=True, stop=True), reads=[t_U[src_nm], t_c], writes=[TP[s1]])
                    _hy_twiddle(k, PSm[s1][:], TP[s1], TW, t_c, m1, m2, t_m, P1[:, 0, :], P1[:, 1, :], t_p1, 256)
                    k.op("pe", lambda en: en.matmul(PSm[sx][:, 0:256], lhsT=G3[:, 0, :], rhs=P1[:, 0, :], start=True, stop=False), reads=[t_p1, t_c], writes=[TP[sx]])
                    k.op("pe", lambda en: en.matmul(PSm[sx][:, 0:256], lhsT=G3[:, 2, :], rhs=P1[:, 1, :], start=False, stop=True), reads=[t_p1, t_c], writes=[TP[sx]])
                    k.op("pe", lambda en: en.matmul(PSm[sx][:, 256:512], lhsT=G3[:, 0, :], rhs=P1[:, 1, :], start=False, stop=False), reads=[t_p1, t_c], writes=[TP[sx]])
                    k.op("pe", lambda en: en.matmul(PSm[sx][:, 256:512], lhsT=G3[:, 1, :], rhs=P1[:, 0, :], start=False, stop=True), reads=[t_p1, t_c], writes=[TP[sx]])
                    (m1, m2), t_m = mm_r.next()
                    Xv = PSm[sx][:].rearrange("p (a n) -> p a n", a=2)
                    k.op("dve", lambda en: en.tensor_tensor(out=m1[:].rearrange("p (a n) -> p a n", a=2), in0=Xv, in1=kh[:, 0:256].unsqueeze(1).broadcast_to([128, 2, 256]), op=ALU.mult), reads=[TP[sx], t_kh], writes=[t_m])
                    k.op("dve", lambda en: en.tensor_tensor(out=m2[:].rearrange("p (a n) -> p a n", a=2), in0=Xv, in1=kh[:, 256:512].unsqueeze(1).broadcast_to([128, 2, 256]), op=ALU.mult), reads=[TP[sx], t_kh, t_m], writes=[t_m])
                    k.op("pool", lambda en: en.tensor_tensor(out=Y[:, 0, :], in0=m1[:, 0:256], in1=m2[:, 256:512], op=ALU.subtract), reads=[t_m], writes=[t_y])
                    k.op("pool", lambda en: en.tensor_tensor(out=Y[:, 1, :], in0=m2[:, 0:256], in1=m1[:, 256:512], op=ALU.add), reads=[t_m, t_y], writes=[t_y])
                    for bc in range(2):
                        pi_ = 4 + bc
                        (m1, m2), t_m = mm_r.next()
                        k.op("pe", lambda en: en.matmul(PSm[pi_][:, 0:256], lhsT=Y[:, 0, bc * 128:(bc + 1) * 128], rhs=GI[:, 0, :], start=True, stop=False), reads=[t_y, t_c], writes=[TP[pi_]])
                        k.op("pe", lambda en: en.matmul(PSm[pi_][:, 0:256], lhsT=Y[:, 1, bc * 128:(bc + 1) * 128], rhs=GI[:, 1, :], start=False, stop=True), reads=[t_y, t_c], writes=[TP[pi_]])
                        k.op("dve", lambda en: en.tensor_tensor(out=m1[:, 0:256], in0=PSm[pi_][:, 0:256], in1=TWI[:, bc, 0, :], op=ALU.mult), reads=[TP[pi_], t_c], writes=[t_m])
                        k.op("dve", lambda en: en.tensor_tensor(out=m2[:, 0:256], in0=PSm[pi_][:, 0:256], in1=TWI[:, bc, 1, :], op=ALU.mult), reads=[TP[pi_], t_c, t_m], writes=[t_m])
                        k.op("pool", lambda en: en.tensor_tensor(out=BW[bc][0][:, grp * 128:(grp + 1) * 128], in0=m1[:, 0:128], in1=m2[:, 128:256], op=ALU.subtract), reads=[t_m], writes=[t_bw])
                        k.op("pool", lambda en: en.tensor_tensor(out=BW[bc][1][:, grp * 128:(grp + 1) * 128], in0=m2[:, 0:128], in1=m1[:, 128:256], op=ALU.add), reads=[t_m, t_bw], writes=[t_bw])
                for ct in range(ncol // 512):
                    pi_ = 4 + ct % 2
                    cs = slice(ct * 512, (ct + 1) * 512)
                    idx = 0
                    for bc in range(2):
                        for ri in range(2):
                            k.op("pe", lambda en: en.matmul(PSm[pi_][:], lhsT=FI[:, bc, ri, :], rhs=BW[bc][ri][:, cs], start=(idx == 0), stop=(idx == 3)), reads=[t_bw, t_c], writes=[TP[pi_]])
                            idx += 1
                    epilogue(ct, cs, PSm[pi_], TP[pi_], o)

            def v4(ap):
                return ap.rearrange("p (g i c) -> p g i c", i=ni, c=cpg)

            def epi1(ct, cs, ps_, tps, o):
                bview = BR[:, o, ct * gpt * cpg:(ct + 1) * gpt * cpg].rearrange("p (g c) -> p g c", c=cpg).unsqueeze(2).broadcast_to([128, gpt, ni, cpg])
                k.op("pool", lambda en: en.tensor_tensor(out=v4(e1[:]), in0=v4(U["v"][:, cs]), in1=bview, op=ALU.mult), reads=[t_U["v"], t_c, t_e], writes=[t_e])
                k.op("dve", lambda en: en.tensor_tensor(out=e2[:], in0=ps_[:], in1=e1[:], op=ALU.add), reads=[tps, t_e], writes=[t_e])
                k.op("pool", lambda en: en.tensor_tensor(out=U["z1"][:, cs], in0=e2[:], in1=U["x1"][:, cs], op=ALU.mult), reads=[t_e, t_U["x1"]], writes=[t_U["z1"]])

            ZN = U["v"]

            def epi2(ct, cs, ps_, tps, o):
                bview = BR[:, o, ct * gpt * cpg:(ct + 1) * gpt * cpg].rearrange("p (g c) -> p g c", c=cpg).unsqueeze(2).broadcast_to([128, gpt, ni, cpg])
                k.op("pool", lambda en: en.tensor_tensor(out=v4(e1[:]), in0=v4(U["z1"][:, cs]), in1=bview, op=ALU.mult), reads=[t_U["z1"], t_c, t_e], writes=[t_e])
                k.op("dve", lambda en: en.tensor_tensor(out=e2[:], in0=ps_[:], in1=e1[:], op=ALU.add), reads=[tps, t_e], writes=[t_e])
                k.op("pool", lambda en: en.tensor_tensor(out=e3[:], in0=e2[:], in1=U["x2"][:, cs], op=ALU.mult), reads=[t_e, t_U["x2"]], writes=[t_e])
                outv = ZN[:].rearrange("p (i g c) -> p g i c", g=ng, c=cpg)[:, ct * gpt:(ct + 1) * gpt, :, :]
                k.op("dve", lambda en: en.tensor_tensor(out=outv, in0=v4(e3[:]), in1=v4(U["gt"][:, cs]), op=ALU.mult), reads=[t_e, t_U["gt"]], writes=[t_U["v"]])

            conv("v", 0, epi1)
            conv("z1", 1, epi2)
            ZT = U["x1"][:].rearrange("p (h t) -> p h t", h=2)
            cnt = 0
            for ch in range(2):
                nb4 = min(4, ni)
                for i0 in range(0, ni, nb4):
                    pt = cnt % 2
                    cnt += 1
                    for ii in range(nb4):
                        i = i0 + ii
                        k.op("pe", lambda en: en.transpose(PT[pt][:, ii, :], ZN[:, i * 256 + ch * 128:i * 256 + ch * 128 + 128], g.ident[:]), reads=[t_U["v"], g.T_ident], writes=[TPT[pt]])
                    outv = ZT[:, ch, :].rearrange("p (j i) -> p i j", i=ni)[:, i0:i0 + nb4, :]
                    k.op("act" if cnt % 2 else "dve",
                         (lambda en: en.activation(out=outv, in_=PT[pt][:, 0:nb4, :], func=AF.Copy)) if cnt % 2 else (lambda en: en.tensor_copy(out=outv, in_=PT[pt][:, 0:nb4, :])),
                         reads=[TPT[pt], t_U["x1"]], writes=[t_U["x1"]])
            k.dma("pool", g.CATT[:, 6:8, tok0:tok0 + n], ZT, reads=[t_U["x1"]], writes=[g.T_CATT])
            k.barrier()


TC = 16
NCH = T // TC
NDBL = 9


def ssm_host_layouts(inputs):
    f = lambda n: np.asarray(inputs[n], np.float32)
    lr, li, ls = f("ssm_lambda_re"), f("ssm_lambda_im"), f("ssm_log_step")
    br, bi, cr, ci = f("ssm_b_re"), f("ssm_b_im"), f("ssm_c_re"), f("ssm_c_im")
    lam = np.zeros((DEPTH, 128, 2, 16), np.float32)
    lsb = np.zeros((DEPTH, 128, 16), np.float32)
    Bp = np.zeros((DEPTH, 128, 2, 16, 64), np.float32)
    Cp = np.zeros((DEPTH, 128, 2, 16, 64), np.float32)
    for d in range(2):
        for q in range(8):
            for e in range(2):
                grp = 2 * q + e
                rows = slice(e * 64, (e + 1) * 64)
                dq = d * 8 + q
                lam[:, rows, 0, dq] = lr[:, d, grp, :]
                lam[:, rows, 1, dq] = li[:, d, grp, :]
                lsb[:, rows, dq] = ls[:, d, grp][:, None]
                pp = q % 2
                c0 = pp * 32 + e * 16
                Bp[:, rows, 0, dq, c0:c0 + 16] = br[:, d, grp, :, :]
                Bp[:, rows, 1, dq, c0:c0 + 16] = bi[:, d, grp, :, :]
                Cp[:, rows, 0, dq, c0:c0 + 16] = cr[:, d, grp, :, :].transpose(0, 2, 1)
                Cp[:, rows, 1, dq, c0:c0 + 16] = ci[:, d, grp, :, :].transpose(0, 2, 1)
    out = {"ssm_lam": lam, "ssm_ls": lsb, "ssm_Bp": Bp, "ssm_Cp": Cp}
    dd = f("ssm_d")
    gb = f("ssm_glu_b")
    out["ssm_cols"] = np.ascontiguousarray(np.stack([dd.reshape(DEPTH, 2, 128).transpose(0, 2, 1), gb.reshape(DEPTH, 2, 128).transpose(0, 2, 1)], -1))
    out["ssm_glu_w"] = f("ssm_glu_w")
    return out


def phase_ssm_weights(g, l):
    nc, k, W = g.nc, g.k, g.W
    PI = math.pi
    with ExitStack() as es:
        sb = lambda nm, s_, d=F32: es.enter_context(nc.sbuf_tensor(_nm(nm), s_, d))
        ps = lambda nm, s_, d=F32: es.enter_context(nc.psum_tensor(_nm(nm), s_, d))
        t = Tok()
        lam = sb("slam", [128, 2, 16]); ls = sb("sls", [128, 16])
        Bp = sb("sBp", [128, 2, 16, 64]); Cp = sb("sCp", [128, 2, 16, 64])
        k.dma("sp", lam[:], W["ssm_lam"][l], writes=[t])
        k.dma("sp", ls[:], W["ssm_ls"][l], writes=[t])
        k.dma("sp", Bp[:], W["ssm_Bp"][l], writes=[t])
        k.dma("sp", Cp[:], W["ssm_Cp"][l], writes=[t])
        sc = sb("ssc", [128, 24, 16])
        R = lambda i: sc[:, i, :]
        STEP, RHO, TH, MK, SIN, COS, AR, AI, DEN, NR, NI_, CRc, CIc, T1, T2 = range(15)

        def dv(fn, eng="dve"):
            k.op(eng, fn, reads=[t], writes=[t])

        dv(lambda en: en.activation(out=R(STEP), in_=ls[:], func=AF.Exp), "act")
        dv(lambda en: en.tensor_tensor(out=R(T1), in0=lam[:, 0, :], in1=R(STEP), op=ALU.mult))
        dv(lambda en: en.activation(out=R(RHO), in_=R(T1), func=AF.Exp), "act")
        dv(lambda en: en.tensor_tensor(out=R(TH), in0=lam[:, 1, :], in1=R(STEP), op=ALU.mult))
        for thr in (PI, 3 * PI, 5 * PI, 7 * PI):
            dv(lambda en: en.tensor_scalar(out=R(MK), in0=R(TH), scalar1=thr, scalar2=-2 * PI, op0=ALU.is_gt, op1=ALU.mult))
            if thr == PI:
                dv(lambda en: en.tensor_copy(out=R(T1), in_=R(MK)))
            else:
                dv(lambda en: en.tensor_tensor(out=R(T1), in0=R(T1), in1=R(MK), op=ALU.add))
        dv(lambda en: en.tensor_tensor(out=R(TH), in0=R(TH), in1=R(T1), op=ALU.add))
        dv(lambda en: en.activation(out=R(SIN), in_=R(TH), func=AF.Sin), "act")
        dv(lambda en: en.tensor_scalar(out=R(T2), in0=R(TH), scalar1=-1.0, scalar2=None, op0=ALU.mult))
        dv(lambda en: en.tensor_tensor(out=R(T1), in0=R(TH), in1=R(T2), op=ALU.max))
        dv(lambda en: en.tensor_scalar(out=R(T1), in0=R(T1), scalar1=-1.0, scalar2=PI / 2, op0=ALU.mult, op1=ALU.add))
        dv(lambda en: en.activation(out=R(COS), in_=R(T1), func=AF.Sin), "act")
        dv(lambda en: en.tensor_tensor(out=R(AR), in0=R(RHO), in1=R(COS), op=ALU.mult))
        dv(lambda en: en.tensor_tensor(out=R(AI), in0=R(RHO), in1=R(SIN), op=ALU.mult))
        dv(lambda en: en.tensor_tensor(out=R(DEN), in0=lam[:, 0, :], in1=lam[:, 0, :], op=ALU.mult))
        dv(lambda en: en.tensor_tensor(out=R(T1), in0=lam[:, 1, :], in1=lam[:, 1, :], op=ALU.mult))
        dv(lambda en: en.tensor_tensor(out=R(DEN), in0=R(DEN), in1=R(T1), op=ALU.add))
        dv(lambda en: en.reciprocal(out=R(DEN), in_=R(DEN)))
        dv(lambda en: en.tensor_scalar(out=R(T2), in0=R(AR), scalar1=-1.0, scalar2=None, op0=ALU.add))
        dv(lambda en: en.tensor_tensor(out=R(NR), in0=R(T2), in1=lam[:, 0, :], op=ALU.mult))
        dv(lambda en: en.tensor_tensor(out=R(T1), in0=R(AI), in1=lam[:, 1, :], op=ALU.mult))
        dv(lambda en: en.tensor_tensor(out=R(NR), in0=R(NR), in1=R(T1), op=ALU.add))
        dv(lambda en: en.tensor_tensor(out=R(NI_), in0=R(AI), in1=lam[:, 0, :], op=ALU.mult))
        dv(lambda en: en.tensor_tensor(out=R(T1), in0=R(T2), in1=lam[:, 1, :], op=ALU.mult))
        dv(lambda en: en.tensor_tensor(out=R(NI_), in0=R(NI_), in1=R(T1), op=ALU.subtract))
        dv(lambda en: en.tensor_tensor(out=R(CRc), in0=R(NR), in1=R(DEN), op=ALU.mult))
        dv(lambda en: en.tensor_tensor(out=R(CIc), in0=R(NI_), in1=R(DEN), op=ALU.mult))
        PW = sb("sPW", [128, 2, 17, 16])
        dv(lambda en: en.memset(PW[:, 0, 0, :], 1.0))
        dv(lambda en: en.memset(PW[:, 1, 0, :], 0.0))
        for n_ in range(16):
            dv(lambda en: en.tensor_tensor(out=R(T1), in0=PW[:, 0, n_, :], in1=R(AR), op=ALU.mult))
            dv(lambda en: en.tensor_tensor(out=R(T2), in0=PW[:, 1, n_, :], in1=R(AI), op=ALU.mult))
            dv(lambda en: en.tensor_tensor(out=PW[:, 0, n_ + 1, :], in0=R(T1), in1=R(T2), op=ALU.subtract))
            dv(lambda en: en.tensor_tensor(out=R(T1), in0=PW[:, 0, n_, :], in1=R(AI), op=ALU.mult))
            dv(lambda en: en.tensor_tensor(out=R(T2), in0=PW[:, 1, n_, :], in1=R(AR), op=ALU.mult))
            dv(lambda en: en.tensor_tensor(out=PW[:, 1, n_ + 1, :], in0=R(T1), in1=R(T2), op=ALU.add))
        A2 = g.lw.A2
        dv(lambda en: en.tensor_copy(out=A2[:, 0, 0, :], in_=PW[:, 0, 16, :]))
        dv(lambda en: en.tensor_copy(out=A2[:, 0, 1, :], in_=PW[:, 1, 16, :]))
        for kk in range(NDBL):
            if kk > 0:
                dv(lambda en: en.tensor_tensor(out=R(T1), in0=A2[:, kk - 1, 0, :], in1=A2[:, kk - 1, 0, :], op=ALU.mult))
                dv(lambda en: en.tensor_tensor(out=R(T2), in0=A2[:, kk - 1, 1, :], in1=A2[:, kk - 1, 1, :], op=ALU.mult))
                dv(lambda en: en.tensor_tensor(out=A2[:, kk, 0, :], in0=R(T1), in1=R(T2), op=ALU.subtract))
                dv(lambda en: en.tensor_tensor(out=R(T1), in0=A2[:, kk - 1, 0, :], in1=A2[:, kk - 1, 1, :], op=ALU.mult))
                dv(lambda en: en.tensor_scalar(out=A2[:, kk, 1, :], in0=R(T1), scalar1=2.0, scalar2=None, op0=ALU.mult))
            dv(lambda en: en.tensor_scalar(out=A2[:, kk, 2, :], in0=A2[:, kk, 1, :], scalar1=-1.0, scalar2=None, op0=ALU.mult))
        bc3 = lambda ap: ap.unsqueeze(2).broadcast_to([128, 16, 64])
        Bb = sb("sBb", [128, 2, 16, 64])
        big = [sb("sbig%d" % i, [128, 16, 64]) for i in range(4)]
        dv(lambda en: en.tensor_tensor(out=big[0][:], in0=Bp[:, 0], in1=bc3(R(CRc)), op=ALU.mult))
        dv(lambda en: en.tensor_tensor(out=big[1][:], in0=Bp[:, 1], in1=bc3(R(CIc)), op=ALU.mult))
        dv(lambda en: en.tensor_tensor(out=Bb[:, 0], in0=big[0][:], in1=big[1][:], op=ALU.subtract))
        dv(lambda en: en.tensor_tensor(out=big[0][:], in0=Bp[:, 1], in1=bc3(R(CRc)), op=ALU.mult))
        dv(lambda en: en.tensor_tensor(out=big[1][:], in0=Bp[:, 0], in1=bc3(R(CIc)), op=ALU.mult))
        dv(lambda en: en.tensor_tensor(out=Bb[:, 1], in0=big[0][:], in1=big[1][:], op=ALU.add))
        Bbb = sb("sBbb", [128, 2, 16, 64], BF16)
        dv(lambda en: en.tensor_copy(out=Bbb[:], in_=Bb[:]))
        CR = sb("sCR", [128, 2, 16, 17, 64], BF16)
        engs = ("dve", "pool")
        for n_ in range(17):
            e1 = engs[n_ % 2]
            dv(lambda en: en.tensor_tensor(out=big[0][:], in0=Cp[:, 0], in1=bc3(PW[:, 0, n_, :]), op=ALU.mult), e1)
            dv(lambda en: en.tensor_tensor(out=big[1][:], in0=Cp[:, 1], in1=bc3(PW[:, 1, n_, :]), op=ALU.mult), e1)
            dv(lambda en: en.tensor_tensor(out=CR[:, 0, :, n_, :], in0=big[0][:], in1=big[1][:], op=ALU.subtract), e1)
            dv(lambda en: en.tensor_tensor(out=big[2][:], in0=Cp[:, 0], in1=bc3(PW[:, 1, n_, :]), op=ALU.mult), e1)
            dv(lambda en: en.tensor_tensor(out=big[3][:], in0=Cp[:, 1], in1=bc3(PW[:, 0, n_, :]), op=ALU.mult), e1)
            dv(lambda en: en.tensor_tensor(out=big[2][:], in0=big[2][:], in1=big[3][:], op=ALU.add), e1)
            dv(lambda en: en.tensor_scalar(out=CR[:, 1, :, n_, :], in0=big[2][:], scalar1=-1.0, scalar2=None, op0=ALU.mult), e1)
        k.dma("pool", g.SSM_L3, CR[:], reads=[t], writes=[g.T_SSMW])
        L1W = sb("sL1W", [128, 2, 16, 2, 128], BF16)
        dv(lambda en: en.memset(L1W[:], 0.0), "pool")
        PS_ = [ps("sps%d" % i, [128, 512]) for i in range(2)]
        TPS_ = [Tok(True) for _ in range(2)]
        for d in range(2):
            for Q in range(4):
                hc, Ql = Q // 2, Q % 2
                for half in range(2):
                    first = True
                    for pp in range(2):
                        dq = d * 8 + 2 * Q + pp
                        for ri in range(2):
                            rhs = CR[:, ri, dq, half * 8:(half + 1) * 8, :].rearrange("p n c -> p (n c)")
                            k.op("pe", lambda en: en.matmul(PS_[half][Ql * 64:(Ql + 1) * 64, :], lhsT=Bbb[:, ri, dq, :], rhs=rhs, start=first, stop=(pp == 1 and ri == 1)), reads=[t], writes=[TPS_[half]])
                            first = False
                    k.op("act" if half else "dve",
                         (lambda en: en.activation(out=L1W[Ql * 64:(Ql + 1) * 64, d, half * 8:(half + 1) * 8, hc, Ql * 64:(Ql + 1) * 64], in_=PS_[half][Ql * 64:(Ql + 1) * 64, :].rearrange("p (n c) -> p n c", c=64), func=AF.Copy)) if half else
                         (lambda en: en.tensor_copy(out=L1W[Ql * 64:(Ql + 1) * 64, d, half * 8:(half + 1) * 8, hc, Ql * 64:(Ql + 1) * 64], in_=PS_[half][Ql * 64:(Ql + 1) * 64, :].rearrange("p (n c) -> p n c", c=64))),
                         reads=[TPS_[half], t], writes=[t])
        k.dma("pool", g.SSM_L1, L1W[:], reads=[t], writes=[g.T_SSMW])
        ZZ = sb("sZZ", [128, 2, 16, 64], BF16)
        PT = [ps("spt%d" % i, [128, 8, 128], BF16) for i in range(2)]
        TPT = [Tok(True) for _ in range(2)]
        sg_r = Rot([sb("ssg%d" % i, [128, 2, 2, 2, 128], BF16) for i in range(2)])
        for n_ in range(16):
            dv(lambda en: en.tensor_tensor(out=big[0][:], in0=Bb[:, 0], in1=bc3(PW[:, 0, n_, :]), op=ALU.mult))
            dv(lambda en: en.tensor_tensor(out=big[1][:], in0=Bb[:, 1], in1=bc3(PW[:, 1, n_, :]), op=ALU.mult))
            dv(lambda en: en.tensor_tensor(out=ZZ[:, 0], in0=big[0][:], in1=big[1][:], op=ALU.subtract))
            dv(lambda en: en.tensor_tensor(out=big[2][:], in0=Bb[:, 0], in1=bc3(PW[:, 1, n_, :]), op=ALU.mult), "pool")
            dv(lambda en: en.tensor_tensor(out=big[3][:], in0=Bb[:, 1], in1=bc3(PW[:, 0, n_, :]), op=ALU.mult), "pool")
            dv(lambda en: en.tensor_tensor(out=ZZ[:, 1], in0=big[2][:], in1=big[3][:], op=ALU.add), "pool")
            for d in range(2):
                sg, t_sg = sg_r.next()
                for hc in range(2):
                    pt = hc
                    for ri in range(2):
                        for Ql in range(2):
                            for pp in range(2):
                                dq = d * 8 + 2 * (hc * 2 + Ql) + pp
                                k.op("pe", lambda en: en.transpose(PT[pt][Ql * 64:(Ql + 1) * 64, ri * 2 + pp, :], ZZ[:, ri, dq, :], g.ident[:]), reads=[t, g.T_ident], writes=[TPT[pt]])
                    k.op("act" if hc else "dve",
                         (lambda en: en.activation(out=sg[:, hc].rearrange("p r q n -> p (r q) n"), in_=PT[pt][:, 0:4, :], func=AF.Copy)) if hc else
                         (lambda en: en.tensor_copy(out=sg[:, hc].rearrange("p r q n -> p (r q) n"), in_=PT[pt][:, 0:4, :])),
                         reads=[TPT[pt]], writes=[t_sg])
                k.dma("pool", g.SSM_SG[:, d, n_], sg[:], reads=[t_sg], writes=[g.T_SSMW])
        k.barrier()


def phase_ssm(g, l, s, need_ctx):
    nc, k, W, lw = g.nc, g.k, g.W, g.lw
    A2 = lw.A2
    with ExitStack() as es:
        sb = lambda nm, s_, d=F32: es.enter_context(nc.sbuf_tensor(_nm(nm), s_, d))
        ps = lambda nm, s_, d=F32: es.enter_context(nc.psum_tensor(_nm(nm), s_, d))
        PSm = [ps("mps%d" % i, [128, 512]) for i in range(8)]
        TP = [Tok(True) for _ in range(8)]
        ujm = sb("mujm", [128, 2, TC, NCH], BF16)
        gjm = sb("mgjm", [128, 2, TC, NCH], BF16)
        Sb = [[sb("mSb%d%d" % (d, ri), [128, 8, NCH], BF16) for ri in range(2)] for d in range(2)]
        t_u = Tok(); t_g = Tok(); t_sb = Tok()
        cols = sb("mcols", [128, 2, 2])
        GW = sb("mGW", [128, 2, 256], BF16)
        t_c = Tok()
        k.dma("sp", cols[:], W["ssm_cols"][l], writes=[t_c])
        with ExitStack() as es1:
            sb1 = lambda nm, s_, d=F32: es1.enter_context(nc.sbuf_tensor(_nm(nm), s_, d))
            wss = sb1("mwss", [128, 8, 512], BF16)
            gwf = sb1("mgwf", [128, 2, 256])
            t_w = Tok()
            k.dma("sp", wss[:], g.WINB[:, :, O_SU:O_SU + 512], reads=[g.T_WINB], writes=[t_w])
            k.dma("sp", gwf[:], W["ssm_glu_w"][l].rearrange("(kc p) n -> p kc n", p=128), writes=[t_w])
            k.op("dve", lambda en: en.tensor_copy(out=GW[:], in_=gwf[:]), reads=[t_w], writes=[t_c])
            hT_r = Rot([sb1("mhT%d" % i, [128, 8, 512], BF16) for i in range(2)])
            blocks = [(0, 256)] + [(256 + 512 * b, 512) for b in range(8)]
            cnt = 0
            for (t0, nb) in blocks:
                hT, t_h = hT_r.next()
                k.dma("sp", hT[:, :, 0:nb], g.HT[:, :, t0:t0 + nb], reads=[g.T_HT], writes=[t_h])
                c0, ncb = t0 // TC, nb // TC
                for cc in range(4):
                    pi_ = cnt % 4
                    cnt += 1
                    for kc in range(8):
                        k.op("pe", lambda en: en.matmul(PSm[pi_][:, 0:nb], lhsT=wss[:, kc, cc * 128:(cc + 1) * 128], rhs=hT[:, kc, 0:nb], start=(kc == 0), stop=(kc == 7)), reads=[t_w, t_h], writes=[TP[pi_]])
                    src = PSm[pi_][:, 0:nb].rearrange("p (c j) -> p j c", j=TC)
                    if cc < 2:
                        k.op("dve", lambda en: en.tensor_copy(out=ujm[:, cc, :, c0:c0 + ncb], in_=src), reads=[TP[pi_]], writes=[t_u])
                    else:
                        k.op("act", lambda en: en.activation(out=gjm[:, cc - 2, :, c0:c0 + ncb], in_=src, func=AF.Silu), reads=[TP[pi_]], writes=[t_g])
            k.barrier()
        with ExitStack() as es2:
            sb2 = lambda nm, s_, d=F32: es2.enter_context(nc.sbuf_tensor(_nm(nm), s_, d))
            SG = sb2("mSG", [128, 2, 16, 2, 2, 2, 128], BF16)
            t_sg = Tok()
            k.dma("sp", SG[:, 0], g.SSM_SG[:, 0], reads=[g.T_SSMW], writes=[t_sg])
            k.dma("sp", SG[:, 1], g.SSM_SG[:, 1], reads=[g.T_SSMW], writes=[t_sg])
            S = [[sb2("mS%d%d" % (d, ri), [128, 8, NCH]) for ri in range(2)] for d in range(2)]
            t_S = [[Tok() for q in range(8)] for d in range(2)]
            Tt = [[sb2("mT%d%d" % (i, ri), [128, NCH]) for ri in range(2)] for i in range(4)]
            t_T = [Tok() for _ in range(4)]
            cnt = 0
            for d in range(2):
                for q in range(8):
                    Q, pp = q // 2, q % 2
                    hc, Ql = Q // 2, Q % 2
                    rows = slice(Ql * 64, (Ql + 1) * 64)
                    for ri in range(2):
                        pi_ = cnt % 4
                        cnt += 1
                        for i in range(TC):
                            n_ = (TC - 1 - i) if d == 0 else i
                            k.op("pe", lambda en: en.matmul(PSm[pi_][:, 0:NCH], lhsT=SG[rows, d, n_, hc, ri, pp, :], rhs=ujm[rows, hc, i, :], start=(i == 0), stop=(i == TC - 1)), reads=[t_sg, t_u], writes=[TP[pi_]])
                        if d == 0:
                            k.op("act" if ri else "dve", (lambda en: en.activation(out=S[d][ri][:, q, :], in_=PSm[pi_][:, 0:NCH], func=AF.Copy)) if ri else (lambda en: en.tensor_copy(out=S[d][ri][:, q, :], in_=PSm[pi_][:, 0:NCH])), reads=[TP[pi_]], writes=[t_S[d][q]])
                        else:
                            k.op("dve", lambda en: en.tensor_copy(out=S[d][ri][:, q, 0:256], in_=PSm[pi_][:, 16:NCH]), reads=[TP[pi_]], writes=[t_S[d][q]])
                            k.op("act", lambda en: en.activation(out=S[d][ri][:, q, 256:NCH], in_=PSm[pi_][:, 0:16], func=AF.Copy), reads=[TP[pi_]], writes=[t_S[d][q]])
            for kk in range(NDBL):
                sh = 1 << kk
                w_ = NCH - sh
                it = 0
                for d in range(2):
                    dst = slice(sh, NCH) if d == 0 else slice(0, w_)
                    srcs = slice(0, w_) if d == 0 else slice(sh, NCH)
                    for q in range(8):
                        dq = d * 8 + q
                        Ar, Ai, nAi = A2[:, kk, 0, dq:dq + 1], A2[:, kk, 1, dq:dq + 1], A2[:, kk, 2, dq:dq + 1]
                        Tr, Ti = Tt[it % 4]
                        tT = t_T[it % 4]
                        it += 1
                        Sr, Si = S[d][0], S[d][1]
                        ts = t_S[d][q]
                        k.op("act", lambda en: en.activation(out=Tr[:, 0:w_], in_=Sr[:, q, srcs], func=AF.Copy, scale=Ar), reads=[ts, lw.tok], writes=[tT])
                        k.op("dve", lambda en: en.scalar_tensor_tensor(out=Tr[:, 0:w_], in0=Si[:, q, srcs], scalar=nAi, in1=Tr[:, 0:w_], op0=ALU.mult, op1=ALU.add), reads=[ts, tT, lw.tok], writes=[tT])
                        k.op("act", lambda en: en.activation(out=Ti[:, 0:w_], in_=Si[:, q, srcs], func=AF.Copy, scale=Ar), reads=[ts, lw.tok, tT], writes=[tT])
                        k.op("dve", lambda en: en.scalar_tensor_tensor(out=Ti[:, 0:w_], in0=Sr[:, q, srcs], scalar=Ai, in1=Ti[:, 0:w_], op0=ALU.mult, op1=ALU.add), reads=[ts, tT, lw.tok], writes=[tT])
                        k.op("pool", lambda en: en.tensor_tensor(out=Sr[:, q, dst], in0=Sr[:, q, dst], in1=Tr[:, 0:w_], op=ALU.add), reads=[tT, ts], writes=[ts])
                        k.op("dve", lambda en: en.tensor_tensor(out=Si[:, q, dst], in0=Si[:, q, dst], in1=Ti[:, 0:w_], op=ALU.add), reads=[tT, ts], writes=[ts])
            for d in range(2):
                for ri in range(2):
                    k.op("act" if ri else "dve", (lambda en: en.activation(out=Sb[d][ri][:], in_=S[d][ri][:], func=AF.Copy)) if ri else (lambda en: en.tensor_copy(out=Sb[d][ri][:], in_=S[d][ri][:])), reads=[t_S[d][q] for q in range(8)], writes=[t_sb])
            k.barrier()
        with ExitStack() as es3:
            sb3 = lambda nm, s_, d=F32: es3.enter_context(nc.sbuf_tensor(_nm(nm), s_, d))
            L3 = sb3("mL3", [128, 2, 16, 17, 64], BF16)
            L1W = sb3("mL1W", [128, 2, 16, 2, 128], BF16)
            t_l = Tok()
            k.dma("sp", L3[:, 0], g.SSM_L3[:, 0], reads=[g.T_SSMW], writes=[t_l])
            k.dma("sp", L3[:, 1], g.SSM_L3[:, 1], reads=[g.T_SSMW], writes=[t_l])
            k.dma("sp", L1W[:], g.SSM_L1, reads=[g.T_SSMW], writes=[t_l])
            Yjm = sb3("mYjm", [128, 2, TC, NCH])
            t_y = Tok()
            cnt = 0
            for j in range(TC):
                for hc in range(2):
                    pi_ = cnt % 4
                    cnt += 1
                    yp = PSm[pi_]
                    first = True
                    for tau in range(j + 1):
                        k.op("pe", lambda en: en.matmul(yp[:, 0:NCH], lhsT=L1W[:, 0, tau, hc, :], rhs=ujm[:, hc, j - tau, :], start=first, stop=False), reads=[t_l, t_u], writes=[TP[pi_]])
                        first = False
                    for tau in range(TC - j):
                        k.op("pe", lambda en: en.matmul(yp[:, 0:NCH], lhsT=L1W[:, 1, tau, hc, :], rhs=ujm[:, hc, j + tau, :], start=False, stop=False), reads=[t_l, t_u], writes=[TP[pi_]])
                    for Ql in range(2):
                        rows = slice(Ql * 64, (Ql + 1) * 64)
                        for pp in range(2):
                            q = 2 * (2 * hc + Ql) + pp
                            for ri in range(2):
                                k.op("pe", lambda en: en.matmul(yp[rows, 1:NCH], lhsT=L3[:, ri, q, j + 1, :], rhs=Sb[0][ri][:, q, 0:NCH - 1], start=False, stop=False), reads=[t_l, t_sb], writes=[TP[pi_]])
                                k.op("pe", lambda en: en.matmul(yp[rows, 16:NCH], lhsT=L3[:, ri, 8 + q, TC - j, :], rhs=Sb[1][ri][:, q, 1:257], start=False, stop=False), reads=[t_l, t_sb], writes=[TP[pi_]])
                                lastm = (Ql == 1 and pp == 1 and ri == 1)
                                k.op("pe", lambda en: en.matmul(yp[rows, 0:15], lhsT=L3[:, ri, 8 + q, TC - j, :], rhs=Sb[1][ri][:, q, 257:NCH], start=False, stop=lastm), reads=[t_l, t_sb], writes=[TP[pi_]])
                    k.op("dve", lambda en: en.scalar_tensor_tensor(out=Yjm[:, hc, j, :], in0=ujm[:, hc, j, :], scalar=cols[:, hc, 0:1], in1=yp[:, 0:NCH], op0=ALU.mult, op1=ALU.add), reads=[TP[pi_], t_u, t_c], writes=[t_y])
            k.barrier()
            FL = TC * NCH
            Yf = [Yjm[:, hc].rearrange("p j c -> p (j c)") for hc in range(2)]
            Gf = [gjm[:, hc].rearrange("p j c -> p (j c)") for hc in range(2)]
            Zb = ujm
            Zf = [Zb[:, hc].rearrange("p j c -> p (j c)") for hc in range(2)]
            w1 = sb3("mw1", [128, 512]); w2 = sb3("mw2", [128, 512]); w3 = sb3("mw3", [128, 2, 512])
            t_w1 = Tok(); t_z = Tok(); t_w3 = Tok()
            CG = 1.5957691216057308
            pieces = [(c0, min(512, FL - c0)) for c0 in range(0, FL, 512)]
            for (c0, w_) in pieces:
                cs = slice(c0, c0 + w_)
                for hc in range(2):
                    k.op("pool", lambda en: en.tensor_tensor(out=w1[:, 0:w_], in0=Yf[hc][:, cs], in1=Yf[hc][:, cs], op=ALU.mult), reads=[t_y, t_w1], writes=[t_w1])
                    k.op("dve", lambda en: en.tensor_scalar(out=w1[:, 0:w_], in0=w1[:, 0:w_], scalar1=0.044715, scalar2=1.0, op0=ALU.mult, op1=ALU.add), reads=[t_w1], writes=[t_w1])
                    k.op("pool", lambda en: en.tensor_tensor(out=w1[:, 0:w_], in0=w1[:, 0:w_], in1=Yf[hc][:, cs], op=ALU.mult), reads=[t_w1, t_y], writes=[t_w1])
                    k.op("act", lambda en: en.activation(out=w2[:, 0:w_], in_=w1[:, 0:w_], func=AF.Sigmoid, scale=CG), reads=[t_w1], writes=[t_w1])
                    k.op("dve", lambda en: en.tensor_tensor(out=w3[:, hc, 0:w_], in0=w2[:, 0:w_], in1=Yf[hc][:, cs], op=ALU.mult), reads=[t_w1, t_y, t_w3], writes=[t_w3])
                    k.op("pool", lambda en: en.tensor_copy(out=Zf[hc][:, cs], in_=w3[:, hc, 0:w_]), reads=[t_w3, t_u], writes=[t_z])
                for oc in range(2):
                    pi_ = 4 + oc
                    for kc in range(2):
                        k.op("pe", lambda en: en.matmul(PSm[pi_][:, 0:w_], lhsT=GW[:, kc, oc * 128:(oc + 1) * 128], rhs=Zf[kc][:, cs], start=(kc == 0), stop=(kc == 1)), reads=[t_z, t_c], writes=[TP[pi_]])
                    k.op("act", lambda en: en.activation(out=w2[:, 0:w_], in_=PSm[pi_][:, 0:w_], func=AF.Sigmoid, bias=cols[:, oc, 1:2]), reads=[TP[pi_], t_c, t_w1], writes=[t_w1])
                    k.op("dve", lambda en: en.tensor_tensor(out=w2[:, 0:w_], in0=w2[:, 0:w_], in1=w3[:, oc, 0:w_], op=ALU.mult), reads=[t_w1, t_w3], writes=[t_w1])
                    k.op("pool", lambda en: en.tensor_tensor(out=Gf[oc][:, cs], in0=w2[:, 0:w_], in1=Gf[oc][:, cs], op=ALU.mult), reads=[t_w1, t_g], writes=[t_g])
            on_t = sb3("mON", [128, 2, T], BF16)
            t_on = Tok()
            for hc in range(2):
                k.op("dve" if hc else "pool", lambda en: en.tensor_copy(out=on_t[:, hc, :].rearrange("p (c j) -> p j c", j=TC), in_=gjm[:, hc]), reads=[t_g], writes=[t_on])
            if need_ctx:
                k.dma("pool", g.CATT[:, 4:6, :], on_t[:], reads=[t_on], writes=[g.T_CATT])
            else:
                k.dma("pool", g.CATT[:, 4:6, C:T], on_t[:, :, C:T], reads=[t_on], writes=[g.T_CATT])
            k.barrier()
```

```python
import math
import numpy as np
from contextlib import ExitStack
import concourse.bass as bass
import concourse.mybir as mybir
from concourse.bass_utils import run_bass_kernel_spmd

F32 = mybir.dt.float32
BF16 = mybir.dt.bfloat16
AF = mybir.ActivationFunctionType
ALU = mybir.AluOpType
AX = mybir.AxisListType

D = 1024
L = 4096
C = 256
T = L + C
NT = T // 128
DEPTH = 4
EPS = 1e-6
NCORES = 8

O_CQ, O_CKV, O_KR, O_GM = 0, 192, 320, 352
O1 = 608
O_GQ, O_GK, O_GV, O_GG = O1, O1 + 256, O1 + 384, O1 + 512
O2 = O1 + 768
O_SU, O_SG = O2, O2 + 256
O3 = O2 + 512
O_HY, O_HG = O3, O3 + 768
NIN = 2912
O_KRP = NIN
O_QM = O_KRP + 32
O_QP = O_QM + 256
O_KP = O_QP + 256
NCB = O_KP + 128

EPOCH = 30000
NDMASLOT = 8


class Tok:
    __slots__ = ("w", "r", "excl")

    def __init__(self, excl=False):
        self.w = []
        self.r = []
        self.excl = excl


class KB:
    def __init__(self, nc, es):
        self.nc = nc
        self.es = es
        self.eng = {"pe": nc.tensor, "act": nc.scalar, "dve": nc.vector, "pool": nc.gpsimd, "sp": nc.sync}
        self.cnt = {e: 0 for e in self.eng}
        self.epoch = {e: 0 for e in self.eng}
        self.sems = {}
        self.seen = {e: {} for e in self.eng}
        self.dma_slots = {}
        self.dma_rr = {e: 0 for e in self.eng}
        self.ninst = 0

    def _sem(self, key):
        if key not in self.sems:
            self.sems[key] = self.es.enter_context(self.nc.semaphore("s_%s_%s" % key))
        return self.sems[key]

    def _wait(self, e, ev):
        key, val = ev
        if self.seen[e].get(key, 0) >= val:
            return
        self.eng[e].wait_ge(self._sem(key), val)
        self.seen[e][key] = val

    def _deps(self, e, reads, writes):
        best = {}

        def add(k_, v):
            if best.get(k_, 0) < v:
                best[k_] = v
        for t in reads:
            for k_, v in t.w:
                add(k_, v)
            if t.excl:
                for k_, v in t.r:
                    if k_[0] != e:
                        add(k_, v)
        for t in writes:
            for k_, v in t.w:
                if k_[0] != e:
                    add(k_, v)
            for k_, v in t.r:
                if k_[0] != e:
                    add(k_, v)
        for k_, v in best.items():
            if e == "pe" and k_[0] == "pe":
                continue
            self._wait(e, (k_, v))

    def _record(self, ev, reads, writes):
        for t in reads:
            t.r.append(ev)
            if len(t.r) > 16:
                best = {}
                for k_, v in t.r:
                    if best.get(k_, 0) < v:
                        best[k_] = v
                t.r = list(best.items())
        for t in writes:
            t.w = [ev]
            t.r = []

    def op(self, e, fn, reads=(), writes=()):
        self._deps(e, reads, writes)
        if self.cnt[e] >= EPOCH:
            self.epoch[e] += 1
            self.cnt[e] = 0
        key = (e, self.epoch[e])
        ins = fn(self.eng[e])
        self.cnt[e] += 1
        ins.then_inc(self._sem(key), 1)
        ev = (key, self.cnt[e])
        self._record(ev, reads, writes)
        self.ninst += 1
        return ev

    def dma(self, e, out, in_, reads=(), writes=(), **kw):
        self._deps(e, reads, writes)
        if e not in self.dma_slots:
            self.dma_slots[e] = [[("d" + e, i), 0] for i in range(NDMASLOT)]
        i = self.dma_rr[e]
        self.dma_rr[e] = (i + 1) % NDMASLOT
        slot = self.dma_slots[e][i]
        key = slot[0]
        if slot[1] > 0:
            self._wait(e, (key, 16 * slot[1]))
        slot[1] += 1
        ins = self.eng[e].dma_start(out=out, in_=in_, **kw)
        ins.then_inc(self._sem(key), 16)
        ev = (key, 16 * slot[1])
        self._record(ev, reads, writes)
        self.ninst += 1
        return ev

    def all_events(self):
        evs = []
        for e in self.eng:
            for ep in range(self.epoch[e] + 1):
                v = self.cnt[e] if ep == self.epoch[e] else EPOCH
                if v > 0:
                    evs.append(((e, ep), v))
        for e, slots in self.dma_slots.items():
            for key, uses in slots:
                if uses:
                    evs.append((key, 16 * uses))
        return evs

    def barrier(self):
        evs = self.all_events()
        for e in ("pe", "act", "dve", "pool", "sp"):
            for ev in evs:
                if ev[0][0] == e:
                    continue
                self._wait(e, ev)

    def drain(self, e="sp"):
        for ev in self.all_events():
            self._wait(e, ev)


_NMC = [0]


def _nm(n):
    _NMC[0] += 1
    return "%s_%d" % (n, _NMC[0])


class Rot:
    def __init__(self, tiles, excl=False):
        self.tiles = [(t, Tok(excl)) for t in tiles]
        self.i = 0

    def next(self):
        r = self.tiles[self.i]
        self.i = (self.i + 1) % len(self.tiles)
        return r


def _rope_table(d):
    hh = d // 2
    qq = hh // 2
    inv = (np.float32(10000.0) ** (-np.arange(0, hh, 2, dtype=np.float32) / np.float32(hh))).astype(np.float32)
    t = np.arange(L)
    row = (t // 64).astype(np.float32)
    col = (t % 64).astype(np.float32)
    cos = np.ones((d, T), np.float32)
    sin = np.zeros((d, T), np.float32)
    for i in range(d):
        hf, within = divmod(i, hh)
        fi = within % qq
        pos = row if hf == 0 else col
        ang = (pos * inv[fi]).astype(np.float32)
        cos[i, C:] = np.cos(ang).astype(np.float32)
        sin[i, C:] = np.sin(ang).astype(np.float32)
    return cos, sin


def _partner_index(d):
    hh = d // 2
    qq = hh // 2
    idx = np.zeros(d, np.int64)
    sg = np.zeros(d, np.float32)
    for i in range(d):
        hf, within = divmod(i, hh)
        if within < qq:
            idx[i] = i + qq
            sg[i] = -1.0
        else:
            idx[i] = i - qq
            sg[i] = 1.0
    return idx, sg


def host_constants():
    cst = {}
    cst["k_ident"] = np.eye(128, dtype=np.float32)
    c32, s32 = _rope_table(32)
    c64, s64 = _rope_table(64)
    mc = np.ones((128, T), np.float32)
    ms = np.zeros((128, T), np.float32)
    mc[64:96] = c32
    ms[64:96] = s32
    cst["k_ropeM"] = np.stack([mc, ms], 0)
    cst["k_ropeG"] = np.stack([np.concatenate([c64, c64], 0), np.concatenate([s64, s64], 0)], 0)
    cst.update(hyena_constants())
    return cst


class G:
    pass


def build_program(layers, final_lat_only, dbg=None):
    nc = bass.Bass("TRN2", target_bir_lowering=False)
    g = G()
    g.nc = nc
    g.dbg = dbg or {}

    def din(name, shape, dt=F32):
        return nc.dram_tensor(name, list(shape), dt, kind="ExternalInput").ap()

    def dscr(name, shape, dt=F32):
        return nc.dram_tensor(name, list(shape), dt).ap()

    g.xs = din("xs", [2, T, D])
    g.cT = din("cT", [128, 8, 3])
    W = {}
    W["w_mod"] = din("w_mod", [DEPTH, D, 3 * D])
    W["b_mod"] = din("b_mod", [DEPTH, 3 * D])
    W["g_pre"] = din("g_pre", [DEPTH, D])
    W["g_post"] = din("g_post", [DEPTH, D])
    W["w_in"] = din("w_in", [DEPTH, D, NIN])
    W["w_out"] = din("w_out", [DEPTH, D, D])
    W["mla_g_cq"] = din("mla_g_cq", [DEPTH, 192])
    W["mla_w_uq"] = din("mla_w_uq", [DEPTH, 192, 384])
    W["mla_g_ckv"] = din("mla_g_ckv", [DEPTH, 128])
    W["mla_w_ukv"] = din("mla_w_ukv", [DEPTH, 128, 512])
    W["gq_cols"] = din("gq_cols", [DEPTH, 128, 4])
    for nm, shp in (("hy_conv_w", [DEPTH, 3, 768]), ("hy_conv_b", [DEPTH, 768]), ("hy_f_w1", [DEPTH, 33, 64]), ("hy_f_b1", [DEPTH, 64]),
                    ("hy_f_freq1", [DEPTH, 64]), ("hy_f_w2", [DEPTH, 64, 64]), ("hy_f_b2", [DEPTH, 64]), ("hy_f_freq2", [DEPTH, 64]),
                    ("hy_f_w3", [DEPTH, 64, 1024]), ("hy_bias", [DEPTH, 2, 256])):
        W[nm] = din(nm, shp)
    for nm, shp in (("ssm_lam", [DEPTH, 128, 2, 16]), ("ssm_ls", [DEPTH, 128, 16]), ("ssm_Bp", [DEPTH, 128, 2, 16, 64]), ("ssm_Cp", [DEPTH, 128, 2, 16, 64]),
                    ("ssm_cols", [DEPTH, 128, 2, 2]), ("ssm_glu_w", [DEPTH, 256, 256])):
        W[nm] = din(nm, shp)
    g.W = W
    K = {}
    K["k_ident"] = din("k_ident", [128, 128])
    K["k_ropeM"] = din("k_ropeM", [2, 128, T])
    K["k_ropeG"] = din("k_ropeG", [2, 128, T])
    for nm, arr in hyena_constants().items():
        K[nm] = din(nm, list(arr.shape))
    g.K = K
    if final_lat_only:
        g.y = nc.dram_tensor("y", [2, L, D], F32, kind="ExternalOutput").ap()
    else:
        g.y = nc.dram_tensor("y", [2, T, D], F32, kind="ExternalOutput").ap()
    for name, (shape, dt) in g.dbg.items():
        g.dbg[name] = nc.dram_tensor(name, list(shape), dt, kind="ExternalOutput").ap()

    g.XS = dscr("XS", [2, T, D]) if len(layers) > 1 else None
    g.WINB = dscr("WINB", [128, 8, NCB], BF16)
    g.WOUTB = dscr("WOUTB", [128, 8, D], BF16)
    g.MODROWS = dscr("MODROWS", [3, 3 * D])
    g.HT = dscr("HT", [128, 8, T], BF16)
    g.CATT = dscr("CATT", [128, 8, T], BF16)
    g.KHAT = {"L": dscr("KHATL", [2, HY_LAT.ng, 128, 512]), "C": dscr("KHATC", [2, HY_CTX.ng, 128, 512])}
    g.T_KHAT = Tok()
    g.SSM_L3 = dscr("SSM_L3", [128, 2, 16, 17, 64], BF16)
    g.SSM_L1 = dscr("SSM_L1", [128, 2, 16, 2, 128], BF16)
    g.SSM_SG = dscr("SSM_SG", [128, 2, 16, 2, 2, 2, 128], BF16)
    g.T_SSMW = Tok()
    g.T_XS = [Tok(), Tok()]
    g.T_WINB = Tok()
    g.T_WOUTB = Tok()
    g.T_MOD = Tok()
    g.T_HT = Tok()
    g.T_CATT = Tok()
    g.T_Y = Tok()

    with ExitStack() as es:
        k = KB(nc, es)
        g.k = k
        g.es = es
        g.ident = es.enter_context(nc.sbuf_tensor("ident", [128, 128], BF16))
        g.T_ident = Tok()
        g.onesf = es.enter_context(nc.sbuf_tensor("onesf", [128, 128], F32))
        g.ones128 = es.enter_context(nc.sbuf_tensor("ones128", [128, 128], BF16))
        g.ones192 = es.enter_context(nc.sbuf_tensor("ones192", [128, 128], BF16))
        g.blk64 = es.enter_context(nc.sbuf_tensor("blk64", [128, 128], BF16))
        g.T_const = Tok()
        with ExitStack() as es2:
            tmp = es2.enter_context(nc.sbuf_tensor("idtmp", [128, 128], F32))
            tt = Tok()
            k.dma("sp", tmp[:], K["k_ident"], writes=[tt])
            k.op("dve", lambda e: e.tensor_copy(out=g.ident[:], in_=tmp[:]), reads=[tt], writes=[g.T_ident])
            k.op("dve", lambda e: e.memset(g.onesf[:], 1.0), writes=[g.T_const])
            k.op("dve", lambda e: e.memset(g.ones128[:], 1.0 / 128), writes=[g.T_const])
            k.op("dve", lambda e: e.memset(g.ones192[:], 1.0 / 192), writes=[g.T_const])
            k.op("dve", lambda e: e.memset(g.blk64[:], 0.0), writes=[g.T_const])
            k.op("dve", lambda e: e.memset(g.blk64[0:64, 0:64], 1.0 / 64), reads=[g.T_const], writes=[g.T_const])
            k.op("dve", lambda e: e.memset(g.blk64[64:128, 64:128], 1.0 / 64), reads=[g.T_const], writes=[g.T_const])
            k.barrier()

        for li, l in enumerate(layers):
            need_ctx = l < DEPTH - 1
            src = g.xs if li == 0 else g.XS
            last = li == len(layers) - 1
            dst = g.y if last else g.XS
            phase_weights(g, l)
            k.barrier()
            phase_ssm_weights(g, l)
            phase_hy_filter(g, l, HY_LAT)
            if need_ctx:
                phase_hy_filter(g, l, HY_CTX)
            for s in range(2):
                phase_P1(g, l, s, src)
                k.barrier()
                phase_hyena(g, l, s, HY_LAT, C)
                if need_ctx:
                    phase_hyena(g, l, s, HY_CTX, 0)
                phase_ssm(g, l, s, need_ctx)
                phase_attn(g, l, s, need_ctx)
                k.barrier()
                phase_P6(g, l, s, src, dst, need_ctx, last and final_lat_only)
                k.barrier()
        k.drain("sp")
    return nc, g


def phase_weights(g, l):
    nc, k, W = g.nc, g.k, g.W
    with ExitStack() as es:
        sb = lambda n, s, d=F32: es.enter_context(nc.sbuf_tensor(_nm(n), s, d))
        ps = lambda n, s, d=F32: es.enter_context(nc.psum_tensor(_nm(n), s, d))
        p32, s32 = _partner_index(32)
        p64, s64 = _partner_index(64)
        fin = Rot([sb("wf%d" % i, [128, NIN]) for i in range(2)])
        fob = Rot([sb("wb%d" % i, [128, NCB], BF16) for i in range(2)])
        qorder = (0, 2, 1, 3)
        engs = ["act", "pool", "dve"]
        ei = 0

        def cp(dst, src, neg=False):
            nonlocal ei
            e = engs[ei % 3]
            ei += 1
            if e == "act":
                k.op("act", lambda en: en.activation(out=dst, in_=src, func=AF.Copy, scale=(-1.0 if neg else 1.0)), reads=[tf], writes=[tb])
            else:
                k.op(e, lambda en: en.tensor_scalar(out=dst, in0=src, scalar1=(-1.0 if neg else 1.0), scalar2=None, op0=ALU.mult), reads=[tf], writes=[tb])

        def partner_cols(dstb, srcb, d):
            hh, qq = d // 2, d // 4
            for hf in range(2):
                b0 = hf * hh
                cp(wb[:, dstb + b0:dstb + b0 + qq], wf[:, srcb + b0 + qq:srcb + b0 + hh], neg=True)
                cp(wb[:, dstb + b0 + qq:dstb + b0 + hh], wf[:, srcb + b0:srcb + b0 + qq], neg=False)

        for kc in range(8):
            wf, tf = fin.next()
            wb, tb = fob.next()
            k.dma("sp", wf[:], W["w_in"][l, kc * 128:(kc + 1) * 128, :], writes=[tf])
            k.op("act", lambda en: en.activation(out=wb[:, 0:1456], in_=wf[:, 0:1456], func=AF.Copy), reads=[tf], writes=[tb])
            k.op("dve", lambda en: en.tensor_copy(out=wb[:, 1456:NIN], in_=wf[:, 1456:NIN]), reads=[tf], writes=[tb])
            partner_cols(O_KRP, O_KR, 32)
            for pos, h in enumerate(qorder):
                cp(wb[:, O_QM + pos * 64:O_QM + pos * 64 + 64], wf[:, O_GQ + h * 64:O_GQ + h * 64 + 64])
                partner_cols(O_QP + pos * 64, O_GQ + h * 64, 64)
            for h in range(2):
                partner_cols(O_KP + h * 64, O_GK + h * 64, 64)
            k.dma("pool", g.WINB[:, kc, :], wb[:], reads=[tb], writes=[g.T_WINB])
        fo = Rot([sb("wof%d" % i, [128, D]) for i in range(2)])
        fb = Rot([sb("wob%d" % i, [128, D], BF16) for i in range(2)])
        for kc in range(8):
            wf, tf = fo.next()
            wb, tb = fb.next()
            k.dma("sp", wf[:], W["w_out"][l, kc * 128:(kc + 1) * 128, :], writes=[tf])
            k.op("act" if kc % 2 else "dve", (lambda en: en.activation(out=wb[:], in_=wf[:], func=AF.Copy)) if kc % 2 else (lambda en: en.tensor_copy(out=wb[:], in_=wf[:])), reads=[tf], writes=[tb])
            k.dma("pool", g.WOUTB[:, kc, :], wb[:], reads=[tb], writes=[g.T_WOUTB])

        cT = sb("cTs", [128, 8, 3])
        scT = sb("scT", [128, 8, 3])
        t_c = Tok()
        k.dma("sp", cT[:], g.cT, writes=[t_c])
        k.op("act", lambda en: en.activation(out=scT[:], in_=cT[:], func=AF.Silu), reads=[t_c], writes=[t_c])
        mrow = sb("mrow", [3, 3 * D])
        brow = sb("brow", [3, 3 * D])
        gpre = sb("gpre", [3, D])
        gpost = sb("gpost", [3, D])
        t_m = Tok()
        t_b = Tok()
        k.dma("sp", brow[:], W["b_mod"][l:l + 1, :].broadcast_to([3, 3 * D]), writes=[t_b])
        k.dma("sp", gpre[:], W["g_pre"][l:l + 1, :].broadcast_to([3, D]), writes=[t_b])
        k.dma("sp", gpost[:], W["g_post"][l:l + 1, :].broadcast_to([3, D]), writes=[t_b])
        wm = Rot([sb("wm%d" % i, [128, 8, 512]) for i in range(2)])
        pm = Rot([ps("pm%d" % i, [3, 512]) for i in range(2)], excl=True)
        for cc in range(6):
            wt, tw = wm.next()
            pt, tp = pm.next()
            k.dma("sp", wt[:], W["w_mod"][l, :, cc * 512:(cc + 1) * 512].rearrange("(kc p) n -> p kc n", p=128), writes=[tw])
            for kc in range(8):
                k.op("pe", lambda en: en.matmul(pt[:], lhsT=scT[:, kc, :], rhs=wt[:, kc, :], start=(kc == 0), stop=(kc == 7)), reads=[t_c, tw], writes=[tp])
            k.op("dve", lambda en: en.tensor_tensor(out=mrow[:, cc * 512:(cc + 1) * 512], in0=pt[:], in1=brow[:, cc * 512:(cc + 1) * 512], op=ALU.add), reads=[tp, t_b], writes=[t_m])
        orow = sb("orow", [3, 3 * D])
        t_o = Tok()
        k.op("dve", lambda en: en.scalar_tensor_tensor(out=orow[:, 0:D], in0=mrow[:, D:2 * D], scalar=1.0, in1=gpre[:], op0=ALU.add, op1=ALU.mult), reads=[t_m, t_b], writes=[t_o])
        k.op("dve", lambda en: en.tensor_copy(out=orow[:, D:2 * D], in_=mrow[:, 0:D]), reads=[t_m, t_o], writes=[t_o])
        k.op("dve", lambda en: en.tensor_tensor(out=orow[:, 2 * D:3 * D], in0=mrow[:, 2 * D:3 * D], in1=gpost[:], op=ALU.mult), reads=[t_m, t_b, t_o], writes=[t_o])
        k.dma("pool", g.MODROWS, orow[:], reads=[t_o], writes=[g.T_MOD])
        k.barrier()

    if not hasattr(g, "lw"):
        lw = G()
        es = g.es
        sbp = lambda n, s, d=F32: es.enter_context(nc.sbuf_tensor(_nm(n), s, d))
        lw.wuq_a = sbp("wuq_a", [128, 384], BF16)
        lw.wuq_b = sbp("wuq_b", [64, 384], BF16)
        lw.wuqp_a = sbp("wuqp_a", [128, 384], BF16)
        lw.wuqp_b = sbp("wuqp_b", [64, 384], BF16)
        lw.wuk = sbp("wuk", [128, 256], BF16)
        lw.wuv = sbp("wuv", [128, 256], BF16)
        lw.gq = sbp("gqc", [128, 4])
        lw.A2 = sbp("ssmA2", [128, NDBL, 3, 16])
        lw.tok = Tok()
        g.lw = lw
    lw = g.lw
    with ExitStack() as es:
        sb = lambda n, s, d=F32: es.enter_context(nc.sbuf_tensor(_nm(n), s, d))
        uqa = sb("uqa", [128, 384])
        uqb = sb("uqb", [64, 384])
        ukv = sb("ukv", [128, 512])
        gcq = sb("gcq", [128, 2])
        gckv = sb("gckv", [128, 1])
        tl = Tok()
        k.dma("sp", uqa[:], W["mla_w_uq"][l, 0:128, :], writes=[tl])
        k.dma("sp", uqb[:], W["mla_w_uq"][l, 128:192, :], writes=[tl])
        k.dma("sp", ukv[:], W["mla_w_ukv"][l], writes=[tl])
        k.dma("sp", gcq[:, 0:1], W["mla_g_cq"][l, 0:128].rearrange("(p o) -> p o", o=1), writes=[tl])
        k.dma("sp", gcq[0:64, 1:2], W["mla_g_cq"][l, 128:192].rearrange("(p o) -> p o", o=1), writes=[tl])
        k.dma("sp", gckv[:], W["mla_g_ckv"][l].rearrange("(p o) -> p o", o=1), writes=[tl])
        k.dma("sp", lw.gq[:], W["gq_cols"][l], writes=[lw.tok])
        k.op("dve", lambda en: en.tensor_scalar(out=uqa[:], in0=uqa[:], scalar1=gcq[:, 0:1], scalar2=None, op0=ALU.mult), reads=[tl], writes=[tl])
        k.op("dve", lambda en: en.tensor_scalar(out=uqb[:], in0=uqb[:], scalar1=gcq[0:64, 1:2], scalar2=None, op0=ALU.mult), reads=[tl], writes=[tl])
        k.op("dve", lambda en: en.tensor_scalar(out=ukv[:], in0=ukv[:], scalar1=gckv[:, 0:1], scalar2=None, op0=ALU.mult), reads=[tl], writes=[tl])
        k.op("dve", lambda en: en.tensor_copy(out=lw.wuq_a[:], in_=uqa[:]), reads=[tl], writes=[lw.tok])
        k.op("dve", lambda en: en.tensor_copy(out=lw.wuq_b[:], in_=uqb[:]), reads=[tl, lw.tok], writes=[lw.tok])
        k.op("dve", lambda en: en.memset(lw.wuqp_a[:], 0.0), reads=[lw.tok], writes=[lw.tok])
        k.op("dve", lambda en: en.memset(lw.wuqp_b[:], 0.0), reads=[lw.tok], writes=[lw.tok])
        for h in range(4):
            for hf in range(2):
                b0 = h * 96 + 64 + hf * 16
                for (dst, src, tile_src) in ((lw.wuqp_a, uqa, 128), (lw.wuqp_b, uqb, 64)):
                    k.op("dve", lambda en: en.tensor_scalar(out=dst[:, b0:b0 + 8], in0=src[:, b0 + 8:b0 + 16], scalar1=-1.0, scalar2=None, op0=ALU.mult), reads=[tl, lw.tok], writes=[lw.tok])
                    k.op("dve", lambda en: en.tensor_copy(out=dst[:, b0 + 8:b0 + 16], in_=src[:, b0:b0 + 8]), reads=[tl, lw.tok], writes=[lw.tok])
            k.op("dve", lambda en: en.tensor_copy(out=lw.wuk[:, h * 64:(h + 1) * 64], in_=ukv[:, h * 128:h * 128 + 64]), reads=[tl, lw.tok], writes=[lw.tok])
            k.op("dve", lambda en: en.tensor_copy(out=lw.wuv[:, h * 64:(h + 1) * 64], in_=ukv[:, h * 128 + 64:h * 128 + 128]), reads=[tl, lw.tok], writes=[lw.tok])
        k.barrier()


def phase_P1(g, l, s, src):
    nc, k = g.nc, g.k
    with ExitStack() as es:
        sb = lambda n, s_, d=F32: es.enter_context(nc.sbuf_tensor(_nm(n), s_, d))
        ps = lambda n, s_, d=F32: es.enter_context(nc.psum_tensor(_nm(n), s_, d))
        mods = {}
        t_mod = Tok()
        for v in (s, 2):
            mods[v] = sb("mod%d" % v, [128, 2 * D])
            k.dma("sp", mods[v][:], g.MODROWS[v:v + 1, 0:2 * D].broadcast_to([128, 2 * D]), reads=[g.T_MOD], writes=[t_mod])
        neghalf = sb("neghalf", [128, 1])
        k.op("pool", lambda en: en.memset(neghalf[:], -0.5), writes=[t_mod])
        xr = Rot([sb("x%d" % i, [128, D]) for i in range(3)])
        junk = sb("junk", [128, D], BF16)
        t_junk = Tok()
        hb_r = Rot([sb("hb%d" % i, [128, D], BF16) for i in range(2)])
        tmp_r = Rot([sb("tmp%d" % i, [128, D]) for i in range(2)])
        st_r = Rot([sb("st%d" % i, [128, 4]) for i in range(3)])
        tp_r = Rot([ps("tp%d" % i, [128, 8, 128], BF16) for i in range(2)], excl=True)
        hs_r = Rot([sb("hs%d" % i, [128, 8, 512], BF16) for i in range(2)])
        hs, t_hs = None, None
        for ti in range(NT):
            if ti == 0 or (ti >= 2 and (ti - 2) % 4 == 0):
                hs, t_hs = hs_r.next()
            off = (ti * 128) if ti < 2 else (((ti - 2) % 4) * 128)
            xt, t_x = xr.next()
            k.dma("sp", xt[:], src[s, ti * 128:(ti + 1) * 128, :], reads=[g.T_XS[s]], writes=[t_x])
            st, t_st = st_r.next()
            k.op("dve", lambda en: en.scalar_tensor_tensor(out=junk[:], in0=xt[:], scalar=1.0, in1=xt[:], op0=ALU.mult, op1=ALU.mult, accum_out=st[:, 0:1]), reads=[t_x], writes=[t_junk, t_st])
            k.op("dve", lambda en: en.tensor_scalar(out=st[:, 1:2], in0=st[:, 0:1], scalar1=1.0 / D, scalar2=EPS, op0=ALU.mult, op1=ALU.add), reads=[t_st], writes=[t_st])
            k.op("pool", lambda en: en.tensor_tensor(out=st[:, 2:3], in0=st[:, 1:2], in1=neghalf[:], op=ALU.pow), reads=[t_st, t_mod], writes=[t_st])
            md = mods[2] if ti < 2 else mods[s]
            tmp, t_tmp = tmp_r.next()
            hb, t_hb = hb_r.next()
            k.op("dve", lambda en: en.scalar_tensor_tensor(out=tmp[:], in0=xt[:], scalar=st[:, 2:3], in1=md[:, 0:D], op0=ALU.mult, op1=ALU.mult), reads=[t_x, t_st, t_mod], writes=[t_tmp])
            k.op("pool", lambda en: en.tensor_tensor(out=hb[:], in0=tmp[:], in1=md[:, D:2 * D], op=ALU.add), reads=[t_tmp, t_mod], writes=[t_hb])
            tp, t_tp = tp_r.next()
            for kc in range(8):
                k.op("pe", lambda en: en.transpose(tp[:, kc, :], hb[:, kc * 128:(kc + 1) * 128], g.ident[:]), reads=[t_hb, g.T_ident], writes=[t_tp])
            k.op("act", lambda en: en.activation(out=hs[:, :, off:off + 128], in_=tp[:], func=AF.Copy), reads=[t_tp], writes=[t_hs])
            if ti == 1:
                k.dma("pool", g.HT[:, :, 0:256], hs[:, :, 0:256], reads=[t_hs], writes=[g.T_HT])
            elif ti >= 2 and (ti - 2) % 4 == 3:
                t0 = (ti - 3) * 128
                k.dma("pool", g.HT[:, :, t0:t0 + 512], hs[:, :, 0:512], reads=[t_hs], writes=[g.T_HT])
        k.barrier()


def phase_attn(g, l, s, need_ctx):
    nc, k, lw = g.nc, g.k, g.lw
    with ExitStack() as es:
        sb = lambda n, s_, d=F32: es.enter_context(nc.sbuf_tensor(_nm(n), s_, d))
        ps = lambda n, s_, d=F32: es.enter_context(nc.psum_tensor(_nm(n), s_, d))
        KTm = [sb("KTm%d" % h, [96, T], BF16) for h in range(4)]
        VM = sb("VM", [128, NT, 4 * 65], BF16)
        KTg = sb("KTg", [128, T], BF16)
        VG = sb("VG", [128, NT, 2 * 65], BF16)
        t_K = Tok()
        k.op("pool", lambda en: en.memset(VM[:], 1.0), writes=[t_K])
        k.op("pool", lambda en: en.memset(VG[:], 1.0), writes=[t_K])
        t_w = Tok()
        epsc = sb("epsc", [128, 1])
        k.op("pool", lambda en: en.memset(epsc[:], EPS), writes=[t_w])

        hT_r = Rot([sb("hT%d" % i, [128, 8, 512], BF16) for i in range(2)])
        rope_r = Rot([sb("rp%d" % i, [128, 4, 512]) for i in range(2)])
        esA = ExitStack()
        sbA = lambda n, s_, d=F32: esA.enter_context(nc.sbuf_tensor(_nm(n), s_, d))
        wkv = sbA("wkv", [128, 8, 576], BF16)
        k.dma("sp", wkv[:, :, 0:160], g.WINB[:, :, O_CKV:O_CKV + 160], reads=[g.T_WINB], writes=[t_w])
        k.dma("sp", wkv[:, :, 160:192], g.WINB[:, :, O_KRP:O_KRP + 32], reads=[g.T_WINB], writes=[t_w])
        k.dma("sp", wkv[:, :, 192:448], g.WINB[:, :, O_GK:O_GK + 256], reads=[g.T_WINB], writes=[t_w])
        k.dma("sp", wkv[:, :, 448:576], g.WINB[:, :, O_KP:O_KP + 128], reads=[g.T_WINB], writes=[t_w])
        SB2 = [ps("psS%d" % i, [128, 2, 512]) for i in range(3)]
        TSB = [Tok(True) for _ in range(3)]
        PS = [SB2[i // 2][:, i % 2, :] for i in range(6)] + [ps("ps%d" % i, [128, 512]) for i in range(6, 8)]
        TPS = [TSB[i // 2] for i in range(6)] + [Tok(True) for _ in range(2)]

        blocks = [(0, 256)] + [(256 + 512 * b, 512) for b in range(8)]

        def load_block(t0, nb):
            hT, t_h = hT_r.next()
            k.dma("sp", hT[:, :, 0:nb], g.HT[:, :, t0:t0 + nb], reads=[g.T_HT], writes=[t_h])
            rp, t_rp = rope_r.next()
            k.dma("sp", rp[:, 0:2, 0:nb], g.K["k_ropeM"][:, :, t0:t0 + nb].rearrange("a p n -> p a n"), writes=[t_rp])
            k.dma("sp", rp[:, 2:4, 0:nb], g.K["k_ropeG"][:, :, t0:t0 + nb].rearrange("a p n -> p a n"), writes=[t_rp])
            return hT, t_h, rp, t_rp

        def proj(pi, M, wt, c0, hT, t_h, nb, pbase=0):
            for kc in range(8):
                k.op("pe", lambda en: en.matmul(PS[pi][pbase:pbase + M, 0:nb], lhsT=wt[:, kc, c0:c0 + M], rhs=hT[:, kc, 0:nb], start=(kc == 0), stop=(kc == 7)), reads=[t_w, t_h], writes=[TPS[pi]])

        def rstd_from_ms(out_ap, ms_ap, reads, wtok, tmp_ap):
            k.op("act", lambda en: en.activation(out=tmp_ap, in_=ms_ap, func=AF.Ln, bias=epsc[0:tmp_ap.shape[0], 0:1]), reads=reads + [t_w], writes=[wtok])
            k.op("act", lambda en: en.activation(out=out_ap, in_=tmp_ap, func=AF.Exp, scale=-0.5), reads=[wtok], writes=[wtok])

        wk_r = Rot([sbA("wk%d" % i, [128, 6, 512]) for i in range(1)])
        wkb_r = Rot([sbA("wkb%d" % i, [128, 3, 512], BF16) for i in range(2)])
        for (t0, nb) in blocks:
            hT, t_h, rp, t_rp = load_block(t0, nb)
            wk, t_wk = wk_r.next()
            wkb, t_wkb = wkb_r.next()
            proj(0, 128, wkv, 0, hT, t_h, nb)
            k.op("act", lambda en: en.activation(out=wkb[:, 0, 0:nb], in_=PS[0][:, 0:nb], func=AF.Square), reads=[TPS[0]], writes=[t_wkb])
            k.op("dve", lambda en: en.tensor_copy(out=wk[:, 0, 0:nb], in_=PS[0][:, 0:nb]), reads=[TPS[0]], writes=[t_wk])
            k.op("pe", lambda en: en.matmul(PS[1][:, 0:nb], lhsT=g.ones128[:], rhs=wkb[:, 0, 0:nb], start=True, stop=True), reads=[t_wkb, g.T_const], writes=[TPS[1]])
            rstd_from_ms(wk[:, 1, 0:nb], PS[1][:, 0:nb], [TPS[1]], t_wk, wk[:, 1, 0:nb])
            k.op("dve", lambda en: en.tensor_tensor(out=wkb[:, 1, 0:nb], in0=wk[:, 0, 0:nb], in1=wk[:, 1, 0:nb], op=ALU.mult), reads=[t_wk], writes=[t_wkb])
            for h in range(4):
                pi = 2 + (h % 2)
                k.op("pe", lambda en: en.matmul(PS[pi][0:64, 0:nb], lhsT=lw.wuk[:, h * 64:(h + 1) * 64], rhs=wkb[:, 1, 0:nb], start=True, stop=True), reads=[t_wkb, lw.tok], writes=[TPS[pi]])
                k.op("act" if h % 2 else "dve", (lambda en: en.activation(out=KTm[h][0:64, t0:t0 + nb], in_=PS[pi][0:64, 0:nb], func=AF.Copy)) if h % 2 else (lambda en: en.tensor_copy(out=KTm[h][0:64, t0:t0 + nb], in_=PS[pi][0:64, 0:nb])), reads=[TPS[pi]], writes=[t_K])
            for j in range(nb // 128):
                ti = t0 // 128 + j
                k.op("pe", lambda en: en.matmul(PS[4][:, 0:256], lhsT=wkb[:, 1, j * 128:(j + 1) * 128], rhs=lw.wuv[:], start=True, stop=True), reads=[t_wkb, lw.tok], writes=[TPS[4]])
                k.op("dve", lambda en: en.tensor_copy(out=VM[:, ti, :].rearrange("p (h d) -> p h d", d=65)[:, :, 0:64], in_=PS[4][:, 0:256].rearrange("p (h d) -> p h d", d=64)), reads=[TPS[4]], writes=[t_K])
            proj(5, 32, wkv, 128, hT, t_h, nb, pbase=64)
            proj(6, 32, wkv, 160, hT, t_h, nb, pbase=64)
            k.op("dve", lambda en: en.tensor_tensor(out=wk[64:96, 2, 0:nb], in0=PS[5][64:96, 0:nb], in1=rp[64:96, 0, 0:nb], op=ALU.mult), reads=[TPS[5], t_rp], writes=[t_wk])
            k.op("dve", lambda en: en.tensor_tensor(out=wk[64:96, 3, 0:nb], in0=PS[6][64:96, 0:nb], in1=rp[64:96, 1, 0:nb], op=ALU.mult), reads=[TPS[6], t_rp], writes=[t_wk])
            for h in range(4):
                k.op("pool" if h % 2 else "dve", lambda en: en.tensor_tensor(out=KTm[h][64:96, t0:t0 + nb], in0=wk[64:96, 2, 0:nb], in1=wk[64:96, 3, 0:nb], op=ALU.add), reads=[t_wk], writes=[t_K])
            proj(7, 128, wkv, 192, hT, t_h, nb)
            proj(0, 128, wkv, 448, hT, t_h, nb)
            k.op("act", lambda en: en.activation(out=wkb[:, 2, 0:nb], in_=PS[7][:, 0:nb], func=AF.Square), reads=[TPS[7]], writes=[t_wkb])
            k.op("pe", lambda en: en.matmul(PS[1][:, 0:nb], lhsT=g.blk64[:], rhs=wkb[:, 2, 0:nb], start=True, stop=True), reads=[t_wkb, g.T_const], writes=[TPS[1]])
            rstd_from_ms(wk[:, 4, 0:nb], PS[1][:, 0:nb], [TPS[1]], t_wk, wk[:, 4, 0:nb])
            k.op("dve", lambda en: en.scalar_tensor_tensor(out=wk[:, 0, 0:nb], in0=PS[7][:, 0:nb], scalar=lw.gq[:, 2:3], in1=rp[:, 2, 0:nb], op0=ALU.mult, op1=ALU.mult), reads=[TPS[7], t_rp, lw.tok, t_wk], writes=[t_wk])
            k.op("dve", lambda en: en.scalar_tensor_tensor(out=wk[:, 5, 0:nb], in0=PS[0][:, 0:nb], scalar=lw.gq[:, 3:4], in1=rp[:, 3, 0:nb], op0=ALU.mult, op1=ALU.mult), reads=[TPS[0], t_rp, lw.tok, t_wk], writes=[t_wk])
            k.op("pool", lambda en: en.tensor_tensor(out=wk[:, 0, 0:nb], in0=wk[:, 0, 0:nb], in1=wk[:, 5, 0:nb], op=ALU.add), reads=[t_wk], writes=[t_wk])
            k.op("dve", lambda en: en.tensor_tensor(out=KTg[:, t0:t0 + nb], in0=wk[:, 0, 0:nb], in1=wk[:, 4, 0:nb], op=ALU.mult), reads=[t_wk], writes=[t_K])
            for j in range(nb // 128):
                ti = t0 // 128 + j
                for kc in range(8):
                    k.op("pe", lambda en: en.matmul(PS[4][:, 0:128], lhsT=hT[:, kc, j * 128:(j + 1) * 128], rhs=wkv[:, kc, 320:448], start=(kc == 0), stop=(kc == 7)), reads=[t_h, t_w], writes=[TPS[4]])
                k.op("act", lambda en: en.activation(out=VG[:, ti, :].rearrange("p (h d) -> p h d", d=65)[:, :, 0:64], in_=PS[4][:, 0:128].rearrange("p (h d) -> p h d", d=64), func=AF.Copy), reads=[TPS[4]], writes=[t_K])
        if "KTm0" in g.dbg:
            k.dma("pool", g.dbg["KTm0"], KTm[0][:], reads=[t_K])
            k.dma("pool", g.dbg["KTg"], KTg[:], reads=[t_K])
            k.dma("pool", g.dbg["VM"], VM[:], reads=[t_K])
            k.dma("pool", g.dbg["VG"], VG[:], reads=[t_K])

        k.barrier()
        esA.close()
        wq = sb("wq", [128, 8, 192 + 256 + 256 + 256 + 256], BF16)
        k.dma("sp", wq[:, :, 0:192], g.WINB[:, :, O_CQ:O_CQ + 192], reads=[g.T_WINB], writes=[t_w])
        k.dma("sp", wq[:, :, 192:448], g.WINB[:, :, O_GM:O_GM + 256], reads=[g.T_WINB], writes=[t_w])
        k.dma("sp", wq[:, :, 448:960], g.WINB[:, :, O_QM:O_QM + 512], reads=[g.T_WINB], writes=[t_w])
        k.dma("sp", wq[:, :, 960:1216], g.WINB[:, :, O_GG:O_GG + 256], reads=[g.T_WINB], writes=[t_w])
        qm_r = Rot([[sb("qm%d_%d" % (i, h), [96, 512], BF16) for h in range(4)] for i in range(2)])
        qg_r = Rot([[sb("qg%d_%d" % (i, j), [128, 512], BF16) for j in range(2)] for i in range(2)])
        gate_r = Rot([sb("gate%d" % i, [64, 8, 512], BF16) for i in range(1)])
        cq_r = Rot([sb("cq%d" % i, [128, 4, 512]) for i in range(1)])
        cqb_r = Rot([sb("cqb%d" % i, [128, 4, 512], BF16) for i in range(1)])
        P_r = Rot([sb("P%d" % i, [128, 2, 512], BF16) for i in range(5)])
        osb_r = Rot([sb("osb%d" % i, [65, 512]) for i in range(3)])
        res_r = Rot([sb("res%d" % i, [64, 512], BF16) for i in range(3)])
        O_bufs = [6, 7]
        for (t0, nb) in blocks:
            if t0 == 0 and not need_ctx:
                continue
            hT, t_h, rp, t_rp = load_block(t0, nb)
            kts = list(range(2)) if t0 == 0 else list(range(NT))
            qm, t_qm = qm_r.next()
            qg, t_qg = qg_r.next()
            gate, t_gate = gate_r.next()
            cq, t_cq = cq_r.next()
            cqb, t_cqb = cqb_r.next()
            for hh in range(8):
                c0 = (192 + hh * 64) if hh < 4 else (960 + (hh - 4) * 64)
                pi = 2 * (hh % 2)
                proj(pi, 64, wq, c0, hT, t_h, nb)
                k.op("act", lambda en: en.activation(out=gate[:, hh, 0:nb], in_=PS[pi][0:64, 0:nb], func=AF.Silu), reads=[TPS[pi]], writes=[t_gate])
            proj(0, 128, wq, 0, hT, t_h, nb)
            proj(2, 64, wq, 128, hT, t_h, nb)
            k.op("act", lambda en: en.activation(out=cqb[:, 0, 0:nb], in_=PS[0][:, 0:nb], func=AF.Square), reads=[TPS[0]], writes=[t_cqb])
            k.op("act", lambda en: en.activation(out=cqb[0:64, 1, 0:nb], in_=PS[2][0:64, 0:nb], func=AF.Square), reads=[TPS[2]], writes=[t_cqb])
            k.op("dve", lambda en: en.tensor_copy(out=cq[:, 0, 0:nb], in_=PS[0][:, 0:nb]), reads=[TPS[0]], writes=[t_cq])
            k.op("dve", lambda en: en.tensor_copy(out=cq[0:64, 1, 0:nb], in_=PS[2][0:64, 0:nb]), reads=[TPS[2]], writes=[t_cq])
            k.op("pe", lambda en: en.matmul(PS[0][:, 0:nb], lhsT=g.ones192[:], rhs=cqb[:, 0, 0:nb], start=True, stop=False), reads=[t_cqb, g.T_const, t_cq], writes=[TPS[0]])
            k.op("pe", lambda en: en.matmul(PS[0][:, 0:nb], lhsT=g.ones192[0:64, :], rhs=cqb[0:64, 1, 0:nb], start=False, stop=True), reads=[t_cqb, g.T_const], writes=[TPS[0]])
            rstd_from_ms(cq[:, 2, 0:nb], PS[0][:, 0:nb], [TPS[0]], t_cq, cq[:, 2, 0:nb])
            k.op("dve", lambda en: en.tensor_tensor(out=cqb[:, 2, 0:nb], in0=cq[:, 0, 0:nb], in1=cq[:, 2, 0:nb], op=ALU.mult), reads=[t_cq], writes=[t_cqb])
            k.op("dve", lambda en: en.tensor_tensor(out=cqb[0:64, 3, 0:nb], in0=cq[0:64, 1, 0:nb], in1=cq[0:64, 2, 0:nb], op=ALU.mult), reads=[t_cq], writes=[t_cqb])
            for h in range(4):
                for (pi, wa, wb_) in ((0, lw.wuq_a, lw.wuq_b), (2, lw.wuqp_a, lw.wuqp_b)):
                    k.op("pe", lambda en: en.matmul(PS[pi][0:96, 0:nb], lhsT=wa[:, h * 96:(h + 1) * 96], rhs=cqb[:, 2, 0:nb], start=True, stop=False), reads=[t_cqb, lw.tok], writes=[TPS[pi]])
                    k.op("pe", lambda en: en.matmul(PS[pi][0:96, 0:nb], lhsT=wb_[:, h * 96:(h + 1) * 96], rhs=cqb[0:64, 3, 0:nb], start=False, stop=True), reads=[t_cqb, lw.tok], writes=[TPS[pi]])
                k.op("dve", lambda en: en.tensor_tensor(out=cq[0:96, 3, 0:nb], in0=PS[0][0:96, 0:nb], in1=rp[0:96, 0, 0:nb], op=ALU.mult), reads=[TPS[0], t_rp, t_cq], writes=[t_cq])
                k.op("dve", lambda en: en.tensor_tensor(out=cq[0:96, 1, 0:nb], in0=PS[2][0:96, 0:nb], in1=rp[0:96, 1, 0:nb], op=ALU.mult), reads=[TPS[2], t_rp, t_cq], writes=[t_cq])
                k.op("pool", lambda en: en.tensor_tensor(out=qm[h][:, 0:nb], in0=cq[0:96, 3, 0:nb], in1=cq[0:96, 1, 0:nb], op=ALU.add), reads=[t_cq], writes=[t_qm])
            for j in range(2):
                proj(0, 128, wq, 448 + j * 128, hT, t_h, nb)
                proj(2, 128, wq, 704 + j * 128, hT, t_h, nb)
                k.op("act", lambda en: en.activation(out=cqb[:, 0, 0:nb], in_=PS[0][:, 0:nb], func=AF.Square), reads=[TPS[0], t_cqb], writes=[t_cqb])
                k.op("dve", lambda en: en.scalar_tensor_tensor(out=cq[:, 0, 0:nb], in0=PS[0][:, 0:nb], scalar=lw.gq[:, 0:1], in1=rp[:, 2, 0:nb], op0=ALU.mult, op1=ALU.mult), reads=[TPS[0], t_rp, lw.tok, t_cq], writes=[t_cq])
                k.op("dve", lambda en: en.scalar_tensor_tensor(out=cq[:, 1, 0:nb], in0=PS[2][:, 0:nb], scalar=lw.gq[:, 1:2], in1=rp[:, 3, 0:nb], op0=ALU.mult, op1=ALU.mult), reads=[TPS[2], t_rp, lw.tok, t_cq], writes=[t_cq])
                k.op("pe", lambda en: en.matmul(PS[0][:, 0:nb], lhsT=g.blk64[:], rhs=cqb[:, 0, 0:nb], start=True, stop=True), reads=[t_cqb, g.T_const, t_cq], writes=[TPS[0]])
                rstd_from_ms(cq[:, 2, 0:nb], PS[0][:, 0:nb], [TPS[0]], t_cq, cq[:, 2, 0:nb])
                k.op("pool", lambda en: en.tensor_tensor(out=cq[:, 0, 0:nb], in0=cq[:, 0, 0:nb], in1=cq[:, 1, 0:nb], op=ALU.add), reads=[t_cq], writes=[t_cq])
                k.op("dve", lambda en: en.tensor_tensor(out=qg[j][:, 0:nb], in0=cq[:, 0, 0:nb], in1=cq[:, 2, 0:nb], op=ALU.mult), reads=[t_cq], writes=[t_qg])
            if "qm0" in g.dbg and t0 == 256:
                k.dma("pool", g.dbg["qm0"], qm[0][:], reads=[t_qm])
                k.dma("pool", g.dbg["qg0"], qg[0][:], reads=[t_qg])
            pending = []
            for hh in range(8):
                while len(pending) > 1:
                    pending.pop(0)()
                if hh < 4:
                    dk, scale = 96, 96 ** -0.5
                    Kt = KTm[hh]
                    kb0 = 0
                    Qt = qm[hh]
                    qb0 = 0
                    Vt, vc0 = VM, hh * 65
                    tq = t_qm
                else:
                    hq = hh - 4
                    kv = hq // 2
                    dk, scale = 64, 64 ** -0.5
                    Kt, kb0 = KTg, kv * 64
                    Qt, qb0 = qg[hq % 2], kv * 64
                    Vt, vc0 = VG, kv * 65
                    tq = t_qg
                oi = O_bufs[hh % 2]
                pairs = [kts[i:i + 2] for i in range(0, len(kts), 2)]

                def emit_S(pidx):
                    sbi = pidx % 3
                    for j, kt in enumerate(pairs[pidx]):
                        k.op("pe", lambda en: en.matmul(SB2[sbi][:, j, 0:nb], lhsT=Kt[kb0:kb0 + dk, kt * 128:(kt + 1) * 128], rhs=Qt[qb0:qb0 + dk, 0:nb], start=True, stop=True), reads=[t_K, tq], writes=[TSB[sbi]])

                emit_S(0)
                if len(pairs) > 1:
                    emit_S(1)
                for pidx in range(len(pairs)):
                    if pidx == 3 and pending:
                        pending.pop(0)()
                    if pidx + 2 < len(pairs):
                        emit_S(pidx + 2)
                    sbi = pidx % 3
                    npr = len(pairs[pidx])
                    Pt, t_P = P_r.next()
                    k.op("act", lambda en: en.activation(out=Pt[:, 0:npr, 0:nb], in_=SB2[sbi][:, 0:npr, 0:nb], func=AF.Exp, scale=scale), reads=[TSB[sbi]], writes=[t_P])
                    for j, kt in enumerate(pairs[pidx]):
                        first = (pidx == 0 and j == 0)
                        lastm = (pidx == len(pairs) - 1 and j == npr - 1)
                        k.op("pe", lambda en: en.matmul(PS[oi][0:65, 0:nb], lhsT=Vt[:, kt, vc0:vc0 + 65], rhs=Pt[:, j, 0:nb], start=first, stop=lastm), reads=[t_P, t_K], writes=[TPS[oi]])
                osb, t_osb = osb_r.next()
                k.op("dve", lambda en: en.tensor_copy(out=osb[:, 0:nb], in_=PS[oi][0:65, 0:nb]), reads=[TPS[oi]], writes=[t_osb])
                k.op("dve", lambda en: en.reciprocal(out=osb[64:65, 0:nb], in_=osb[64:65, 0:nb]), reads=[t_osb], writes=[t_osb])

                def finish(hh=hh, oi=oi, osb=osb, t_osb=t_osb):
                    k.op("pe", lambda en: en.matmul(PS[oi][0:64, 0:nb], lhsT=g.onesf[64:65, 0:64], rhs=osb[64:65, 0:nb], start=True, stop=True), reads=[t_osb, g.T_const], writes=[TPS[oi]])
                    k.op("dve", lambda en: en.tensor_tensor(out=osb[0:64, 0:nb], in0=osb[0:64, 0:nb], in1=PS[oi][0:64, 0:nb], op=ALU.mult), reads=[t_osb, TPS[oi]], writes=[t_osb])
                    res, t_res = res_r.next()
                    k.op("pool", lambda en: en.tensor_tensor(out=res[:, 0:nb], in0=osb[0:64, 0:nb], in1=gate[:, hh, 0:nb], op=ALU.mult), reads=[t_osb, t_gate], writes=[t_res])
                    kc = hh // 2
                    p0 = (hh % 2) * 64
                    k.dma("pool", g.CATT[p0:p0 + 64, kc, t0:t0 + nb], res[:, 0:nb], reads=[t_res], writes=[g.T_CATT])
                pending.append(finish)
            while pending:
                pending.pop(0)()
        k.barrier()


def phase_P6(g, l, s, src, dst, need_ctx, lat_only_out):
    nc, k = g.nc, g.k
    with ExitStack() as es:
        sb = lambda n, s_, d=F32: es.enter_context(nc.sbuf_tensor(_nm(n), s_, d))
        ps = lambda n, s_, d=F32: es.enter_context(nc.psum_tensor(_nm(n), s_, d))
        wo = sb("wo", [128, 8, D], BF16)
        t_w = Tok()
        k.dma("sp", wo[:], g.WOUTB, reads=[g.T_WOUTB], writes=[t_w])
        Gb = {}
        for v in ((s, 2) if need_ctx else (s,)):
            Gb[v] = sb("Gb%d" % v, [128, D])
            k.dma("sp", Gb[v][:], g.MODROWS[v:v + 1, 2 * D:3 * D].broadcast_to([128, D]), reads=[g.T_MOD], writes=[t_w])
        neghalf = sb("neghalf6", [128, 1])
        k.op("pool", lambda en: en.memset(neghalf[:], -0.5), writes=[t_w])
        cat_r = Rot([sb("cat%d" % i, [128, 8, 512], BF16) for i in range(2)])
        x_r = Rot([sb("x6_%d" % i, [128, D]) for i in range(3)])
        o_r = Rot([sb("o6_%d" % i, [128, D]) for i in range(3)])
        st_r = Rot([sb("st6_%d" % i, [128, 4]) for i in range(3)])
        junk = sb("junk6", [128, D], BF16)
        t_junk = Tok()
        po_r = Rot([ps("po%d" % i, [128, D]) for i in range(3)], excl=True)
        blocks = ([(0, 256)] if need_ctx else []) + [(256 + 512 * b, 512) for b in range(8)]
        for (t0, nb) in blocks:
            cat, t_cat = cat_r.next()
            k.dma("sp", cat[:, :, 0:nb], g.CATT[:, :, t0:t0 + nb], reads=[g.T_CATT], writes=[t_cat])
            for j in range(nb // 128):
                tok0 = t0 + j * 128
                po, t_po = po_r.next()
                for hf in range(2):
                    for kc in range(8):
                        k.op("pe", lambda en: en.matmul(po[:, hf * 512:(hf + 1) * 512], lhsT=cat[:, kc, j * 128:(j + 1) * 128], rhs=wo[:, kc, hf * 512:(hf + 1) * 512], start=(kc == 0), stop=(kc == 7)), reads=[t_cat, t_w], writes=[t_po])
                xt, t_x = x_r.next()
                k.dma("sp", xt[:], src[s, tok0:tok0 + 128, :], reads=[g.T_XS[s]], writes=[t_x])
                st, t_st = st_r.next()
                k.op("act", lambda en: en.activation(out=junk[:], in_=po[:], func=AF.Square, accum_out=st[:, 0:1]), reads=[t_po], writes=[t_junk, t_st])
                k.op("dve", lambda en: en.tensor_scalar(out=st[:, 1:2], in0=st[:, 0:1], scalar1=1.0 / D, scalar2=EPS, op0=ALU.mult, op1=ALU.add), reads=[t_st], writes=[t_st])
                k.op("pool", lambda en: en.tensor_tensor(out=st[:, 2:3], in0=st[:, 1:2], in1=neghalf[:], op=ALU.pow), reads=[t_st, t_w], writes=[t_st])
                ot, t_o = o_r.next()
                gb = Gb[2] if t0 == 0 else Gb[s]
                k.op("dve", lambda en: en.scalar_tensor_tensor(out=ot[:], in0=po[:], scalar=st[:, 2:3], in1=gb[:], op0=ALU.mult, op1=ALU.mult), reads=[t_po, t_st, t_w], writes=[t_o])
                k.op("pool", lambda en: en.tensor_tensor(out=ot[:], in0=ot[:], in1=xt[:], op=ALU.add), reads=[t_o, t_x], writes=[t_o])
                if lat_only_out:
                    if t0 == 0:
                        continue
                    k.dma("pool", dst[s, tok0 - C:tok0 - C + 128, :], ot[:], reads=[t_o], writes=[g.T_Y])
                else:
                    wt = g.T_XS[s] if dst is g.XS else g.T_Y
                    k.dma("pool", dst[s, tok0:tok0 + 128, :], ot[:], reads=[t_o], writes=[wt])
        k.barrier()


_CACHE = {}


def _weights_host(inputs):
    w = {}
    for n in ("w_mod", "b_mod", "g_pre", "g_post", "w_in", "w_out", "mla_g_cq", "mla_w_uq", "mla_g_ckv", "mla_w_ukv",
              "hy_conv_w", "hy_conv_b", "hy_f_w1", "hy_f_b1", "hy_f_freq1", "hy_f_w2", "hy_f_b2", "hy_f_freq2", "hy_f_w3", "hy_bias"):
        w[n] = np.ascontiguousarray(inputs[n], dtype=np.float32)
    p64, _ = _partner_index(64)
    gq = np.asarray(inputs["gqa_g_q"], np.float32)
    gk = np.asarray(inputs["gqa_g_k"], np.float32)
    cols = np.zeros((DEPTH, 128, 4), np.float32)
    for l in range(DEPTH):
        cols[l, :, 0] = np.concatenate([gq[l], gq[l]])
        cols[l, :, 1] = np.concatenate([gq[l][p64], gq[l][p64]])
        cols[l, :, 2] = np.concatenate([gk[l], gk[l]])
        cols[l, :, 3] = np.concatenate([gk[l][p64], gk[l][p64]])
    w["gq_cols"] = cols
    w.update(ssm_host_layouts(inputs))
    return w


def make_in_maps(inputs, xs_per_core):
    w = _weights_host(inputs)
    cst = host_constants()
    c = np.asarray(inputs["c"], np.float32)
    c_ctx = np.asarray(inputs["c_ctx"], np.float32)
    maps = []
    for i in range(NCORES):
        cv = np.stack([c[2 * i], c[2 * i + 1], c_ctx], -1)
        cT = np.ascontiguousarray(cv.reshape(8, 128, 3).transpose(1, 0, 2))
        m = {"xs": xs_per_core[i], "cT": cT}
        m.update(w)
        m.update(cst)
        maps.append(m)
    return maps


def kernel(**inputs):
    x = np.asarray(inputs["x"], np.float32)
    ctx = np.asarray(inputs["ctx"], np.float32)
    xs = [np.ascontiguousarray(np.concatenate([ctx[2 * i:2 * i + 2], x[2 * i:2 * i + 2]], axis=1)) for i in range(NCORES)]
    key = "fused"
    if key not in _CACHE:
        _CACHE[key] = build_program(list(range(DEPTH)), True)[0]
    nc = _CACHE[key]
    res = run_bass_kernel_spmd(nc, make_in_maps(inputs, xs), core_ids=list(range(NCORES)))
    out = np.concatenate([r["y"] for r in res.results], axis=0)
    return out.astype(np.float32)


HY_BANDS = 16
HY_DECAY = (math.log(1e-2) / 1.5, math.log(1e-2) / 0.3)


class HyCfg:
    def __init__(self, name, n, ni):
        self.name = name
        self.n = n
        self.ni = ni
        self.N = 2 * n
        self.cpg = 128 // ni
        self.ng = 256 // self.cpg
        self.ncol = 256 * ni


HY_LAT = HyCfg("L", 4096, 32)
HY_CTX = HyCfg("C", 256, 2)


def _hy_features(n, pos):
    f32 = np.float32
    t = np.linspace(0.0, 1.0, n, dtype=f32)
    omega = (f32(2.0 * math.pi) * np.arange(n, dtype=f32) / f32(n)).astype(f32)
    bands = np.linspace(1e-4, HY_BANDS - 1, HY_BANDS, dtype=f32)
    tt = t[pos]
    om = omega[pos]
    ang = (bands[:, None] * om[None, :]).astype(f32)
    z = np.concatenate([tt[None, :], np.cos(ang), -np.sin(ang)], axis=0).astype(f32)
    return z, tt


def hyena_constants():
    cst = {}
    f64 = np.float64
    j = np.arange(128)[:, None]
    b = np.arange(256)[None, :]
    ang = 2 * np.pi * j * b / 256.0
    F1 = np.concatenate([np.cos(ang), -np.sin(ang)], 1)
    sgn = np.where(b % 2 == 0, 1.0, -1.0)
    F1b = np.concatenate([np.cos(ang) * sgn, -np.sin(ang) * sgn], 1)
    cst["hk_F1"] = np.stack([F1, F1b], 0).astype(np.float32)
    for cfg in (HY_LAT, HY_CTX):
        p = np.arange(128)
        i_of_p = (p // cfg.cpg)[:, None]
        ang = 2 * np.pi * i_of_p * b / cfg.N
        TW = np.stack([np.concatenate([np.cos(ang), np.cos(ang)], 1), np.concatenate([-np.sin(ang), -np.sin(ang)], 1)], 0)
        cst["hk_TW" + cfg.name] = TW.astype(np.float32)
        ii = (p // cfg.cpg)[:, None]
        ci = (p % cfg.cpg)[:, None]
        aa = (p // cfg.cpg)[None, :]
        ca = (p % cfg.cpg)[None, :]
        dl = (ci == ca).astype(f64)
        ang = 2 * np.pi * ii * aa / cfg.ni
        Gr = dl * np.cos(ang)
        Gi = -dl * np.sin(ang)
        cst["hk_G" + cfg.name] = np.stack([Gr, Gi, -Gi], 0).astype(np.float32)
        ang = 2 * np.pi * aa.T * ii.T / cfg.ni
        dl2 = (ci.T == ca.T)
        angm = 2 * np.pi * (p // cfg.cpg)[:, None] * (p // cfg.cpg)[None, :] / cfg.ni
        dlm = ((p % cfg.cpg)[:, None] == (p % cfg.cpg)[None, :]).astype(f64)
        GIr = dlm * np.cos(angm)
        GIi = dlm * np.sin(angm)
        cst["hk_GI" + cfg.name] = np.stack([np.concatenate([GIr, GIi], 1), np.concatenate([-GIi, GIr], 1)], 0).astype(np.float32)
        TWI = np.zeros((2, 2, 128, 256), f64)
        for bc in range(2):
            bb = (bc * 128 + np.arange(128))[:, None]
            ang = 2 * np.pi * bb * (p // cfg.cpg)[None, :] / cfg.N
            TWI[bc, 0] = np.concatenate([np.cos(ang), np.cos(ang)], 1)
            TWI[bc, 1] = np.concatenate([np.sin(ang), np.sin(ang)], 1)
        cst["hk_TWI" + cfg.name] = TWI.astype(np.float32)
        FI = np.zeros((2, 2, 128, 128), f64)
        for bc in range(2):
            bb = (bc * 128 + np.arange(128))[:, None]
            ang = 2 * np.pi * bb * np.arange(128)[None, :] / 256.0
            FI[bc, 0] = np.cos(ang) / cfg.N
            FI[bc, 1] = -np.sin(ang) / cfg.N
        cst["hk_FI" + cfg.name] = FI.astype(np.float32)
        n = cfg.n
        u = np.arange(n)
        posr = np.where(u == 0, 0, n - u)
        zf, tf_ = _hy_features(n, u)
        zb, tb_ = _hy_features(n, posr)
        cst["hk_Z" + cfg.name] = np.stack([zf, zb], 0)
        deltas = np.abs(np.linspace(HY_DECAY[0], HY_DECAY[1], 256, dtype=np.float32))
        decf = np.exp(-tf_[:, None] * deltas[None, :]).astype(np.float32)
        decb = np.exp(-tb_[:, None] * deltas[None, :]).astype(np.float32)
        cst["hk_DEC" + cfg.name] = np.stack([decf.reshape(128, cfg.ni, 256), decb.reshape(128, cfg.ni, 256)], 0)
        msk = (p[:, None] % cfg.cpg == np.arange(cfg.cpg)[None, :]).astype(np.float32)
        cst["hk_MSK" + cfg.name] = msk
    return cst


def _hy_twiddle(k, A_ps, t_A, TW, t_TW, m1, m2, t_m, outr, outi, t_out, n2, sub_first=True):
    k.op("dve", lambda en: en.tensor_tensor(out=m1[:, 0:2 * n2], in0=A_ps, in1=TW[:, 0, 0:2 * n2], op=ALU.mult), reads=[t_A, t_TW], writes=[t_m])
    k.op("dve", lambda en: en.tensor_tensor(out=m2[:, 0:2 * n2], in0=A_ps, in1=TW[:, 1, 0:2 * n2], op=ALU.mult), reads=[t_A, t_TW, t_m], writes=[t_m])
    k.op("pool", lambda en: en.tensor_tensor(out=outr, in0=m1[:, 0:n2], in1=m2[:, n2:2 * n2], op=ALU.subtract), reads=[t_m], writes=[t_out])
    k.op("pool", lambda en: en.tensor_tensor(out=outi, in0=m2[:, 0:n2], in1=m1[:, n2:2 * n2], op=ALU.add), reads=[t_m, t_out], writes=[t_out])


def phase_hy_filter(g, l, cfg):
    nc, k, W, K = g.nc, g.k, g.W, g.K
    n, ni, ng, cpg, ncol = cfg.n, cfg.ni, cfg.ng, cfg.cpg, cfg.ncol
    KH = g.KHAT[cfg.name]
    with ExitStack() as es:
        sb = lambda nm, s_, d=F32: es.enter_context(nc.sbuf_tensor(_nm(nm), s_, d))
        ps = lambda nm, s_, d=F32: es.enter_context(nc.psum_tensor(_nm(nm), s_, d))
        t_w = Tok()
        w1 = sb("hw1", [33, 64]); w2 = sb("hw2", [64, 64]); w3 = sb("hw3", [64, 1024])
        cols = sb("hcols", [64, 8])
        k.dma("sp", w1[:], W["hy_f_w1"][l], writes=[t_w])
        k.dma("sp", w2[:], W["hy_f_w2"][l], writes=[t_w])
        k.dma("sp", w3[:], W["hy_f_w3"][l], writes=[t_w])
        for ci, nm in enumerate(("hy_f_b1", "hy_f_freq1", "hy_f_b2", "hy_f_freq2")):
            k.dma("sp", cols[:, ci:ci + 1], W[nm][l].rearrange("(p o) -> p o", o=1), writes=[t_w])
        k.op("dve", lambda en: en.tensor_tensor(out=cols[:, 4:5], in0=cols[:, 0:1], in1=cols[:, 1:2], op=ALU.mult), reads=[t_w], writes=[t_w])
        k.op("dve", lambda en: en.tensor_tensor(out=cols[:, 5:6], in0=cols[:, 2:3], in1=cols[:, 3:4], op=ALU.mult), reads=[t_w], writes=[t_w])
        F1 = sb("hF1", [128, 2, 512]); TW = sb("hTW", [128, 2, 512]); G3 = sb("hG", [128, 3, 128]); MSK = sb("hmsk", [128, cpg])
        t_c = Tok()
        k.dma("sp", F1[:], K["hk_F1"].rearrange("a p n -> p a n"), writes=[t_c])
        k.dma("sp", TW[:], K["hk_TW" + cfg.name].rearrange("a p n -> p a n"), writes=[t_c])
        k.dma("sp", G3[:], K["hk_G" + cfg.name].rearrange("a p n -> p a n"), writes=[t_c])
        k.dma("sp", MSK[:], K["hk_MSK" + cfg.name], writes=[t_c])
        epsc = sb("hyeps", [128, 1])
        k.op("pool", lambda en: en.memset(epsc[:], EPS), writes=[t_c])
        h2T = [sb("h2T%d" % d_, [64, n]) for d_ in range(2)]
        t_h2 = Tok()
        PSm = [ps("hps%d" % i, [128, 512]) for i in range(4)]
        TP = [Tok(True) for _ in range(4)]
        PI = math.pi

        def sin_layer(out_ap, ps_ap, t_ps, fr_col, fb_col, tmp, msk_, t_tmp, wtok):
            k.op("dve", lambda en: en.tensor_scalar(out=tmp, in0=ps_ap, scalar1=fr_col, scalar2=fb_col, op0=ALU.mult, op1=ALU.add), reads=[t_ps, t_w], writes=[t_tmp])
            k.op("dve", lambda en: en.tensor_scalar(out=msk_, in0=tmp, scalar1=PI, scalar2=-2 * PI, op0=ALU.is_gt, op1=ALU.mult), reads=[t_tmp], writes=[t_tmp])
            k.op("dve", lambda en: en.tensor_tensor(out=tmp, in0=tmp, in1=msk_, op=ALU.add), reads=[t_tmp], writes=[t_tmp])
            k.op("dve", lambda en: en.tensor_scalar(out=msk_, in0=tmp, scalar1=-PI, scalar2=2 * PI, op0=ALU.is_lt, op1=ALU.mult), reads=[t_tmp], writes=[t_tmp])
            k.op("dve", lambda en: en.tensor_tensor(out=tmp, in0=tmp, in1=msk_, op=ALU.add), reads=[t_tmp], writes=[t_tmp])
            k.op("act", lambda en: en.activation(out=out_ap, in_=tmp, func=AF.Sin), reads=[t_tmp], writes=[wtok])

        with ExitStack() as es2:
            sb2 = lambda nm, s_, d=F32: es2.enter_context(nc.sbuf_tensor(_nm(nm), s_, d))
            Zt = sb2("hZ", [33, n])
            t_z = Tok()
            h1 = sb2("hh1", [64, 512]); tmp = sb2("htmp", [64, 512]); msk_ = sb2("hmk", [64, 512])
            t_h1 = Tok(); t_tmp = Tok()
            bw = min(512, n)
            for d_ in range(2):
                k.dma("sp", Zt[:], K["hk_Z" + cfg.name][d_], reads=[], writes=[t_z])
                for b0 in range(0, n, bw):
                    k.op("pe", lambda en: en.matmul(PSm[0][0:64, 0:bw], lhsT=w1[:], rhs=Zt[:, b0:b0 + bw], start=True, stop=True), reads=[t_w, t_z], writes=[TP[0]])
                    sin_layer(h1[:, 0:bw], PSm[0][0:64, 0:bw], TP[0], cols[:, 1:2], cols[:, 4:5], tmp[:, 0:bw], msk_[:, 0:bw], t_tmp, t_h1)
                    k.op("pe", lambda en: en.matmul(PSm[1][0:64, 0:bw], lhsT=w2[:], rhs=h1[:, 0:bw], start=True, stop=True), reads=[t_w, t_h1], writes=[TP[1]])
                    sin_layer(h2T[d_][:, b0:b0 + bw], PSm[1][0:64, 0:bw], TP[1], cols[:, 3:4], cols[:, 5:6], tmp[:, 0:bw], msk_[:, 0:bw], t_tmp, t_h2)
            k.barrier()
        UF = [sb("hUF%d" % d_, [128, ncol]) for d_ in range(2)]
        t_uf = Tok()
        DEC = sb("hDEC", [128, ni, 256])
        t_dec = Tok()
        HSQ = sb("hHSQ", [128, 256]); sq = sb("hsq", [128, 256]); hh = sb("hhh", [128, 256])
        t_hsq = Tok(); t_hh = Tok(); t_sq = Tok()
        RS = sb("hRS", [128, 256]); SC = sb("hSC", [128, ng]); rtmp = sb("hrtmp", [128, 256])
        t_rs = Tok()
        m1 = sb("hm1", [128, 512]); m2 = sb("hm2", [128, 512])
        P1 = sb("hP1", [128, 2, 256])
        t_m = Tok(); t_p1 = Tok()
        ko_r = Rot([sb("hko%d" % i, [128, 512]) for i in range(2)])
        for o in range(2):
            k.op("pool", lambda en: en.memset(HSQ[:], 0.0), reads=[t_rs], writes=[t_hsq])
            for d_ in range(2):
                k.dma("sp", DEC[:], K["hk_DEC" + cfg.name][d_], writes=[t_dec])
                c0 = o * 512 + d_ * 256
                for i in range(ni):
                    pi_ = i % 2
                    k.op("pe", lambda en: en.matmul(PSm[pi_][:, 0:256], lhsT=h2T[d_][:, i:n:ni], rhs=w3[:, c0:c0 + 256], start=True, stop=True), reads=[t_h2, t_w], writes=[TP[pi_]])
                    k.op("dve", lambda en: en.tensor_tensor(out=hh[:], in0=PSm[pi_][:, 0:256], in1=DEC[:, i, :], op=ALU.mult), reads=[TP[pi_], t_dec], writes=[t_hh])
                    k.op("act", lambda en: en.activation(out=UF[d_][:].rearrange("p (g i c) -> p g i c", i=ni, c=cpg)[:, :, i, :], in_=hh[:].rearrange("p (g c) -> p g c", c=cpg), func=AF.Copy), reads=[t_hh], writes=[t_uf])
                    k.op("act", lambda en: en.activation(out=sq[:], in_=hh[:], func=AF.Square), reads=[t_hh], writes=[t_sq])
                    k.op("pool", lambda en: en.tensor_tensor(out=HSQ[:], in0=HSQ[:], in1=sq[:], op=ALU.add), reads=[t_sq, t_hsq], writes=[t_hsq])
            k.op("pe", lambda en: en.matmul(PSm[2][:, 0:256], lhsT=g.onesf[:], rhs=HSQ[:], start=True, stop=True), reads=[t_hsq, g.T_const], writes=[TP[2]])
            k.op("act", lambda en: en.activation(out=rtmp[:], in_=PSm[2][:, 0:256], func=AF.Ln, bias=epsc[:, 0:1]), reads=[TP[2], t_c], writes=[t_rs])
            k.op("act", lambda en: en.activation(out=RS[:], in_=rtmp[:], func=AF.Exp, scale=-0.5), reads=[t_rs], writes=[t_rs])
            k.op("dve", lambda en: en.tensor_tensor(out=rtmp[:].rearrange("p (g c) -> p g c", c=cpg), in0=RS[:].rearrange("p (g c) -> p g c", c=cpg), in1=MSK[:].unsqueeze(1).broadcast_to([128, ng, cpg]), op=ALU.mult), reads=[t_rs, t_c], writes=[t_rs])
            k.op("dve", lambda en: en.tensor_reduce(out=SC[:], in_=rtmp[:].rearrange("p (g c) -> p g c", c=cpg), axis=AX.X, op=ALU.add), reads=[t_rs], writes=[t_rs])
            k.op("dve", lambda en: en.memset(UF[1][0:1, :].rearrange("p (g i c) -> p g i c", i=ni, c=cpg)[:, :, 0, :], 0.0), reads=[t_uf], writes=[t_uf])
            for grp in range(ng):
                k.op("pe", lambda en: en.matmul(PSm[0][:], lhsT=UF[0][:, grp * 128:(grp + 1) * 128], rhs=F1[:, 0, :], start=True, stop=False), reads=[t_uf, t_c], writes=[TP[0]])
                k.op("pe", lambda en: en.matmul(PSm[0][:], lhsT=UF[1][:, grp * 128:(grp + 1) * 128], rhs=F1[:, 1, :], start=False, stop=True), reads=[t_uf, t_c], writes=[TP[0]])
                _hy_twiddle(k, PSm[0][:], TP[0], TW, t_c, m1, m2, t_m, P1[:, 0, :], P1[:, 1, :], t_p1, 256)
                k.op("pe", lambda en: en.matmul(PSm[1][:, 0:256], lhsT=G3[:, 0, :], rhs=P1[:, 0, :], start=True, stop=False), reads=[t_p1, t_c], writes=[TP[1]])
                k.op("pe", lambda en: en.matmul(PSm[1][:, 0:256], lhsT=G3[:, 2, :], rhs=P1[:, 1, :], start=False, stop=True), reads=[t_p1, t_c], writes=[TP[1]])
                k.op("pe", lambda en: en.matmul(PSm[1][:, 256:512], lhsT=G3[:, 0, :], rhs=P1[:, 1, :], start=False, stop=False), reads=[t_p1, t_c], writes=[TP[1]])
                k.op("pe", lambda en: en.matmul(PSm[1][:, 256:512], lhsT=G3[:, 1, :], rhs=P1[:, 0, :], start=False, stop=True), reads=[t_p1, t_c], writes=[TP[1]])
                ko, t_ko = ko_r.next()
                k.op("dve", lambda en: en.tensor_scalar(out=ko[:], in0=PSm[1][:], scalar1=SC[:, grp:grp + 1], scalar2=None, op0=ALU.mult), reads=[TP[1], t_rs], writes=[t_ko])
                k.dma("pool", KH[o, grp], ko[:], reads=[t_ko], writes=[g.T_KHAT])
        k.barrier()


def phase_hyena(g, l, s, cfg, tok0):
    nc, k, W, K = g.nc, g.k, g.W, g.K
    n, ni, ng, cpg, ncol = cfg.n, cfg.ni, cfg.ng, cfg.cpg, cfg.ncol
    KH = g.KHAT[cfg.name]
    with ExitStack() as es:
        sb = lambda nm, s_, d=F32: es.enter_context(nc.sbuf_tensor(_nm(nm), s_, d))
        ps = lambda nm, s_, d=F32: es.enter_context(nc.psum_tensor(_nm(nm), s_, d))
        PSm = [ps("yps%d" % i, [128, 512]) for i in range(6)]
        TP = [Tok(True) for _ in range(6)]
        PT = [ps("ypt%d" % i, [128, 8, 128], BF16) for i in range(2)]
        TPT = [Tok(True) for _ in range(2)]
        t_c = Tok()
        cf = sb("ycf", [128, 1024])
        F1b = sb("yF1", [128, 512], BF16); G3 = sb("yG", [128, 3, 128], BF16); GI = sb("yGI", [128, 2, 256], BF16); FI = sb("yFI", [128, 2, 2, 128], BF16)
        TW = sb("yTW", [128, 2, 512]); TWI = sb("yTWI", [128, 2, 2, 256])
        k.dma("sp", cf[:, 0:512], K["hk_F1"][0], writes=[t_c])
        k.op("dve", lambda en: en.tensor_copy(out=F1b[:], in_=cf[:, 0:512]), reads=[t_c], writes=[t_c])
        k.dma("sp", cf[:, 0:384].rearrange("p (a n) -> p a n", a=3), K["hk_G" + cfg.name].rearrange("a p n -> p a n"), reads=[t_c], writes=[t_c])
        k.op("dve", lambda en: en.tensor_copy(out=G3[:], in_=cf[:, 0:384].rearrange("p (a n) -> p a n", a=3)), reads=[t_c], writes=[t_c])
        k.dma("sp", cf[:, 0:512].rearrange("p (a n) -> p a n", a=2), K["hk_GI" + cfg.name].rearrange("a p n -> p a n"), reads=[t_c], writes=[t_c])
        k.op("dve", lambda en: en.tensor_copy(out=GI[:], in_=cf[:, 0:512].rearrange("p (a n) -> p a n", a=2)), reads=[t_c], writes=[t_c])
        k.dma("sp", cf[:, 0:512].rearrange("p (b a n) -> p b a n", b=2, a=2), K["hk_FI" + cfg.name].rearrange("b a p n -> p b a n"), reads=[t_c], writes=[t_c])
        k.op("dve", lambda en: en.tensor_copy(out=FI[:], in_=cf[:, 0:512].rearrange("p (b a n) -> p b a n", b=2, a=2)), reads=[t_c], writes=[t_c])
        k.dma("sp", TW[:], K["hk_TW" + cfg.name].rearrange("a p n -> p a n"), writes=[t_c])
        k.dma("sp", TWI[:], K["hk_TWI" + cfg.name].rearrange("b a p n -> p b a n"), writes=[t_c])
        cw = sb("ycw", [128, 6, 3]); cb = sb("ycb", [128, 6]); BR = sb("yBR", [128, 2, 256])
        for kk in range(3):
            k.dma("sp", cw[:, :, kk:kk + 1], W["hy_conv_w"][l, kk, :].rearrange("(c p o) -> p c o", p=128, o=1), writes=[t_c], allow_slow_non_contiguous=True)
        k.dma("sp", cb[:].unsqueeze(2), W["hy_conv_b"][l].rearrange("(c p o) -> p c o", p=128, o=1), writes=[t_c], allow_slow_non_contiguous=True)
        k.dma("sp", BR[:], W["hy_bias"][l:l + 1].broadcast_to([128, 2, 256]), writes=[t_c])
        U = {nm: sb("yU" + nm, [128, ncol], BF16) for nm in ("gt", "x2", "v", "x1")}
        t_U = {nm: Tok() for nm in ("gt", "x2", "v", "x1", "z1")}
        names = ["v", "v", "x1", "x1", "x2", "x2", "gt", "gt"]
        with ExitStack() as es1:
            sb1 = lambda nm, s_, d=F32: es1.enter_context(nc.sbuf_tensor(_nm(nm), s_, d))
            hT = sb1("yhT", [128, 8, n], BF16)
            whb_r = Rot([sb1("ywhb%d" % i, [128, 8, 128], BF16) for i in range(2)])
            t_h = Tok()
            k.dma("sp", hT[:], g.HT[:, :, tok0:tok0 + n], reads=[g.T_HT], writes=[t_h])
            PJ = sb1("yPJ", [128, n]); CV = sb1("yCV", [128, n]); CVb = sb1("yCVb", [128, n], BF16)
            t_pj = Tok(); t_cv = Tok(); t_cvb = Tok()
            bw = min(512, n)
            for c8 in range(8):
                whb, t_whb = whb_r.next()
                k.dma("sp", whb[:], g.WINB[:, :, O_HY + c8 * 128:O_HY + (c8 + 1) * 128], reads=[g.T_WINB], writes=[t_whb])
                for bi, b0 in enumerate(range(0, n, bw)):
                    pi_ = bi % 2
                    for kc in range(8):
                        k.op("pe", lambda en: en.matmul(PSm[pi_][:, 0:bw], lhsT=whb[:, kc, :], rhs=hT[:, kc, b0:b0 + bw], start=(kc == 0), stop=(kc == 7)), reads=[t_h, t_whb], writes=[TP[pi_]])
                    if c8 < 6:
                        k.op("act", lambda en: en.activation(out=PJ[:, b0:b0 + bw], in_=PSm[pi_][:, 0:bw], func=AF.Copy), reads=[TP[pi_]], writes=[t_pj])
                    else:
                        k.op("act", lambda en: en.activation(out=CVb[:, b0:b0 + bw], in_=PSm[pi_][:, 0:bw], func=AF.Silu), reads=[TP[pi_]], writes=[t_cvb])
                if c8 < 6:
                    k.op("dve", lambda en: en.tensor_scalar(out=CV[:], in0=PJ[:], scalar1=cw[:, c8, 1:2], scalar2=cb[:, c8:c8 + 1], op0=ALU.mult, op1=ALU.add), reads=[t_pj, t_c], writes=[t_cv])
                    k.op("dve", lambda en: en.scalar_tensor_tensor(out=CV[:, 1:n], in0=PJ[:, 0:n - 1], scalar=cw[:, c8, 0:1], in1=CV[:, 1:n], op0=ALU.mult, op1=ALU.add), reads=[t_pj, t_c, t_cv], writes=[t_cv])
                    k.op("dve", lambda en: en.scalar_tensor_tensor(out=CVb[:, 0:n - 1], in0=PJ[:, 1:n], scalar=cw[:, c8, 2:3], in1=CV[:, 0:n - 1], op0=ALU.mult, op1=ALU.add), reads=[t_pj, t_c, t_cv], writes=[t_cvb])
                    k.op("dve", lambda en: en.tensor_copy(out=CVb[:, n - 1:n], in_=CV[:, n - 1:n]), reads=[t_cv, t_cvb], writes=[t_cvb])
                nm = names[c8]
                g0 = (c8 % 2) * (128 // cpg)
                Uv = U[nm][:].rearrange("p (g i c) -> p g i c", i=ni, c=cpg)
                nb4 = min(4, ni)
                for i0 in range(0, ni, nb4):
                    pt = (i0 // nb4) % 2
                    for ii in range(nb4):
                        k.op("pe", lambda en: en.transpose(PT[pt][:, ii, :], CVb[:, i0 + ii:n:ni], g.ident[:]), reads=[t_cvb, g.T_ident], writes=[TPT[pt]])
                    k.op("act" if (i0 // nb4) % 2 else "dve",
                         (lambda en: en.activation(out=Uv[:, g0:g0 + 128 // cpg, i0:i0 + nb4, :].rearrange("p g i c -> p i g c"), in_=PT[pt][:, 0:nb4, :].rearrange("p i (g c) -> p i g c", c=cpg), func=AF.Copy)) if (i0 // nb4) % 2 else
                         (lambda en: en.tensor_copy(out=Uv[:, g0:g0 + 128 // cpg, i0:i0 + nb4, :].rearrange("p g i c -> p i g c"), in_=PT[pt][:, 0:nb4, :].rearrange("p i (g c) -> p i g c", c=cpg))),
                         reads=[TPT[pt]], writes=[t_U[nm]])
            k.barrier()
        with ExitStack() as es2:
            sb2 = lambda nm, s_, d=F32: es2.enter_context(nc.sbuf_tensor(_nm(nm), s_, d))
            U["z1"] = sb2("yUz1", [128, ncol], BF16)
            BW = [[sb2("yBW%d%d" % (bc, ri), [128, ncol], BF16) for ri in range(2)] for bc in range(2)]
            t_bw = Tok()
            mm_r = Rot([(sb2("ym1_%d" % i, [128, 512]), sb2("ym2_%d" % i, [128, 512])) for i in range(5)])
            P1_r = Rot([sb2("yP1_%d" % i, [128, 2, 256], BF16) for i in range(3)])
            Y_r = Rot([sb2("yY_%d" % i, [128, 2, 256], BF16) for i in range(3)])
            kh_r = Rot([sb2("ykh%d" % i, [128, 512]) for i in range(3)])
            e1 = sb2("ye1", [128, 512]); e2 = sb2("ye2", [128, 512]); e3 = sb2("ye3", [128, 512]); t_e = Tok()
            gpt = 512 // (ni * cpg)

            def conv(src_nm, o, epilogue):
                Uin = U[src_nm]
                for grp in range(ng):
                    kh, t_kh = kh_r.next()
                    k.dma("sp", kh[:], KH[o, grp], reads=[g.T_KHAT], writes=[t_kh])
                    s1 = grp % 2
                    sx = 2 + grp % 2
                    (m1, m2), t_m = mm_r.next()
                    P1, t_p1 = P1_r.next()
                    Y, t_y = Y_r.next()
                    k.op("pe", lambda en: en.matmul(PSm[s1][:], lhsT=Uin[:, grp * 128:(grp + 1) * 128], rhs=F1b[:], start=True, stop=True), reads=[t_U[src_nm], t_c], writes=[TP[s1]])
                    _hy_twiddle(k, PSm[s1][:], TP[s1], TW, t_c, m1, m2, t_m, P1[:, 0, :], P1[:, 1, :], t_p1, 256)
                    k.op("pe", lambda en: en.matmul(PSm[sx][:, 0:256], lhsT=G3[:, 0, :], rhs=P1[:, 0, :], start=True, stop=False), reads=[t_p1, t_c], writes=[TP[sx]])
                    k.op("pe", lambda en: en.matmul(PSm[sx][:, 0:256], lhsT=G3[:, 2, :], rhs=P1[:, 1, :], start=False, stop=True), reads=[t_p1, t_c], writes=[TP[sx]])
                    k.op("pe", lambda en: en.matmul(PSm[sx][:, 256:512], lhsT=G3[:, 0, :], rhs=P1[:, 1, :], start=False, stop=False), reads=[t_p1, t_c], writes=[TP[sx]])
                    k.op("pe", lambda en: en.matmul(PSm[sx][:, 256:512], lhsT=G3[:, 1, :], rhs=P1[:, 0, :], start=False, stop=True), reads=[t_p1, t_c], writes=[TP[sx]])
                    (m1, m2), t_m = mm_r.next()
                    Xv = PSm[sx][:].rearrange("p (a n) -> p a n", a=2)
                    k.op("dve", lambda en: en.tensor_tensor(out=m1[:].rearrange("p (a n) -> p a n", a=2), in0=Xv, in1=kh[:, 0:256].unsqueeze(1).broadcast_to([128, 2, 256]), op=ALU.mult), reads=[TP[sx], t_kh], writes=[t_m])
                    k.op("dve", lambda en: en.tensor_tensor(out=m2[:].rearrange("p (a n) -> p a n", a=2), in0=Xv, in1=kh[:, 256:512].unsqueeze(1).broadcast_to([128, 2, 256]), op=ALU.mult), reads=[TP[sx], t_kh, t_m], writes=[t_m])
                    k.op("pool", lambda en: en.tensor_tensor(out=Y[:, 0, :], in0=m1[:, 0:256], in1=m2[:, 256:512], op=ALU.subtract), reads=[t_m], writes=[t_y])
                    k.op("pool", lambda en: en.tensor_tensor(out=Y[:, 1, :], in0=m2[:, 0:256], in1=m1[:, 256:512], op=ALU.add), reads=[t_m, t_y], writes=[t_y])
                    for bc in range(2):
                        pi_ = 4 + bc
                        (m1, m2), t_m = mm_r.next()
                        k.op("pe", lambda en: en.matmul(PSm[pi_][:, 0:256], lhsT=Y[:, 0, bc * 128:(bc + 1) * 128], rhs=GI[:, 0, :], start=True, stop=False), reads=[t_y, t_c], writes=[TP[pi_]])
                        k.op("pe", lambda en: en.matmul(PSm[pi_][:, 0:256], lhsT=Y[:, 1, bc * 128:(bc + 1) * 128], rhs=GI[:, 1, :], start=False, stop=True), reads=[t_y, t_c], writes=[TP[pi_]])
                        k.op("dve", lambda en: en.tensor_tensor(out=m1[:, 0:256], in0=PSm[pi_][:, 0:256], in1=TWI[:, bc, 0, :], op=ALU.mult), reads=[TP[pi_], t_c], writes=[t_m])
                        k.op("dve", lambda en: en.tensor_tensor(out=m2[:, 0:256], in0=PSm[pi_][:, 0:256], in1=TWI[:, bc, 1, :], op=ALU.mult), reads=[TP[pi_], t_c, t_m], writes=[t_m])
                        k.op("pool", lambda en: en.tensor_tensor(out=BW[bc][0][:, grp * 128:(grp + 1) * 128], in0=m1[:, 0:128], in1=m2[:, 128:256], op=ALU.subtract), reads=[t_m], writes=[t_bw])
                        k.op("pool", lambda en: en.tensor_tensor(out=BW[bc][1][:, grp * 128:(grp + 1) * 128], in0=m2[:, 0:128], in1=m1[:, 128:256], op=ALU.add), reads=[t_m, t_bw], writes=[t_bw])
                for ct in range(ncol // 512):
                    pi_ = 4 + ct % 2
                    cs = slice(ct * 512, (ct + 1) * 512)
                    idx = 0
                    for bc in range(2):
                        for ri in range(2):
                            k.op("pe", lambda en: en.matmul(PSm[pi_][:], lhsT=FI[:, bc, ri, :], rhs=BW[bc][ri][:, cs], start=(idx == 0), stop=(idx == 3)), reads=[t_bw, t_c], writes=[TP[pi_]])
                            idx += 1
                    epilogue(ct, cs, PSm[pi_], TP[pi_], o)

            def v4(ap):
                return ap.rearrange("p (g i c) -> p g i c", i=ni, c=cpg)

            def epi1(ct, cs, ps_, tps, o):
                bview = BR[:, o, ct * gpt * cpg:(ct + 1) * gpt * cpg].rearrange("p (g c) -> p g c", c=cpg).unsqueeze(2).broadcast_to([128, gpt, ni, cpg])
                k.op("pool", lambda en: en.tensor_tensor(out=v4(e1[:]), in0=v4(U["v"][:, cs]), in1=bview, op=ALU.mult), reads=[t_U["v"], t_c, t_e], writes=[t_e])
                k.op("dve", lambda en: en.tensor_tensor(out=e2[:], in0=ps_[:], in1=e1[:], op=ALU.add), reads=[tps, t_e], writes=[t_e])
                k.op("pool", lambda en: en.tensor_tensor(out=U["z1"][:, cs], in0=e2[:], in1=U["x1"][:, cs], op=ALU.mult), reads=[t_e, t_U["x1"]], writes=[t_U["z1"]])

            ZN = U["v"]

            def epi2(ct, cs, ps_, tps, o):
                bview = BR[:, o, ct * gpt * cpg:(ct + 1) * gpt * cpg].rearrange("p (g c) -> p g c", c=cpg).unsqueeze(2).broadcast_to([128, gpt, ni, cpg])
                k.op("pool", lambda en: en.tensor_tensor(out=v4(e1[:]), in0=v4(U["z1"][:, cs]), in1=bview, op=ALU.mult), reads=[t_U["z1"], t_c, t_e], writes=[t_e])
                k.op("dve", lambda en: en.tensor_tensor(out=e2[:], in0=ps_[:], in1=e1[:], op=ALU.add), reads=[tps, t_e], writes=[t_e])
                k.op("pool", lambda en: en.tensor_tensor(out=e3[:], in0=e2[:], in1=U["x2"][:, cs], op=ALU.mult), reads=[t_e, t_U["x2"]], writes=[t_e])
                outv = ZN[:].rearrange("p (i g c) -> p g i c", g=ng, c=cpg)[:, ct * gpt:(ct + 1) * gpt, :, :]
                k.op("dve", lambda en: en.tensor_tensor(out=outv, in0=v4(e3[:]), in1=v4(U["gt"][:, cs]), op=ALU.mult), reads=[t_e, t_U["gt"]], writes=[t_U["v"]])

            conv("v", 0, epi1)
            conv("z1", 1, epi2)
            ZT = U["x1"][:].rearrange("p (h t) -> p h t", h=2)
            cnt = 0
            for ch in range(2):
                nb4 = min(4, ni)
                for i0 in range(0, ni, nb4):
                    pt = cnt % 2
                    cnt += 1
                    for ii in range(nb4):
                        i = i0 + ii
                        k.op("pe", lambda en: en.transpose(PT[pt][:, ii, :], ZN[:, i * 256 + ch * 128:i * 256 + ch * 128 + 128], g.ident[:]), reads=[t_U["v"], g.T_ident], writes=[TPT[pt]])
                    outv = ZT[:, ch, :].rearrange("p (j i) -> p i j", i=ni)[:, i0:i0 + nb4, :]
                    k.op("act" if cnt % 2 else "dve",
                         (lambda en: en.activation(out=outv, in_=PT[pt][:, 0:nb4, :], func=AF.Copy)) if cnt % 2 else (lambda en: en.tensor_copy(out=outv, in_=PT[pt][:, 0:nb4, :])),
                         reads=[TPT[pt], t_U["x1"]], writes=[t_U["x1"]])
            k.dma("pool", g.CATT[:, 6:8, tok0:tok0 + n], ZT, reads=[t_U["x1"]], writes=[g.T_CATT])
            k.barrier()


TC = 16
NCH = T // TC
NDBL = 9


def ssm_host_layouts(inputs):
    f = lambda n: np.asarray(inputs[n], np.float32)
    lr, li, ls = f("ssm_lambda_re"), f("ssm_lambda_im"), f("ssm_log_step")
    br, bi, cr, ci = f("ssm_b_re"), f("ssm_b_im"), f("ssm_c_re"), f("ssm_c_im")
    lam = np.zeros((DEPTH, 128, 2, 16), np.float32)
    lsb = np.zeros((DEPTH, 128, 16), np.float32)
    Bp = np.zeros((DEPTH, 128, 2, 16, 64), np.float32)
    Cp = np.zeros((DEPTH, 128, 2, 16, 64), np.float32)
    for d in range(2):
        for q in range(8):
            for e in range(2):
                grp = 2 * q + e
                rows = slice(e * 64, (e + 1) * 64)
                dq = d * 8 + q
                lam[:, rows, 0, dq] = lr[:, d, grp, :]
                lam[:, rows, 1, dq] = li[:, d, grp, :]
                lsb[:, rows, dq] = ls[:, d, grp][:, None]
                pp = q % 2
                c0 = pp * 32 + e * 16
                Bp[:, rows, 0, dq, c0:c0 + 16] = br[:, d, grp, :, :]
                Bp[:, rows, 1, dq, c0:c0 + 16] = bi[:, d, grp, :, :]
                Cp[:, rows, 0, dq, c0:c0 + 16] = cr[:, d, grp, :, :].transpose(0, 2, 1)
                Cp[:, rows, 1, dq, c0:c0 + 16] = ci[:, d, grp, :, :].transpose(0, 2, 1)
    out = {"ssm_lam": lam, "ssm_ls": lsb, "ssm_Bp": Bp, "ssm_Cp": Cp}
    dd = f("ssm_d")
    gb = f("ssm_glu_b")
    out["ssm_cols"] = np.ascontiguousarray(np.stack([dd.reshape(DEPTH, 2, 128).transpose(0, 2, 1), gb.reshape(DEPTH, 2, 128).transpose(0, 2, 1)], -1))
    out["ssm_glu_w"] = f("ssm_glu_w")
    return out


def phase_ssm_weights(g, l):
    nc, k, W = g.nc, g.k, g.W
    PI = math.pi
    with ExitStack() as es:
        sb = lambda nm, s_, d=F32: es.enter_context(nc.sbuf_tensor(_nm(nm), s_, d))
        ps = lambda nm, s_, d=F32: es.enter_context(nc.psum_tensor(_nm(nm), s_, d))
        t = Tok()
        lam = sb("slam", [128, 2, 16]); ls = sb("sls", [128, 16])
        Bp = sb("sBp", [128, 2, 16, 64]); Cp = sb("sCp", [128, 2, 16, 64])
        k.dma("sp", lam[:], W["ssm_lam"][l], writes=[t])
        k.dma("sp", ls[:], W["ssm_ls"][l], writes=[t])
        k.dma("sp", Bp[:], W["ssm_Bp"][l], writes=[t])
        k.dma("sp", Cp[:], W["ssm_Cp"][l], writes=[t])
        sc = sb("ssc", [128, 24, 16])
        R = lambda i: sc[:, i, :]
        STEP, RHO, TH, MK, SIN, COS, AR, AI, DEN, NR, NI_, CRc, CIc, T1, T2 = range(15)

        def dv(fn, eng="dve"):
            k.op(eng, fn, reads=[t], writes=[t])

        dv(lambda en: en.activation(out=R(STEP), in_=ls[:], func=AF.Exp), "act")
        dv(lambda en: en.tensor_tensor(out=R(T1), in0=lam[:, 0, :], in1=R(STEP), op=ALU.mult))
        dv(lambda en: en.activation(out=R(RHO), in_=R(T1), func=AF.Exp), "act")
        dv(lambda en: en.tensor_tensor(out=R(TH), in0=lam[:, 1, :], in1=R(STEP), op=ALU.mult))
        for thr in (PI, 3 * PI, 5 * PI, 7 * PI):
            dv(lambda en: en.tensor_scalar(out=R(MK), in0=R(TH), scalar1=thr, scalar2=-2 * PI, op0=ALU.is_gt, op1=ALU.mult))
            if thr == PI:
                dv(lambda en: en.tensor_copy(out=R(T1), in_=R(MK)))
            else:
                dv(lambda en: en.tensor_tensor(out=R(T1), in0=R(T1), in1=R(MK), op=ALU.add))
        dv(lambda en: en.tensor_tensor(out=R(TH), in0=R(TH), in1=R(T1), op=ALU.add))
        dv(lambda en: en.activation(out=R(SIN), in_=R(TH), func=AF.Sin), "act")
        dv(lambda en: en.tensor_scalar(out=R(T2), in0=R(TH), scalar1=-1.0, scalar2=None, op0=ALU.mult))
        dv(lambda en: en.tensor_tensor(out=R(T1), in0=R(TH), in1=R(T2), op=ALU.max))
        dv(lambda en: en.tensor_scalar(out=R(T1), in0=R(T1), scalar1=-1.0, scalar2=PI / 2, op0=ALU.mult, op1=ALU.add))
        dv(lambda en: en.activation(out=R(COS), in_=R(T1), func=AF.Sin), "act")
        dv(lambda en: en.tensor_tensor(out=R(AR), in0=R(RHO), in1=R(COS), op=ALU.mult))
        dv(lambda en: en.tensor_tensor(out=R(AI), in0=R(RHO), in1=R(SIN), op=ALU.mult))
        dv(lambda en: en.tensor_tensor(out=R(DEN), in0=lam[:, 0, :], in1=lam[:, 0, :], op=ALU.mult))
        dv(lambda en: en.tensor_tensor(out=R(T1), in0=lam[:, 1, :], in1=lam[:, 1, :], op=ALU.mult))
        dv(lambda en: en.tensor_tensor(out=R(DEN), in0=R(DEN), in1=R(T1), op=ALU.add))
        dv(lambda en: en.reciprocal(out=R(DEN), in_=R(DEN)))
        dv(lambda en: en.tensor_scalar(out=R(T2), in0=R(AR), scalar1=-1.0, scalar2=None, op0=ALU.add))
        dv(lambda en: en.tensor_tensor(out=R(NR), in0=R(T2), in1=lam[:, 0, :], op=ALU.mult))
        dv(lambda en: en.tensor_tensor(out=R(T1), in0=R(AI), in1=lam[:, 1, :], op=ALU.mult))
        dv(lambda en: en.tensor_tensor(out=R(NR), in0=R(NR), in1=R(T1), op=ALU.add))
        dv(lambda en: en.tensor_tensor(out=R(NI_), in0=R(AI), in1=lam[:, 0, :], op=ALU.mult))
        dv(lambda en: en.tensor_tensor(out=R(T1), in0=R(T2), in1=lam[:, 1, :], op=ALU.mult))
        dv(lambda en: en.tensor_tensor(out=R(NI_), in0=R(NI_), in1=R(T1), op=ALU.subtract))
        dv(lambda en: en.tensor_tensor(out=R(CRc), in0=R(NR), in1=R(DEN), op=ALU.mult))
        dv(lambda en: en.tensor_tensor(out=R(CIc), in0=R(NI_), in1=R(DEN), op=ALU.mult))
        PW = sb("sPW", [128, 2, 17, 16])
        dv(lambda en: en.memset(PW[:, 0, 0, :], 1.0))
        dv(lambda en: en.memset(PW[:, 1, 0, :], 0.0))
        for n_ in range(16):
            dv(lambda en: en.tensor_tensor(out=R(T1), in0=PW[:, 0, n_, :], in1=R(AR), op=ALU.mult))
            dv(lambda en: en.tensor_tensor(out=R(T2), in0=PW[:, 1, n_, :], in1=R(AI), op=ALU.mult))
            dv(lambda en: en.tensor_tensor(out=PW[:, 0, n_ + 1, :], in0=R(T1), in1=R(T2), op=ALU.subtract))
            dv(lambda en: en.tensor_tensor(out=R(T1), in0=PW[:, 0, n_, :], in1=R(AI), op=ALU.mult))
            dv(lambda en: en.tensor_tensor(out=R(T2), in0=PW[:, 1, n_, :], in1=R(AR), op=ALU.mult))
            dv(lambda en: en.tensor_tensor(out=PW[:, 1, n_ + 1, :], in0=R(T1), in1=R(T2), op=ALU.add))
        A2 = g.lw.A2
        dv(lambda en: en.tensor_copy(out=A2[:, 0, 0, :], in_=PW[:, 0, 16, :]))
        dv(lambda en: en.tensor_copy(out=A2[:, 0, 1, :], in_=PW[:, 1, 16, :]))
        for kk in range(NDBL):
            if kk > 0:
                dv(lambda en: en.tensor_tensor(out=R(T1), in0=A2[:, kk - 1, 0, :], in1=A2[:, kk - 1, 0, :], op=ALU.mult))
                dv(lambda en: en.tensor_tensor(out=R(T2), in0=A2[:, kk - 1, 1, :], in1=A2[:, kk - 1, 1, :], op=ALU.mult))
                dv(lambda en: en.tensor_tensor(out=A2[:, kk, 0, :], in0=R(T1), in1=R(T2), op=ALU.subtract))
                dv(lambda en: en.tensor_tensor(out=R(T1), in0=A2[:, kk - 1, 0, :], in1=A2[:, kk - 1, 1, :], op=ALU.mult))
                dv(lambda en: en.tensor_scalar(out=A2[:, kk, 1, :], in0=R(T1), scalar1=2.0, scalar2=None, op0=ALU.mult))
            dv(lambda en: en.tensor_scalar(out=A2[:, kk, 2, :], in0=A2[:, kk, 1, :], scalar1=-1.0, scalar2=None, op0=ALU.mult))
        bc3 = lambda ap: ap.unsqueeze(2).broadcast_to([128, 16, 64])
        Bb = sb("sBb", [128, 2, 16, 64])
        big = [sb("sbig%d" % i, [128, 16, 64]) for i in range(4)]
        dv(lambda en: en.tensor_tensor(out=big[0][:], in0=Bp[:, 0], in1=bc3(R(CRc)), op=ALU.mult))
        dv(lambda en: en.tensor_tensor(out=big[1][:], in0=Bp[:, 1], in1=bc3(R(CIc)), op=ALU.mult))
        dv(lambda en: en.tensor_tensor(out=Bb[:, 0], in0=big[0][:], in1=big[1][:], op=ALU.subtract))
        dv(lambda en: en.tensor_tensor(out=big[0][:], in0=Bp[:, 1], in1=bc3(R(CRc)), op=ALU.mult))
        dv(lambda en: en.tensor_tensor(out=big[1][:], in0=Bp[:, 0], in1=bc3(R(CIc)), op=ALU.mult))
        dv(lambda en: en.tensor_tensor(out=Bb[:, 1], in0=big[0][:], in1=big[1][:], op=ALU.add))
        Bbb = sb("sBbb", [128, 2, 16, 64], BF16)
        dv(lambda en: en.tensor_copy(out=Bbb[:], in_=Bb[:]))
        CR = sb("sCR", [128, 2, 16, 17, 64], BF16)
        engs = ("dve", "pool")
        for n_ in range(17):
            e1 = engs[n_ % 2]
            dv(lambda en: en.tensor_tensor(out=big[0][:], in0=Cp[:, 0], in1=bc3(PW[:, 0, n_, :]), op=ALU.mult), e1)
            dv(lambda en: en.tensor_tensor(out=big[1][:], in0=Cp[:, 1], in1=bc3(PW[:, 1, n_, :]), op=ALU.mult), e1)
            dv(lambda en: en.tensor_tensor(out=CR[:, 0, :, n_, :], in0=big[0][:], in1=big[1][:], op=ALU.subtract), e1)
            dv(lambda en: en.tensor_tensor(out=big[2][:], in0=Cp[:, 0], in1=bc3(PW[:, 1, n_, :]), op=ALU.mult), e1)
            dv(lambda en: en.tensor_tensor(out=big[3][:], in0=Cp[:, 1], in1=bc3(PW[:, 0, n_, :]), op=ALU.mult), e1)
            dv(lambda en: en.tensor_tensor(out=big[2][:], in0=big[2][:], in1=big[3][:], op=ALU.add), e1)
            dv(lambda en: en.tensor_scalar(out=CR[:, 1, :, n_, :], in0=big[2][:], scalar1=-1.0, scalar2=None, op0=ALU.mult), e1)
        k.dma("pool", g.SSM_L3, CR[:], reads=[t], writes=[g.T_SSMW])
        L1W = sb("sL1W", [128, 2, 16, 2, 128], BF16)
        dv(lambda en: en.memset(L1W[:], 0.0), "pool")
        PS_ = [ps("sps%d" % i, [128, 512]) for i in range(2)]
        TPS_ = [Tok(True) for _ in range(2)]
        for d in range(2):
            for Q in range(4):
                hc, Ql = Q // 2, Q % 2
                for half in range(2):
                    first = True
                    for pp in range(2):
                        dq = d * 8 + 2 * Q + pp
                        for ri in range(2):
                            rhs = CR[:, ri, dq, half * 8:(half + 1) * 8, :].rearrange("p n c -> p (n c)")
                            k.op("pe", lambda en: en.matmul(PS_[half][Ql * 64:(Ql + 1) * 64, :], lhsT=Bbb[:, ri, dq, :], rhs=rhs, start=first, stop=(pp == 1 and ri == 1)), reads=[t], writes=[TPS_[half]])
                            first = False
                    k.op("act" if half else "dve",
                         (lambda en: en.activation(out=L1W[Ql * 64:(Ql + 1) * 64, d, half * 8:(half + 1) * 8, hc, Ql * 64:(Ql + 1) * 64], in_=PS_[half][Ql * 64:(Ql + 1) * 64, :].rearrange("p (n c) -> p n c", c=64), func=AF.Copy)) if half else
                         (lambda en: en.tensor_copy(out=L1W[Ql * 64:(Ql + 1) * 64, d, half * 8:(half + 1) * 8, hc, Ql * 64:(Ql + 1) * 64], in_=PS_[half][Ql * 64:(Ql + 1) * 64, :].rearrange("p (n c) -> p n c", c=64))),
                         reads=[TPS_[half], t], writes=[t])
        k.dma("pool", g.SSM_L1, L1W[:], reads=[t], writes=[g.T_SSMW])
        ZZ = sb("sZZ", [128, 2, 16, 64], BF16)
        PT = [ps("spt%d" % i, [128, 8, 128], BF16) for i in range(2)]
        TPT = [Tok(True) for _ in range(2)]
        sg_r = Rot([sb("ssg%d" % i, [128, 2, 2, 2, 128], BF16) for i in range(2)])
        for n_ in range(16):
            dv(lambda en: en.tensor_tensor(out=big[0][:], in0=Bb[:, 0], in1=bc3(PW[:, 0, n_, :]), op=ALU.mult))
            dv(lambda en: en.tensor_tensor(out=big[1][:], in0=Bb[:, 1], in1=bc3(PW[:, 1, n_, :]), op=ALU.mult))
            dv(lambda en: en.tensor_tensor(out=ZZ[:, 0], in0=big[0][:], in1=big[1][:], op=ALU.subtract))
            dv(lambda en: en.tensor_tensor(out=big[2][:], in0=Bb[:, 0], in1=bc3(PW[:, 1, n_, :]), op=ALU.mult), "pool")
            dv(lambda en: en.tensor_tensor(out=big[3][:], in0=Bb[:, 1], in1=bc3(PW[:, 0, n_, :]), op=ALU.mult), "pool")
            dv(lambda en: en.tensor_tensor(out=ZZ[:, 1], in0=big[2][:], in1=big[3][:], op=ALU.add), "pool")
            for d in range(2):
                sg, t_sg = sg_r.next()
                for hc in range(2):
                    pt = hc
                    for ri in range(2):
                        for Ql in range(2):
                            for pp in range(2):
                                dq = d * 8 + 2 * (hc * 2 + Ql) + pp
                                k.op("pe", lambda en: en.transpose(PT[pt][Ql * 64:(Ql + 1) * 64, ri * 2 + pp, :], ZZ[:, ri, dq, :], g.ident[:]), reads=[t, g.T_ident], writes=[TPT[pt]])
                    k.op("act" if hc else "dve",
                         (lambda en: en.activation(out=sg[:, hc].rearrange("p r q n -> p (r q) n"), in_=PT[pt][:, 0:4, :], func=AF.Copy)) if hc else
                         (lambda en: en.tensor_copy(out=sg[:, hc].rearrange("p r q n -> p (r q) n"), in_=PT[pt][:, 0:4, :])),
                         reads=[TPT[pt]], writes=[t_sg])
                k.dma("pool", g.SSM_SG[:, d, n_], sg[:], reads=[t_sg], writes=[g.T_SSMW])
        k.barrier()


def phase_ssm(g, l, s, need_ctx):
    nc, k, W, lw = g.nc, g.k, g.W, g.lw
    A2 = lw.A2
    with ExitStack() as es:
        sb = lambda nm, s_, d=F32: es.enter_context(nc.sbuf_tensor(_nm(nm), s_, d))
        ps = lambda nm, s_, d=F32: es.enter_context(nc.psum_tensor(_nm(nm), s_, d))
        PSm = [ps("mps%d" % i, [128, 512]) for i in range(8)]
        TP = [Tok(True) for _ in range(8)]
        ujm = sb("mujm", [128, 2, TC, NCH], BF16)
        gjm = sb("mgjm", [128, 2, TC, NCH], BF16)
        Sb = [[sb("mSb%d%d" % (d, ri), [128, 8, NCH], BF16) for ri in range(2)] for d in range(2)]
        t_u = Tok(); t_g = Tok(); t_sb = Tok()
        cols = sb("mcols", [128, 2, 2])
        GW = sb("mGW", [128, 2, 256], BF16)
        t_c = Tok()
        k.dma("sp", cols[:], W["ssm_cols"][l], writes=[t_c])
        with ExitStack() as es1:
            sb1 = lambda nm, s_, d=F32: es1.enter_context(nc.sbuf_tensor(_nm(nm), s_, d))
            wss = sb1("mwss", [128, 8, 512], BF16)
            gwf = sb1("mgwf", [128, 2, 256])
            t_w = Tok()
            k.dma("sp", wss[:], g.WINB[:, :, O_SU:O_SU + 512], reads=[g.T_WINB], writes=[t_w])
            k.dma("sp", gwf[:], W["ssm_glu_w"][l].rearrange("(kc p) n -> p kc n", p=128), writes=[t_w])
            k.op("dve", lambda en: en.tensor_copy(out=GW[:], in_=gwf[:]), reads=[t_w], writes=[t_c])
            hT_r = Rot([sb1("mhT%d" % i, [128, 8, 512], BF16) for i in range(2)])
            blocks = [(0, 256)] + [(256 + 512 * b, 512) for b in range(8)]
            cnt = 0
            for (t0, nb) in blocks:
                hT, t_h = hT_r.next()
                k.dma("sp", hT[:, :, 0:nb], g.HT[:, :, t0:t0 + nb], reads=[g.T_HT], writes=[t_h])
                c0, ncb = t0 // TC, nb // TC
                for cc in range(4):
                    pi_ = cnt % 4
                    cnt += 1
                    for kc in range(8):
                        k.op("pe", lambda en: en.matmul(PSm[pi_][:, 0:nb], lhsT=wss[:, kc, cc * 128:(cc + 1) * 128], rhs=hT[:, kc, 0:nb], start=(kc == 0), stop=(kc == 7)), reads=[t_w, t_h], writes=[TP[pi_]])
                    src = PSm[pi_][:, 0:nb].rearrange("p (c j) -> p j c", j=TC)
                    if cc < 2:
                        k.op("dve", lambda en: en.tensor_copy(out=ujm[:, cc, :, c0:c0 + ncb], in_=src), reads=[TP[pi_]], writes=[t_u])
                    else:
                        k.op("act", lambda en: en.activation(out=gjm[:, cc - 2, :, c0:c0 + ncb], in_=src, func=AF.Silu), reads=[TP[pi_]], writes=[t_g])
            k.barrier()
        with ExitStack() as es2:
            sb2 = lambda nm, s_, d=F32: es2.enter_context(nc.sbuf_tensor(_nm(nm), s_, d))
            SG = sb2("mSG", [128, 2, 16, 2, 2, 2, 128], BF16)
            t_sg = Tok()
            k.dma("sp", SG[:, 0], g.SSM_SG[:, 0], reads=[g.T_SSMW], writes=[t_sg])
            k.dma("sp", SG[:, 1], g.SSM_SG[:, 1], reads=[g.T_SSMW], writes=[t_sg])
            S = [[sb2("mS%d%d" % (d, ri), [128, 8, NCH]) for ri in range(2)] for d in range(2)]
            t_S = [[Tok() for q in range(8)] for d in range(2)]
            Tt = [[sb2("mT%d%d" % (i, ri), [128, NCH]) for ri in range(2)] for i in range(4)]
            t_T = [Tok() for _ in range(4)]
            cnt = 0
            for d in range(2):
                for q in range(8):
                    Q, pp = q // 2, q % 2
                    hc, Ql = Q // 2, Q % 2
                    rows = slice(Ql * 64, (Ql + 1) * 64)
                    for ri in range(2):
                        pi_ = cnt % 4
                        cnt += 1
                        for i in range(TC):
                            n_ = (TC - 1 - i) if d == 0 else i
                            k.op("pe", lambda en: en.matmul(PSm[pi_][:, 0:NCH], lhsT=SG[rows, d, n_, hc, ri, pp, :], rhs=ujm[rows, hc, i, :], start=(i == 0), stop=(i == TC - 1)), reads=[t_sg, t_u], writes=[TP[pi_]])
                        if d == 0:
                            k.op("act" if ri else "dve", (lambda en: en.activation(out=S[d][ri][:, q, :], in_=PSm[pi_][:, 0:NCH], func=AF.Copy)) if ri else (lambda en: en.tensor_copy(out=S[d][ri][:, q, :], in_=PSm[pi_][:, 0:NCH])), reads=[TP[pi_]], writes=[t_S[d][q]])
                        else:
                            k.op("dve", lambda en: en.tensor_copy(out=S[d][ri][:, q, 0:256], in_=PSm[pi_][:, 16:NCH]), reads=[TP[pi_]], writes=[t_S[d][q]])
                            k.op("act", lambda en: en.activation(out=S[d][ri][:, q, 256:NCH], in_=PSm[pi_][:, 0:16], func=AF.Copy), reads=[TP[pi_]], writes=[t_S[d][q]])
            for kk in range(NDBL):
                sh = 1 << kk
                w_ = NCH - sh
                it = 0
                for d in range(2):
                    dst = slice(sh, NCH) if d == 0 else slice(0, w_)
                    srcs = slice(0, w_) if d == 0 else slice(sh, NCH)
                    for q in range(8):
                        dq = d * 8 + q
                        Ar, Ai, nAi = A2[:, kk, 0, dq:dq + 1], A2[:, kk, 1, dq:dq + 1], A2[:, kk, 2, dq:dq + 1]
                        Tr, Ti = Tt[it % 4]
                        tT = t_T[it % 4]
                        it += 1
                        Sr, Si = S[d][0], S[d][1]
                        ts = t_S[d][q]
                        k.op("act", lambda en: en.activation(out=Tr[:, 0:w_], in_=Sr[:, q, srcs], func=AF.Copy, scale=Ar), reads=[ts, lw.tok], writes=[tT])
                        k.op("dve", lambda en: en.scalar_tensor_tensor(out=Tr[:, 0:w_], in0=Si[:, q, srcs], scalar=nAi, in1=Tr[:, 0:w_], op0=ALU.mult, op1=ALU.add), reads=[ts, tT, lw.tok], writes=[tT])
                        k.op("act", lambda en: en.activation(out=Ti[:, 0:w_], in_=Si[:, q, srcs], func=AF.Copy, scale=Ar), reads=[ts, lw.tok, tT], writes=[tT])
                        k.op("dve", lambda en: en.scalar_tensor_tensor(out=Ti[:, 0:w_], in0=Sr[:, q, srcs], scalar=Ai, in1=Ti[:, 0:w_], op0=ALU.mult, op1=ALU.add), reads=[ts, tT, lw.tok], writes=[tT])
                        k.op("pool", lambda en: en.tensor_tensor(out=Sr[:, q, dst], in0=Sr[:, q, dst], in1=Tr[:, 0:w_], op=ALU.add), reads=[tT, ts], writes=[ts])
                        k.op("dve", lambda en: en.tensor_tensor(out=Si[:, q, dst], in0=Si[:, q, dst], in1=Ti[:, 0:w_], op=ALU.add), reads=[tT, ts], writes=[ts])
            for d in range(2):
                for ri in range(2):
                    k.op("act" if ri else "dve", (lambda en: en.activation(out=Sb[d][ri][:], in_=S[d][ri][:], func=AF.Copy)) if ri else (lambda en: en.tensor_copy(out=Sb[d][ri][:], in_=S[d][ri][:])), reads=[t_S[d][q] for q in range(8)], writes=[t_sb])
            k.barrier()
        with ExitStack() as es3:
            sb3 = lambda nm, s_, d=F32: es3.enter_context(nc.sbuf_tensor(_nm(nm), s_, d))
            L3 = sb3("mL3", [128, 2, 16, 17, 64], BF16)
            L1W = sb3("mL1W", [128, 2, 16, 2, 128], BF16)
            t_l = Tok()
            k.dma("sp", L3[:, 0], g.SSM_L3[:, 0], reads=[g.T_SSMW], writes=[t_l])
            k.dma("sp", L3[:, 1], g.SSM_L3[:, 1], reads=[g.T_SSMW], writes=[t_l])
            k.dma("sp", L1W[:], g.SSM_L1, reads=[g.T_SSMW], writes=[t_l])
            Yjm = sb3("mYjm", [128, 2, TC, NCH])
            t_y = Tok()
            cnt = 0
            for j in range(TC):
                for hc in range(2):
                    pi_ = cnt % 4
                    cnt += 1
                    yp = PSm[pi_]
                    first = True
                    for tau in range(j + 1):
                        k.op("pe", lambda en: en.matmul(yp[:, 0:NCH], lhsT=L1W[:, 0, tau, hc, :], rhs=ujm[:, hc, j - tau, :], start=first, stop=False), reads=[t_l, t_u], writes=[TP[pi_]])
                        first = False
                    for tau in range(TC - j):
                        k.op("pe", lambda en: en.matmul(yp[:, 0:NCH], lhsT=L1W[:, 1, tau, hc, :], rhs=ujm[:, hc, j + tau, :], start=False, stop=False), reads=[t_l, t_u], writes=[TP[pi_]])
                    for Ql in range(2):
                        rows = slice(Ql * 64, (Ql + 1) * 64)
                        for pp in range(2):
                            q = 2 * (2 * hc + Ql) + pp
                            for ri in range(2):
                                k.op("pe", lambda en: en.matmul(yp[rows, 1:NCH], lhsT=L3[:, ri, q, j + 1, :], rhs=Sb[0][ri][:, q, 0:NCH - 1], start=False, stop=False), reads=[t_l, t_sb], writes=[TP[pi_]])
                                k.op("pe", lambda en: en.matmul(yp[rows, 16:NCH], lhsT=L3[:, ri, 8 + q, TC - j, :], rhs=Sb[1][ri][:, q, 1:257], start=False, stop=False), reads=[t_l, t_sb], writes=[TP[pi_]])
                                lastm = (Ql == 1 and pp == 1 and ri == 1)
                                k.op("pe", lambda en: en.matmul(yp[rows, 0:15], lhsT=L3[:, ri, 8 + q, TC - j, :], rhs=Sb[1][ri][:, q, 257:NCH], start=False, stop=lastm), reads=[t_l, t_sb], writes=[TP[pi_]])
                    k.op("dve", lambda en: en.scalar_tensor_tensor(out=Yjm[:, hc, j, :], in0=ujm[:, hc, j, :], scalar=cols[:, hc, 0:1], in1=yp[:, 0:NCH], op0=ALU.mult, op1=ALU.add), reads=[TP[pi_], t_u, t_c], writes=[t_y])
            k.barrier()
            FL = TC * NCH
            Yf = [Yjm[:, hc].rearrange("p j c -> p (j c)") for hc in range(2)]
            Gf = [gjm[:, hc].rearrange("p j c -> p (j c)") for hc in range(2)]
            Zb = ujm
            Zf = [Zb[:, hc].rearrange("p j c -> p (j c)") for hc in range(2)]
            w1 = sb3("mw1", [128, 512]); w2 = sb3("mw2", [128, 512]); w3 = sb3("mw3", [128, 2, 512])
            t_w1 = Tok(); t_z = Tok(); t_w3 = Tok()
            CG = 1.5957691216057308
            pieces = [(c0, min(512, FL - c0)) for c0 in range(0, FL, 512)]
            for (c0, w_) in pieces:
                cs = slice(c0, c0 + w_)
                for hc in range(2):
                    k.op("pool", lambda en: en.tensor_tensor(out=w1[:, 0:w_], in0=Yf[hc][:, cs], in1=Yf[hc][:, cs], op=ALU.mult), reads=[t_y, t_w1], writes=[t_w1])
                    k.op("dve", lambda en: en.tensor_scalar(out=w1[:, 0:w_], in0=w1[:, 0:w_], scalar1=0.044715, scalar2=1.0, op0=ALU.mult, op1=ALU.add), reads=[t_w1], writes=[t_w1])
                    k.op("pool", lambda en: en.tensor_tensor(out=w1[:, 0:w_], in0=w1[:, 0:w_], in1=Yf[hc][:, cs], op=ALU.mult), reads=[t_w1, t_y], writes=[t_w1])
                    k.op("act", lambda en: en.activation(out=w2[:, 0:w_], in_=w1[:, 0:w_], func=AF.Sigmoid, scale=CG), reads=[t_w1], writes=[t_w1])
                    k.op("dve", lambda en: en.tensor_tensor(out=w3[:, hc, 0:w_], in0=w2[:, 0:w_], in1=Yf[hc][:, cs], op=ALU.mult), reads=[t_w1, t_y, t_w3], writes=[t_w3])
                    k.op("pool", lambda en: en.tensor_copy(out=Zf[hc][:, cs], in_=w3[:, hc, 0:w_]), reads=[t_w3, t_u], writes=[t_z])
                for oc in range(2):
                    pi_ = 4 + oc
                    for kc in range(2):
                        k.op("pe", lambda en: en.matmul(PSm[pi_][:, 0:w_], lhsT=GW[:, kc, oc * 128:(oc + 1) * 128], rhs=Zf[kc][:, cs], start=(kc == 0), stop=(kc == 1)), reads=[t_z, t_c], writes=[TP[pi_]])
                    k.op("act", lambda en: en.activation(out=w2[:, 0:w_], in_=PSm[pi_][:, 0:w_], func=AF.Sigmoid, bias=cols[:, oc, 1:2]), reads=[TP[pi_], t_c, t_w1], writes=[t_w1])
                    k.op("dve", lambda en: en.tensor_tensor(out=w2[:, 0:w_], in0=w2[:, 0:w_], in1=w3[:, oc, 0:w_], op=ALU.mult), reads=[t_w1, t_w3], writes=[t_w1])
                    k.op("pool", lambda en: en.tensor_tensor(out=Gf[oc][:, cs], in0=w2[:, 0:w_], in1=Gf[oc][:, cs], op=ALU.mult), reads=[t_w1, t_g], writes=[t_g])
            on_t = sb3("mON", [128, 2, T], BF16)
            t_on = Tok()
            for hc in range(2):
                k.op("dve" if hc else "pool", lambda en: en.tensor_copy(out=on_t[:, hc, :].rearrange("p (c j) -> p j c", j=TC), in_=gjm[:, hc]), reads=[t_g], writes=[t_on])
            if need_ctx:
                k.dma("pool", g.CATT[:, 4:6, :], on_t[:], reads=[t_on], writes=[g.T_CATT])
            else:
                k.dma("pool", g.CATT[:, 4:6, C:T], on_t[:, :, C:T], reads=[t_on], writes=[g.T_CATT])
            k.barrier()
```

```python
import math
import numpy as np
from contextlib import ExitStack
import concourse.bass as bass
import concourse.mybir as mybir
from concourse.bass_utils import run_bass_kernel_spmd

F32 = mybir.dt.float32
BF16 = mybir.dt.bfloat16
AF = mybir.ActivationFunctionType
ALU = mybir.AluOpType
AX = mybir.AxisListType

D = 1024
L = 4096
C = 256
T = L + C
NT = T // 128
DEPTH = 4
EPS = 1e-6
NCORES = 8

O_CQ, O_CKV, O_KR, O_GM = 0, 192, 320, 352
O1 = 608
O_GQ, O_GK, O_GV, O_GG = O1, O1 + 256, O1 + 384, O1 + 512
O2 = O1 + 768
O_SU, O_SG = O2, O2 + 256
O3 = O2 + 512
O_HY, O_HG = O3, O3 + 768
NIN = 2912
O_KRP = NIN
O_QM = O_KRP + 32
O_QP = O_QM + 256
O_KP = O_QP + 256
NCB = O_KP + 128

EPOCH = 30000
NDMASLOT = 8


class Tok:
    __slots__ = ("w", "r", "excl")

    def __init__(self, excl=False):
        self.w = []
        self.r = []
        self.excl = excl


class KB:
    def __init__(self, nc, es):
        self.nc = nc
        self.es = es
        self.eng = {"pe": nc.tensor, "act": nc.scalar, "dve": nc.vector, "pool": nc.gpsimd, "sp": nc.sync}
        self.cnt = {e: 0 for e in self.eng}
        self.epoch = {e: 0 for e in self.eng}
        self.sems = {}
        self.seen = {e: {} for e in self.eng}
        self.dma_slots = {}
        self.dma_rr = {e: 0 for e in self.eng}
        self.ninst = 0

    def _sem(self, key):
        if key not in self.sems:
            self.sems[key] = self.es.enter_context(self.nc.semaphore("s_%s_%s" % key))
        return self.sems[key]

    def _wait(self, e, ev):
        key, val = ev
        if self.seen[e].get(key, 0) >= val:
            return
        self.eng[e].wait_ge(self._sem(key), val)
        self.seen[e][key] = val

    def _deps(self, e, reads, writes):
        best = {}

        def add(k_, v):
            if best.get(k_, 0) < v:
                best[k_] = v
        for t in reads:
            for k_, v in t.w:
                add(k_, v)
            if t.excl:
                for k_, v in t.r:
                    if k_[0] != e:
                        add(k_, v)
        for t in writes:
            for k_, v in t.w:
                if k_[0] != e:
                    add(k_, v)
            for k_, v in t.r:
                if k_[0] != e:
                    add(k_, v)
        for k_, v in best.items():
            if e == "pe" and k_[0] == "pe":
                continue
            self._wait(e, (k_, v))

    def _record(self, ev, reads, writes):
        for t in reads:
            t.r.append(ev)
            if len(t.r) > 16:
                best = {}
                for k_, v in t.r:
                    if best.get(k_, 0) < v:
                        best[k_] = v
                t.r = list(best.items())
        for t in writes:
            t.w = [ev]
            t.r = []

    def op(self, e, fn, reads=(), writes=()):
        self._deps(e, reads, writes)
        if self.cnt[e] >= EPOCH:
            self.epoch[e] += 1
            self.cnt[e] = 0
        key = (e, self.epoch[e])
        ins = fn(self.eng[e])
        self.cnt[e] += 1
        ins.then_inc(self._sem(key), 1)
        ev = (key, self.cnt[e])
        self._record(ev, reads, writes)
        self.ninst += 1
        return ev

    def dma(self, e, out, in_, reads=(), writes=(), **kw):
        self._deps(e, reads, writes)
        if e not in self.dma_slots:
            self.dma_slots[e] = [[("d" + e, i), 0] for i in range(NDMASLOT)]
        i = self.dma_rr[e]
        self.dma_rr[e] = (i + 1) % NDMASLOT
        slot = self.dma_slots[e][i]
        key = slot[0]
        if slot[1] > 0:
            self._wait(e, (key, 16 * slot[1]))
        slot[1] += 1
        ins = self.eng[e].dma_start(out=out, in_=in_, **kw)
        ins.then_inc(self._sem(key), 16)
        ev = (key, 16 * slot[1])
        self._record(ev, reads, writes)
        self.ninst += 1
        return ev

    def all_events(self):
        evs = []
        for e in self.eng:
            for ep in range(self.epoch[e] + 1):
                v = self.cnt[e] if ep == self.epoch[e] else EPOCH
                if v > 0:
                    evs.append(((e, ep), v))
        for e, slots in self.dma_slots.items():
            for key, uses in slots:
                if uses:
                    evs.append((key, 16 * uses))
        return evs

    def barrier(self):
        evs = self.all_events()
        for e in ("pe", "act", "dve", "pool", "sp"):
            for ev in evs:
                if ev[0][0] == e:
                    continue
                self._wait(e, ev)

    def drain(self, e="sp"):
        for ev in self.all_events():
            self._wait(e, ev)


_NMC = [0]


def _nm(n):
    _NMC[0] += 1
    return "%s_%d" % (n, _NMC[0])


class Rot:
    def __init__(self, tiles, excl=False):
        self.tiles = [(t, Tok(excl)) for t in tiles]
        self.i = 0

    def next(self):
        r = self.tiles[self.i]
        self.i = (self.i + 1) % len(self.tiles)
        return r


def _rope_table(d):
    hh = d // 2
    qq = hh // 2
    inv = (np.float32(10000.0) ** (-np.arange(0, hh, 2, dtype=np.float32) / np.float32(hh))).astype(np.float32)
    t = np.arange(L)
    row = (t // 64).astype(np.float32)
    col = (t % 64).astype(np.float32)
    cos = np.ones((d, T), np.float32)
    sin = np.zeros((d, T), np.float32)
    for i in range(d):
        hf, within = divmod(i, hh)
        fi = within % qq
        pos = row if hf == 0 else col
        ang = (pos * inv[fi]).astype(np.float32)
        cos[i, C:] = np.cos(ang).astype(np.float32)
        sin[i, C:] = np.sin(ang).astype(np.float32)
    return cos, sin


def _partner_index(d):
    hh = d // 2
    qq = hh // 2
    idx = np.zeros(d, np.int64)
    sg = np.zeros(d, np.float32)
    for i in range(d):
        hf, within = divmod(i, hh)
        if within < qq:
            idx[i] = i + qq
            sg[i] = -1.0
        else:
            idx[i] = i - qq
            sg[i] = 1.0
    return idx, sg


def host_constants():
    cst = {}
    cst["k_ident"] = np.eye(128, dtype=np.float32)
    c32, s32 = _rope_table(32)
    c64, s64 = _rope_table(64)
    mc = np.ones((128, T), np.float32)
    ms = np.zeros((128, T), np.float32)
    mc[64:96] = c32
    ms[64:96] = s32
    cst["k_ropeM"] = np.stack([mc, ms], 0)
    cst["k_ropeG"] = np.stack([np.concatenate([c64, c64], 0), np.concatenate([s64, s64], 0)], 0)
    cst.update(hyena_constants())
    return cst


class G:
    pass


def build_program(layers, final_lat_only, dbg=None):
    nc = bass.Bass("TRN2", target_bir_lowering=False)
    g = G()
    g.nc = nc
    g.dbg = dbg or {}

    def din(name, shape, dt=F32):
        return nc.dram_tensor(name, list(shape), dt, kind="ExternalInput").ap()

    def dscr(name, shape, dt=F32):
        return nc.dram_tensor(name, list(shape), dt).ap()

    g.xs = din("xs", [2, T, D])
    g.cT = din("cT", [128, 8, 3])
    W = {}
    W["w_mod"] = din("w_mod", [DEPTH, D, 3 * D])
    W["b_mod"] = din("b_mod", [DEPTH, 3 * D])
    W["g_pre"] = din("g_pre", [DEPTH, D])
    W["g_post"] = din("g_post", [DEPTH, D])
    W["w_in"] = din("w_in", [DEPTH, D, NIN])
    W["w_out"] = din("w_out", [DEPTH, D, D])
    W["mla_g_cq"] = din("mla_g_cq", [DEPTH, 192])
    W["mla_w_uq"] = din("mla_w_uq", [DEPTH, 192, 384])
    W["mla_g_ckv"] = din("mla_g_ckv", [DEPTH, 128])
    W["mla_w_ukv"] = din("mla_w_ukv", [DEPTH, 128, 512])
    W["gq_cols"] = din("gq_cols", [DEPTH, 128, 4])
    for nm, shp in (("hy_conv_w", [DEPTH, 3, 768]), ("hy_conv_b", [DEPTH, 768]), ("hy_f_w1", [DEPTH, 33, 64]), ("hy_f_b1", [DEPTH, 64]),
                    ("hy_f_freq1", [DEPTH, 64]), ("hy_f_w2", [DEPTH, 64, 64]), ("hy_f_b2", [DEPTH, 64]), ("hy_f_freq2", [DEPTH, 64]),
                    ("hy_f_w3", [DEPTH, 64, 1024]), ("hy_bias", [DEPTH, 2, 256])):
        W[nm] = din(nm, shp)
    for nm, shp in (("ssm_lam", [DEPTH, 128, 2, 16]), ("ssm_ls", [DEPTH, 128, 16]), ("ssm_Bp", [DEPTH, 128, 2, 16, 64]), ("ssm_Cp", [DEPTH, 128, 2, 16, 64]),
                    ("ssm_cols", [DEPTH, 128, 2, 2]), ("ssm_glu_w", [DEPTH, 256, 256])):
        W[nm] = din(nm, shp)
    g.W = W
    K = {}
    K["k_ident"] = din("k_ident", [128, 128])
    K["k_ropeM"] = din("k_ropeM", [2, 128, T])
    K["k_ropeG"] = din("k_ropeG", [2, 128, T])
    for nm, arr in hyena_constants().items():
        K[nm] = din(nm, list(arr.shape))
    g.K = K
    if final_lat_only:
        g.y = nc.dram_tensor("y", [2, L, D], F32, kind="ExternalOutput").ap()
    else:
        g.y = nc.dram_tensor("y", [2, T, D], F32, kind="ExternalOutput").ap()
    for name, (shape, dt) in g.dbg.items():
        g.dbg[name] = nc.dram_tensor(name, list(shape), dt, kind="ExternalOutput").ap()

    g.XS = dscr("XS", [2, T, D]) if len(layers) > 1 else None
    g.WINB = dscr("WINB", [128, 8, NCB], BF16)
    g.WOUTB = dscr("WOUTB", [128, 8, D], BF16)
    g.MODROWS = dscr("MODROWS", [3, 3 * D])
    g.HT = dscr("HT", [128, 8, T], BF16)
    g.CATT = dscr("CATT", [128, 8, T], BF16)
    g.KHAT = {"L": dscr("KHATL", [2, HY_LAT.ng, 128, 512]), "C": dscr("KHATC", [2, HY_CTX.ng, 128, 512])}
    g.T_KHAT = Tok()
    g.SSM_L3 = dscr("SSM_L3", [128, 2, 16, 17, 64], BF16)
    g.SSM_L1 = dscr("SSM_L1", [128, 2, 16, 2, 128], BF16)
    g.SSM_SG = dscr("SSM_SG", [128, 2, 16, 2, 2, 2, 128], BF16)
    g.T_SSMW = Tok()
    g.T_XS = [Tok(), Tok()]
    g.T_WINB = Tok()
    g.T_WOUTB = Tok()
    g.T_MOD = Tok()
    g.T_HT = Tok()
    g.T_CATT = Tok()
    g.T_Y = Tok()

    with ExitStack() as es:
        k = KB(nc, es)
        g.k = k
        g.es = es
        g.ident = es.enter_context(nc.sbuf_tensor("ident", [128, 128], BF16))
        g.T_ident = Tok()
        g.onesf = es.enter_context(nc.sbuf_tensor("onesf", [128, 128], F32))
        g.ones128 = es.enter_context(nc.sbuf_tensor("ones128", [128, 128], BF16))
        g.ones192 = es.enter_context(nc.sbuf_tensor("ones192", [128, 128], BF16))
        g.blk64 = es.enter_context(nc.sbuf_tensor("blk64", [128, 128], BF16))
        g.T_const = Tok()
        with ExitStack() as es2:
            tmp = es2.enter_context(nc.sbuf_tensor("idtmp", [128, 128], F32))
            tt = Tok()
            k.dma("sp", tmp[:], K["k_ident"], writes=[tt])
            k.op("dve", lambda e: e.tensor_copy(out=g.ident[:], in_=tmp[:]), reads=[tt], writes=[g.T_ident])
            k.op("dve", lambda e: e.memset(g.onesf[:], 1.0), writes=[g.T_const])
            k.op("dve", lambda e: e.memset(g.ones128[:], 1.0 / 128), writes=[g.T_const])
            k.op("dve", lambda e: e.memset(g.ones192[:], 1.0 / 192), writes=[g.T_const])
            k.op("dve", lambda e: e.memset(g.blk64[:], 0.0), writes=[g.T_const])
            k.op("dve", lambda e: e.memset(g.blk64[0:64, 0:64], 1.0 / 64), reads=[g.T_const], writes=[g.T_const])
            k.op("dve", lambda e: e.memset(g.blk64[64:128, 64:128], 1.0 / 64), reads=[g.T_const], writes=[g.T_const])
            k.barrier()

        for li, l in enumerate(layers):
            need_ctx = l < DEPTH - 1
            src = g.xs if li == 0 else g.XS
            last = li == len(layers) - 1
            dst = g.y if last else g.XS
            phase_weights(g, l)
            k.barrier()
            phase_ssm_weights(g, l)
            phase_hy_filter(g, l, HY_LAT)
            if need_ctx:
                phase_hy_filter(g, l, HY_CTX)
            for s in range(2):
                phase_P1(g, l, s, src)
                k.barrier()
                phase_hyena(g, l, s, HY_LAT, C)
                if need_ctx:
                    phase_hyena(g, l, s, HY_CTX, 0)
                phase_ssm(g, l, s, need_ctx)
                phase_attn(g, l, s, need_ctx)
                k.barrier()
                phase_P6(g, l, s, src, dst, need_ctx, last and final_lat_only)
                k.barrier()
        k.drain("sp")
    return nc, g


def phase_weights(g, l):
    nc, k, W = g.nc, g.k, g.W
    with ExitStack() as es:
        sb = lambda n, s, d=F32: es.enter_context(nc.sbuf_tensor(_nm(n), s, d))
        ps = lambda n, s, d=F32: es.enter_context(nc.psum_tensor(_nm(n), s, d))
        p32, s32 = _partner_index(32)
        p64, s64 = _partner_index(64)
        fin = Rot([sb("wf%d" % i, [128, NIN]) for i in range(2)])
        fob = Rot([sb("wb%d" % i, [128, NCB], BF16) for i in range(2)])
        qorder = (0, 2, 1, 3)
        engs = ["act", "pool", "dve"]
        ei = 0

        def cp(dst, src, neg=False):
            nonlocal ei
            e = engs[ei % 3]
            ei += 1
            if e == "act":
                k.op("act", lambda en: en.activation(out=dst, in_=src, func=AF.Copy, scale=(-1.0 if neg else 1.0)), reads=[tf], writes=[tb])
            else:
                k.op(e, lambda en: en.tensor_scalar(out=dst, in0=src, scalar1=(-1.0 if neg else 1.0), scalar2=None, op0=ALU.mult), reads=[tf], writes=[tb])

        def partner_cols(dstb, srcb, d):
            hh, qq = d // 2, d // 4
            for hf in range(2):
                b0 = hf * hh
                cp(wb[:, dstb + b0:dstb + b0 + qq], wf[:, srcb + b0 + qq:srcb + b0 + hh], neg=True)
                cp(wb[:, dstb + b0 + qq:dstb + b0 + hh], wf[:, srcb + b0:srcb + b0 + qq], neg=False)

        for kc in range(8):
            wf, tf = fin.next()
            wb, tb = fob.next()
            k.dma("sp", wf[:], W["w_in"][l, kc * 128:(kc + 1) * 128, :], writes=[tf])
            k.op("act", lambda en: en.activation(out=wb[:, 0:1456], in_=wf[:, 0:1456], func=AF.Copy), reads=[tf], writes=[tb])
            k.op("dve", lambda en: en.tensor_copy(out=wb[:, 1456:NIN], in_=wf[:, 1456:NIN]), reads=[tf], writes=[tb])
            partner_cols(O_KRP, O_KR, 32)
            for pos, h in enumerate(qorder):
                cp(wb[:, O_QM + pos * 64:O_QM + pos * 64 + 64], wf[:, O_GQ + h * 64:O_GQ + h * 64 + 64])
                partner_cols(O_QP + pos * 64, O_GQ + h * 64, 64)
            for h in range(2):
                partner_cols(O_KP + h * 64, O_GK + h * 64, 64)
            k.dma("pool", g.WINB[:, kc, :], wb[:], reads=[tb], writes=[g.T_WINB])
        fo = Rot([sb("wof%d" % i, [128, D]) for i in range(2)])
        fb = Rot([sb("wob%d" % i, [128, D], BF16) for i in range(2)])
        for kc in range(8):
            wf, tf = fo.next()
            wb, tb = fb.next()
            k.dma("sp", wf[:], W["w_out"][l, kc * 128:(kc + 1) * 128, :], writes=[tf])
            k.op("act" if kc % 2 else "dve", (lambda en: en.activation(out=wb[:], in_=wf[:], func=AF.Copy)) if kc % 2 else (lambda en: en.tensor_copy(out=wb[:], in_=wf[:])), reads=[tf], writes=[tb])
            k.dma("pool", g.WOUTB[:, kc, :], wb[:], reads=[tb], writes=[g.T_WOUTB])

        cT = sb("cTs", [128, 8, 3])
        scT = sb("scT", [128, 8, 3])
        t_c = Tok()
        k.dma("sp", cT[:], g.cT, writes=[t_c])
        k.op("act", lambda en: en.activation(out=scT[:], in_=cT[:], func=AF.Silu), reads=[t_c], writes=[t_c])
        mrow = sb("mrow", [3, 3 * D])
        brow = sb("brow", [3, 3 * D])
        gpre = sb("gpre", [3, D])
        gpost = sb("gpost", [3, D])
        t_m = Tok()
        t_b = Tok()
        k.dma("sp", brow[:], W["b_mod"][l:l + 1, :].broadcast_to([3, 3 * D]), writes=[t_b])
        k.dma("sp", gpre[:], W["g_pre"][l:l + 1, :].broadcast_to([3, D]), writes=[t_b])
        k.dma("sp", gpost[:], W["g_post"][l:l + 1, :].broadcast_to([3, D]), writes=[t_b])
        wm = Rot([sb("wm%d" % i, [128, 8, 512]) for i in range(2)])
        pm = Rot([ps("pm%d" % i, [3, 512]) for i in range(2)], excl=True)
        for cc in range(6):
            wt, tw = wm.next()
            pt, tp = pm.next()
            k.dma("sp", wt[:], W["w_mod"][l, :, cc * 512:(cc + 1) * 512].rearrange("(kc p) n -> p kc n", p=128), writes=[tw])
            for kc in range(8):
                k.op("pe", lambda en: en.matmul(pt[:], lhsT=scT[:, kc, :], rhs=wt[:, kc, :], start=(kc == 0), stop=(kc == 7)), reads=[t_c, tw], writes=[tp])
            k.op("dve", lambda en: en.tensor_tensor(out=mrow[:, cc * 512:(cc + 1) * 512], in0=pt[:], in1=brow[:, cc * 512:(cc + 1) * 512], op=ALU.add), reads=[tp, t_b], writes=[t_m])
        orow = sb("orow", [3, 3 * D])
        t_o = Tok()
        k.op("dve", lambda en: en.scalar_tensor_tensor(out=orow[:, 0:D], in0=mrow[:, D:2 * D], scalar=1.0, in1=gpre[:], op0=ALU.add, op1=ALU.mult), reads=[t_m, t_b], writes=[t_o])
        k.op("dve", lambda en: en.tensor_copy(out=orow[:, D:2 * D], in_=mrow[:, 0:D]), reads=[t_m, t_o], writes=[t_o])
        k.op("dve", lambda en: en.tensor_tensor(out=orow[:, 2 * D:3 * D], in0=mrow[:, 2 * D:3 * D], in1=gpost[:], op=ALU.mult), reads=[t_m, t_b, t_o], writes=[t_o])
        k.dma("pool", g.MODROWS, orow[:], reads=[t_o], writes=[g.T_MOD])
        k.barrier()

    if not hasattr(g, "lw"):
        lw = G()
        es = g.es
        sbp = lambda n, s, d=F32: es.enter_context(nc.sbuf_tensor(_nm(n), s, d))
        lw.wuq_a = sbp("wuq_a", [128, 384], BF16)
        lw.wuq_b = sbp("wuq_b", [64, 384], BF16)
        lw.wuqp_a = sbp("wuqp_a", [128, 384], BF16)
        lw.wuqp_b = sbp("wuqp_b", [64, 384], BF16)
        lw.wuk = sbp("wuk", [128, 256], BF16)
        lw.wuv = sbp("wuv", [128, 256], BF16)
        lw.gq = sbp("gqc", [128, 4])
        lw.A2 = sbp("ssmA2", [128, NDBL, 3, 16])
        lw.tok = Tok()
        g.lw = lw
    lw = g.lw
    with ExitStack() as es:
        sb = lambda n, s, d=F32: es.enter_context(nc.sbuf_tensor(_nm(n), s, d))
        uqa = sb("uqa", [128, 384])
        uqb = sb("uqb", [64, 384])
        ukv = sb("ukv", [128, 512])
        gcq = sb("gcq", [128, 2])
        gckv = sb("gckv", [128, 1])
        tl = Tok()
        k.dma("sp", uqa[:], W["mla_w_uq"][l, 0:128, :], writes=[tl])
        k.dma("sp", uqb[:], W["mla_w_uq"][l, 128:192, :], writes=[tl])
        k.dma("sp", ukv[:], W["mla_w_ukv"][l], writes=[tl])
        k.dma("sp", gcq[:, 0:1], W["mla_g_cq"][l, 0:128].rearrange("(p o) -> p o", o=1), writes=[tl])
        k.dma("sp", gcq[0:64, 1:2], W["mla_g_cq"][l, 128:192].rearrange("(p o) -> p o", o=1), writes=[tl])
        k.dma("sp", gckv[:], W["mla_g_ckv"][l].rearrange("(p o) -> p o", o=1), writes=[tl])
        k.dma("sp", lw.gq[:], W["gq_cols"][l], writes=[lw.tok])
        k.op("dve", lambda en: en.tensor_scalar(out=uqa[:], in0=uqa[:], scalar1=gcq[:, 0:1], scalar2=None, op0=ALU.mult), reads=[tl], writes=[tl])
        k.op("dve", lambda en: en.tensor_scalar(out=uqb[:], in0=uqb[:], scalar1=gcq[0:64, 1:2], scalar2=None, op0=ALU.mult), reads=[tl], writes=[tl])
        k.op("dve", lambda en: en.tensor_scalar(out=ukv[:], in0=ukv[:], scalar1=gckv[:, 0:1], scalar2=None, op0=ALU.mult), reads=[tl], writes=[tl])
        k.op("dve", lambda en: en.tensor_copy(out=lw.wuq_a[:], in_=uqa[:]), reads=[tl], writes=[lw.tok])
        k.op("dve", lambda en: en.tensor_copy(out=lw.wuq_b[:], in_=uqb[:]), reads=[tl, lw.tok], writes=[lw.tok])
        k.op("dve", lambda en: en.memset(lw.wuqp_a[:], 0.0), reads=[lw.tok], writes=[lw.tok])
        k.op("dve", lambda en: en.memset(lw.wuqp_b[:], 0.0), reads=[lw.tok], writes=[lw.tok])
        for h in range(4):
            for hf in range(2):
                b0 = h * 96 + 64 + hf * 16
                for (dst, src, tile_src) in ((lw.wuqp_a, uqa, 128), (lw.wuqp_b, uqb, 64)):
                    k.op("dve", lambda en: en.tensor_scalar(out=dst[:, b0:b0 + 8], in0=src[:, b0 + 8:b0 + 16], scalar1=-1.0, scalar2=None, op0=ALU.mult), reads=[tl, lw.tok], writes=[lw.tok])
                    k.op("dve", lambda en: en.tensor_copy(out=dst[:, b0 + 8:b0 + 16], in_=src[:, b0:b0 + 8]), reads=[tl, lw.tok], writes=[lw.tok])
            k.op("dve", lambda en: en.tensor_copy(out=lw.wuk[:, h * 64:(h + 1) * 64], in_=ukv[:, h * 128:h * 128 + 64]), reads=[tl, lw.tok], writes=[lw.tok])
            k.op("dve", lambda en: en.tensor_copy(out=lw.wuv[:, h * 64:(h + 1) * 64], in_=ukv[:, h * 128 + 64:h * 128 + 128]), reads=[tl, lw.tok], writes=[lw.tok])
        k.barrier()


def phase_P1(g, l, s, src):
    nc, k = g.nc, g.k
    with ExitStack() as es:
        sb = lambda n, s_, d=F32: es.enter_context(nc.sbuf_tensor(_nm(n), s_, d))
        ps = lambda n, s_, d=F32: es.enter_context(nc.psum_tensor(_nm(n), s_, d))
        mods = {}
        t_mod = Tok()
        for v in (s, 2):
            mods[v] = sb("mod%d" % v, [128, 2 * D])
            k.dma("sp", mods[v][:], g.MODROWS[v:v + 1, 0:2 * D].broadcast_to([128, 2 * D]), reads=[g.T_MOD], writes=[t_mod])
        neghalf = sb("neghalf", [128, 1])
        k.op("pool", lambda en: en.memset(neghalf[:], -0.5), writes=[t_mod])
        xr = Rot([sb("x%d" % i, [128, D]) for i in range(3)])
        junk = sb("junk", [128, D], BF16)
        t_junk = Tok()
        hb_r = Rot([sb("hb%d" % i, [128, D], BF16) for i in range(2)])
        tmp_r = Rot([sb("tmp%d" % i, [128, D]) for i in range(2)])
        st_r = Rot([sb("st%d" % i, [128, 4]) for i in range(3)])
        tp_r = Rot([ps("tp%d" % i, [128, 8, 128], BF16) for i in range(2)], excl=True)
        hs_r = Rot([sb("hs%d" % i, [128, 8, 512], BF16) for i in range(2)])
        hs, t_hs = None, None
        for ti in range(NT):
            if ti == 0 or (ti >= 2 and (ti - 2) % 4 == 0):
                hs, t_hs = hs_r.next()
            off = (ti * 128) if ti < 2 else (((ti - 2) % 4) * 128)
            xt, t_x = xr.next()
            k.dma("sp", xt[:], src[s, ti * 128:(ti + 1) * 128, :], reads=[g.T_XS[s]], writes=[t_x])
            st, t_st = st_r.next()
            k.op("dve", lambda en: en.scalar_tensor_tensor(out=junk[:], in0=xt[:], scalar=1.0, in1=xt[:], op0=ALU.mult, op1=ALU.mult, accum_out=st[:, 0:1]), reads=[t_x], writes=[t_junk, t_st])
            k.op("dve", lambda en: en.tensor_scalar(out=st[:, 1:2], in0=st[:, 0:1], scalar1=1.0 / D, scalar2=EPS, op0=ALU.mult, op1=ALU.add), reads=[t_st], writes=[t_st])
            k.op("pool", lambda en: en.tensor_tensor(out=st[:, 2:3], in0=st[:, 1:2], in1=neghalf[:], op=ALU.pow), reads=[t_st, t_mod], writes=[t_st])
            md = mods[2] if ti < 2 else mods[s]
            tmp, t_tmp = tmp_r.next()
            hb, t_hb = hb_r.next()
            k.op("dve", lambda en: en.scalar_tensor_tensor(out=tmp[:], in0=xt[:], scalar=st[:, 2:3], in1=md[:, 0:D], op0=ALU.mult, op1=ALU.mult), reads=[t_x, t_st, t_mod], writes=[t_tmp])
            k.op("pool", lambda en: en.tensor_tensor(out=hb[:], in0=tmp[:], in1=md[:, D:2 * D], op=ALU.add), reads=[t_tmp, t_mod], writes=[t_hb])
            tp, t_tp = tp_r.next()
            for kc in range(8):
                k.op("pe", lambda en: en.transpose(tp[:, kc, :], hb[:, kc * 128:(kc + 1) * 128], g.ident[:]), reads=[t_hb, g.T_ident], writes=[t_tp])
            k.op("act", lambda en: en.activation(out=hs[:, :, off:off + 128], in_=tp[:], func=AF.Copy), reads=[t_tp], writes=[t_hs])
            if ti == 1:
                k.dma("pool", g.HT[:, :, 0:256], hs[:, :, 0:256], reads=[t_hs], writes=[g.T_HT])
            elif ti >= 2 and (ti - 2) % 4 == 3:
                t0 = (ti - 3) * 128
                k.dma("pool", g.HT[:, :, t0:t0 + 512], hs[:, :, 0:512], reads=[t_hs], writes=[g.T_HT])
        k.barrier()


def phase_attn(g, l, s, need_ctx):
    nc, k, lw = g.nc, g.k, g.lw
    with ExitStack() as es:
        sb = lambda n, s_, d=F32: es.enter_context(nc.sbuf_tensor(_nm(n), s_, d))
        ps = lambda n, s_, d=F32: es.enter_context(nc.psum_tensor(_nm(n), s_, d))
        KTm = [sb("KTm%d" % h, [96, T], BF16) for h in range(4)]
        VM = sb("VM", [128, NT, 4 * 65], BF16)
        KTg = sb("KTg", [128, T], BF16)
        VG = sb("VG", [128, NT, 2 * 65], BF16)
        t_K = Tok()
        k.op("pool", lambda en: en.memset(VM[:], 1.0), writes=[t_K])
        k.op("pool", lambda en: en.memset(VG[:], 1.0), writes=[t_K])
        t_w = Tok()
        epsc = sb("epsc", [128, 1])
        k.op("pool", lambda en: en.memset(epsc[:], EPS), writes=[t_w])

        hT_r = Rot([sb("hT%d" % i, [128, 8, 512], BF16) for i in range(2)])
        rope_r = Rot([sb("rp%d" % i, [128, 4, 512]) for i in range(2)])
        esA = ExitStack()
        sbA = lambda n, s_, d=F32: esA.enter_context(nc.sbuf_tensor(_nm(n), s_, d))
        wkv = sbA("wkv", [128, 8, 576], BF16)
        k.dma("sp", wkv[:, :, 0:160], g.WINB[:, :, O_CKV:O_CKV + 160], reads=[g.T_WINB], writes=[t_w])
        k.dma("sp", wkv[:, :, 160:192], g.WINB[:, :, O_KRP:O_KRP + 32], reads=[g.T_WINB], writes=[t_w])
        k.dma("sp", wkv[:, :, 192:448], g.WINB[:, :, O_GK:O_GK + 256], reads=[g.T_WINB], writes=[t_w])
        k.dma("sp", wkv[:, :, 448:576], g.WINB[:, :, O_KP:O_KP + 128], reads=[g.T_WINB], writes=[t_w])
        SB2 = [ps("psS%d" % i, [128, 2, 512]) for i in range(3)]
        TSB = [Tok(True) for _ in range(3)]
        PS = [SB2[i // 2][:, i % 2, :] for i in range(6)] + [ps("ps%d" % i, [128, 512]) for i in range(6, 8)]
        TPS = [TSB[i // 2] for i in range(6)] + [Tok(True) for _ in range(2)]

        blocks = [(0, 256)] + [(256 + 512 * b, 512) for b in range(8)]

        def load_block(t0, nb):
            hT, t_h = hT_r.next()
            k.dma("sp", hT[:, :, 0:nb], g.HT[:, :, t0:t0 + nb], reads=[g.T_HT], writes=[t_h])
            rp, t_rp = rope_r.next()
            k.dma("sp", rp[:, 0:2, 0:nb], g.K["k_ropeM"][:, :, t0:t0 + nb].rearrange("a p n -> p a n"), writes=[t_rp])
            k.dma("sp", rp[:, 2:4, 0:nb], g.K["k_ropeG"][:, :, t0:t0 + nb].rearrange("a p n -> p a n"), writes=[t_rp])
            return hT, t_h, rp, t_rp

        def proj(pi, M, wt, c0, hT, t_h, nb, pbase=0):
            for kc in range(8):
                k.op("pe", lambda en: en.matmul(PS[pi][pbase:pbase + M, 0:nb], lhsT=wt[:, kc, c0:c0 + M], rhs=hT[:, kc, 0:nb], start=(kc == 0), stop=(kc == 7)), reads=[t_w, t_h], writes=[TPS[pi]])

        def rstd_from_ms(out_ap, ms_ap, reads, wtok, tmp_ap):
            k.op("act", lambda en: en.activation(out=tmp_ap, in_=ms_ap, func=AF.Ln, bias=epsc[0:tmp_ap.shape[0], 0:1]), reads=reads + [t_w], writes=[wtok])
            k.op("act", lambda en: en.activation(out=out_ap, in_=tmp_ap, func=AF.Exp, scale=-0.5), reads=[wtok], writes=[wtok])

        wk_r = Rot([sbA("wk%d" % i, [128, 6, 512]) for i in range(1)])
        wkb_r = Rot([sbA("wkb%d" % i, [128, 3, 512], BF16) for i in range(2)])
        for (t0, nb) in blocks:
            hT, t_h, rp, t_rp = load_block(t0, nb)
            wk, t_wk = wk_r.next()
            wkb, t_wkb = wkb_r.next()
            proj(0, 128, wkv, 0, hT, t_h, nb)
            k.op("act", lambda en: en.activation(out=wkb[:, 0, 0:nb], in_=PS[0][:, 0:nb], func=AF.Square), reads=[TPS[0]], writes=[t_wkb])
            k.op("dve", lambda en: en.tensor_copy(out=wk[:, 0, 0:nb], in_=PS[0][:, 0:nb]), reads=[TPS[0]], writes=[t_wk])
            k.op("pe", lambda en: en.matmul(PS[1][:, 0:nb], lhsT=g.ones128[:], rhs=wkb[:, 0, 0:nb], start=True, stop=True), reads=[t_wkb, g.T_const], writes=[TPS[1]])
            rstd_from_ms(wk[:, 1, 0:nb], PS[1][:, 0:nb], [TPS[1]], t_wk, wk[:, 1, 0:nb])
            k.op("dve", lambda en: en.tensor_tensor(out=wkb[:, 1, 0:nb], in0=wk[:, 0, 0:nb], in1=wk[:, 1, 0:nb], op=ALU.mult), reads=[t_wk], writes=[t_wkb])
            for h in range(4):
                pi = 2 + (h % 2)
                k.op("pe", lambda en: en.matmul(PS[pi][0:64, 0:nb], lhsT=lw.wuk[:, h * 64:(h + 1) * 64], rhs=wkb[:, 1, 0:nb], start=True, stop=True), reads=[t_wkb, lw.tok], writes=[TPS[pi]])
                k.op("act" if h % 2 else "dve", (lambda en: en.activation(out=KTm[h][0:64, t0:t0 + nb], in_=PS[pi][0:64, 0:nb], func=AF.Copy)) if h % 2 else (lambda en: en.tensor_copy(out=KTm[h][0:64, t0:t0 + nb], in_=PS[pi][0:64, 0:nb])), reads=[TPS[pi]], writes=[t_K])
            for j in range(nb // 128):
                ti = t0 // 128 + j
                k.op("pe", lambda en: en.matmul(PS[4][:, 0:256], lhsT=wkb[:, 1, j * 128:(j + 1) * 128], rhs=lw.wuv[:], start=True, stop=True), reads=[t_wkb, lw.tok], writes=[TPS[4]])
                k.op("dve", lambda en: en.tensor_copy(out=VM[:, ti, :].rearrange("p (h d) -> p h d", d=65)[:, :, 0:64], in_=PS[4][:, 0:256].rearrange("p (h d) -> p h d", d=64)), reads=[TPS[4]], writes=[t_K])
            proj(5, 32, wkv, 128, hT, t_h, nb, pbase=64)
            proj(6, 32, wkv, 160, hT, t_h, nb, pbase=64)
            k.op("dve", lambda en: en.tensor_tensor(out=wk[64:96, 2, 0:nb], in0=PS[5][64:96, 0:nb], in1=rp[64:96, 0, 0:nb], op=ALU.mult), reads=[TPS[5], t_rp], writes=[t_wk])
            k.op("dve", lambda en: en.tensor_tensor(out=wk[64:96, 3, 0:nb], in0=PS[6][64:96, 0:nb], in1=rp[64:96, 1, 0:nb], op=ALU.mult), reads=[TPS[6], t_rp], writes=[t_wk])
            for h in range(4):
                k.op("pool" if h % 2 else "dve", lambda en: en.tensor_tensor(out=KTm[h][64:96, t0:t0 + nb], in0=wk[64:96, 2, 0:nb], in1=wk[64:96, 3, 0:nb], op=ALU.add), reads=[t_wk], writes=[t_K])
            proj(7, 128, wkv, 192, hT, t_h, nb)
            proj(0, 128, wkv, 448, hT, t_h, nb)
            k.op("act", lambda en: en.activation(out=wkb[:, 2, 0:nb], in_=PS[7][:, 0:nb], func=AF.Square), reads=[TPS[7]], writes=[t_wkb])
            k.op("pe", lambda en: en.matmul(PS[1][:, 0:nb], lhsT=g.blk64[:], rhs=wkb[:, 2, 0:nb], start=True, stop=True), reads=[t_wkb, g.T_const], writes=[TPS[1]])
            rstd_from_ms(wk[:, 4, 0:nb], PS[1][:, 0:nb], [TPS[1]], t_wk, wk[:, 4, 0:nb])
            k.op("dve", lambda en: en.scalar_tensor_tensor(out=wk[:, 0, 0:nb], in0=PS[7][:, 0:nb], scalar=lw.gq[:, 2:3], in1=rp[:, 2, 0:nb], op0=ALU.mult, op1=ALU.mult), reads=[TPS[7], t_rp, lw.tok, t_wk], writes=[t_wk])
            k.op("dve", lambda en: en.scalar_tensor_tensor(out=wk[:, 5, 0:nb], in0=PS[0][:, 0:nb], scalar=lw.gq[:, 3:4], in1=rp[:, 3, 0:nb], op0=ALU.mult, op1=ALU.mult), reads=[TPS[0], t_rp, lw.tok, t_wk], writes=[t_wk])
            k.op("pool", lambda en: en.tensor_tensor(out=wk[:, 0, 0:nb], in0=wk[:, 0, 0:nb], in1=wk[:, 5, 0:nb], op=ALU.add), reads=[t_wk], writes=[t_wk])
            k.op("dve", lambda en: en.tensor_tensor(out=KTg[:, t0:t0 + nb], in0=wk[:, 0, 0:nb], in1=wk[:, 4, 0:nb], op=ALU.mult), reads=[t_wk], writes=[t_K])
            for j in range(nb // 128):
                ti = t0 // 128 + j
                for kc in range(8):
                    k.op("pe", lambda en: en.matmul(PS[4][:, 0:128], lhsT=hT[:, kc, j * 128:(j + 1) * 128], rhs=wkv[:, kc, 320:448], start=(kc == 0), stop=(kc == 7)), reads=[t_h, t_w], writes=[TPS[4]])
                k.op("act", lambda en: en.activation(out=VG[:, ti, :].rearrange("p (h d) -> p h d", d=65)[:, :, 0:64], in_=PS[4][:, 0:128].rearrange("p (h d) -> p h d", d=64), func=AF.Copy), reads=[TPS[4]], writes=[t_K])
        if "KTm0" in g.dbg:
            k.dma("pool", g.dbg["KTm0"], KTm[0][:], reads=[t_K])
            k.dma("pool", g.dbg["KTg"], KTg[:], reads=[t_K])
            k.dma("pool", g.dbg["VM"], VM[:], reads=[t_K])
            k.dma("pool", g.dbg["VG"], VG[:], reads=[t_K])

        k.barrier()
        esA.close()
        wq = sb("wq", [128, 8, 192 + 256 + 256 + 256 + 256], BF16)
        k.dma("sp", wq[:, :, 0:192], g.WINB[:, :, O_CQ:O_CQ + 192], reads=[g.T_WINB], writes=[t_w])
        k.dma("sp", wq[:, :, 192:448], g.WINB[:, :, O_GM:O_GM + 256], reads=[g.T_WINB], writes=[t_w])
        k.dma("sp", wq[:, :, 448:960], g.WINB[:, :, O_QM:O_QM + 512], reads=[g.T_WINB], writes=[t_w])
        k.dma("sp", wq[:, :, 960:1216], g.WINB[:, :, O_GG:O_GG + 256], reads=[g.T_WINB], writes=[t_w])
        qm_r = Rot([[sb("qm%d_%d" % (i, h), [96, 512], BF16) for h in range(4)] for i in range(2)])
        qg_r = Rot([[sb("qg%d_%d" % (i, j), [128, 512], BF16) for j in range(2)] for i in range(2)])
        gate_r = Rot([sb("gate%d" % i, [64, 8, 512], BF16) for i in range(1)])
        cq_r = Rot([sb("cq%d" % i, [128, 4, 512]) for i in range(1)])
        cqb_r = Rot([sb("cqb%d" % i, [128, 4, 512], BF16) for i in range(1)])
        P_r = Rot([sb("P%d" % i, [128, 2, 512], BF16) for i in range(5)])
        osb_r = Rot([sb("osb%d" % i, [65, 512]) for i in range(3)])
        res_r = Rot([sb("res%d" % i, [64, 512], BF16) for i in range(3)])
        O_bufs = [6, 7]
        for (t0, nb) in blocks:
            if t0 == 0 and not need_ctx:
                continue
            hT, t_h, rp, t_rp = load_block(t0, nb)
            kts = list(range(2)) if t0 == 0 else list(range(NT))
            qm, t_qm = qm_r.next()
            qg, t_qg = qg_r.next()
            gate, t_gate = gate_r.next()
            cq, t_cq = cq_r.next()
            cqb, t_cqb = cqb_r.next()
            for hh in range(8):
                c0 = (192 + hh * 64) if hh < 4 else (960 + (hh - 4) * 64)
                pi = 2 * (hh % 2)
                proj(pi, 64, wq, c0, hT, t_h, nb)
                k.op("act", lambda en: en.activation(out=gate[:, hh, 0:nb], in_=PS[pi][0:64, 0:nb], func=AF.Silu), reads=[TPS[pi]], writes=[t_gate])
            proj(0, 128, wq, 0, hT, t_h, nb)
            proj(2, 64, wq, 128, hT, t_h, nb)
            k.op("act", lambda en: en.activation(out=cqb[:, 0, 0:nb], in_=PS[0][:, 0:nb], func=AF.Square), reads=[TPS[0]], writes=[t_cqb])
            k.op("act", lambda en: en.activation(out=cqb[0:64, 1, 0:nb], in_=PS[2][0:64, 0:nb], func=AF.Square), reads=[TPS[2]], writes=[t_cqb])
            k.op("dve", lambda en: en.tensor_copy(out=cq[:, 0, 0:nb], in_=PS[0][:, 0:nb]), reads=[TPS[0]], writes=[t_cq])
            k.op("dve", lambda en: en.tensor_copy(out=cq[0:64, 1, 0:nb], in_=PS[2][0:64, 0:nb]), reads=[TPS[2]], writes=[t_cq])
            k.op("pe", lambda en: en.matmul(PS[0][:, 0:nb], lhsT=g.ones192[:], rhs=cqb[:, 0, 0:nb], start=True, stop=False), reads=[t_cqb, g.T_const, t_cq], writes=[TPS[0]])
            k.op("pe", lambda en: en.matmul(PS[0][:, 0:nb], lhsT=g.ones192[0:64, :], rhs=cqb[0:64, 1, 0:nb], start=False, stop=True), reads=[t_cqb, g.T_const], writes=[TPS[0]])
            rstd_from_ms(cq[:, 2, 0:nb], PS[0][:, 0:nb], [TPS[0]], t_cq, cq[:, 2, 0:nb])
            k.op("dve", lambda en: en.tensor_tensor(out=cqb[:, 2, 0:nb], in0=cq[:, 0, 0:nb], in1=cq[:, 2, 0:nb], op=ALU.mult), reads=[t_cq], writes=[t_cqb])
            k.op("dve", lambda en: en.tensor_tensor(out=cqb[0:64, 3, 0:nb], in0=cq[0:64, 1, 0:nb], in1=cq[0:64, 2, 0:nb], op=ALU.mult), reads=[t_cq], writes=[t_cqb])
            for h in range(4):
                for (pi, wa, wb_) in ((0, lw.wuq_a, lw.wuq_b), (2, lw.wuqp_a, lw.wuqp_b)):
                    k.op("pe", lambda en: en.matmul(PS[pi][0:96, 0:nb], lhsT=wa[:, h * 96:(h + 1) * 96], rhs=cqb[:, 2, 0:nb], start=True, stop=False), reads=[t_cqb, lw.tok], writes=[TPS[pi]])
                    k.op("pe", lambda en: en.matmul(PS[pi][0:96, 0:nb], lhsT=wb_[:, h * 96:(h + 1) * 96], rhs=cqb[0:64, 3, 0:nb], start=False, stop=True), reads=[t_cqb, lw.tok], writes=[TPS[pi]])
                k.op("dve", lambda en: en.tensor_tensor(out=cq[0:96, 3, 0:nb], in0=PS[0][0:96, 0:nb], in1=rp[0:96, 0, 0:nb], op=ALU.mult), reads=[TPS[0], t_rp, t_cq], writes=[t_cq])
                k.op("dve", lambda en: en.tensor_tensor(out=cq[0:96, 1, 0:nb], in0=PS[2][0:96, 0:nb], in1=rp[0:96, 1, 0:nb], op=ALU.mult), reads=[TPS[2], t_rp, t_cq], writes=[t_cq])
                k.op("pool", lambda en: en.tensor_tensor(out=qm[h][:, 0:nb], in0=cq[0:96, 3, 0:nb], in1=cq[0:96, 1, 0:nb], op=ALU.add), reads=[t_cq], writes=[t_qm])
            for j in range(2):
                proj(0, 128, wq, 448 + j * 128, hT, t_h, nb)
                proj(2, 128, wq, 704 + j * 128, hT, t_h, nb)
                k.op("act", lambda en: en.activation(out=cqb[:, 0, 0:nb], in_=PS[0][:, 0:nb], func=AF.Square), reads=[TPS[0], t_cqb], writes=[t_cqb])
                k.op("dve", lambda en: en.scalar_tensor_tensor(out=cq[:, 0, 0:nb], in0=PS[0][:, 0:nb], scalar=lw.gq[:, 0:1], in1=rp[:, 2, 0:nb], op0=ALU.mult, op1=ALU.mult), reads=[TPS[0], t_rp, lw.tok, t_cq], writes=[t_cq])
                k.op("dve", lambda en: en.scalar_tensor_tensor(out=cq[:, 1, 0:nb], in0=PS[2][:, 0:nb], scalar=lw.gq[:, 1:2], in1=rp[:, 3, 0:nb], op0=ALU.mult, op1=ALU.mult), reads=[TPS[2], t_rp, lw.tok, t_cq], writes=[t_cq])
                k.op("pe", lambda en: en.matmul(PS[0][:, 0:nb], lhsT=g.blk64[:], rhs=cqb[:, 0, 0:nb], start=True, stop=True), reads=[t_cqb, g.T_const, t_cq], writes=[TPS[0]])
                rstd_from_ms(cq[:, 2, 0:nb], PS[0][:, 0:nb], [TPS[0]], t_cq, cq[:, 2, 0:nb])
                k.op("pool", lambda en: en.tensor_tensor(out=cq[:, 0, 0:nb], in0=cq[:, 0, 0:nb], in1=cq[:, 1, 0:nb], op=ALU.add), reads=[t_cq], writes=[t_cq])
                k.op("dve", lambda en: en.tensor_tensor(out=qg[j][:, 0:nb], in0=cq[:, 0, 0:nb], in1=cq[:, 2, 0:nb], op=ALU.mult), reads=[t_cq], writes=[t_qg])
            if "qm0" in g.dbg and t0 == 256:
                k.dma("pool", g.dbg["qm0"], qm[0][:], reads=[t_qm])
                k.dma("pool", g.dbg["qg0"], qg[0][:], reads=[t_qg])
            pending = []
            for hh in range(8):
                while len(pending) > 1:
                    pending.pop(0)()
                if hh < 4:
                    dk, scale = 96, 96 ** -0.5
                    Kt = KTm[hh]
                    kb0 = 0
                    Qt = qm[hh]
                    qb0 = 0
                    Vt, vc0 = VM, hh * 65
                    tq = t_qm
                else:
                    hq = hh - 4
                    kv = hq // 2
                    dk, scale = 64, 64 ** -0.5
                    Kt, kb0 = KTg, kv * 64
                    Qt, qb0 = qg[hq % 2], kv * 64
                    Vt, vc0 = VG, kv * 65
                    tq = t_qg
                oi = O_bufs[hh % 2]
                pairs = [kts[i:i + 2] for i in range(0, len(kts), 2)]

                def emit_S(pidx):
                    sbi = pidx % 3
                    for j, kt in enumerate(pairs[pidx]):
                        k.op("pe", lambda en: en.matmul(SB2[sbi][:, j, 0:nb], lhsT=Kt[kb0:kb0 + dk, kt * 128:(kt + 1) * 128], rhs=Qt[qb0:qb0 + dk, 0:nb], start=True, stop=True), reads=[t_K, tq], writes=[TSB[sbi]])

                emit_S(0)
                if len(pairs) > 1:
                    emit_S(1)
                for pidx in range(len(pairs)):
                    if pidx == 3 and pending:
                        pending.pop(0)()
                    if pidx + 2 < len(pairs):
                        emit_S(pidx + 2)
                    sbi = pidx % 3
                    npr = len(pairs[pidx])
                    Pt, t_P = P_r.next()
                    k.op("act", lambda en: en.activation(out=Pt[:, 0:npr, 0:nb], in_=SB2[sbi][:, 0:npr, 0:nb], func=AF.Exp, scale=scale), reads=[TSB[sbi]], writes=[t_P])
                    for j, kt in enumerate(pairs[pidx]):
                        first = (pidx == 0 and j == 0)
                        lastm = (pidx == len(pairs) - 1 and j == npr - 1)
                        k.op("pe", lambda en: en.matmul(PS[oi][0:65, 0:nb], lhsT=Vt[:, kt, vc0:vc0 + 65], rhs=Pt[:, j, 0:nb], start=first, stop=lastm), reads=[t_P, t_K], writes=[TPS[oi]])
                osb, t_osb = osb_r.next()
                k.op("dve", lambda en: en.tensor_copy(out=osb[:, 0:nb], in_=PS[oi][0:65, 0:nb]), reads=[TPS[oi]], writes=[t_osb])
                k.op("dve", lambda en: en.reciprocal(out=osb[64:65, 0:nb], in_=osb[64:65, 0:nb]), reads=[t_osb], writes=[t_osb])

                def finish(hh=hh, oi=oi, osb=osb, t_osb=t_osb):
                    k.op("pe", lambda en: en.matmul(PS[oi][0:64, 0:nb], lhsT=g.onesf[64:65, 0:64], rhs=osb[64:65, 0:nb], start=True, stop=True), reads=[t_osb, g.T_const], writes=[TPS[oi]])
                    k.op("dve", lambda en: en.tensor_tensor(out=osb[0:64, 0:nb], in0=osb[0:64, 0:nb], in1=PS[oi][0:64, 0:nb], op=ALU.mult), reads=[t_osb, TPS[oi]], writes=[t_osb])
                    res, t_res = res_r.next()
                    k.op("pool", lambda en: en.tensor_tensor(out=res[:, 0:nb], in0=osb[0:64, 0:nb], in1=gate[:, hh, 0:nb], op=ALU.mult), reads=[t_osb, t_gate], writes=[t_res])
                    kc = hh // 2
                    p0 = (hh % 2) * 64
                    k.dma("pool", g.CATT[p0:p0 + 64, kc, t0:t0 + nb], res[:, 0:nb], reads=[t_res], writes=[g.T_CATT])
                pending.append(finish)
            while pending:
                pending.pop(0)()
        k.barrier()


def phase_P6(g, l, s, src, dst, need_ctx, lat_only_out):
    nc, k = g.nc, g.k
    with ExitStack() as es:
        sb = lambda n, s_, d=F32: es.enter_context(nc.sbuf_tensor(_nm(n), s_, d))
        ps = lambda n, s_, d=F32: es.enter_context(nc.psum_tensor(_nm(n), s_, d))
        wo = sb("wo", [128, 8, D], BF16)
        t_w = Tok()
        k.dma("sp", wo[:], g.WOUTB, reads=[g.T_WOUTB], writes=[t_w])
        Gb = {}
        for v in ((s, 2) if need_ctx else (s,)):
            Gb[v] = sb("Gb%d" % v, [128, D])
            k.dma("sp", Gb[v][:], g.MODROWS[v:v + 1, 2 * D:3 * D].broadcast_to([128, D]), reads=[g.T_MOD], writes=[t_w])
        neghalf = sb("neghalf6", [128, 1])
        k.op("pool", lambda en: en.memset(neghalf[:], -0.5), writes=[t_w])
        cat_r = Rot([sb("cat%d" % i, [128, 8, 512], BF16) for i in range(2)])
        x_r = Rot([sb("x6_%d" % i, [128, D]) for i in range(3)])
        o_r = Rot([sb("o6_%d" % i, [128, D]) for i in range(3)])
        st_r = Rot([sb("st6_%d" % i, [128, 4]) for i in range(3)])
        junk = sb("junk6", [128, D], BF16)
        t_junk = Tok()
        po_r = Rot([ps("po%d" % i, [128, D]) for i in range(3)], excl=True)
        blocks = ([(0, 256)] if need_ctx else []) + [(256 + 512 * b, 512) for b in range(8)]
        for (t0, nb) in blocks:
            cat, t_cat = cat_r.next()
            k.dma("sp", cat[:, :, 0:nb], g.CATT[:, :, t0:t0 + nb], reads=[g.T_CATT], writes=[t_cat])
            for j in range(nb // 128):
                tok0 = t0 + j * 128
                po, t_po = po_r.next()
                for hf in range(2):
                    for kc in range(8):
                        k.op("pe", lambda en: en.matmul(po[:, hf * 512:(hf + 1) * 512], lhsT=cat[:, kc, j * 128:(j + 1) * 128], rhs=wo[:, kc, hf * 512:(hf + 1) * 512], start=(kc == 0), stop=(kc == 7)), reads=[t_cat, t_w], writes=[t_po])
                xt, t_x = x_r.next()
                k.dma("sp", xt[:], src[s, tok0:tok0 + 128, :], reads=[g.T_XS[s]], writes=[t_x])
                st, t_st = st_r.next()
                k.op("act", lambda en: en.activation(out=junk[:], in_=po[:], func=AF.Square, accum_out=st[:, 0:1]), reads=[t_po], writes=[t_junk, t_st])
                k.op("dve", lambda en: en.tensor_scalar(out=st[:, 1:2], in0=st[:, 0:1], scalar1=1.0 / D, scalar2=EPS, op0=ALU.mult, op1=ALU.add), reads=[t_st], writes=[t_st])
                k.op("pool", lambda en: en.tensor_tensor(out=st[:, 2:3], in0=st[:, 1:2], in1=neghalf[:], op=ALU.pow), reads=[t_st, t_w], writes=[t_st])
                ot, t_o = o_r.next()
                gb = Gb[2] if t0 == 0 else Gb[s]
                k.op("dve", lambda en: en.scalar_tensor_tensor(out=ot[:], in0=po[:], scalar=st[:, 2:3], in1=gb[:], op0=ALU.mult, op1=ALU.mult), reads=[t_po, t_st, t_w], writes=[t_o])
                k.op("pool", lambda en: en.tensor_tensor(out=ot[:], in0=ot[:], in1=xt[:], op=ALU.add), reads=[t_o, t_x], writes=[t_o])
                if lat_only_out:
                    if t0 == 0:
                        continue
                    k.dma("pool", dst[s, tok0 - C:tok0 - C + 128, :], ot[:], reads=[t_o], writes=[g.T_Y])
                else:
                    wt = g.T_XS[s] if dst is g.XS else g.T_Y
                    k.dma("pool", dst[s, tok0:tok0 + 128, :], ot[:], reads=[t_o], writes=[wt])
        k.barrier()


_CACHE = {}


def _weights_host(inputs):
    w = {}
    for n in ("w_mod", "b_mod", "g_pre", "g_post", "w_in", "w_out", "mla_g_cq", "mla_w_uq", "mla_g_ckv", "mla_w_ukv",
              "hy_conv_w", "hy_conv_b", "hy_f_w1", "hy_f_b1", "hy_f_freq1", "hy_f_w2", "hy_f_b2", "hy_f_freq2", "hy_f_w3", "hy_bias"):
        w[n] = np.ascontiguousarray(inputs[n], dtype=np.float32)
    p64, _ = _partner_index(64)
    gq = np.asarray(inputs["gqa_g_q"], np.float32)
    gk = np.asarray(inputs["gqa_g_k"], np.float32)
    cols = np.zeros((DEPTH, 128, 4), np.float32)
    for l in range(DEPTH):
        cols[l, :, 0] = np.concatenate([gq[l], gq[l]])
        cols[l, :, 1] = np.concatenate([gq[l][p64], gq[l][p64]])
        cols[l, :, 2] = np.concatenate([gk[l], gk[l]])
        cols[l, :, 3] = np.concatenate([gk[l][p64], gk[l][p64]])
    w["gq_cols"] = cols
    w.update(ssm_host_layouts(inputs))
    return w


def make_in_maps(inputs, xs_per_core):
    w = _weights_host(inputs)
    cst = host_constants()
    c = np.asarray(inputs["c"], np.float32)
    c_ctx = np.asarray(inputs["c_ctx"], np.float32)
    maps = []
    for i in range(NCORES):
        cv = np.stack([c[2 * i], c[2 * i + 1], c_ctx], -1)
        cT = np.ascontiguousarray(cv.reshape(8, 128, 3).transpose(1, 0, 2))
        m = {"xs": xs_per_core[i], "cT": cT}
        m.update(w)
        m.update(cst)
        maps.append(m)
    return maps


def kernel(**inputs):
    x = np.asarray(inputs["x"], np.float32)
    ctx = np.asarray(inputs["ctx"], np.float32)
    xs = [np.ascontiguousarray(np.concatenate([ctx[2 * i:2 * i + 2], x[2 * i:2 * i + 2]], axis=1)) for i in range(NCORES)]
    key = "fused"
    if key not in _CACHE:
        _CACHE[key] = build_program(list(range(DEPTH)), True)[0]
    nc = _CACHE[key]
    res = run_bass_kernel_spmd(nc, make_in_maps(inputs, xs), core_ids=list(range(NCORES)))
    out = np.concatenate([r["y"] for r in res.results], axis=0)
    return out.astype(np.float32)


HY_BANDS = 16
HY_DECAY = (math.log(1e-2) / 1.5, math.log(1e-2) / 0.3)


class HyCfg:
    def __init__(self, name, n, ni):
        self.name = name
        self.n = n
        self.ni = ni
        self.N = 2 * n
        self.cpg = 128 // ni
        self.ng = 256 // self.cpg
        self.ncol = 256 * ni


HY_LAT = HyCfg("L", 4096, 32)
HY_CTX = HyCfg("C", 256, 2)


def _hy_features(n, pos):
    f32 = np.float32
    t = np.linspace(0.0, 1.0, n, dtype=f32)
    omega = (f32(2.0 * math.pi) * np.arange(n, dtype=f32) / f32(n)).astype(f32)
    bands = np.linspace(1e-4, HY_BANDS - 1, HY_BANDS, dtype=f32)
    tt = t[pos]
    om = omega[pos]
    ang = (bands[:, None] * om[None, :]).astype(f32)
    z = np.concatenate([tt[None, :], np.cos(ang), -np.sin(ang)], axis=0).astype(f32)
    return z, tt


def hyena_constants():
    cst = {}
    f64 = np.float64
    j = np.arange(128)[:, None]
    b = np.arange(256)[None, :]
    ang = 2 * np.pi * j * b / 256.0
    F1 = np.concatenate([np.cos(ang), -np.sin(ang)], 1)
    sgn = np.where(b % 2 == 0, 1.0, -1.0)
    F1b = np.concatenate([np.cos(ang) * sgn, -np.sin(ang) * sgn], 1)
    cst["hk_F1"] = np.stack([F1, F1b], 0).astype(np.float32)
    for cfg in (HY_LAT, HY_CTX):
        p = np.arange(128)
        i_of_p = (p // cfg.cpg)[:, None]
        ang = 2 * np.pi * i_of_p * b / cfg.N
        TW = np.stack([np.concatenate([np.cos(ang), np.cos(ang)], 1), np.concatenate([-np.sin(ang), -np.sin(ang)], 1)], 0)
        cst["hk_TW" + cfg.name] = TW.astype(np.float32)
        ii = (p // cfg.cpg)[:, None]
        ci = (p % cfg.cpg)[:, None]
        aa = (p // cfg.cpg)[None, :]
        ca = (p % cfg.cpg)[None, :]
        dl = (ci == ca).astype(f64)
        ang = 2 * np.pi * ii * aa / cfg.ni
        Gr = dl * np.cos(ang)
        Gi = -dl * np.sin(ang)
        cst["hk_G" + cfg.name] = np.stack([Gr, Gi, -Gi], 0).astype(np.float32)
        ang = 2 * np.pi * aa.T * ii.T / cfg.ni
        dl2 = (ci.T == ca.T)
        angm = 2 * np.pi * (p // cfg.cpg)[:, None] * (p // cfg.cpg)[None, :] / cfg.ni
        dlm = ((p % cfg.cpg)[:, None] == (p % cfg.cpg)[None, :]).astype(f64)
        GIr = dlm * np.cos(angm)
        GIi = dlm * np.sin(angm)
        cst["hk_GI" + cfg.name] = np.stack([np.concatenate([GIr, GIi], 1), np.concatenate([-GIi, GIr], 1)], 0).astype(np.float32)
        TWI = np.zeros((2, 2, 128, 256), f64)
        for bc in range(2):
            bb = (bc * 128 + np.arange(128))[:, None]
            ang = 2 * np.pi * bb * (p // cfg.cpg)[None, :] / cfg.N
            TWI[bc, 0] = np.concatenate([np.cos(ang), np.cos(ang)], 1)
            TWI[bc, 1] = np.concatenate([np.sin(ang), np.sin(ang)], 1)
        cst["hk_TWI" + cfg.name] = TWI.astype(np.float32)
        FI = np.zeros((2, 2, 128, 128), f64)
        for bc in range(2):
            bb = (bc * 128 + np.arange(128))[:, None]
            ang = 2 * np.pi * bb * np.arange(128)[None, :] / 256.0
            FI[bc, 0] = np.cos(ang) / cfg.N
            FI[bc, 1] = -np.sin(ang) / cfg.N
        cst["hk_FI" + cfg.name] = FI.astype(np.float32)
        n = cfg.n
        u = np.arange(n)
        posr = np.where(u == 0, 0, n - u)
        zf, tf_ = _hy_features(n, u)
        zb, tb_ = _hy_features(n, posr)
        cst["hk_Z" + cfg.name] = np.stack([zf, zb], 0)
        deltas = np.abs(np.linspace(HY_DECAY[0], HY_DECAY[1], 256, dtype=np.float32))
        decf = np.exp(-tf_[:, None] * deltas[None, :]).astype(np.float32)
        decb = np.exp(-tb_[:, None] * deltas[None, :]).astype(np.float32)
        cst["hk_DEC" + cfg.name] = np.stack([decf.reshape(128, cfg.ni, 256), decb.reshape(128, cfg.ni, 256)], 0)
        msk = (p[:, None] % cfg.cpg == np.arange(cfg.cpg)[None, :]).astype(np.float32)
        cst["hk_MSK" + cfg.name] = msk
    return cst


def _hy_twiddle(k, A_ps, t_A, TW, t_TW, m1, m2, t_m, outr, outi, t_out, n2, sub_first=True):
    k.op("dve", lambda en: en.tensor_tensor(out=m1[:, 0:2 * n2], in0=A_ps, in1=TW[:, 0, 0:2 * n2], op=ALU.mult), reads=[t_A, t_TW], writes=[t_m])
    k.op("dve", lambda en: en.tensor_tensor(out=m2[:, 0:2 * n2], in0=A_ps, in1=TW[:, 1, 0:2 * n2], op=ALU.mult), reads=[t_A, t_TW, t_m], writes=[t_m])
    k.op("pool", lambda en: en.tensor_tensor(out=outr, in0=m1[:, 0:n2], in1=m2[:, n2:2 * n2], op=ALU.subtract), reads=[t_m], writes=[t_out])
    k.op("pool", lambda en: en.tensor_tensor(out=outi, in0=m2[:, 0:n2], in1=m1[:, n2:2 * n2], op=ALU.add), reads=[t_m, t_out], writes=[t_out])


def phase_hy_filter(g, l, cfg):
    nc, k, W, K = g.nc, g.k, g.W, g.K
    n, ni, ng, cpg, ncol = cfg.n, cfg.ni, cfg.ng, cfg.cpg, cfg.ncol
    KH = g.KHAT[cfg.name]
    with ExitStack() as es:
        sb = lambda nm, s_, d=F32: es.enter_context(nc.sbuf_tensor(_nm(nm), s_, d))
        ps = lambda nm, s_, d=F32: es.enter_context(nc.psum_tensor(_nm(nm), s_, d))
        t_w = Tok()
        w1 = sb("hw1", [33, 64]); w2 = sb("hw2", [64, 64]); w3 = sb("hw3", [64, 1024])
        cols = sb("hcols", [64, 8])
        k.dma("sp", w1[:], W["hy_f_w1"][l], writes=[t_w])
        k.dma("sp", w2[:], W["hy_f_w2"][l], writes=[t_w])
        k.dma("sp", w3[:], W["hy_f_w3"][l], writes=[t_w])
        for ci, nm in enumerate(("hy_f_b1", "hy_f_freq1", "hy_f_b2", "hy_f_freq2")):
            k.dma("sp", cols[:, ci:ci + 1], W[nm][l].rearrange("(p o) -> p o", o=1), writes=[t_w])
        k.op("dve", lambda en: en.tensor_tensor(out=cols[:, 4:5], in0=cols[:, 0:1], in1=cols[:, 1:2], op=ALU.mult), reads=[t_w], writes=[t_w])
        k.op("dve", lambda en: en.tensor_tensor(out=cols[:, 5:6], in0=cols[:, 2:3], in1=cols[:, 3:4], op=ALU.mult), reads=[t_w], writes=[t_w])
        F1 = sb("hF1", [128, 2, 512]); TW = sb("hTW", [128, 2, 512]); G3 = sb("hG", [128, 3, 128]); MSK = sb("hmsk", [128, cpg])
        t_c = Tok()
        k.dma("sp", F1[:], K["hk_F1"].rearrange("a p n -> p a n"), writes=[t_c])
        k.dma("sp", TW[:], K["hk_TW" + cfg.name].rearrange("a p n -> p a n"), writes=[t_c])
        k.dma("sp", G3[:], K["hk_G" + cfg.name].rearrange("a p n -> p a n"), writes=[t_c])
        k.dma("sp", MSK[:], K["hk_MSK" + cfg.name], writes=[t_c])
        epsc = sb("hyeps", [128, 1])
        k.op("pool", lambda en: en.memset(epsc[:], EPS), writes=[t_c])
        h2T = [sb("h2T%d" % d_, [64, n]) for d_ in range(2)]
        t_h2 = Tok()
        PSm = [ps("hps%d" % i, [128, 512]) for i in range(4)]
        TP = [Tok(True) for _ in range(4)]
        PI = math.pi

        def sin_layer(out_ap, ps_ap, t_ps, fr_col, fb_col, tmp, msk_, t_tmp, wtok):
            k.op("dve", lambda en: en.tensor_scalar(out=tmp, in0=ps_ap, scalar1=fr_col, scalar2=fb_col, op0=ALU.mult, op1=ALU.add), reads=[t_ps, t_w], writes=[t_tmp])
            k.op("dve", lambda en: en.tensor_scalar(out=msk_, in0=tmp, scalar1=PI, scalar2=-2 * PI, op0=ALU.is_gt, op1=ALU.mult), reads=[t_tmp], writes=[t_tmp])
            k.op("dve", lambda en: en.tensor_tensor(out=tmp, in0=tmp, in1=msk_, op=ALU.add), reads=[t_tmp], writes=[t_tmp])
            k.op("dve", lambda en: en.tensor_scalar(out=msk_, in0=tmp, scalar1=-PI, scalar2=2 * PI, op0=ALU.is_lt, op1=ALU.mult), reads=[t_tmp], writes=[t_tmp])
            k.op("dve", lambda en: en.tensor_tensor(out=tmp, in0=tmp, in1=msk_, op=ALU.add), reads=[t_tmp], writes=[t_tmp])
            k.op("act", lambda en: en.activation(out=out_ap, in_=tmp, func=AF.Sin), reads=[t_tmp], writes=[wtok])

        with ExitStack() as es2:
            sb2 = lambda nm, s_, d=F32: es2.enter_context(nc.sbuf_tensor(_nm(nm), s_, d))
            Zt = sb2("hZ", [33, n])
            t_z = Tok()
            h1 = sb2("hh1", [64, 512]); tmp = sb2("htmp", [64, 512]); msk_ = sb2("hmk", [64, 512])
            t_h1 = Tok(); t_tmp = Tok()
            bw = min(512, n)
            for d_ in range(2):
                k.dma("sp", Zt[:], K["hk_Z" + cfg.name][d_], reads=[], writes=[t_z])
                for b0 in range(0, n, bw):
                    k.op("pe", lambda en: en.matmul(PSm[0][0:64, 0:bw], lhsT=w1[:], rhs=Zt[:, b0:b0 + bw], start=True, stop=True), reads=[t_w, t_z], writes=[TP[0]])
                    sin_layer(h1[:, 0:bw], PSm[0][0:64, 0:bw], TP[0], cols[:, 1:2], cols[:, 4:5], tmp[:, 0:bw], msk_[:, 0:bw], t_tmp, t_h1)
                    k.op("pe", lambda en: en.matmul(PSm[1][0:64, 0:bw], lhsT=w2[:], rhs=h1[:, 0:bw], start=True, stop=True), reads=[t_w, t_h1], writes=[TP[1]])
                    sin_layer(h2T[d_][:, b0:b0 + bw], PSm[1][0:64, 0:bw], TP[1], cols[:, 3:4], cols[:, 5:6], tmp[:, 0:bw], msk_[:, 0:bw], t_tmp, t_h2)
            k.barrier()
        UF = [sb("hUF%d" % d_, [128, ncol]) for d_ in range(2)]
        t_uf = Tok()
        DEC = sb("hDEC", [128, ni, 256])
        t_dec = Tok()
        HSQ = sb("hHSQ", [128, 256]); sq = sb("hsq", [128, 256]); hh = sb("hhh", [128, 256])
        t_hsq = Tok(); t_hh = Tok(); t_sq = Tok()
        RS = sb("hRS", [128, 256]); SC = sb("hSC", [128, ng]); rtmp = sb("hrtmp", [128, 256])
        t_rs = Tok()
        fmm_r = Rot([(sb("hm1_%d" % i, [128, 512]), sb("hm2_%d" % i, [128, 512])) for i in range(3)])
        fP1_r = Rot([sb("hP1_%d" % i, [128, 2, 256]) for i in range(3)])
        ko_r = Rot([sb("hko%d" % i, [128, 512]) for i in range(3)])
        for o in range(2):
            k.op("pool", lambda en: en.memset(HSQ[:], 0.0), reads=[t_rs], writes=[t_hsq])
            for d_ in range(2):
                k.dma("sp", DEC[:], K["hk_DEC" + cfg.name][d_], writes=[t_dec])
                c0 = o * 512 + d_ * 256
                for i in range(ni):
                    pi_ = i % 2
                    k.op("pe", lambda en: en.matmul(PSm[pi_][:, 0:256], lhsT=h2T[d_][:, i:n:ni], rhs=w3[:, c0:c0 + 256], start=True, stop=True), reads=[t_h2, t_w], writes=[TP[pi_]])
                    k.op("dve", lambda en: en.tensor_tensor(out=hh[:], in0=PSm[pi_][:, 0:256], in1=DEC[:, i, :], op=ALU.mult), reads=[TP[pi_], t_dec], writes=[t_hh])
                    k.op("act", lambda en: en.activation(out=UF[d_][:].rearrange("p (g i c) -> p g i c", i=ni, c=cpg)[:, :, i, :], in_=hh[:].rearrange("p (g c) -> p g c", c=cpg), func=AF.Copy), reads=[t_hh], writes=[t_uf])
                    k.op("act", lambda en: en.activation(out=sq[:], in_=hh[:], func=AF.Square), reads=[t_hh], writes=[t_sq])
                    k.op("pool", lambda en: en.tensor_tensor(out=HSQ[:], in0=HSQ[:], in1=sq[:], op=ALU.add), reads=[t_sq, t_hsq], writes=[t_hsq])
            k.op("pe", lambda en: en.matmul(PSm[2][:, 0:256], lhsT=g.onesf[:], rhs=HSQ[:], start=True, stop=True), reads=[t_hsq, g.T_const], writes=[TP[2]])
            k.op("act", lambda en: en.activation(out=rtmp[:], in_=PSm[2][:, 0:256], func=AF.Ln, bias=epsc[:, 0:1]), reads=[TP[2], t_c], writes=[t_rs])
            k.op("act", lambda en: en.activation(out=RS[:], in_=rtmp[:], func=AF.Exp, scale=-0.5), reads=[t_rs], writes=[t_rs])
            k.op("dve", lambda en: en.tensor_tensor(out=rtmp[:].rearrange("p (g c) -> p g c", c=cpg), in0=RS[:].rearrange("p (g c) -> p g c", c=cpg), in1=MSK[:].unsqueeze(1).broadcast_to([128, ng, cpg]), op=ALU.mult), reads=[t_rs, t_c], writes=[t_rs])
            k.op("dve", lambda en: en.tensor_reduce(out=SC[:], in_=rtmp[:].rearrange("p (g c) -> p g c", c=cpg), axis=AX.X, op=ALU.add), reads=[t_rs], writes=[t_rs])
            k.op("dve", lambda en: en.memset(UF[1][0:1, :].rearrange("p (g i c) -> p g i c", i=ni, c=cpg)[:, :, 0, :], 0.0), reads=[t_uf], writes=[t_uf])
            fst = {}

            def F_S1(grp):
                b0 = grp % 2
                (m1_, m2_), t_m_ = fmm_r.next()
                P1_, t_p1_ = fP1_r.next()
                fst[grp] = (P1_, t_p1_)
                k.op("pe", lambda en: en.matmul(PSm[b0][:], lhsT=UF[0][:, grp * 128:(grp + 1) * 128], rhs=F1[:, 0, :], start=True, stop=False), reads=[t_uf, t_c], writes=[TP[b0]])
                k.op("pe", lambda en: en.matmul(PSm[b0][:], lhsT=UF[1][:, grp * 128:(grp + 1) * 128], rhs=F1[:, 1, :], start=False, stop=True), reads=[t_uf, t_c], writes=[TP[b0]])
                _hy_twiddle(k, PSm[b0][:], TP[b0], TW, t_c, m1_, m2_, t_m_, P1_[:, 0, :], P1_[:, 1, :], t_p1_, 256)

            def F_S2(grp):
                b1 = 2 + grp % 2
                P1_, t_p1_ = fst.pop(grp)
                k.op("pe", lambda en: en.matmul(PSm[b1][:, 0:256], lhsT=G3[:, 0, :], rhs=P1_[:, 0, :], start=True, stop=False), reads=[t_p1_, t_c], writes=[TP[b1]])
                k.op("pe", lambda en: en.matmul(PSm[b1][:, 0:256], lhsT=G3[:, 2, :], rhs=P1_[:, 1, :], start=False, stop=True), reads=[t_p1_, t_c], writes=[TP[b1]])
                k.op("pe", lambda en: en.matmul(PSm[b1][:, 256:512], lhsT=G3[:, 0, :], rhs=P1_[:, 1, :], start=False, stop=False), reads=[t_p1_, t_c], writes=[TP[b1]])
                k.op("pe", lambda en: en.matmul(PSm[b1][:, 256:512], lhsT=G3[:, 1, :], rhs=P1_[:, 0, :], start=False, stop=True), reads=[t_p1_, t_c], writes=[TP[b1]])
                ko, t_ko = ko_r.next()
                k.op("dve", lambda en: en.tensor_scalar(out=ko[:], in0=PSm[b1][:], scalar1=SC[:, grp:grp + 1], scalar2=None, op0=ALU.mult), reads=[TP[b1], t_rs], writes=[t_ko])
                k.dma("pool", KH[o, grp], ko[:], reads=[t_ko], writes=[g.T_KHAT])

            for t_ in range(ng + 1):
                if t_ < ng:
                    F_S1(t_)
                if t_ >= 1:
                    F_S2(t_ - 1)
        k.barrier()


def phase_hyena(g, l, s, cfg, tok0):
    nc, k, W, K = g.nc, g.k, g.W, g.K
    n, ni, ng, cpg, ncol = cfg.n, cfg.ni, cfg.ng, cfg.cpg, cfg.ncol
    KH = g.KHAT[cfg.name]
    with ExitStack() as es:
        sb = lambda nm, s_, d=F32: es.enter_context(nc.sbuf_tensor(_nm(nm), s_, d))
        ps = lambda nm, s_, d=F32: es.enter_context(nc.psum_tensor(_nm(nm), s_, d))
        PSm = [ps("yps%d" % i, [128, 512]) for i in range(6)]
        TP = [Tok(True) for _ in range(6)]
        PT = [ps("ypt%d" % i, [128, 8, 128], BF16) for i in range(2)]
        TPT = [Tok(True) for _ in range(2)]
        t_c = Tok()
        cf = sb("ycf", [128, 1024])
        F1b = sb("yF1", [128, 512], BF16); G3 = sb("yG", [128, 3, 128], BF16); GI = sb("yGI", [128, 2, 256], BF16); FI = sb("yFI", [128, 2, 2, 128], BF16)
        TW = sb("yTW", [128, 2, 512]); TWI = sb("yTWI", [128, 2, 2, 256])
        k.dma("sp", cf[:, 0:512], K["hk_F1"][0], writes=[t_c])
        k.op("dve", lambda en: en.tensor_copy(out=F1b[:], in_=cf[:, 0:512]), reads=[t_c], writes=[t_c])
        k.dma("sp", cf[:, 0:384].rearrange("p (a n) -> p a n", a=3), K["hk_G" + cfg.name].rearrange("a p n -> p a n"), reads=[t_c], writes=[t_c])
        k.op("dve", lambda en: en.tensor_copy(out=G3[:], in_=cf[:, 0:384].rearrange("p (a n) -> p a n", a=3)), reads=[t_c], writes=[t_c])
        k.dma("sp", cf[:, 0:512].rearrange("p (a n) -> p a n", a=2), K["hk_GI" + cfg.name].rearrange("a p n -> p a n"), reads=[t_c], writes=[t_c])
        k.op("dve", lambda en: en.tensor_copy(out=GI[:], in_=cf[:, 0:512].rearrange("p (a n) -> p a n", a=2)), reads=[t_c], writes=[t_c])
        k.dma("sp", cf[:, 0:512].rearrange("p (b a n) -> p b a n", b=2, a=2), K["hk_FI" + cfg.name].rearrange("b a p n -> p b a n"), reads=[t_c], writes=[t_c])
        k.op("dve", lambda en: en.tensor_copy(out=FI[:], in_=cf[:, 0:512].rearrange("p (b a n) -> p b a n", b=2, a=2)), reads=[t_c], writes=[t_c])
        k.dma("sp", TW[:], K["hk_TW" + cfg.name].rearrange("a p n -> p a n"), writes=[t_c])
        k.dma("sp", TWI[:], K["hk_TWI" + cfg.name].rearrange("b a p n -> p b a n"), writes=[t_c])
        cw = sb("ycw", [128, 6, 3]); cb = sb("ycb", [128, 6]); BR = sb("yBR", [128, 2, 256])
        for kk in range(3):
            k.dma("sp", cw[:, :, kk:kk + 1], W["hy_conv_w"][l, kk, :].rearrange("(c p o) -> p c o", p=128, o=1), writes=[t_c], allow_slow_non_contiguous=True)
        k.dma("sp", cb[:].unsqueeze(2), W["hy_conv_b"][l].rearrange("(c p o) -> p c o", p=128, o=1), writes=[t_c], allow_slow_non_contiguous=True)
        k.dma("sp", BR[:], W["hy_bias"][l:l + 1].broadcast_to([128, 2, 256]), writes=[t_c])
        U = {nm: sb("yU" + nm, [128, ncol], BF16) for nm in ("gt", "x2", "v", "x1")}
        t_U = {nm: Tok() for nm in ("gt", "x2", "v", "x1", "z1")}
        names = ["v", "v", "x1", "x1", "x2", "x2", "gt", "gt"]
        with ExitStack() as es1:
            sb1 = lambda nm, s_, d=F32: es1.enter_context(nc.sbuf_tensor(_nm(nm), s_, d))
            hT = sb1("yhT", [128, 8, n], BF16)
            whb_r = Rot([sb1("ywhb%d" % i, [128, 8, 128], BF16) for i in range(2)])
            t_h = Tok()
            k.dma("sp", hT[:], g.HT[:, :, tok0:tok0 + n], reads=[g.T_HT], writes=[t_h])
            PJ = sb1("yPJ", [128, n]); CV = sb1("yCV", [128, n]); CVb = sb1("yCVb", [128, n], BF16)
            t_pj = Tok(); t_cv = Tok(); t_cvb = Tok()
            bw = min(512, n)
            for c8 in range(8):
                whb, t_whb = whb_r.next()
                k.dma("sp", whb[:], g.WINB[:, :, O_HY + c8 * 128:O_HY + (c8 + 1) * 128], reads=[g.T_WINB], writes=[t_whb])
                for bi, b0 in enumerate(range(0, n, bw)):
                    pi_ = bi % 2
                    for kc in range(8):
                        k.op("pe", lambda en: en.matmul(PSm[pi_][:, 0:bw], lhsT=whb[:, kc, :], rhs=hT[:, kc, b0:b0 + bw], start=(kc == 0), stop=(kc == 7)), reads=[t_h, t_whb], writes=[TP[pi_]])
                    if c8 < 6:
                        k.op("act", lambda en: en.activation(out=PJ[:, b0:b0 + bw], in_=PSm[pi_][:, 0:bw], func=AF.Copy), reads=[TP[pi_]], writes=[t_pj])
                    else:
                        k.op("act", lambda en: en.activation(out=CVb[:, b0:b0 + bw], in_=PSm[pi_][:, 0:bw], func=AF.Silu), reads=[TP[pi_]], writes=[t_cvb])
                if c8 < 6:
                    k.op("dve", lambda en: en.tensor_scalar(out=CV[:], in0=PJ[:], scalar1=cw[:, c8, 1:2], scalar2=cb[:, c8:c8 + 1], op0=ALU.mult, op1=ALU.add), reads=[t_pj, t_c], writes=[t_cv])
                    k.op("dve", lambda en: en.scalar_tensor_tensor(out=CV[:, 1:n], in0=PJ[:, 0:n - 1], scalar=cw[:, c8, 0:1], in1=CV[:, 1:n], op0=ALU.mult, op1=ALU.add), reads=[t_pj, t_c, t_cv], writes=[t_cv])
                    k.op("dve", lambda en: en.scalar_tensor_tensor(out=CVb[:, 0:n - 1], in0=PJ[:, 1:n], scalar=cw[:, c8, 2:3], in1=CV[:, 0:n - 1], op0=ALU.mult, op1=ALU.add), reads=[t_pj, t_c, t_cv], writes=[t_cvb])
                    k.op("dve", lambda en: en.tensor_copy(out=CVb[:, n - 1:n], in_=CV[:, n - 1:n]), reads=[t_cv, t_cvb], writes=[t_cvb])
                nm = names[c8]
                g0 = (c8 % 2) * (128 // cpg)
                Uv = U[nm][:].rearrange("p (g i c) -> p g i c", i=ni, c=cpg)
                nb4 = min(4, ni)
                for i0 in range(0, ni, nb4):
                    pt = (i0 // nb4) % 2
                    for ii in range(nb4):
                        k.op("pe", lambda en: en.transpose(PT[pt][:, ii, :], CVb[:, i0 + ii:n:ni], g.ident[:]), reads=[t_cvb, g.T_ident], writes=[TPT[pt]])
                    k.op("act" if (i0 // nb4) % 2 else "dve",
                         (lambda en: en.activation(out=Uv[:, g0:g0 + 128 // cpg, i0:i0 + nb4, :].rearrange("p g i c -> p i g c"), in_=PT[pt][:, 0:nb4, :].rearrange("p i (g c) -> p i g c", c=cpg), func=AF.Copy)) if (i0 // nb4) % 2 else
                         (lambda en: en.tensor_copy(out=Uv[:, g0:g0 + 128 // cpg, i0:i0 + nb4, :].rearrange("p g i c -> p i g c"), in_=PT[pt][:, 0:nb4, :].rearrange("p i (g c) -> p i g c", c=cpg))),
                         reads=[TPT[pt]], writes=[t_U[nm]])
            k.barrier()
        with ExitStack() as es2:
            sb2 = lambda nm, s_, d=F32: es2.enter_context(nc.sbuf_tensor(_nm(nm), s_, d))
            U["z1"] = sb2("yUz1", [128, ncol], BF16)
            BW = [[sb2("yBW%d%d" % (bc, ri), [128, ncol], BF16) for ri in range(2)] for bc in range(2)]
            t_bw = Tok()
            mm_r = Rot([(sb2("ym1_%d" % i, [128, 512]), sb2("ym2_%d" % i, [128, 512])) for i in range(5)])
            P1_r = Rot([sb2("yP1_%d" % i, [128, 2, 256], BF16) for i in range(3)])
            Y_r = Rot([sb2("yY_%d" % i, [128, 2, 256], BF16) for i in range(3)])
            kh_r = Rot([sb2("ykh%d" % i, [128, 512]) for i in range(3)])
            e1 = sb2("ye1", [128, 512]); e2 = sb2("ye2", [128, 512]); e3 = sb2("ye3", [128, 512]); t_e = Tok()
            gpt = 512 // (ni * cpg)

            def conv(src_nm, o, epilogue):
                Uin = U[src_nm]
                st = {}

                def S1(grp):
                    s1 = grp % 2
                    (m1, m2), t_m = mm_r.next()
                    P1, t_p1 = P1_r.next()
                    st[grp] = [P1, t_p1]
                    k.op("pe", lambda en: en.matmul(PSm[s1][:], lhsT=Uin[:, grp * 128:(grp + 1) * 128], rhs=F1b[:], start=True, stop=True), reads=[t_U[src_nm], t_c], writes=[TP[s1]])
                    _hy_twiddle(k, PSm[s1][:], TP[s1], TW, t_c, m1, m2, t_m, P1[:, 0, :], P1[:, 1, :], t_p1, 256)

                def S2(grp):
                    sx = 2 + grp % 2
                    P1, t_p1 = st[grp]
                    kh, t_kh = kh_r.next()
                    k.dma("sp", kh[:], KH[o, grp], reads=[g.T_KHAT], writes=[t_kh])
                    Y, t_y = Y_r.next()
                    st[grp] = [Y, t_y]
                    k.op("pe", lambda en: en.matmul(PSm[sx][:, 0:256], lhsT=G3[:, 0, :], rhs=P1[:, 0, :], start=True, stop=False), reads=[t_p1, t_c], writes=[TP[sx]])
                    k.op("pe", lambda en: en.matmul(PSm[sx][:, 0:256], lhsT=G3[:, 2, :], rhs=P1[:, 1, :], start=False, stop=True), reads=[t_p1, t_c], writes=[TP[sx]])
                    k.op("pe", lambda en: en.matmul(PSm[sx][:, 256:512], lhsT=G3[:, 0, :], rhs=P1[:, 1, :], start=False, stop=False), reads=[t_p1, t_c], writes=[TP[sx]])
                    k.op("pe", lambda en: en.matmul(PSm[sx][:, 256:512], lhsT=G3[:, 1, :], rhs=P1[:, 0, :], start=False, stop=True), reads=[t_p1, t_c], writes=[TP[sx]])
                    (m1, m2), t_m = mm_r.next()
                    Xv = PSm[sx][:].rearrange("p (a n) -> p a n", a=2)
                    k.op("dve", lambda en: en.tensor_tensor(out=m1[:].rearrange("p (a n) -> p a n", a=2), in0=Xv, in1=kh[:, 0:256].unsqueeze(1).broadcast_to([128, 2, 256]), op=ALU.mult), reads=[TP[sx], t_kh], writes=[t_m])
                    k.op("dve", lambda en: en.tensor_tensor(out=m2[:].rearrange("p (a n) -> p a n", a=2), in0=Xv, in1=kh[:, 256:512].unsqueeze(1).broadcast_to([128, 2, 256]), op=ALU.mult), reads=[TP[sx], t_kh, t_m], writes=[t_m])
                    k.op("pool", lambda en: en.tensor_tensor(out=Y[:, 0, :], in0=m1[:, 0:256], in1=m2[:, 256:512], op=ALU.subtract), reads=[t_m], writes=[t_y])
                    k.op("pool", lambda en: en.tensor_tensor(out=Y[:, 1, :], in0=m2[:, 0:256], in1=m1[:, 256:512], op=ALU.add), reads=[t_m, t_y], writes=[t_y])

                def S3(grp):
                    Y, t_y = st.pop(grp)
                    for bc in range(2):
                        pi_ = 4 + bc
                        (m1, m2), t_m = mm_r.next()
                        k.op("pe", lambda en: en.matmul(PSm[pi_][:, 0:256], lhsT=Y[:, 0, bc * 128:(bc + 1) * 128], rhs=GI[:, 0, :], start=True, stop=False), reads=[t_y, t_c], writes=[TP[pi_]])
                        k.op("pe", lambda en: en.matmul(PSm[pi_][:, 0:256], lhsT=Y[:, 1, bc * 128:(bc + 1) * 128], rhs=GI[:, 1, :], start=False, stop=True), reads=[t_y, t_c], writes=[TP[pi_]])
                        k.op("dve", lambda en: en.tensor_tensor(out=m1[:, 0:256], in0=PSm[pi_][:, 0:256], in1=TWI[:, bc, 0, :], op=ALU.mult), reads=[TP[pi_], t_c], writes=[t_m])
                        k.op("dve", lambda en: en.tensor_tensor(out=m2[:, 0:256], in0=PSm[pi_][:, 0:256], in1=TWI[:, bc, 1, :], op=ALU.mult), reads=[TP[pi_], t_c, t_m], writes=[t_m])
                        k.op("pool", lambda en: en.tensor_tensor(out=BW[bc][0][:, grp * 128:(grp + 1) * 128], in0=m1[:, 0:128], in1=m2[:, 128:256], op=ALU.subtract), reads=[t_m], writes=[t_bw])
                        k.op("pool", lambda en: en.tensor_tensor(out=BW[bc][1][:, grp * 128:(grp + 1) * 128], in0=m2[:, 0:128], in1=m1[:, 128:256], op=ALU.add), reads=[t_m, t_bw], writes=[t_bw])

                for t_ in range(ng + 2):
                    if t_ < ng:
                        S1(t_)
                    if 0 <= t_ - 1 < ng:
                        S2(t_ - 1)
                    if 0 <= t_ - 2 < ng:
                        S3(t_ - 2)
                for ct in range(ncol // 512):
                    pi_ = 4 + ct % 2
                    cs = slice(ct * 512, (ct + 1) * 512)
                    idx = 0
                    for bc in range(2):
                        for ri in range(2):
                            k.op("pe", lambda en: en.matmul(PSm[pi_][:], lhsT=FI[:, bc, ri, :], rhs=BW[bc][ri][:, cs], start=(idx == 0), stop=(idx == 3)), reads=[t_bw, t_c], writes=[TP[pi_]])
                            idx += 1
                    epilogue(ct, cs, PSm[pi_], TP[pi_], o)

            def v4(ap):
                return ap.rearrange("p (g i c) -> p g i c", i=ni, c=cpg)

            def epi1(ct, cs, ps_, tps, o):
                bview = BR[:, o, ct * gpt * cpg:(ct + 1) * gpt * cpg].rearrange("p (g c) -> p g c", c=cpg).unsqueeze(2).broadcast_to([128, gpt, ni, cpg])
                k.op("pool", lambda en: en.tensor_tensor(out=v4(e1[:]), in0=v4(U["v"][:, cs]), in1=bview, op=ALU.mult), reads=[t_U["v"], t_c, t_e], writes=[t_e])
                k.op("dve", lambda en: en.tensor_tensor(out=e2[:], in0=ps_[:], in1=e1[:], op=ALU.add), reads=[tps, t_e], writes=[t_e])
                k.op("pool", lambda en: en.tensor_tensor(out=U["z1"][:, cs], in0=e2[:], in1=U["x1"][:, cs], op=ALU.mult), reads=[t_e, t_U["x1"]], writes=[t_U["z1"]])

            ZN = U["v"]

            def epi2(ct, cs, ps_, tps, o):
                bview = BR[:, o, ct * gpt * cpg:(ct + 1) * gpt * cpg].rearrange("p (g c) -> p g c", c=cpg).unsqueeze(2).broadcast_to([128, gpt, ni, cpg])
                k.op("pool", lambda en: en.tensor_tensor(out=v4(e1[:]), in0=v4(U["z1"][:, cs]), in1=bview, op=ALU.mult), reads=[t_U["z1"], t_c, t_e], writes=[t_e])
                k.op("dve", lambda en: en.tensor_tensor(out=e2[:], in0=ps_[:], in1=e1[:], op=ALU.add), reads=[tps, t_e], writes=[t_e])
                k.op("pool", lambda en: en.tensor_tensor(out=e3[:], in0=e2[:], in1=U["x2"][:, cs], op=ALU.mult), reads=[t_e, t_U["x2"]], writes=[t_e])
                outv = ZN[:].rearrange("p (i g c) -> p g i c", g=ng, c=cpg)[:, ct * gpt:(ct + 1) * gpt, :, :]
                k.op("dve", lambda en: en.tensor_tensor(out=outv, in0=v4(e3[:]), in1=v4(U["gt"][:, cs]), op=ALU.mult), reads=[t_e, t_U["gt"]], writes=[t_U["v"]])

            conv("v", 0, epi1)
            conv("z1", 1, epi2)
            ZT = U["x1"][:].rearrange("p (h t) -> p h t", h=2)
            cnt = 0
            for ch in range(2):
                nb4 = min(4, ni)
                for i0 in range(0, ni, nb4):
                    pt = cnt % 2
                    cnt += 1
                    for ii in range(nb4):
                        i = i0 + ii
                        k.op("pe", lambda en: en.transpose(PT[pt][:, ii, :], ZN[:, i * 256 + ch * 128:i * 256 + ch * 128 + 128], g.ident[:]), reads=[t_U["v"], g.T_ident], writes=[TPT[pt]])
                    outv = ZT[:, ch, :].rearrange("p (j i) -> p i j", i=ni)[:, i0:i0 + nb4, :]
                    k.op("act" if cnt % 2 else "dve",
                         (lambda en: en.activation(out=outv, in_=PT[pt][:, 0:nb4, :], func=AF.Copy)) if cnt % 2 else (lambda en: en.tensor_copy(out=outv, in_=PT[pt][:, 0:nb4, :])),
                         reads=[TPT[pt], t_U["x1"]], writes=[t_U["x1"]])
            k.dma("pool", g.CATT[:, 6:8, tok0:tok0 + n], ZT, reads=[t_U["x1"]], writes=[g.T_CATT])
            k.barrier()


TC = 16
NCH = T // TC
NDBL = 9


def ssm_host_layouts(inputs):
    f = lambda n: np.asarray(inputs[n], np.float32)
    lr, li, ls = f("ssm_lambda_re"), f("ssm_lambda_im"), f("ssm_log_step")
    br, bi, cr, ci = f("ssm_b_re"), f("ssm_b_im"), f("ssm_c_re"), f("ssm_c_im")
    lam = np.zeros((DEPTH, 128, 2, 16), np.float32)
    lsb = np.zeros((DEPTH, 128, 16), np.float32)
    Bp = np.zeros((DEPTH, 128, 2, 16, 64), np.float32)
    Cp = np.zeros((DEPTH, 128, 2, 16, 64), np.float32)
    for d in range(2):
        for q in range(8):
            for e in range(2):
                grp = 2 * q + e
                rows = slice(e * 64, (e + 1) * 64)
                dq = d * 8 + q
                lam[:, rows, 0, dq] = lr[:, d, grp, :]
                lam[:, rows, 1, dq] = li[:, d, grp, :]
                lsb[:, rows, dq] = ls[:, d, grp][:, None]
                pp = q % 2
                c0 = pp * 32 + e * 16
                Bp[:, rows, 0, dq, c0:c0 + 16] = br[:, d, grp, :, :]
                Bp[:, rows, 1, dq, c0:c0 + 16] = bi[:, d, grp, :, :]
                Cp[:, rows, 0, dq, c0:c0 + 16] = cr[:, d, grp, :, :].transpose(0, 2, 1)
                Cp[:, rows, 1, dq, c0:c0 + 16] = ci[:, d, grp, :, :].transpose(0, 2, 1)
    out = {"ssm_lam": lam, "ssm_ls": lsb, "ssm_Bp": Bp, "ssm_Cp": Cp}
    dd = f("ssm_d")
    gb = f("ssm_glu_b")
    out["ssm_cols"] = np.ascontiguousarray(np.stack([dd.reshape(DEPTH, 2, 128).transpose(0, 2, 1), gb.reshape(DEPTH, 2, 128).transpose(0, 2, 1)], -1))
    out["ssm_glu_w"] = f("ssm_glu_w")
    return out


def phase_ssm_weights(g, l):
    nc, k, W = g.nc, g.k, g.W
    PI = math.pi
    with ExitStack() as es:
        sb = lambda nm, s_, d=F32: es.enter_context(nc.sbuf_tensor(_nm(nm), s_, d))
        ps = lambda nm, s_, d=F32: es.enter_context(nc.psum_tensor(_nm(nm), s_, d))
        t = Tok()
        lam = sb("slam", [128, 2, 16]); ls = sb("sls", [128, 16])
        Bp = sb("sBp", [128, 2, 16, 64]); Cp = sb("sCp", [128, 2, 16, 64])
        k.dma("sp", lam[:], W["ssm_lam"][l], writes=[t])
        k.dma("sp", ls[:], W["ssm_ls"][l], writes=[t])
        k.dma("sp", Bp[:], W["ssm_Bp"][l], writes=[t])
        k.dma("sp", Cp[:], W["ssm_Cp"][l], writes=[t])
        sc = sb("ssc", [128, 24, 16])
        R = lambda i: sc[:, i, :]
        STEP, RHO, TH, MK, SIN, COS, AR, AI, DEN, NR, NI_, CRc, CIc, T1, T2 = range(15)

        def dv(fn, eng="dve"):
            k.op(eng, fn, reads=[t], writes=[t])

        dv(lambda en: en.activation(out=R(STEP), in_=ls[:], func=AF.Exp), "act")
        dv(lambda en: en.tensor_tensor(out=R(T1), in0=lam[:, 0, :], in1=R(STEP), op=ALU.mult))
        dv(lambda en: en.activation(out=R(RHO), in_=R(T1), func=AF.Exp), "act")
        dv(lambda en: en.tensor_tensor(out=R(TH), in0=lam[:, 1, :], in1=R(STEP), op=ALU.mult))
        for thr in (PI, 3 * PI, 5 * PI, 7 * PI):
            dv(lambda en: en.tensor_scalar(out=R(MK), in0=R(TH), scalar1=thr, scalar2=-2 * PI, op0=ALU.is_gt, op1=ALU.mult))
            if thr == PI:
                dv(lambda en: en.tensor_copy(out=R(T1), in_=R(MK)))
            else:
                dv(lambda en: en.tensor_tensor(out=R(T1), in0=R(T1), in1=R(MK), op=ALU.add))
        dv(lambda en: en.tensor_tensor(out=R(TH), in0=R(TH), in1=R(T1), op=ALU.add))
        dv(lambda en: en.activation(out=R(SIN), in_=R(TH), func=AF.Sin), "act")
        dv(lambda en: en.tensor_scalar(out=R(T2), in0=R(TH), scalar1=-1.0, scalar2=None, op0=ALU.mult))
        dv(lambda en: en.tensor_tensor(out=R(T1), in0=R(TH), in1=R(T2), op=ALU.max))
        dv(lambda en: en.tensor_scalar(out=R(T1), in0=R(T1), scalar1=-1.0, scalar2=PI / 2, op0=ALU.mult, op1=ALU.add))
        dv(lambda en: en.activation(out=R(COS), in_=R(T1), func=AF.Sin), "act")
        dv(lambda en: en.tensor_tensor(out=R(AR), in0=R(RHO), in1=R(COS), op=ALU.mult))
        dv(lambda en: en.tensor_tensor(out=R(AI), in0=R(RHO), in1=R(SIN), op=ALU.mult))
        dv(lambda en: en.tensor_tensor(out=R(DEN), in0=lam[:, 0, :], in1=lam[:, 0, :], op=ALU.mult))
        dv(lambda en: en.tensor_tensor(out=R(T1), in0=lam[:, 1, :], in1=lam[:, 1, :], op=ALU.mult))
        dv(lambda en: en.tensor_tensor(out=R(DEN), in0=R(DEN), in1=R(T1), op=ALU.add))
        dv(lambda en: en.reciprocal(out=R(DEN), in_=R(DEN)))
        dv(lambda en: en.tensor_scalar(out=R(T2), in0=R(AR), scalar1=-1.0, scalar2=None, op0=ALU.add))
        dv(lambda en: en.tensor_tensor(out=R(NR), in0=R(T2), in1=lam[:, 0, :], op=ALU.mult))
        dv(lambda en: en.tensor_tensor(out=R(T1), in0=R(AI), in1=lam[:, 1, :], op=ALU.mult))
        dv(lambda en: en.tensor_tensor(out=R(NR), in0=R(NR), in1=R(T1), op=ALU.add))
        dv(lambda en: en.tensor_tensor(out=R(NI_), in0=R(AI), in1=lam[:, 0, :], op=ALU.mult))
        dv(lambda en: en.tensor_tensor(out=R(T1), in0=R(T2), in1=lam[:, 1, :], op=ALU.mult))
        dv(lambda en: en.tensor_tensor(out=R(NI_), in0=R(NI_), in1=R(T1), op=ALU.subtract))
        dv(lambda en: en.tensor_tensor(out=R(CRc), in0=R(NR), in1=R(DEN), op=ALU.mult))
        dv(lambda en: en.tensor_tensor(out=R(CIc), in0=R(NI_), in1=R(DEN), op=ALU.mult))
        PW = sb("sPW", [128, 2, 17, 16])
        dv(lambda en: en.memset(PW[:, 0, 0, :], 1.0))
        dv(lambda en: en.memset(PW[:, 1, 0, :], 0.0))
        for n_ in range(16):
            dv(lambda en: en.tensor_tensor(out=R(T1), in0=PW[:, 0, n_, :], in1=R(AR), op=ALU.mult))
            dv(lambda en: en.tensor_tensor(out=R(T2), in0=PW[:, 1, n_, :], in1=R(AI), op=ALU.mult))
            dv(lambda en: en.tensor_tensor(out=PW[:, 0, n_ + 1, :], in0=R(T1), in1=R(T2), op=ALU.subtract))
            dv(lambda en: en.tensor_tensor(out=R(T1), in0=PW[:, 0, n_, :], in1=R(AI), op=ALU.mult))
            dv(lambda en: en.tensor_tensor(out=R(T2), in0=PW[:, 1, n_, :], in1=R(AR), op=ALU.mult))
            dv(lambda en: en.tensor_tensor(out=PW[:, 1, n_ + 1, :], in0=R(T1), in1=R(T2), op=ALU.add))
        A2 = g.lw.A2
        dv(lambda en: en.tensor_copy(out=A2[:, 0, 0, :], in_=PW[:, 0, 16, :]))
        dv(lambda en: en.tensor_copy(out=A2[:, 0, 1, :], in_=PW[:, 1, 16, :]))
        for kk in range(NDBL):
            if kk > 0:
                dv(lambda en: en.tensor_tensor(out=R(T1), in0=A2[:, kk - 1, 0, :], in1=A2[:, kk - 1, 0, :], op=ALU.mult))
                dv(lambda en: en.tensor_tensor(out=R(T2), in0=A2[:, kk - 1, 1, :], in1=A2[:, kk - 1, 1, :], op=ALU.mult))
                dv(lambda en: en.tensor_tensor(out=A2[:, kk, 0, :], in0=R(T1), in1=R(T2), op=ALU.subtract))
                dv(lambda en: en.tensor_tensor(out=R(T1), in0=A2[:, kk - 1, 0, :], in1=A2[:, kk - 1, 1, :], op=ALU.mult))
                dv(lambda en: en.tensor_scalar(out=A2[:, kk, 1, :], in0=R(T1), scalar1=2.0, scalar2=None, op0=ALU.mult))
            dv(lambda en: en.tensor_scalar(out=A2[:, kk, 2, :], in0=A2[:, kk, 1, :], scalar1=-1.0, scalar2=None, op0=ALU.mult))
        bc3 = lambda ap: ap.unsqueeze(2).broadcast_to([128, 16, 64])
        Bb = sb("sBb", [128, 2, 16, 64])
        big = [sb("sbig%d" % i, [128, 16, 64]) for i in range(4)]
        dv(lambda en: en.tensor_tensor(out=big[0][:], in0=Bp[:, 0], in1=bc3(R(CRc)), op=ALU.mult))
        dv(lambda en: en.tensor_tensor(out=big[1][:], in0=Bp[:, 1], in1=bc3(R(CIc)), op=ALU.mult))
        dv(lambda en: en.tensor_tensor(out=Bb[:, 0], in0=big[0][:], in1=big[1][:], op=ALU.subtract))
        dv(lambda en: en.tensor_tensor(out=big[0][:], in0=Bp[:, 1], in1=bc3(R(CRc)), op=ALU.mult))
        dv(lambda en: en.tensor_tensor(out=big[1][:], in0=Bp[:, 0], in1=bc3(R(CIc)), op=ALU.mult))
        dv(lambda en: en.tensor_tensor(out=Bb[:, 1], in0=big[0][:], in1=big[1][:], op=ALU.add))
        Bbb = sb("sBbb", [128, 2, 16, 64], BF16)
        dv(lambda en: en.tensor_copy(out=Bbb[:], in_=Bb[:]))
        CR = sb("sCR", [128, 2, 16, 17, 64], BF16)
        engs = ("dve", "pool")
        for n_ in range(17):
            e1 = engs[n_ % 2]
            dv(lambda en: en.tensor_tensor(out=big[0][:], in0=Cp[:, 0], in1=bc3(PW[:, 0, n_, :]), op=ALU.mult), e1)
            dv(lambda en: en.tensor_tensor(out=big[1][:], in0=Cp[:, 1], in1=bc3(PW[:, 1, n_, :]), op=ALU.mult), e1)
            dv(lambda en: en.tensor_tensor(out=CR[:, 0, :, n_, :], in0=big[0][:], in1=big[1][:], op=ALU.subtract), e1)
            dv(lambda en: en.tensor_tensor(out=big[2][:], in0=Cp[:, 0], in1=bc3(PW[:, 1, n_, :]), op=ALU.mult), e1)
            dv(lambda en: en.tensor_tensor(out=big[3][:], in0=Cp[:, 1], in1=bc3(PW[:, 0, n_, :]), op=ALU.mult), e1)
            dv(lambda en: en.tensor_tensor(out=big[2][:], in0=big[2][:], in1=big[3][:], op=ALU.add), e1)
            dv(lambda en: en.tensor_scalar(out=CR[:, 1, :, n_, :], in0=big[2][:], scalar1=-1.0, scalar2=None, op0=ALU.mult), e1)
        k.dma("pool", g.SSM_L3, CR[:], reads=[t], writes=[g.T_SSMW])
        L1W = sb("sL1W", [128, 2, 16, 2, 128], BF16)
        dv(lambda en: en.memset(L1W[:], 0.0), "pool")
        PS_ = [ps("sps%d" % i, [128, 512]) for i in range(2)]
        TPS_ = [Tok(True) for _ in range(2)]
        for d in range(2):
            for Q in range(4):
                hc, Ql = Q // 2, Q % 2
                for half in range(2):
                    first = True
                    for pp in range(2):
                        dq = d * 8 + 2 * Q + pp
                        for ri in range(2):
                            rhs = CR[:, ri, dq, half * 8:(half + 1) * 8, :].rearrange("p n c -> p (n c)")
                            k.op("pe", lambda en: en.matmul(PS_[half][Ql * 64:(Ql + 1) * 64, :], lhsT=Bbb[:, ri, dq, :], rhs=rhs, start=first, stop=(pp == 1 and ri == 1)), reads=[t], writes=[TPS_[half]])
                            first = False
                    k.op("act" if half else "dve",
                         (lambda en: en.activation(out=L1W[Ql * 64:(Ql + 1) * 64, d, half * 8:(half + 1) * 8, hc, Ql * 64:(Ql + 1) * 64], in_=PS_[half][Ql * 64:(Ql + 1) * 64, :].rearrange("p (n c) -> p n c", c=64), func=AF.Copy)) if half else
                         (lambda en: en.tensor_copy(out=L1W[Ql * 64:(Ql + 1) * 64, d, half * 8:(half + 1) * 8, hc, Ql * 64:(Ql + 1) * 64], in_=PS_[half][Ql * 64:(Ql + 1) * 64, :].rearrange("p (n c) -> p n c", c=64))),
                         reads=[TPS_[half], t], writes=[t])
        k.dma("pool", g.SSM_L1, L1W[:], reads=[t], writes=[g.T_SSMW])
        ZZ = sb("sZZ", [128, 2, 16, 64], BF16)
        PT = [ps("spt%d" % i, [128, 8, 128], BF16) for i in range(2)]
        TPT = [Tok(True) for _ in range(2)]
        sg_r = Rot([sb("ssg%d" % i, [128, 2, 2, 2, 128], BF16) for i in range(2)])
        for n_ in range(16):
            dv(lambda en: en.tensor_tensor(out=big[0][:], in0=Bb[:, 0], in1=bc3(PW[:, 0, n_, :]), op=ALU.mult))
            dv(lambda en: en.tensor_tensor(out=big[1][:], in0=Bb[:, 1], in1=bc3(PW[:, 1, n_, :]), op=ALU.mult))
            dv(lambda en: en.tensor_tensor(out=ZZ[:, 0], in0=big[0][:], in1=big[1][:], op=ALU.subtract))
            dv(lambda en: en.tensor_tensor(out=big[2][:], in0=Bb[:, 0], in1=bc3(PW[:, 1, n_, :]), op=ALU.mult), "pool")
            dv(lambda en: en.tensor_tensor(out=big[3][:], in0=Bb[:, 1], in1=bc3(PW[:, 0, n_, :]), op=ALU.mult), "pool")
            dv(lambda en: en.tensor_tensor(out=ZZ[:, 1], in0=big[2][:], in1=big[3][:], op=ALU.add), "pool")
            for d in range(2):
                sg, t_sg = sg_r.next()
                for hc in range(2):
                    pt = hc
                    for ri in range(2):
                        for Ql in range(2):
                            for pp in range(2):
                                dq = d * 8 + 2 * (hc * 2 + Ql) + pp
                                k.op("pe", lambda en: en.transpose(PT[pt][Ql * 64:(Ql + 1) * 64, ri * 2 + pp, :], ZZ[:, ri, dq, :], g.ident[:]), reads=[t, g.T_ident], writes=[TPT[pt]])
                    k.op("act" if hc else "dve",
                         (lambda en: en.activation(out=sg[:, hc].rearrange("p r q n -> p (r q) n"), in_=PT[pt][:, 0:4, :], func=AF.Copy)) if hc else
                         (lambda en: en.tensor_copy(out=sg[:, hc].rearrange("p r q n -> p (r q) n"), in_=PT[pt][:, 0:4, :])),
                         reads=[TPT[pt]], writes=[t_sg])
                k.dma("pool", g.SSM_SG[:, d, n_], sg[:], reads=[t_sg], writes=[g.T_SSMW])
        k.barrier()


def phase_ssm(g, l, s, need_ctx):
    nc, k, W, lw = g.nc, g.k, g.W, g.lw
    A2 = lw.A2
    with ExitStack() as es:
        sb = lambda nm, s_, d=F32: es.enter_context(nc.sbuf_tensor(_nm(nm), s_, d))
        ps = lambda nm, s_, d=F32: es.enter_context(nc.psum_tensor(_nm(nm), s_, d))
        PSm = [ps("mps%d" % i, [128, 512]) for i in range(8)]
        TP = [Tok(True) for _ in range(8)]
        ujm = sb("mujm", [128, 2, TC, NCH], BF16)
        gjm = sb("mgjm", [128, 2, TC, NCH], BF16)
        Sb = [[sb("mSb%d%d" % (d, ri), [128, 8, NCH], BF16) for ri in range(2)] for d in range(2)]
        t_u = Tok(); t_g = Tok(); t_sb = Tok()
        cols = sb("mcols", [128, 2, 2])
        GW = sb("mGW", [128, 2, 256], BF16)
        t_c = Tok()
        k.dma("sp", cols[:], W["ssm_cols"][l], writes=[t_c])
        with ExitStack() as es1:
            sb1 = lambda nm, s_, d=F32: es1.enter_context(nc.sbuf_tensor(_nm(nm), s_, d))
            wss = sb1("mwss", [128, 8, 512], BF16)
            gwf = sb1("mgwf", [128, 2, 256])
            t_w = Tok()
            k.dma("sp", wss[:], g.WINB[:, :, O_SU:O_SU + 512], reads=[g.T_WINB], writes=[t_w])
            k.dma("sp", gwf[:], W["ssm_glu_w"][l].rearrange("(kc p) n -> p kc n", p=128), writes=[t_w])
            k.op("dve", lambda en: en.tensor_copy(out=GW[:], in_=gwf[:]), reads=[t_w], writes=[t_c])
            hT_r = Rot([sb1("mhT%d" % i, [128, 8, 512], BF16) for i in range(2)])
            blocks = [(0, 256)] + [(256 + 512 * b, 512) for b in range(8)]
            cnt = 0
            for (t0, nb) in blocks:
                hT, t_h = hT_r.next()
                k.dma("sp", hT[:, :, 0:nb], g.HT[:, :, t0:t0 + nb], reads=[g.T_HT], writes=[t_h])
                c0, ncb = t0 // TC, nb // TC
                for cc in range(4):
                    pi_ = cnt % 4
                    cnt += 1
                    for kc in range(8):
                        k.op("pe", lambda en: en.matmul(PSm[pi_][:, 0:nb], lhsT=wss[:, kc, cc * 128:(cc + 1) * 128], rhs=hT[:, kc, 0:nb], start=(kc == 0), stop=(kc == 7)), reads=[t_w, t_h], writes=[TP[pi_]])
                    src = PSm[pi_][:, 0:nb].rearrange("p (c j) -> p j c", j=TC)
                    if cc < 2:
                        k.op("dve", lambda en: en.tensor_copy(out=ujm[:, cc, :, c0:c0 + ncb], in_=src), reads=[TP[pi_]], writes=[t_u])
                    else:
                        k.op("act", lambda en: en.activation(out=gjm[:, cc - 2, :, c0:c0 + ncb], in_=src, func=AF.Silu), reads=[TP[pi_]], writes=[t_g])
            k.barrier()
        with ExitStack() as es2:
            sb2 = lambda nm, s_, d=F32: es2.enter_context(nc.sbuf_tensor(_nm(nm), s_, d))
            SG = sb2("mSG", [128, 2, 16, 2, 2, 2, 128], BF16)
            t_sg = Tok()
            k.dma("sp", SG[:, 0], g.SSM_SG[:, 0], reads=[g.T_SSMW], writes=[t_sg])
            k.dma("sp", SG[:, 1], g.SSM_SG[:, 1], reads=[g.T_SSMW], writes=[t_sg])
            S = [[sb2("mS%d%d" % (d, ri), [128, 8, NCH]) for ri in range(2)] for d in range(2)]
            t_S = [[Tok() for q in range(8)] for d in range(2)]
            Tt = [[sb2("mT%d%d" % (i, ri), [128, NCH]) for ri in range(2)] for i in range(4)]
            t_T = [Tok() for _ in range(4)]
            cnt = 0
            for d in range(2):
                for q in range(8):
                    Q, pp = q // 2, q % 2
                    hc, Ql = Q // 2, Q % 2
                    rows = slice(Ql * 64, (Ql + 1) * 64)
                    for ri in range(2):
                        pi_ = cnt % 4
                        cnt += 1
                        for i in range(TC):
                            n_ = (TC - 1 - i) if d == 0 else i
                            k.op("pe", lambda en: en.matmul(PSm[pi_][:, 0:NCH], lhsT=SG[rows, d, n_, hc, ri, pp, :], rhs=ujm[rows, hc, i, :], start=(i == 0), stop=(i == TC - 1)), reads=[t_sg, t_u], writes=[TP[pi_]])
                        if d == 0:
                            k.op("act" if ri else "dve", (lambda en: en.activation(out=S[d][ri][:, q, :], in_=PSm[pi_][:, 0:NCH], func=AF.Copy)) if ri else (lambda en: en.tensor_copy(out=S[d][ri][:, q, :], in_=PSm[pi_][:, 0:NCH])), reads=[TP[pi_]], writes=[t_S[d][q]])
                        else:
                            k.op("dve", lambda en: en.tensor_copy(out=S[d][ri][:, q, 0:256], in_=PSm[pi_][:, 16:NCH]), reads=[TP[pi_]], writes=[t_S[d][q]])
                            k.op("act", lambda en: en.activation(out=S[d][ri][:, q, 256:NCH], in_=PSm[pi_][:, 0:16], func=AF.Copy), reads=[TP[pi_]], writes=[t_S[d][q]])
            for kk in range(NDBL):
                sh = 1 << kk
                w_ = NCH - sh
                it = 0
                for d in range(2):
                    dst = slice(sh, NCH) if d == 0 else slice(0, w_)
                    srcs = slice(0, w_) if d == 0 else slice(sh, NCH)
                    for q in range(8):
                        dq = d * 8 + q
                        Ar, Ai, nAi = A2[:, kk, 0, dq:dq + 1], A2[:, kk, 1, dq:dq + 1], A2[:, kk, 2, dq:dq + 1]
                        Tr, Ti = Tt[it % 4]
                        tT = t_T[it % 4]
                        it += 1
                        Sr, Si = S[d][0], S[d][1]
                        ts = t_S[d][q]
                        k.op("act", lambda en: en.activation(out=Tr[:, 0:w_], in_=Sr[:, q, srcs], func=AF.Copy, scale=Ar), reads=[ts, lw.tok], writes=[tT])
                        k.op("dve", lambda en: en.scalar_tensor_tensor(out=Tr[:, 0:w_], in0=Si[:, q, srcs], scalar=nAi, in1=Tr[:, 0:w_], op0=ALU.mult, op1=ALU.add), reads=[ts, tT, lw.tok], writes=[tT])
                        k.op("act", lambda en: en.activation(out=Ti[:, 0:w_], in_=Si[:, q, srcs], func=AF.Copy, scale=Ar), reads=[ts, lw.tok, tT], writes=[tT])
                        k.op("dve", lambda en: en.scalar_tensor_tensor(out=Ti[:, 0:w_], in0=Sr[:, q, srcs], scalar=Ai, in1=Ti[:, 0:w_], op0=ALU.mult, op1=ALU.add), reads=[ts, tT, lw.tok], writes=[tT])
                        k.op("pool", lambda en: en.tensor_tensor(out=Sr[:, q, dst], in0=Sr[:, q, dst], in1=Tr[:, 0:w_], op=ALU.add), reads=[tT, ts], writes=[ts])
                        k.op("dve", lambda en: en.tensor_tensor(out=Si[:, q, dst], in0=Si[:, q, dst], in1=Ti[:, 0:w_], op=ALU.add), reads=[tT, ts], writes=[ts])
            for d in range(2):
                for ri in range(2):
                    k.op("act" if ri else "dve", (lambda en: en.activation(out=Sb[d][ri][:], in_=S[d][ri][:], func=AF.Copy)) if ri else (lambda en: en.tensor_copy(out=Sb[d][ri][:], in_=S[d][ri][:])), reads=[t_S[d][q] for q in range(8)], writes=[t_sb])
            k.barrier()
        with ExitStack() as es3:
            sb3 = lambda nm, s_, d=F32: es3.enter_context(nc.sbuf_tensor(_nm(nm), s_, d))
            L3 = sb3("mL3", [128, 2, 16, 17, 64], BF16)
            L1W = sb3("mL1W", [128, 2, 16, 2, 128], BF16)
            t_l = Tok()
            k.dma("sp", L3[:, 0], g.SSM_L3[:, 0], reads=[g.T_SSMW], writes=[t_l])
            k.dma("sp", L3[:, 1], g.SSM_L3[:, 1], reads=[g.T_SSMW], writes=[t_l])
            k.dma("sp", L1W[:], g.SSM_L1, reads=[g.T_SSMW], writes=[t_l])
            Yjm = sb3("mYjm", [128, 2, TC, NCH])
            t_y = Tok()
            cnt = 0
            for j in range(TC):
                for hc in range(2):
                    pi_ = cnt % 4
                    cnt += 1
                    yp = PSm[pi_]
                    first = True
                    for tau in range(j + 1):
                        k.op("pe", lambda en: en.matmul(yp[:, 0:NCH], lhsT=L1W[:, 0, tau, hc, :], rhs=ujm[:, hc, j - tau, :], start=first, stop=False), reads=[t_l, t_u], writes=[TP[pi_]])
                        first = False
                    for tau in range(TC - j):
                        k.op("pe", lambda en: en.matmul(yp[:, 0:NCH], lhsT=L1W[:, 1, tau, hc, :], rhs=ujm[:, hc, j + tau, :], start=False, stop=False), reads=[t_l, t_u], writes=[TP[pi_]])
                    for Ql in range(2):
                        rows = slice(Ql * 64, (Ql + 1) * 64)
                        for pp in range(2):
                            q = 2 * (2 * hc + Ql) + pp
                            for ri in range(2):
                                k.op("pe", lambda en: en.matmul(yp[rows, 1:NCH], lhsT=L3[:, ri, q, j + 1, :], rhs=Sb[0][ri][:, q, 0:NCH - 1], start=False, stop=False), reads=[t_l, t_sb], writes=[TP[pi_]])
                                k.op("pe", lambda en: en.matmul(yp[rows, 16:NCH], lhsT=L3[:, ri, 8 + q, TC - j, :], rhs=Sb[1][ri][:, q, 1:257], start=False, stop=False), reads=[t_l, t_sb], writes=[TP[pi_]])
                                lastm = (Ql == 1 and pp == 1 and ri == 1)
                                k.op("pe", lambda en: en.matmul(yp[rows, 0:15], lhsT=L3[:, ri, 8 + q, TC - j, :], rhs=Sb[1][ri][:, q, 257:NCH], start=False, stop=lastm), reads=[t_l, t_sb], writes=[TP[pi_]])
                    k.op("dve", lambda en: en.scalar_tensor_tensor(out=Yjm[:, hc, j, :], in0=ujm[:, hc, j, :], scalar=cols[:, hc, 0:1], in1=yp[:, 0:NCH], op0=ALU.mult, op1=ALU.add), reads=[TP[pi_], t_u, t_c], writes=[t_y])
            k.barrier()
            FL = TC * NCH
            Yf = [Yjm[:, hc].rearrange("p j c -> p (j c)") for hc in range(2)]
            Gf = [gjm[:, hc].rearrange("p j c -> p (j c)") for hc in range(2)]
            Zb = ujm
            Zf = [Zb[:, hc].rearrange("p j c -> p (j c)") for hc in range(2)]
            w1 = sb3("mw1", [128, 512]); w2 = sb3("mw2", [128, 512]); w3 = sb3("mw3", [128, 2, 512])
            t_w1 = Tok(); t_z = Tok(); t_w3 = Tok()
            CG = 1.5957691216057308
            pieces = [(c0, min(512, FL - c0)) for c0 in range(0, FL, 512)]
            for (c0, w_) in pieces:
                cs = slice(c0, c0 + w_)
                for hc in range(2):
                    k.op("pool", lambda en: en.tensor_tensor(out=w1[:, 0:w_], in0=Yf[hc][:, cs], in1=Yf[hc][:, cs], op=ALU.mult), reads=[t_y, t_w1], writes=[t_w1])
                    k.op("dve", lambda en: en.tensor_scalar(out=w1[:, 0:w_], in0=w1[:, 0:w_], scalar1=0.044715, scalar2=1.0, op0=ALU.mult, op1=ALU.add), reads=[t_w1], writes=[t_w1])
                    k.op("pool", lambda en: en.tensor_tensor(out=w1[:, 0:w_], in0=w1[:, 0:w_], in1=Yf[hc][:, cs], op=ALU.mult), reads=[t_w1, t_y], writes=[t_w1])
                    k.op("act", lambda en: en.activation(out=w2[:, 0:w_], in_=w1[:, 0:w_], func=AF.Sigmoid, scale=CG), reads=[t_w1], writes=[t_w1])
                    k.op("dve", lambda en: en.tensor_tensor(out=w3[:, hc, 0:w_], in0=w2[:, 0:w_], in1=Yf[hc][:, cs], op=ALU.mult), reads=[t_w1, t_y, t_w3], writes=[t_w3])
                    k.op("pool", lambda en: en.tensor_copy(out=Zf[hc][:, cs], in_=w3[:, hc, 0:w_]), reads=[t_w3, t_u], writes=[t_z])
                for oc in range(2):
                    pi_ = 4 + oc
                    for kc in range(2):
                        k.op("pe", lambda en: en.matmul(PSm[pi_][:, 0:w_], lhsT=GW[:, kc, oc * 128:(oc + 1) * 128], rhs=Zf[kc][:, cs], start=(kc == 0), stop=(kc == 1)), reads=[t_z, t_c], writes=[TP[pi_]])
                    k.op("act", lambda en: en.activation(out=w2[:, 0:w_], in_=PSm[pi_][:, 0:w_], func=AF.Sigmoid, bias=cols[:, oc, 1:2]), reads=[TP[pi_], t_c, t_w1], writes=[t_w1])
                    k.op("dve", lambda en: en.tensor_tensor(out=w2[:, 0:w_], in0=w2[:, 0:w_], in1=w3[:, oc, 0:w_], op=ALU.mult), reads=[t_w1, t_w3], writes=[t_w1])
                    k.op("pool", lambda en: en.tensor_tensor(out=Gf[oc][:, cs], in0=w2[:, 0:w_], in1=Gf[oc][:, cs], op=ALU.mult), reads=[t_w1, t_g], writes=[t_g])
            on_t = sb3("mON", [128, 2, T], BF16)
            t_on = Tok()
            for hc in range(2):
                k.op("dve" if hc else "pool", lambda en: en.tensor_copy(out=on_t[:, hc, :].rearrange("p (c j) -> p j c", j=TC), in_=gjm[:, hc]), reads=[t_g], writes=[t_on])
            if need_ctx:
                k.dma("pool", g.CATT[:, 4:6, :], on_t[:], reads=[t_on], writes=[g.T_CATT])
            else:
                k.dma("pool", g.CATT[:, 4:6, C:T], on_t[:, :, C:T], reads=[t_on], writes=[g.T_CATT])
            k.barrier()
```

```python
import math
import numpy as np
from contextlib import ExitStack
import concourse.bass as bass
import concourse.mybir as mybir
from concourse.bass_utils import run_bass_kernel_spmd

F32 = mybir.dt.float32
BF16 = mybir.dt.bfloat16
AF = mybir.ActivationFunctionType
ALU = mybir.AluOpType
AX = mybir.AxisListType

D = 1024
L = 4096
C = 256
T = L + C
NT = T // 128
DEPTH = 4
EPS = 1e-6
NCORES = 8

O_CQ, O_CKV, O_KR, O_GM = 0, 192, 320, 352
O1 = 608
O_GQ, O_GK, O_GV, O_GG = O1, O1 + 256, O1 + 384, O1 + 512
O2 = O1 + 768
O_SU, O_SG = O2, O2 + 256
O3 = O2 + 512
O_HY, O_HG = O3, O3 + 768
NIN = 2912
O_KRP = NIN
O_QM = O_KRP + 32
O_QP = O_QM + 256
O_KP = O_QP + 256
NCB = O_KP + 128

EPOCH = 30000
NDMASLOT = 8


class Tok:
    __slots__ = ("w", "r", "excl")

    def __init__(self, excl=False):
        self.w = []
        self.r = []
        self.excl = excl


class KB:
    def __init__(self, nc, es):
        self.nc = nc
        self.es = es
        self.eng = {"pe": nc.tensor, "act": nc.scalar, "dve": nc.vector, "pool": nc.gpsimd, "sp": nc.sync}
        self.cnt = {e: 0 for e in self.eng}
        self.epoch = {e: 0 for e in self.eng}
        self.sems = {}
        self.seen = {e: {} for e in self.eng}
        self.dma_slots = {}
        self.dma_rr = {e: 0 for e in self.eng}
        self.ninst = 0

    def _sem(self, key):
        if key not in self.sems:
            self.sems[key] = self.es.enter_context(self.nc.semaphore("s_%s_%s" % key))
        return self.sems[key]

    def _wait(self, e, ev):
        key, val = ev
        if self.seen[e].get(key, 0) >= val:
            return
        self.eng[e].wait_ge(self._sem(key), val)
        self.seen[e][key] = val

    def _deps(self, e, reads, writes):
        best = {}

        def add(k_, v):
            if best.get(k_, 0) < v:
                best[k_] = v
        for t in reads:
            for k_, v in t.w:
                add(k_, v)
            if t.excl:
                for k_, v in t.r:
                    if k_[0] != e:
                        add(k_, v)
        for t in writes:
            for k_, v in t.w:
                if k_[0] != e:
                    add(k_, v)
            for k_, v in t.r:
                if k_[0] != e:
                    add(k_, v)
        for k_, v in best.items():
            if e == "pe" and k_[0] == "pe":
                continue
            self._wait(e, (k_, v))

    def _record(self, ev, reads, writes):
        for t in reads:
            t.r.append(ev)
            if len(t.r) > 16:
                best = {}
                for k_, v in t.r:
                    if best.get(k_, 0) < v:
                        best[k_] = v
                t.r = list(best.items())
        for t in writes:
            t.w = [ev]
            t.r = []

    def op(self, e, fn, reads=(), writes=()):
        self._deps(e, reads, writes)
        if self.cnt[e] >= EPOCH:
            self.epoch[e] += 1
            self.cnt[e] = 0
        key = (e, self.epoch[e])
        ins = fn(self.eng[e])
        self.cnt[e] += 1
        ins.then_inc(self._sem(key), 1)
        ev = (key, self.cnt[e])
        self._record(ev, reads, writes)
        self.ninst += 1
        return ev

    def dma(self, e, out, in_, reads=(), writes=(), **kw):
        self._deps(e, reads, writes)
        if e not in self.dma_slots:
            self.dma_slots[e] = [[("d" + e, i), 0] for i in range(NDMASLOT)]
        i = self.dma_rr[e]
        self.dma_rr[e] = (i + 1) % NDMASLOT
        slot = self.dma_slots[e][i]
        key = slot[0]
        if slot[1] > 0:
            self._wait(e, (key, 16 * slot[1]))
        slot[1] += 1
        ins = self.eng[e].dma_start(out=out, in_=in_, **kw)
        ins.then_inc(self._sem(key), 16)
        ev = (key, 16 * slot[1])
        self._record(ev, reads, writes)
        self.ninst += 1
        return ev

    def all_events(self):
        evs = []
        for e in self.eng:
            for ep in range(self.epoch[e] + 1):
                v = self.cnt[e] if ep == self.epoch[e] else EPOCH
                if v > 0:
                    evs.append(((e, ep), v))
        for e, slots in self.dma_slots.items():
            for key, uses in slots:
                if uses:
                    evs.append((key, 16 * uses))
        return evs

    def barrier(self):
        evs = self.all_events()
        for e in ("pe", "act", "dve", "pool", "sp"):
            for ev in evs:
                if ev[0][0] == e:
                    continue
                self._wait(e, ev)

    def drain(self, e="sp"):
        for ev in self.all_events():
            self._wait(e, ev)


_NMC = [0]


def _nm(n):
    _NMC[0] += 1
    return "%s_%d" % (n, _NMC[0])


class Rot:
    def __init__(self, tiles, excl=False):
        self.tiles = [(t, Tok(excl)) for t in tiles]
        self.i = 0

    def next(self):
        r = self.tiles[self.i]
        self.i = (self.i + 1) % len(self.tiles)
        return r


def _rope_table(d):
    hh = d // 2
    qq = hh // 2
    inv = (np.float32(10000.0) ** (-np.arange(0, hh, 2, dtype=np.float32) / np.float32(hh))).astype(np.float32)
    t = np.arange(L)
    row = (t // 64).astype(np.float32)
    col = (t % 64).astype(np.float32)
    cos = np.ones((d, T), np.float32)
    sin = np.zeros((d, T), np.float32)
    for i in range(d):
        hf, within = divmod(i, hh)
        fi = within % qq
        pos = row if hf == 0 else col
        ang = (pos * inv[fi]).astype(np.float32)
        cos[i, C:] = np.cos(ang).astype(np.float32)
        sin[i, C:] = np.sin(ang).astype(np.float32)
    return cos, sin


def _partner_index(d):
    hh = d // 2
    qq = hh // 2
    idx = np.zeros(d, np.int64)
    sg = np.zeros(d, np.float32)
    for i in range(d):
        hf, within = divmod(i, hh)
        if within < qq:
            idx[i] = i + qq
            sg[i] = -1.0
        else:
            idx[i] = i - qq
            sg[i] = 1.0
    return idx, sg


def host_constants():
    cst = {}
    cst["k_ident"] = np.eye(128, dtype=np.float32)
    c32, s32 = _rope_table(32)
    c64, s64 = _rope_table(64)
    mc = np.ones((128, T), np.float32)
    ms = np.zeros((128, T), np.float32)
    mc[64:96] = c32
    ms[64:96] = s32
    cst["k_ropeM"] = np.stack([mc, ms], 0)
    cst["k_ropeG"] = np.stack([np.concatenate([c64, c64], 0), np.concatenate([s64, s64], 0)], 0)
    cst.update(hyena_constants())
    return cst


class G:
    pass


def build_program(layers, final_lat_only, dbg=None):
    nc = bass.Bass("TRN2", target_bir_lowering=False)
    g = G()
    g.nc = nc
    g.dbg = dbg or {}

    def din(name, shape, dt=F32):
        return nc.dram_tensor(name, list(shape), dt, kind="ExternalInput").ap()

    def dscr(name, shape, dt=F32):
        return nc.dram_tensor(name, list(shape), dt).ap()

    g.xs = din("xs", [2, T, D])
    g.cT = din("cT", [128, 8, 3])
    W = {}
    W["w_mod"] = din("w_mod", [DEPTH, D, 3 * D])
    W["b_mod"] = din("b_mod", [DEPTH, 3 * D])
    W["g_pre"] = din("g_pre", [DEPTH, D])
    W["g_post"] = din("g_post", [DEPTH, D])
    W["w_in"] = din("w_in", [DEPTH, D, NIN])
    W["w_out"] = din("w_out", [DEPTH, D, D])
    W["mla_g_cq"] = din("mla_g_cq", [DEPTH, 192])
    W["mla_w_uq"] = din("mla_w_uq", [DEPTH, 192, 384])
    W["mla_g_ckv"] = din("mla_g_ckv", [DEPTH, 128])
    W["mla_w_ukv"] = din("mla_w_ukv", [DEPTH, 128, 512])
    W["gq_cols"] = din("gq_cols", [DEPTH, 128, 4])
    for nm, shp in (("hy_conv_w", [DEPTH, 3, 768]), ("hy_conv_b", [DEPTH, 768]), ("hy_f_w1", [DEPTH, 33, 64]), ("hy_f_b1", [DEPTH, 64]),
                    ("hy_f_freq1", [DEPTH, 64]), ("hy_f_w2", [DEPTH, 64, 64]), ("hy_f_b2", [DEPTH, 64]), ("hy_f_freq2", [DEPTH, 64]),
                    ("hy_f_w3", [DEPTH, 64, 1024]), ("hy_bias", [DEPTH, 2, 256])):
        W[nm] = din(nm, shp)
    for nm, shp in (("ssm_lam", [DEPTH, 128, 2, 16]), ("ssm_ls", [DEPTH, 128, 16]), ("ssm_Bp", [DEPTH, 128, 2, 16, 64]), ("ssm_Cp", [DEPTH, 128, 2, 16, 64]),
                    ("ssm_cols", [DEPTH, 128, 2, 2]), ("ssm_glu_w", [DEPTH, 256, 256])):
        W[nm] = din(nm, shp)
    g.W = W
    K = {}
    K["k_ident"] = din("k_ident", [128, 128])
    K["k_ropeM"] = din("k_ropeM", [2, 128, T])
    K["k_ropeG"] = din("k_ropeG", [2, 128, T])
    for nm, arr in hyena_constants().items():
        K[nm] = din(nm, list(arr.shape))
    g.K = K
    if final_lat_only:
        g.y = nc.dram_tensor("y", [2, L, D], F32, kind="ExternalOutput").ap()
    else:
        g.y = nc.dram_tensor("y", [2, T, D], F32, kind="ExternalOutput").ap()
    for name, (shape, dt) in g.dbg.items():
        g.dbg[name] = nc.dram_tensor(name, list(shape), dt, kind="ExternalOutput").ap()

    g.XS = dscr("XS", [2, T, D]) if len(layers) > 1 else None
    g.WINB = dscr("WINB", [128, 8, NCB], BF16)
    g.WOUTB = dscr("WOUTB", [128, 8, D], BF16)
    g.MODROWS = dscr("MODROWS", [3, 3 * D])
    g.HT = dscr("HT", [128, 8, T], BF16)
    g.CATT = dscr("CATT", [128, 8, T], BF16)
    g.KHAT = {"L": dscr("KHATL", [2, HY_LAT.ng, 128, 512]), "C": dscr("KHATC", [2, HY_CTX.ng, 128, 512])}
    g.T_KHAT = Tok()
    g.SSM_L3 = dscr("SSM_L3", [128, 2, 16, 17, 64], BF16)
    g.SSM_L1 = dscr("SSM_L1", [128, 2, 16, 2, 128], BF16)
    g.SSM_SG = dscr("SSM_SG", [128, 2, 16, 2, 2, 2, 128], BF16)
    g.T_SSMW = Tok()
    g.T_XS = [Tok(), Tok()]
    g.T_WINB = Tok()
    g.T_WOUTB = Tok()
    g.T_MOD = Tok()
    g.T_HT = Tok()
    g.T_CATT = Tok()
    g.T_Y = Tok()

    with ExitStack() as es:
        k = KB(nc, es)
        g.k = k
        g.es = es
        g.ident = es.enter_context(nc.sbuf_tensor("ident", [128, 128], BF16))
        g.T_ident = Tok()
        g.onesf = es.enter_context(nc.sbuf_tensor("onesf", [128, 128], F32))
        g.ones128 = es.enter_context(nc.sbuf_tensor("ones128", [128, 128], BF16))
        g.ones192 = es.enter_context(nc.sbuf_tensor("ones192", [128, 128], BF16))
        g.blk64 = es.enter_context(nc.sbuf_tensor("blk64", [128, 128], BF16))
        g.T_const = Tok()
        with ExitStack() as es2:
            tmp = es2.enter_context(nc.sbuf_tensor("idtmp", [128, 128], F32))
            tt = Tok()
            k.dma("sp", tmp[:], K["k_ident"], writes=[tt])
            k.op("dve", lambda e: e.tensor_copy(out=g.ident[:], in_=tmp[:]), reads=[tt], writes=[g.T_ident])
            k.op("dve", lambda e: e.memset(g.onesf[:], 1.0), writes=[g.T_const])
            k.op("dve", lambda e: e.memset(g.ones128[:], 1.0 / 128), writes=[g.T_const])
            k.op("dve", lambda e: e.memset(g.ones192[:], 1.0 / 192), writes=[g.T_const])
            k.op("dve", lambda e: e.memset(g.blk64[:], 0.0), writes=[g.T_const])
            k.op("dve", lambda e: e.memset(g.blk64[0:64, 0:64], 1.0 / 64), reads=[g.T_const], writes=[g.T_const])
            k.op("dve", lambda e: e.memset(g.blk64[64:128, 64:128], 1.0 / 64), reads=[g.T_const], writes=[g.T_const])
            k.barrier()

        for li, l in enumerate(layers):
            need_ctx = l < DEPTH - 1
            src = g.xs if li == 0 else g.XS
            last = li == len(layers) - 1
            dst = g.y if last else g.XS
            phase_weights(g, l)
            k.barrier()
            phase_ssm_weights(g, l)
            phase_hy_filter(g, l, HY_LAT)
            if need_ctx:
                phase_hy_filter(g, l, HY_CTX)
            for s in range(2):
                phase_P1(g, l, s, src)
                k.barrier()
                phase_hyena(g, l, s, HY_LAT, C)
                if need_ctx:
                    phase_hyena(g, l, s, HY_CTX, 0)
                phase_ssm(g, l, s, need_ctx)
                phase_attn(g, l, s, need_ctx)
                k.barrier()
                phase_P6(g, l, s, src, dst, need_ctx, last and final_lat_only)
                k.barrier()
        k.drain("sp")
    return nc, g


def phase_weights(g, l):
    nc, k, W = g.nc, g.k, g.W
    with ExitStack() as es:
        sb = lambda n, s, d=F32: es.enter_context(nc.sbuf_tensor(_nm(n), s, d))
        ps = lambda n, s, d=F32: es.enter_context(nc.psum_tensor(_nm(n), s, d))
        p32, s32 = _partner_index(32)
        p64, s64 = _partner_index(64)
        fin = Rot([sb("wf%d" % i, [128, NIN]) for i in range(2)])
        fob = Rot([sb("wb%d" % i, [128, NCB], BF16) for i in range(2)])
        qorder = (0, 2, 1, 3)
        engs = ["act", "pool", "dve"]
        ei = 0

        def cp(dst, src, neg=False):
            nonlocal ei
            e = engs[ei % 3]
            ei += 1
            if e == "act":
                k.op("act", lambda en: en.activation(out=dst, in_=src, func=AF.Copy, scale=(-1.0 if neg else 1.0)), reads=[tf], writes=[tb])
            else:
                k.op(e, lambda en: en.tensor_scalar(out=dst, in0=src, scalar1=(-1.0 if neg else 1.0), scalar2=None, op0=ALU.mult), reads=[tf], writes=[tb])

        def partner_cols(dstb, srcb, d):
            hh, qq = d // 2, d // 4
            for hf in range(2):
                b0 = hf * hh
                cp(wb[:, dstb + b0:dstb + b0 + qq], wf[:, srcb + b0 + qq:srcb + b0 + hh], neg=True)
                cp(wb[:, dstb + b0 + qq:dstb + b0 + hh], wf[:, srcb + b0:srcb + b0 + qq], neg=False)

        for kc in range(8):
            wf, tf = fin.next()
            wb, tb = fob.next()
            k.dma("sp", wf[:], W["w_in"][l, kc * 128:(kc + 1) * 128, :], writes=[tf])
            k.op("act", lambda en: en.activation(out=wb[:, 0:1456], in_=wf[:, 0:1456], func=AF.Copy), reads=[tf], writes=[tb])
            k.op("dve", lambda en: en.tensor_copy(out=wb[:, 1456:NIN], in_=wf[:, 1456:NIN]), reads=[tf], writes=[tb])
            partner_cols(O_KRP, O_KR, 32)
            for pos, h in enumerate(qorder):
                cp(wb[:, O_QM + pos * 64:O_QM + pos * 64 + 64], wf[:, O_GQ + h * 64:O_GQ + h * 64 + 64])
                partner_cols(O_QP + pos * 64, O_GQ + h * 64, 64)
            for h in range(2):
                partner_cols(O_KP + h * 64, O_GK + h * 64, 64)
            k.dma("pool", g.WINB[:, kc, :], wb[:], reads=[tb], writes=[g.T_WINB])
        fo = Rot([sb("wof%d" % i, [128, D]) for i in range(2)])
        fb = Rot([sb("wob%d" % i, [128, D], BF16) for i in range(2)])
        for kc in range(8):
            wf, tf = fo.next()
            wb, tb = fb.next()
            k.dma("sp", wf[:], W["w_out"][l, kc * 128:(kc + 1) * 128, :], writes=[tf])
            k.op("act" if kc % 2 else "dve", (lambda en: en.activation(out=wb[:], in_=wf[:], func=AF.Copy)) if kc % 2 else (lambda en: en.tensor_copy(out=wb[:], in_=wf[:])), reads=[tf], writes=[tb])
            k.dma("pool", g.WOUTB[:, kc, :], wb[:], reads=[tb], writes=[g.T_WOUTB])

        cT = sb("cTs", [128, 8, 3])
        scT = sb("scT", [128, 8, 3])
        t_c = Tok()
        k.dma("sp", cT[:], g.cT, writes=[t_c])
        k.op("act", lambda en: en.activation(out=scT[:], in_=cT[:], func=AF.Silu), reads=[t_c], writes=[t_c])
        mrow = sb("mrow", [3, 3 * D])
        brow = sb("brow", [3, 3 * D])
        gpre = sb("gpre", [3, D])
        gpost = sb("gpost", [3, D])
        t_m = Tok()
        t_b = Tok()
        k.dma("sp", brow[:], W["b_mod"][l:l + 1, :].broadcast_to([3, 3 * D]), writes=[t_b])
        k.dma("sp", gpre[:], W["g_pre"][l:l + 1, :].broadcast_to([3, D]), writes=[t_b])
        k.dma("sp", gpost[:], W["g_post"][l:l + 1, :].broadcast_to([3, D]), writes=[t_b])
        wm = Rot([sb("wm%d" % i, [128, 8, 512]) for i in range(2)])
        pm = Rot([ps("pm%d" % i, [3, 512]) for i in range(2)], excl=True)
        for cc in range(6):
            wt, tw = wm.next()
            pt, tp = pm.next()
            k.dma("sp", wt[:], W["w_mod"][l, :, cc * 512:(cc + 1) * 512].rearrange("(kc p) n -> p kc n", p=128), writes=[tw])
            for kc in range(8):
                k.op("pe", lambda en: en.matmul(pt[:], lhsT=scT[:, kc, :], rhs=wt[:, kc, :], start=(kc == 0), stop=(kc == 7)), reads=[t_c, tw], writes=[tp])
            k.op("dve", lambda en: en.tensor_tensor(out=mrow[:, cc * 512:(cc + 1) * 512], in0=pt[:], in1=brow[:, cc * 512:(cc + 1) * 512], op=ALU.add), reads=[tp, t_b], writes=[t_m])
        orow = sb("orow", [3, 3 * D])
        t_o = Tok()
        k.op("dve", lambda en: en.scalar_tensor_tensor(out=orow[:, 0:D], in0=mrow[:, D:2 * D], scalar=1.0, in1=gpre[:], op0=ALU.add, op1=ALU.mult), reads=[t_m, t_b], writes=[t_o])
        k.op("dve", lambda en: en.tensor_copy(out=orow[:, D:2 * D], in_=mrow[:, 0:D]), reads=[t_m, t_o], writes=[t_o])
        k.op("dve", lambda en: en.tensor_tensor(out=orow[:, 2 * D:3 * D], in0=mrow[:, 2 * D:3 * D], in1=gpost[:], op=ALU.mult), reads=[t_m, t_b, t_o], writes=[t_o])
        k.dma("pool", g.MODROWS, orow[:], reads=[t_o], writes=[g.T_MOD])
        k.barrier()

    if not hasattr(g, "lw"):
        lw = G()
        es = g.es
        sbp = lambda n, s, d=F32: es.enter_context(nc.sbuf_tensor(_nm(n), s, d))
        lw.wuq_a = sbp("wuq_a", [128, 384], BF16)
        lw.wuq_b = sbp("wuq_b", [64, 384], BF16)
        lw.wuqp_a = sbp("wuqp_a", [128, 384], BF16)
        lw.wuqp_b = sbp("wuqp_b", [64, 384], BF16)
        lw.wuk = sbp("wuk", [128, 256], BF16)
        lw.wuv = sbp("wuv", [128, 256], BF16)
        lw.gq = sbp("gqc", [128, 4])
        lw.A2 = sbp("ssmA2", [128, NDBL, 3, 16])
        lw.tok = Tok()
        g.lw = lw
    lw = g.lw
    with ExitStack() as es:
        sb = lambda n, s, d=F32: es.enter_context(nc.sbuf_tensor(_nm(n), s, d))
        uqa = sb("uqa", [128, 384])
        uqb = sb("uqb", [64, 384])
        ukv = sb("ukv", [128, 512])
        gcq = sb("gcq", [128, 2])
        gckv = sb("gckv", [128, 1])
        tl = Tok()
        k.dma("sp", uqa[:], W["mla_w_uq"][l, 0:128, :], writes=[tl])
        k.dma("sp", uqb[:], W["mla_w_uq"][l, 128:192, :], writes=[tl])
        k.dma("sp", ukv[:], W["mla_w_ukv"][l], writes=[tl])
        k.dma("sp", gcq[:, 0:1], W["mla_g_cq"][l, 0:128].rearrange("(p o) -> p o", o=1), writes=[tl])
        k.dma("sp", gcq[0:64, 1:2], W["mla_g_cq"][l, 128:192].rearrange("(p o) -> p o", o=1), writes=[tl])
        k.dma("sp", gckv[:], W["mla_g_ckv"][l].rearrange("(p o) -> p o", o=1), writes=[tl])
        k.dma("sp", lw.gq[:], W["gq_cols"][l], writes=[lw.tok])
        k.op("dve", lambda en: en.tensor_scalar(out=uqa[:], in0=uqa[:], scalar1=gcq[:, 0:1], scalar2=None, op0=ALU.mult), reads=[tl], writes=[tl])
        k.op("dve", lambda en: en.tensor_scalar(out=uqb[:], in0=uqb[:], scalar1=gcq[0:64, 1:2], scalar2=None, op0=ALU.mult), reads=[tl], writes=[tl])
        k.op("dve", lambda en: en.tensor_scalar(out=ukv[:], in0=ukv[:], scalar1=gckv[:, 0:1], scalar2=None, op0=ALU.mult), reads=[tl], writes=[tl])
        k.op("dve", lambda en: en.tensor_copy(out=lw.wuq_a[:], in_=uqa[:]), reads=[tl], writes=[lw.tok])
        k.op("dve", lambda en: en.tensor_copy(out=lw.wuq_b[:], in_=uqb[:]), reads=[tl, lw.tok], writes=[lw.tok])
        k.op("dve", lambda en: en.memset(lw.wuqp_a[:], 0.0), reads=[lw.tok], writes=[lw.tok])
        k.op("dve", lambda en: en.memset(lw.wuqp_b[:], 0.0), reads=[lw.tok], writes=[lw.tok])
        for h in range(4):
            for hf in range(2):
                b0 = h * 96 + 64 + hf * 16
                for (dst, src, tile_src) in ((lw.wuqp_a, uqa, 128), (lw.wuqp_b, uqb, 64)):
                    k.op("dve", lambda en: en.tensor_scalar(out=dst[:, b0:b0 + 8], in0=src[:, b0 + 8:b0 + 16], scalar1=-1.0, scalar2=None, op0=ALU.mult), reads=[tl, lw.tok], writes=[lw.tok])
                    k.op("dve", lambda en: en.tensor_copy(out=dst[:, b0 + 8:b0 + 16], in_=src[:, b0:b0 + 8]), reads=[tl, lw.tok], writes=[lw.tok])
            k.op("dve", lambda en: en.tensor_copy(out=lw.wuk[:, h * 64:(h + 1) * 64], in_=ukv[:, h * 128:h * 128 + 64]), reads=[tl, lw.tok], writes=[lw.tok])
            k.op("dve", lambda en: en.tensor_copy(out=lw.wuv[:, h * 64:(h + 1) * 64], in_=ukv[:, h * 128 + 64:h * 128 + 128]), reads=[tl, lw.tok], writes=[lw.tok])
        k.barrier()


def phase_P1(g, l, s, src):
    nc, k = g.nc, g.k
    with ExitStack() as es:
        sb = lambda n, s_, d=F32: es.enter_context(nc.sbuf_tensor(_nm(n), s_, d))
        ps = lambda n, s_, d=F32: es.enter_context(nc.psum_tensor(_nm(n), s_, d))
        mods = {}
        t_mod = Tok()
        for v in (s, 2):
            mods[v] = sb("mod%d" % v, [128, 2 * D])
            k.dma("sp", mods[v][:], g.MODROWS[v:v + 1, 0:2 * D].broadcast_to([128, 2 * D]), reads=[g.T_MOD], writes=[t_mod])
        neghalf = sb("neghalf", [128, 1])
        k.op("pool", lambda en: en.memset(neghalf[:], -0.5), writes=[t_mod])
        xr = Rot([sb("x%d" % i, [128, D]) for i in range(3)])
        junk = sb("junk", [128, D], BF16)
        t_junk = Tok()
        hb_r = Rot([sb("hb%d" % i, [128, D], BF16) for i in range(2)])
        tmp_r = Rot([sb("tmp%d" % i, [128, D]) for i in range(2)])
        st_r = Rot([sb("st%d" % i, [128, 4]) for i in range(3)])
        tp_r = Rot([ps("tp%d" % i, [128, 8, 128], BF16) for i in range(2)], excl=True)
        hs_r = Rot([sb("hs%d" % i, [128, 8, 512], BF16) for i in range(2)])
        hs, t_hs = None, None
        for ti in range(NT):
            if ti == 0 or (ti >= 2 and (ti - 2) % 4 == 0):
                hs, t_hs = hs_r.next()
            off = (ti * 128) if ti < 2 else (((ti - 2) % 4) * 128)
            xt, t_x = xr.next()
            k.dma("sp", xt[:], src[s, ti * 128:(ti + 1) * 128, :], reads=[g.T_XS[s]], writes=[t_x])
            st, t_st = st_r.next()
            k.op("dve", lambda en: en.scalar_tensor_tensor(out=junk[:], in0=xt[:], scalar=1.0, in1=xt[:], op0=ALU.mult, op1=ALU.mult, accum_out=st[:, 0:1]), reads=[t_x], writes=[t_junk, t_st])
            k.op("dve", lambda en: en.tensor_scalar(out=st[:, 1:2], in0=st[:, 0:1], scalar1=1.0 / D, scalar2=EPS, op0=ALU.mult, op1=ALU.add), reads=[t_st], writes=[t_st])
            k.op("pool", lambda en: en.tensor_tensor(out=st[:, 2:3], in0=st[:, 1:2], in1=neghalf[:], op=ALU.pow), reads=[t_st, t_mod], writes=[t_st])
            md = mods[2] if ti < 2 else mods[s]
            tmp, t_tmp = tmp_r.next()
            hb, t_hb = hb_r.next()
            k.op("dve", lambda en: en.scalar_tensor_tensor(out=tmp[:], in0=xt[:], scalar=st[:, 2:3], in1=md[:, 0:D], op0=ALU.mult, op1=ALU.mult), reads=[t_x, t_st, t_mod], writes=[t_tmp])
            k.op("pool", lambda en: en.tensor_tensor(out=hb[:], in0=tmp[:], in1=md[:, D:2 * D], op=ALU.add), reads=[t_tmp, t_mod], writes=[t_hb])
            tp, t_tp = tp_r.next()
            for kc in range(8):
                k.op("pe", lambda en: en.transpose(tp[:, kc, :], hb[:, kc * 128:(kc + 1) * 128], g.ident[:]), reads=[t_hb, g.T_ident], writes=[t_tp])
            k.op("act", lambda en: en.activation(out=hs[:, :, off:off + 128], in_=tp[:], func=AF.Copy), reads=[t_tp], writes=[t_hs])
            if ti == 1:
                k.dma("pool", g.HT[:, :, 0:256], hs[:, :, 0:256], reads=[t_hs], writes=[g.T_HT])
            elif ti >= 2 and (ti - 2) % 4 == 3:
                t0 = (ti - 3) * 128
                k.dma("pool", g.HT[:, :, t0:t0 + 512], hs[:, :, 0:512], reads=[t_hs], writes=[g.T_HT])
        k.barrier()


def phase_attn(g, l, s, need_ctx):
    nc, k, lw = g.nc, g.k, g.lw
    with ExitStack() as es:
        sb = lambda n, s_, d=F32: es.enter_context(nc.sbuf_tensor(_nm(n), s_, d))
        ps = lambda n, s_, d=F32: es.enter_context(nc.psum_tensor(_nm(n), s_, d))
        KTm = [sb("KTm%d" % h, [96, T], BF16) for h in range(4)]
        VM = sb("VM", [128, NT, 4 * 65], BF16)
        KTg = sb("KTg", [128, T], BF16)
        VG = sb("VG", [128, NT, 2 * 65], BF16)
        t_K = Tok()
        k.op("pool", lambda en: en.memset(VM[:], 1.0), writes=[t_K])
        k.op("pool", lambda en: en.memset(VG[:], 1.0), writes=[t_K])
        t_w = Tok()
        epsc = sb("epsc", [128, 1])
        k.op("pool", lambda en: en.memset(epsc[:], EPS), writes=[t_w])

        hT_r = Rot([sb("hT%d" % i, [128, 8, 512], BF16) for i in range(2)])
        rope_r = Rot([sb("rp%d" % i, [128, 4, 512]) for i in range(2)])
        esA = ExitStack()
        sbA = lambda n, s_, d=F32: esA.enter_context(nc.sbuf_tensor(_nm(n), s_, d))
        wkv = sbA("wkv", [128, 8, 576], BF16)
        k.dma("sp", wkv[:, :, 0:160], g.WINB[:, :, O_CKV:O_CKV + 160], reads=[g.T_WINB], writes=[t_w])
        k.dma("sp", wkv[:, :, 160:192], g.WINB[:, :, O_KRP:O_KRP + 32], reads=[g.T_WINB], writes=[t_w])
        k.dma("sp", wkv[:, :, 192:448], g.WINB[:, :, O_GK:O_GK + 256], reads=[g.T_WINB], writes=[t_w])
        k.dma("sp", wkv[:, :, 448:576], g.WINB[:, :, O_KP:O_KP + 128], reads=[g.T_WINB], writes=[t_w])
        SB2 = [ps("psS%d" % i, [128, 2, 512]) for i in range(3)]
        TSB = [Tok(True) for _ in range(3)]
        PS = [SB2[i // 2][:, i % 2, :] for i in range(6)] + [ps("ps%d" % i, [128, 512]) for i in range(6, 8)]
        TPS = [TSB[i // 2] for i in range(6)] + [Tok(True) for _ in range(2)]

        blocks = [(0, 256)] + [(256 + 512 * b, 512) for b in range(8)]

        def load_block(t0, nb):
            hT, t_h = hT_r.next()
            k.dma("sp", hT[:, :, 0:nb], g.HT[:, :, t0:t0 + nb], reads=[g.T_HT], writes=[t_h])
            rp, t_rp = rope_r.next()
            k.dma("sp", rp[:, 0:2, 0:nb], g.K["k_ropeM"][:, :, t0:t0 + nb].rearrange("a p n -> p a n"), writes=[t_rp])
            k.dma("sp", rp[:, 2:4, 0:nb], g.K["k_ropeG"][:, :, t0:t0 + nb].rearrange("a p n -> p a n"), writes=[t_rp])
            return hT, t_h, rp, t_rp

        def proj(pi, M, wt, c0, hT, t_h, nb, pbase=0):
            for kc in range(8):
                k.op("pe", lambda en: en.matmul(PS[pi][pbase:pbase + M, 0:nb], lhsT=wt[:, kc, c0:c0 + M], rhs=hT[:, kc, 0:nb], start=(kc == 0), stop=(kc == 7)), reads=[t_w, t_h], writes=[TPS[pi]])

        def rstd_from_ms(out_ap, ms_ap, reads, wtok, tmp_ap):
            k.op("act", lambda en: en.activation(out=tmp_ap, in_=ms_ap, func=AF.Ln, bias=epsc[0:tmp_ap.shape[0], 0:1]), reads=reads + [t_w], writes=[wtok])
            k.op("act", lambda en: en.activation(out=out_ap, in_=tmp_ap, func=AF.Exp, scale=-0.5), reads=[wtok], writes=[wtok])

        wk_r = Rot([sbA("wk%d" % i, [128, 6, 512]) for i in range(1)])
        wkb_r = Rot([sbA("wkb%d" % i, [128, 3, 512], BF16) for i in range(2)])
        for (t0, nb) in blocks:
            hT, t_h, rp, t_rp = load_block(t0, nb)
            wk, t_wk = wk_r.next()
            wkb, t_wkb = wkb_r.next()
            proj(0, 128, wkv, 0, hT, t_h, nb)
            k.op("act", lambda en: en.activation(out=wkb[:, 0, 0:nb], in_=PS[0][:, 0:nb], func=AF.Square), reads=[TPS[0]], writes=[t_wkb])
            k.op("dve", lambda en: en.tensor_copy(out=wk[:, 0, 0:nb], in_=PS[0][:, 0:nb]), reads=[TPS[0]], writes=[t_wk])
            k.op("pe", lambda en: en.matmul(PS[1][:, 0:nb], lhsT=g.ones128[:], rhs=wkb[:, 0, 0:nb], start=True, stop=True), reads=[t_wkb, g.T_const], writes=[TPS[1]])
            rstd_from_ms(wk[:, 1, 0:nb], PS[1][:, 0:nb], [TPS[1]], t_wk, wk[:, 1, 0:nb])
            k.op("dve", lambda en: en.tensor_tensor(out=wkb[:, 1, 0:nb], in0=wk[:, 0, 0:nb], in1=wk[:, 1, 0:nb], op=ALU.mult), reads=[t_wk], writes=[t_wkb])
            for h in range(4):
                pi = 2 + (h % 2)
                k.op("pe", lambda en: en.matmul(PS[pi][0:64, 0:nb], lhsT=lw.wuk[:, h * 64:(h + 1) * 64], rhs=wkb[:, 1, 0:nb], start=True, stop=True), reads=[t_wkb, lw.tok], writes=[TPS[pi]])
                k.op("act" if h % 2 else "dve", (lambda en: en.activation(out=KTm[h][0:64, t0:t0 + nb], in_=PS[pi][0:64, 0:nb], func=AF.Copy)) if h % 2 else (lambda en: en.tensor_copy(out=KTm[h][0:64, t0:t0 + nb], in_=PS[pi][0:64, 0:nb])), reads=[TPS[pi]], writes=[t_K])
            for j in range(nb // 128):
                ti = t0 // 128 + j
                k.op("pe", lambda en: en.matmul(PS[4][:, 0:256], lhsT=wkb[:, 1, j * 128:(j + 1) * 128], rhs=lw.wuv[:], start=True, stop=True), reads=[t_wkb, lw.tok], writes=[TPS[4]])
                k.op("dve", lambda en: en.tensor_copy(out=VM[:, ti, :].rearrange("p (h d) -> p h d", d=65)[:, :, 0:64], in_=PS[4][:, 0:256].rearrange("p (h d) -> p h d", d=64)), reads=[TPS[4]], writes=[t_K])
            proj(5, 32, wkv, 128, hT, t_h, nb, pbase=64)
            proj(6, 32, wkv, 160, hT, t_h, nb, pbase=64)
            k.op("dve", lambda en: en.tensor_tensor(out=wk[64:96, 2, 0:nb], in0=PS[5][64:96, 0:nb], in1=rp[64:96, 0, 0:nb], op=ALU.mult), reads=[TPS[5], t_rp], writes=[t_wk])
            k.op("dve", lambda en: en.tensor_tensor(out=wk[64:96, 3, 0:nb], in0=PS[6][64:96, 0:nb], in1=rp[64:96, 1, 0:nb], op=ALU.mult), reads=[TPS[6], t_rp], writes=[t_wk])
            for h in range(4):
                k.op("pool" if h % 2 else "dve", lambda en: en.tensor_tensor(out=KTm[h][64:96, t0:t0 + nb], in0=wk[64:96, 2, 0:nb], in1=wk[64:96, 3, 0:nb], op=ALU.add), reads=[t_wk], writes=[t_K])
            proj(7, 128, wkv, 192, hT, t_h, nb)
            proj(0, 128, wkv, 448, hT, t_h, nb)
            k.op("act", lambda en: en.activation(out=wkb[:, 2, 0:nb], in_=PS[7][:, 0:nb], func=AF.Square), reads=[TPS[7]], writes=[t_wkb])
            k.op("pe", lambda en: en.matmul(PS[1][:, 0:nb], lhsT=g.blk64[:], rhs=wkb[:, 2, 0:nb], start=True, stop=True), reads=[t_wkb, g.T_const], writes=[TPS[1]])
            rstd_from_ms(wk[:, 4, 0:nb], PS[1][:, 0:nb], [TPS[1]], t_wk, wk[:, 4, 0:nb])
            k.op("dve", lambda en: en.scalar_tensor_tensor(out=wk[:, 0, 0:nb], in0=PS[7][:, 0:nb], scalar=lw.gq[:, 2:3], in1=rp[:, 2, 0:nb], op0=ALU.mult, op1=ALU.mult), reads=[TPS[7], t_rp, lw.tok, t_wk], writes=[t_wk])
            k.op("dve", lambda en: en.scalar_tensor_tensor(out=wk[:, 5, 0:nb], in0=PS[0][:, 0:nb], scalar=lw.gq[:, 3:4], in1=rp[:, 3, 0:nb], op0=ALU.mult, op1=ALU.mult), reads=[TPS[0], t_rp, lw.tok, t_wk], writes=[t_wk])
            k.op("pool", lambda en: en.tensor_tensor(out=wk[:, 0, 0:nb], in0=wk[:, 0, 0:nb], in1=wk[:, 5, 0:nb], op=ALU.add), reads=[t_wk], writes=[t_wk])
            k.op("dve", lambda en: en.tensor_tensor(out=KTg[:, t0:t0 + nb], in0=wk[:, 0, 0:nb], in1=wk[:, 4, 0:nb], op=ALU.mult), reads=[t_wk], writes=[t_K])
            for j in range(nb // 128):
                ti = t0 // 128 + j
                for kc in range(8):
                    k.op("pe", lambda en: en.matmul(PS[4][:, 0:128], lhsT=hT[:, kc, j * 128:(j + 1) * 128], rhs=wkv[:, kc, 320:448], start=(kc == 0), stop=(kc == 7)), reads=[t_h, t_w], writes=[TPS[4]])
                k.op("act", lambda en: en.activation(out=VG[:, ti, :].rearrange("p (h d) -> p h d", d=65)[:, :, 0:64], in_=PS[4][:, 0:128].rearrange("p (h d) -> p h d", d=64), func=AF.Copy), reads=[TPS[4]], writes=[t_K])
        if "KTm0" in g.dbg:
            k.dma("pool", g.dbg["KTm0"], KTm[0][:], reads=[t_K])
            k.dma("pool", g.dbg["KTg"], KTg[:], reads=[t_K])
            k.dma("pool", g.dbg["VM"], VM[:], reads=[t_K])
            k.dma("pool", g.dbg["VG"], VG[:], reads=[t_K])

        k.barrier()
        esA.close()
        wq = sb("wq", [128, 8, 192 + 256 + 256 + 256 + 256], BF16)
        k.dma("sp", wq[:, :, 0:192], g.WINB[:, :, O_CQ:O_CQ + 192], reads=[g.T_WINB], writes=[t_w])
        k.dma("sp", wq[:, :, 192:448], g.WINB[:, :, O_GM:O_GM + 256], reads=[g.T_WINB], writes=[t_w])
        k.dma("sp", wq[:, :, 448:960], g.WINB[:, :, O_QM:O_QM + 512], reads=[g.T_WINB], writes=[t_w])
        k.dma("sp", wq[:, :, 960:1216], g.WINB[:, :, O_GG:O_GG + 256], reads=[g.T_WINB], writes=[t_w])
        qm_r = Rot([[sb("qm%d_%d" % (i, h), [96, 512], BF16) for h in range(4)] for i in range(2)])
        qg_r = Rot([[sb("qg%d_%d" % (i, j), [128, 512], BF16) for j in range(2)] for i in range(2)])
        gate_r = Rot([sb("gate%d" % i, [64, 8, 512], BF16) for i in range(1)])
        cq_r = Rot([sb("cq%d" % i, [128, 4, 512]) for i in range(1)])
        cqb_r = Rot([sb("cqb%d" % i, [128, 4, 512], BF16) for i in range(1)])
        P_r = Rot([sb("P%d" % i, [128, 2, 512], BF16) for i in range(5)])
        osb_r = Rot([sb("osb%d" % i, [65, 512]) for i in range(3)])
        res_r = Rot([sb("res%d" % i, [64, 512], BF16) for i in range(3)])
        O_bufs = [6, 7]
        for (t0, nb) in blocks:
            if t0 == 0 and not need_ctx:
                continue
            hT, t_h, rp, t_rp = load_block(t0, nb)
            kts = list(range(2)) if t0 == 0 else list(range(NT))
            qm, t_qm = qm_r.next()
            qg, t_qg = qg_r.next()
            gate, t_gate = gate_r.next()
            cq, t_cq = cq_r.next()
            cqb, t_cqb = cqb_r.next()
            for hh in range(8):
                c0 = (192 + hh * 64) if hh < 4 else (960 + (hh - 4) * 64)
                pi = 2 * (hh % 2)
                proj(pi, 64, wq, c0, hT, t_h, nb)
                k.op("act", lambda en: en.activation(out=gate[:, hh, 0:nb], in_=PS[pi][0:64, 0:nb], func=AF.Silu), reads=[TPS[pi]], writes=[t_gate])
            proj(0, 128, wq, 0, hT, t_h, nb)
            proj(2, 64, wq, 128, hT, t_h, nb)
            k.op("act", lambda en: en.activation(out=cqb[:, 0, 0:nb], in_=PS[0][:, 0:nb], func=AF.Square), reads=[TPS[0]], writes=[t_cqb])
            k.op("act", lambda en: en.activation(out=cqb[0:64, 1, 0:nb], in_=PS[2][0:64, 0:nb], func=AF.Square), reads=[TPS[2]], writes=[t_cqb])
            k.op("dve", lambda en: en.tensor_copy(out=cq[:, 0, 0:nb], in_=PS[0][:, 0:nb]), reads=[TPS[0]], writes=[t_cq])
            k.op("dve", lambda en: en.tensor_copy(out=cq[0:64, 1, 0:nb], in_=PS[2][0:64, 0:nb]), reads=[TPS[2]], writes=[t_cq])
            k.op("pe", lambda en: en.matmul(PS[0][:, 0:nb], lhsT=g.ones192[:], rhs=cqb[:, 0, 0:nb], start=True, stop=False), reads=[t_cqb, g.T_const, t_cq], writes=[TPS[0]])
            k.op("pe", lambda en: en.matmul(PS[0][:, 0:nb], lhsT=g.ones192[0:64, :], rhs=cqb[0:64, 1, 0:nb], start=False, stop=True), reads=[t_cqb, g.T_const], writes=[TPS[0]])
            rstd_from_ms(cq[:, 2, 0:nb], PS[0][:, 0:nb], [TPS[0]], t_cq, cq[:, 2, 0:nb])
            k.op("dve", lambda en: en.tensor_tensor(out=cqb[:, 2, 0:nb], in0=cq[:, 0, 0:nb], in1=cq[:, 2, 0:nb], op=ALU.mult), reads=[t_cq], writes=[t_cqb])
            k.op("dve", lambda en: en.tensor_tensor(out=cqb[0:64, 3, 0:nb], in0=cq[0:64, 1, 0:nb], in1=cq[0:64, 2, 0:nb], op=ALU.mult), reads=[t_cq], writes=[t_cqb])
            for h in range(4):
                for (pi, wa, wb_) in ((0, lw.wuq_a, lw.wuq_b), (2, lw.wuqp_a, lw.wuqp_b)):
                    k.op("pe", lambda en: en.matmul(PS[pi][0:96, 0:nb], lhsT=wa[:, h * 96:(h + 1) * 96], rhs=cqb[:, 2, 0:nb], start=True, stop=False), reads=[t_cqb, lw.tok], writes=[TPS[pi]])
                    k.op("pe", lambda en: en.matmul(PS[pi][0:96, 0:nb], lhsT=wb_[:, h * 96:(h + 1) * 96], rhs=cqb[0:64, 3, 0:nb], start=False, stop=True), reads=[t_cqb, lw.tok], writes=[TPS[pi]])
                k.op("dve", lambda en: en.tensor_tensor(out=cq[0:96, 3, 0:nb], in0=PS[0][0:96, 0:nb], in1=rp[0:96, 0, 0:nb], op=ALU.mult), reads=[TPS[0], t_rp, t_cq], writes=[t_cq])
                k.op("dve", lambda en: en.tensor_tensor(out=cq[0:96, 1, 0:nb], in0=PS[2][0:96, 0:nb], in1=rp[0:96, 1, 0:nb], op=ALU.mult), reads=[TPS[2], t_rp, t_cq], writes=[t_cq])
                k.op("pool", lambda en: en.tensor_tensor(out=qm[h][:, 0:nb], in0=cq[0:96, 3, 0:nb], in1=cq[0:96, 1, 0:nb], op=ALU.add), reads=[t_cq], writes=[t_qm])
            for j in range(2):
                proj(0, 128, wq, 448 + j * 128, hT, t_h, nb)
                proj(2, 128, wq, 704 + j * 128, hT, t_h, nb)
                k.op("act", lambda en: en.activation(out=cqb[:, 0, 0:nb], in_=PS[0][:, 0:nb], func=AF.Square), reads=[TPS[0], t_cqb], writes=[t_cqb])
                k.op("dve", lambda en: en.scalar_tensor_tensor(out=cq[:, 0, 0:nb], in0=PS[0][:, 0:nb], scalar=lw.gq[:, 0:1], in1=rp[:, 2, 0:nb], op0=ALU.mult, op1=ALU.mult), reads=[TPS[0], t_rp, lw.tok, t_cq], writes=[t_cq])
                k.op("dve", lambda en: en.scalar_tensor_tensor(out=cq[:, 1, 0:nb], in0=PS[2][:, 0:nb], scalar=lw.gq[:, 1:2], in1=rp[:, 3, 0:nb], op0=ALU.mult, op1=ALU.mult), reads=[TPS[2], t_rp, lw.tok, t_cq], writes=[t_cq])
                k.op("pe", lambda en: en.matmul(PS[0][:, 0:nb], lhsT=g.blk64[:], rhs=cqb[:, 0, 0:nb], start=True, stop=True), reads=[t_cqb, g.T_const, t_cq], writes=[TPS[0]])
                rstd_from_ms(cq[:, 2, 0:nb], PS[0][:, 0:nb], [TPS[0]], t_cq, cq[:, 2, 0:nb])
                k.op("pool", lambda en: en.tensor_tensor(out=cq[:, 0, 0:nb], in0=cq[:, 0, 0:nb], in1=cq[:, 1, 0:nb], op=ALU.add), reads=[t_cq], writes=[t_cq])
                k.op("dve", lambda en: en.tensor_tensor(out=qg[j][:, 0:nb], in0=cq[:, 0, 0:nb], in1=cq[:, 2, 0:nb], op=ALU.mult), reads=[t_cq], writes=[t_qg])
            if "qm0" in g.dbg and t0 == 256:
                k.dma("pool", g.dbg["qm0"], qm[0][:], reads=[t_qm])
                k.dma("pool", g.dbg["qg0"], qg[0][:], reads=[t_qg])
            pending = []
            for hh in range(8):
                while len(pending) > 1:
                    pending.pop(0)()
                if hh < 4:
                    dk, scale = 96, 96 ** -0.5
                    Kt = KTm[hh]
                    kb0 = 0
                    Qt = qm[hh]
                    qb0 = 0
                    Vt, vc0 = VM, hh * 65
                    tq = t_qm
                else:
                    hq = hh - 4
                    kv = hq // 2
                    dk, scale = 64, 64 ** -0.5
                    Kt, kb0 = KTg, kv * 64
                    Qt, qb0 = qg[hq % 2], kv * 64
                    Vt, vc0 = VG, kv * 65
                    tq = t_qg
                oi = O_bufs[hh % 2]
                pairs = [kts[i:i + 2] for i in range(0, len(kts), 2)]

                def emit_S(pidx):
                    sbi = pidx % 3
                    for j, kt in enumerate(pairs[pidx]):
                        k.op("pe", lambda en: en.matmul(SB2[sbi][:, j, 0:nb], lhsT=Kt[kb0:kb0 + dk, kt * 128:(kt + 1) * 128], rhs=Qt[qb0:qb0 + dk, 0:nb], start=True, stop=True), reads=[t_K, tq], writes=[TSB[sbi]])

                emit_S(0)
                if len(pairs) > 1:
                    emit_S(1)
                for pidx in range(len(pairs)):
                    if pidx == 3 and pending:
                        pending.pop(0)()
                    if pidx + 2 < len(pairs):
                        emit_S(pidx + 2)
                    sbi = pidx % 3
                    npr = len(pairs[pidx])
                    Pt, t_P = P_r.next()
                    k.op("act", lambda en: en.activation(out=Pt[:, 0:npr, 0:nb], in_=SB2[sbi][:, 0:npr, 0:nb], func=AF.Exp, scale=scale), reads=[TSB[sbi]], writes=[t_P])
                    for j, kt in enumerate(pairs[pidx]):
                        first = (pidx == 0 and j == 0)
                        lastm = (pidx == len(pairs) - 1 and j == npr - 1)
                        k.op("pe", lambda en: en.matmul(PS[oi][0:65, 0:nb], lhsT=Vt[:, kt, vc0:vc0 + 65], rhs=Pt[:, j, 0:nb], start=first, stop=lastm), reads=[t_P, t_K], writes=[TPS[oi]])
                osb, t_osb = osb_r.next()
                k.op("dve", lambda en: en.tensor_copy(out=osb[:, 0:nb], in_=PS[oi][0:65, 0:nb]), reads=[TPS[oi]], writes=[t_osb])
                k.op("dve", lambda en: en.reciprocal(out=osb[64:65, 0:nb], in_=osb[64:65, 0:nb]), reads=[t_osb], writes=[t_osb])

                def finish(hh=hh, oi=oi, osb=osb, t_osb=t_osb):
                    k.op("pe", lambda en: en.matmul(PS[oi][0:64, 0:nb], lhsT=g.onesf[64:65, 0:64], rhs=osb[64:65, 0:nb], start=True, stop=True), reads=[t_osb, g.T_const], writes=[TPS[oi]])
                    k.op("dve", lambda en: en.tensor_tensor(out=osb[0:64, 0:nb], in0=osb[0:64, 0:nb], in1=PS[oi][0:64, 0:nb], op=ALU.mult), reads=[t_osb, TPS[oi]], writes=[t_osb])
                    res, t_res = res_r.next()
                    k.op("pool", lambda en: en.tensor_tensor(out=res[:, 0:nb], in0=osb[0:64, 0:nb], in1=gate[:, hh, 0:nb], op=ALU.mult), reads=[t_osb, t_gate], writes=[t_res])
                    kc = hh // 2
                    p0 = (hh % 2) * 64
                    k.dma("pool", g.CATT[p0:p0 + 64, kc, t0:t0 + nb], res[:, 0:nb], reads=[t_res], writes=[g.T_CATT])
                pending.append(finish)
            while pending:
                pending.pop(0)()
        k.barrier()


def phase_P6(g, l, s, src, dst, need_ctx, lat_only_out):
    nc, k = g.nc, g.k
    with ExitStack() as es:
        sb = lambda n, s_, d=F32: es.enter_context(nc.sbuf_tensor(_nm(n), s_, d))
        ps = lambda n, s_, d=F32: es.enter_context(nc.psum_tensor(_nm(n), s_, d))
        wo = sb("wo", [128, 8, D], BF16)
        t_w = Tok()
        k.dma("sp", wo[:], g.WOUTB, reads=[g.T_WOUTB], writes=[t_w])
        Gb = {}
        for v in ((s, 2) if need_ctx else (s,)):
            Gb[v] = sb("Gb%d" % v, [128, D])
            k.dma("sp", Gb[v][:], g.MODROWS[v:v + 1, 2 * D:3 * D].broadcast_to([128, D]), reads=[g.T_MOD], writes=[t_w])
        neghalf = sb("neghalf6", [128, 1])
        k.op("pool", lambda en: en.memset(neghalf[:], -0.5), writes=[t_w])
        cat_r = Rot([sb("cat%d" % i, [128, 8, 512], BF16) for i in range(2)])
        x_r = Rot([sb("x6_%d" % i, [128, D]) for i in range(3)])
        o_r = Rot([sb("o6_%d" % i, [128, D]) for i in range(3)])
        st_r = Rot([sb("st6_%d" % i, [128, 4]) for i in range(3)])
        junk = sb("junk6", [128, D], BF16)
        t_junk = Tok()
        po_r = Rot([ps("po%d" % i, [128, D]) for i in range(3)], excl=True)
        blocks = ([(0, 256)] if need_ctx else []) + [(256 + 512 * b, 512) for b in range(8)]
        for (t0, nb) in blocks:
            cat, t_cat = cat_r.next()
            k.dma("sp", cat[:, :, 0:nb], g.CATT[:, :, t0:t0 + nb], reads=[g.T_CATT], writes=[t_cat])
            for j in range(nb // 128):
                tok0 = t0 + j * 128
                po, t_po = po_r.next()
                for hf in range(2):
                    for kc in range(8):
                        k.op("pe", lambda en: en.matmul(po[:, hf * 512:(hf + 1) * 512], lhsT=cat[:, kc, j * 128:(j + 1) * 128], rhs=wo[:, kc, hf * 512:(hf + 1) * 512], start=(kc == 0), stop=(kc == 7)), reads=[t_cat, t_w], writes=[t_po])
                xt, t_x = x_r.next()
                k.dma("sp", xt[:], src[s, tok0:tok0 + 128, :], reads=[g.T_XS[s]], writes=[t_x])
                st, t_st = st_r.next()
                k.op("act", lambda en: en.activation(out=junk[:], in_=po[:], func=AF.Square, accum_out=st[:, 0:1]), reads=[t_po], writes=[t_junk, t_st])
                k.op("dve", lambda en: en.tensor_scalar(out=st[:, 1:2], in0=st[:, 0:1], scalar1=1.0 / D, scalar2=EPS, op0=ALU.mult, op1=ALU.add), reads=[t_st], writes=[t_st])
                k.op("pool", lambda en: en.tensor_tensor(out=st[:, 2:3], in0=st[:, 1:2], in1=neghalf[:], op=ALU.pow), reads=[t_st, t_w], writes=[t_st])
                ot, t_o = o_r.next()
                gb = Gb[2] if t0 == 0 else Gb[s]
                k.op("dve", lambda en: en.scalar_tensor_tensor(out=ot[:], in0=po[:], scalar=st[:, 2:3], in1=gb[:], op0=ALU.mult, op1=ALU.mult), reads=[t_po, t_st, t_w], writes=[t_o])
                k.op("pool", lambda en: en.tensor_tensor(out=ot[:], in0=ot[:], in1=xt[:], op=ALU.add), reads=[t_o, t_x], writes=[t_o])
                if lat_only_out:
                    if t0 == 0:
                        continue
                    k.dma("pool", dst[s, tok0 - C:tok0 - C + 128, :], ot[:], reads=[t_o], writes=[g.T_Y])
                else:
                    wt = g.T_XS[s] if dst is g.XS else g.T_Y
                    k.dma("pool", dst[s, tok0:tok0 + 128, :], ot[:], reads=[t_o], writes=[wt])
        k.barrier()


_CACHE = {}


def _weights_host(inputs):
    w = {}
    for n in ("w_mod", "b_mod", "g_pre", "g_post", "w_in", "w_out", "mla_g_cq", "mla_w_uq", "mla_g_ckv", "mla_w_ukv",
              "hy_conv_w", "hy_conv_b", "hy_f_w1", "hy_f_b1", "hy_f_freq1", "hy_f_w2", "hy_f_b2", "hy_f_freq2", "hy_f_w3", "hy_bias"):
        w[n] = np.ascontiguousarray(inputs[n], dtype=np.float32)
    p64, _ = _partner_index(64)
    gq = np.asarray(inputs["gqa_g_q"], np.float32)
    gk = np.asarray(inputs["gqa_g_k"], np.float32)
    cols = np.zeros((DEPTH, 128, 4), np.float32)
    for l in range(DEPTH):
        cols[l, :, 0] = np.concatenate([gq[l], gq[l]])
        cols[l, :, 1] = np.concatenate([gq[l][p64], gq[l][p64]])
        cols[l, :, 2] = np.concatenate([gk[l], gk[l]])
        cols[l, :, 3] = np.concatenate([gk[l][p64], gk[l][p64]])
    w["gq_cols"] = cols
    w.update(ssm_host_layouts(inputs))
    return w


def make_in_maps(inputs, xs_per_core):
    w = _weights_host(inputs)
    cst = host_constants()
    c = np.asarray(inputs["c"], np.float32)
    c_ctx = np.asarray(inputs["c_ctx"], np.float32)
    maps = []
    for i in range(NCORES):
        cv = np.stack([c[2 * i], c[2 * i + 1], c_ctx], -1)
        cT = np.ascontiguousarray(cv.reshape(8, 128, 3).transpose(1, 0, 2))
        m = {"xs": xs_per_core[i], "cT": cT}
        m.update(w)
        m.update(cst)
        maps.append(m)
    return maps


def kernel(**inputs):
    x = np.asarray(inputs["x"], np.float32)
    ctx = np.asarray(inputs["ctx"], np.float32)
    xs = [np.ascontiguousarray(np.concatenate([ctx[2 * i:2 * i + 2], x[2 * i:2 * i + 2]], axis=1)) for i in range(NCORES)]
    key = "fused"
    if key not in _CACHE:
        _CACHE[key] = build_program(list(range(DEPTH)), True)[0]
    nc = _CACHE[key]
    res = run_bass_kernel_spmd(nc, make_in_maps(inputs, xs), core_ids=list(range(NCORES)))
    out = np.concatenate([r["y"] for r in res.results], axis=0)
    return out.astype(np.float32)


HY_BANDS = 16
HY_DECAY = (math.log(1e-2) / 1.5, math.log(1e-2) / 0.3)


class HyCfg:
    def __init__(self, name, n, ni):
        self.name = name
        self.n = n
        self.ni = ni
        self.N = 2 * n
        self.cpg = 128 // ni
        self.ng = 256 // self.cpg
        self.ncol = 256 * ni


HY_LAT = HyCfg("L", 4096, 32)
HY_CTX = HyCfg("C", 256, 2)


def _hy_features(n, pos):
    f32 = np.float32
    t = np.linspace(0.0, 1.0, n, dtype=f32)
    omega = (f32(2.0 * math.pi) * np.arange(n, dtype=f32) / f32(n)).astype(f32)
    bands = np.linspace(1e-4, HY_BANDS - 1, HY_BANDS, dtype=f32)
    tt = t[pos]
    om = omega[pos]
    ang = (bands[:, None] * om[None, :]).astype(f32)
    z = np.concatenate([tt[None, :], np.cos(ang), -np.sin(ang)], axis=0).astype(f32)
    return z, tt


def hyena_constants():
    cst = {}
    f64 = np.float64
    j = np.arange(128)[:, None]
    b = np.arange(256)[None, :]
    ang = 2 * np.pi * j * b / 256.0
    F1 = np.concatenate([np.cos(ang), -np.sin(ang)], 1)
    sgn = np.where(b % 2 == 0, 1.0, -1.0)
    F1b = np.concatenate([np.cos(ang) * sgn, -np.sin(ang) * sgn], 1)
    cst["hk_F1"] = np.stack([F1, F1b], 0).astype(np.float32)
    for cfg in (HY_LAT, HY_CTX):
        p = np.arange(128)
        i_of_p = (p // cfg.cpg)[:, None]
        ang = 2 * np.pi * i_of_p * b / cfg.N
        TW = np.stack([np.concatenate([np.cos(ang), np.cos(ang)], 1), np.concatenate([-np.sin(ang), -np.sin(ang)], 1)], 0)
        cst["hk_TW" + cfg.name] = TW.astype(np.float32)
        ii = (p // cfg.cpg)[:, None]
        ci = (p % cfg.cpg)[:, None]
        aa = (p // cfg.cpg)[None, :]
        ca = (p % cfg.cpg)[None, :]
        dl = (ci == ca).astype(f64)
        ang = 2 * np.pi * ii * aa / cfg.ni
        Gr = dl * np.cos(ang)
        Gi = -dl * np.sin(ang)
        cst["hk_G" + cfg.name] = np.stack([Gr, Gi, -Gi], 0).astype(np.float32)
        ang = 2 * np.pi * aa.T * ii.T / cfg.ni
        dl2 = (ci.T == ca.T)
        angm = 2 * np.pi * (p // cfg.cpg)[:, None] * (p // cfg.cpg)[None, :] / cfg.ni
        dlm = ((p % cfg.cpg)[:, None] == (p % cfg.cpg)[None, :]).astype(f64)
        GIr = dlm * np.cos(angm)
        GIi = dlm * np.sin(angm)
        cst["hk_GI" + cfg.name] = np.stack([np.concatenate([GIr, GIi], 1), np.concatenate([-GIi, GIr], 1)], 0).astype(np.float32)
        TWI = np.zeros((2, 2, 128, 256), f64)
        for bc in range(2):
            bb = (bc * 128 + np.arange(128))[:, None]
            ang = 2 * np.pi * bb * (p // cfg.cpg)[None, :] / cfg.N
            TWI[bc, 0] = np.concatenate([np.cos(ang), np.cos(ang)], 1)
            TWI[bc, 1] = np.concatenate([np.sin(ang), np.sin(ang)], 1)
        cst["hk_TWI" + cfg.name] = TWI.astype(np.float32)
        FI = np.zeros((2, 2, 128, 128), f64)
        for bc in range(2):
            bb = (bc * 128 + np.arange(128))[:, None]
            ang = 2 * np.pi * bb * np.arange(128)[None, :] / 256.0
            FI[bc, 0] = np.cos(ang) / cfg.N
            FI[bc, 1] = -np.sin(ang) / cfg.N
        cst["hk_FI" + cfg.name] = FI.astype(np.float32)
        n = cfg.n
        u = np.arange(n)
        posr = np.where(u == 0, 0, n - u)
        zf, tf_ = _hy_features(n, u)
        zb, tb_ = _hy_features(n, posr)
        cst["hk_Z" + cfg.name] = np.stack([zf, zb], 0)
        deltas = np.abs(np.linspace(HY_DECAY[0], HY_DECAY[1], 256, dtype=np.float32))
        decf = np.exp(-tf_[:, None] * deltas[None, :]).astype(np.float32)
        decb = np.exp(-tb_[:, None] * deltas[None, :]).astype(np.float32)
        cst["hk_DEC" + cfg.name] = np.stack([decf.reshape(128, cfg.ni, 256), decb.reshape(128, cfg.ni, 256)], 0)
        msk = (p[:, None] % cfg.cpg == np.arange(cfg.cpg)[None, :]).astype(np.float32)
        cst["hk_MSK" + cfg.name] = msk
    return cst


def _hy_twiddle(k, A_ps, t_A, TW, t_TW, m1, m2, t_m, outr, outi, t_out, n2, sub_first=True):
    k.op("dve", lambda en: en.tensor_tensor(out=m1[:, 0:2 * n2], in0=A_ps, in1=TW[:, 0, 0:2 * n2], op=ALU.mult), reads=[t_A, t_TW], writes=[t_m])
    k.op("dve", lambda en: en.tensor_tensor(out=m2[:, 0:2 * n2], in0=A_ps, in1=TW[:, 1, 0:2 * n2], op=ALU.mult), reads=[t_A, t_TW, t_m], writes=[t_m])
    k.op("pool", lambda en: en.tensor_tensor(out=outr, in0=m1[:, 0:n2], in1=m2[:, n2:2 * n2], op=ALU.subtract), reads=[t_m], writes=[t_out])
    k.op("pool", lambda en: en.tensor_tensor(out=outi, in0=m2[:, 0:n2], in1=m1[:, n2:2 * n2], op=ALU.add), reads=[t_m, t_out], writes=[t_out])


def phase_hy_filter(g, l, cfg):
    nc, k, W, K = g.nc, g.k, g.W, g.K
    n, ni, ng, cpg, ncol = cfg.n, cfg.ni, cfg.ng, cfg.cpg, cfg.ncol
    KH = g.KHAT[cfg.name]
    with ExitStack() as es:
        sb = lambda nm, s_, d=F32: es.enter_context(nc.sbuf_tensor(_nm(nm), s_, d))
        ps = lambda nm, s_, d=F32: es.enter_context(nc.psum_tensor(_nm(nm), s_, d))
        t_w = Tok()
        w1 = sb("hw1", [33, 64]); w2 = sb("hw2", [64, 64]); w3 = sb("hw3", [64, 1024])
        cols = sb("hcols", [64, 8])
        k.dma("sp", w1[:], W["hy_f_w1"][l], writes=[t_w])
        k.dma("sp", w2[:], W["hy_f_w2"][l], writes=[t_w])
        k.dma("sp", w3[:], W["hy_f_w3"][l], writes=[t_w])
        for ci, nm in enumerate(("hy_f_b1", "hy_f_freq1", "hy_f_b2", "hy_f_freq2")):
            k.dma("sp", cols[:, ci:ci + 1], W[nm][l].rearrange("(p o) -> p o", o=1), writes=[t_w])
        k.op("dve", lambda en: en.tensor_tensor(out=cols[:, 4:5], in0=cols[:, 0:1], in1=cols[:, 1:2], op=ALU.mult), reads=[t_w], writes=[t_w])
        k.op("dve", lambda en: en.tensor_tensor(out=cols[:, 5:6], in0=cols[:, 2:3], in1=cols[:, 3:4], op=ALU.mult), reads=[t_w], writes=[t_w])
        F1 = sb("hF1", [128, 2, 512]); TW = sb("hTW", [128, 2, 512]); G3 = sb("hG", [128, 3, 128]); MSK = sb("hmsk", [128, cpg])
        t_c = Tok()
        k.dma("sp", F1[:], K["hk_F1"].rearrange("a p n -> p a n"), writes=[t_c])
        k.dma("sp", TW[:], K["hk_TW" + cfg.name].rearrange("a p n -> p a n"), writes=[t_c])
        k.dma("sp", G3[:], K["hk_G" + cfg.name].rearrange("a p n -> p a n"), writes=[t_c])
        k.dma("sp", MSK[:], K["hk_MSK" + cfg.name], writes=[t_c])
        epsc = sb("hyeps", [128, 1])
        k.op("pool", lambda en: en.memset(epsc[:], EPS), writes=[t_c])
        h2T = [sb("h2T%d" % d_, [64, n]) for d_ in range(2)]
        t_h2 = Tok()
        PSm = [ps("hps%d" % i, [128, 512]) for i in range(4)]
        TP = [Tok(True) for _ in range(4)]
        PI = math.pi

        def sin_layer(out_ap, ps_ap, t_ps, fr_col, fb_col, tmp, msk_, t_tmp, wtok):
            k.op("dve", lambda en: en.tensor_scalar(out=tmp, in0=ps_ap, scalar1=fr_col, scalar2=fb_col, op0=ALU.mult, op1=ALU.add), reads=[t_ps, t_w], writes=[t_tmp])
            k.op("dve", lambda en: en.tensor_scalar(out=msk_, in0=tmp, scalar1=PI, scalar2=-2 * PI, op0=ALU.is_gt, op1=ALU.mult), reads=[t_tmp], writes=[t_tmp])
            k.op("dve", lambda en: en.tensor_tensor(out=tmp, in0=tmp, in1=msk_, op=ALU.add), reads=[t_tmp], writes=[t_tmp])
            k.op("dve", lambda en: en.tensor_scalar(out=msk_, in0=tmp, scalar1=-PI, scalar2=2 * PI, op0=ALU.is_lt, op1=ALU.mult), reads=[t_tmp], writes=[t_tmp])
            k.op("dve", lambda en: en.tensor_tensor(out=tmp, in0=tmp, in1=msk_, op=ALU.add), reads=[t_tmp], writes=[t_tmp])
            k.op("act", lambda en: en.activation(out=out_ap, in_=tmp, func=AF.Sin), reads=[t_tmp], writes=[wtok])

        with ExitStack() as es2:
            sb2 = lambda nm, s_, d=F32: es2.enter_context(nc.sbuf_tensor(_nm(nm), s_, d))
            Zt = sb2("hZ", [33, n])
            t_z = Tok()
            h1 = sb2("hh1", [64, 512]); tmp = sb2("htmp", [64, 512]); msk_ = sb2("hmk", [64, 512])
            t_h1 = Tok(); t_tmp = Tok()
            bw = min(512, n)
            for d_ in range(2):
                k.dma("sp", Zt[:], K["hk_Z" + cfg.name][d_], reads=[], writes=[t_z])
                for b0 in range(0, n, bw):
                    k.op("pe", lambda en: en.matmul(PSm[0][0:64, 0:bw], lhsT=w1[:], rhs=Zt[:, b0:b0 + bw], start=True, stop=True), reads=[t_w, t_z], writes=[TP[0]])
                    sin_layer(h1[:, 0:bw], PSm[0][0:64, 0:bw], TP[0], cols[:, 1:2], cols[:, 4:5], tmp[:, 0:bw], msk_[:, 0:bw], t_tmp, t_h1)
                    k.op("pe", lambda en: en.matmul(PSm[1][0:64, 0:bw], lhsT=w2[:], rhs=h1[:, 0:bw], start=True, stop=True), reads=[t_w, t_h1], writes=[TP[1]])
                    sin_layer(h2T[d_][:, b0:b0 + bw], PSm[1][0:64, 0:bw], TP[1], cols[:, 3:4], cols[:, 5:6], tmp[:, 0:bw], msk_[:, 0:bw], t_tmp, t_h2)
            k.barrier()
        UF = [sb("hUF%d" % d_, [128, ncol]) for d_ in range(2)]
        t_uf = Tok()
        DEC = sb("hDEC", [128, ni, 256])
        t_dec = Tok()
        HSQ = sb("hHSQ", [128, 256]); sq = sb("hsq", [128, 256]); hh = sb("hhh", [128, 256])
        t_hsq = Tok(); t_hh = Tok(); t_sq = Tok()
        RS = sb("hRS", [128, 256]); SC = sb("hSC", [128, ng]); rtmp = sb("hrtmp", [128, 256])
        t_rs = Tok()
        fmm_r = Rot([(sb("hm1_%d" % i, [128, 512]), sb("hm2_%d" % i, [128, 512])) for i in range(3)])
        fP1_r = Rot([sb("hP1_%d" % i, [128, 2, 256]) for i in range(3)])
        ko_r = Rot([sb("hko%d" % i, [128, 512]) for i in range(3)])
        for o in range(2):
            k.op("pool", lambda en: en.memset(HSQ[:], 0.0), reads=[t_rs], writes=[t_hsq])
            for d_ in range(2):
                k.dma("sp", DEC[:], K["hk_DEC" + cfg.name][d_], writes=[t_dec])
                c0 = o * 512 + d_ * 256
                for i in range(ni):
                    pi_ = i % 2
                    k.op("pe", lambda en: en.matmul(PSm[pi_][:, 0:256], lhsT=h2T[d_][:, i:n:ni], rhs=w3[:, c0:c0 + 256], start=True, stop=True), reads=[t_h2, t_w], writes=[TP[pi_]])
                    k.op("dve", lambda en: en.tensor_tensor(out=hh[:], in0=PSm[pi_][:, 0:256], in1=DEC[:, i, :], op=ALU.mult), reads=[TP[pi_], t_dec], writes=[t_hh])
                    k.op("act", lambda en: en.activation(out=UF[d_][:].rearrange("p (g i c) -> p g i c", i=ni, c=cpg)[:, :, i, :], in_=hh[:].rearrange("p (g c) -> p g c", c=cpg), func=AF.Copy), reads=[t_hh], writes=[t_uf])
                    k.op("act", lambda en: en.activation(out=sq[:], in_=hh[:], func=AF.Square), reads=[t_hh], writes=[t_sq])
                    k.op("pool", lambda en: en.tensor_tensor(out=HSQ[:], in0=HSQ[:], in1=sq[:], op=ALU.add), reads=[t_sq, t_hsq], writes=[t_hsq])
            k.op("pe", lambda en: en.matmul(PSm[2][:, 0:256], lhsT=g.onesf[:], rhs=HSQ[:], start=True, stop=True), reads=[t_hsq, g.T_const], writes=[TP[2]])
            k.op("act", lambda en: en.activation(out=rtmp[:], in_=PSm[2][:, 0:256], func=AF.Ln, bias=epsc[:, 0:1]), reads=[TP[2], t_c], writes=[t_rs])
            k.op("act", lambda en: en.activation(out=RS[:], in_=rtmp[:], func=AF.Exp, scale=-0.5), reads=[t_rs], writes=[t_rs])
            k.op("dve", lambda en: en.tensor_tensor(out=rtmp[:].rearrange("p (g c) -> p g c", c=cpg), in0=RS[:].rearrange("p (g c) -> p g c", c=cpg), in1=MSK[:].unsqueeze(1).broadcast_to([128, ng, cpg]), op=ALU.mult), reads=[t_rs, t_c], writes=[t_rs])
            k.op("dve", lambda en: en.tensor_reduce(out=SC[:], in_=rtmp[:].rearrange("p (g c) -> p g c", c=cpg), axis=AX.X, op=ALU.add), reads=[t_rs], writes=[t_rs])
            k.op("dve", lambda en: en.memset(UF[1][0:1, :].rearrange("p (g i c) -> p g i c", i=ni, c=cpg)[:, :, 0, :], 0.0), reads=[t_uf], writes=[t_uf])
            fst = {}

            def F_S1(grp):
                b0 = grp % 2
                (m1_, m2_), t_m_ = fmm_r.next()
                P1_, t_p1_ = fP1_r.next()
                fst[grp] = (P1_, t_p1_)
                k.op("pe", lambda en: en.matmul(PSm[b0][:], lhsT=UF[0][:, grp * 128:(grp + 1) * 128], rhs=F1[:, 0, :], start=True, stop=False), reads=[t_uf, t_c], writes=[TP[b0]])
                k.op("pe", lambda en: en.matmul(PSm[b0][:], lhsT=UF[1][:, grp * 128:(grp + 1) * 128], rhs=F1[:, 1, :], start=False, stop=True), reads=[t_uf, t_c], writes=[TP[b0]])
                _hy_twiddle(k, PSm[b0][:], TP[b0], TW, t_c, m1_, m2_, t_m_, P1_[:, 0, :], P1_[:, 1, :], t_p1_, 256)

            def F_S2(grp):
                b1 = 2 + grp % 2
                P1_, t_p1_ = fst.pop(grp)
                k.op("pe", lambda en: en.matmul(PSm[b1][:, 0:256], lhsT=G3[:, 0, :], rhs=P1_[:, 0, :], start=True, stop=False), reads=[t_p1_, t_c], writes=[TP[b1]])
                k.op("pe", lambda en: en.matmul(PSm[b1][:, 0:256], lhsT=G3[:, 2, :], rhs=P1_[:, 1, :], start=False, stop=True), reads=[t_p1_, t_c], writes=[TP[b1]])
                k.op("pe", lambda en: en.matmul(PSm[b1][:, 256:512], lhsT=G3[:, 0, :], rhs=P1_[:, 1, :], start=False, stop=False), reads=[t_p1_, t_c], writes=[TP[b1]])
                k.op("pe", lambda en: en.matmul(PSm[b1][:, 256:512], lhsT=G3[:, 1, :], rhs=P1_[:, 0, :], start=False, stop=True), reads=[t_p1_, t_c], writes=[TP[b1]])
                ko, t_ko = ko_r.next()
                k.op("dve", lambda en: en.tensor_scalar(out=ko[:], in0=PSm[b1][:], scalar1=SC[:, grp:grp + 1], scalar2=None, op0=ALU.mult), reads=[TP[b1], t_rs], writes=[t_ko])
                k.dma("pool", KH[o, grp], ko[:], reads=[t_ko], writes=[g.T_KHAT])

            for t_ in range(ng + 1):
                if t_ < ng:
                    F_S1(t_)
                if t_ >= 1:
                    F_S2(t_ - 1)
        k.barrier()


def phase_hyena(g, l, s, cfg, tok0):
    nc, k, W, K = g.nc, g.k, g.W, g.K
    n, ni, ng, cpg, ncol = cfg.n, cfg.ni, cfg.ng, cfg.cpg, cfg.ncol
    KH = g.KHAT[cfg.name]
    with ExitStack() as es:
        sb = lambda nm, s_, d=F32: es.enter_context(nc.sbuf_tensor(_nm(nm), s_, d))
        ps = lambda nm, s_, d=F32: es.enter_context(nc.psum_tensor(_nm(nm), s_, d))
        PSm = [ps("yps%d" % i, [128, 512]) for i in range(6)]
        TP = [Tok(True) for _ in range(6)]
        PT = [ps("ypt%d" % i, [128, 8, 128], BF16) for i in range(2)]
        TPT = [Tok(True) for _ in range(2)]
        t_c = Tok()
        cf = sb("ycf", [128, 1024])
        F1b = sb("yF1", [128, 512], BF16); G3 = sb("yG", [128, 3, 128], BF16); GI = sb("yGI", [128, 2, 256], BF16); FI = sb("yFI", [128, 2, 2, 128], BF16)
        TW = sb("yTW", [128, 2, 512]); TWI = sb("yTWI", [128, 2, 2, 256])
        k.dma("sp", cf[:, 0:512], K["hk_F1"][0], writes=[t_c])
        k.op("dve", lambda en: en.tensor_copy(out=F1b[:], in_=cf[:, 0:512]), reads=[t_c], writes=[t_c])
        k.dma("sp", cf[:, 0:384].rearrange("p (a n) -> p a n", a=3), K["hk_G" + cfg.name].rearrange("a p n -> p a n"), reads=[t_c], writes=[t_c])
        k.op("dve", lambda en: en.tensor_copy(out=G3[:], in_=cf[:, 0:384].rearrange("p (a n) -> p a n", a=3)), reads=[t_c], writes=[t_c])
        k.dma("sp", cf[:, 0:512].rearrange("p (a n) -> p a n", a=2), K["hk_GI" + cfg.name].rearrange("a p n -> p a n"), reads=[t_c], writes=[t_c])
        k.op("dve", lambda en: en.tensor_copy(out=GI[:], in_=cf[:, 0:512].rearrange("p (a n) -> p a n", a=2)), reads=[t_c], writes=[t_c])
        k.dma("sp", cf[:, 0:512].rearrange("p (b a n) -> p b a n", b=2, a=2), K["hk_FI" + cfg.name].rearrange("b a p n -> p b a n"), reads=[t_c], writes=[t_c])
        k.op("dve", lambda en: en.tensor_copy(out=FI[:], in_=cf[:, 0:512].rearrange("p (b a n) -> p b a n", b=2, a=2)), reads=[t_c], writes=[t_c])
        k.dma("sp", TW[:], K["hk_TW" + cfg.name].rearrange("a p n -> p a n"), writes=[t_c])
        k.dma("sp", TWI[:], K["hk_TWI" + cfg.name].rearrange("b a p n -> p b a n"), writes=[t_c])
        cw = sb("ycw", [128, 6, 3]); cb = sb("ycb", [128, 6]); BR = sb("yBR", [128, 2, 256])
        for kk in range(3):
            k.dma("sp", cw[:, :, kk:kk + 1], W["hy_conv_w"][l, kk, :].rearrange("(c p o) -> p c o", p=128, o=1), writes=[t_c], allow_slow_non_contiguous=True)
        k.dma("sp", cb[:].unsqueeze(2), W["hy_conv_b"][l].rearrange("(c p o) -> p c o", p=128, o=1), writes=[t_c], allow_slow_non_contiguous=True)
        k.dma("sp", BR[:], W["hy_bias"][l:l + 1].broadcast_to([128, 2, 256]), writes=[t_c])
        U = {nm: sb("yU" + nm, [128, ncol], BF16) for nm in ("gt", "x2", "v", "x1")}
        t_U = {nm: Tok() for nm in ("gt", "x2", "v", "x1", "z1")}
        names = ["v", "v", "x1", "x1", "x2", "x2", "gt", "gt"]
        with ExitStack() as es1:
            sb1 = lambda nm, s_, d=F32: es1.enter_context(nc.sbuf_tensor(_nm(nm), s_, d))
            hT = sb1("yhT", [128, 8, n], BF16)
            whb_r = Rot([sb1("ywhb%d" % i, [128, 8, 128], BF16) for i in range(2)])
            t_h = Tok()
            k.dma("sp", hT[:], g.HT[:, :, tok0:tok0 + n], reads=[g.T_HT], writes=[t_h])
            PJ = sb1("yPJ", [128, n]); CV = sb1("yCV", [128, n]); CVb = sb1("yCVb", [128, n], BF16)
            t_pj = Tok(); t_cv = Tok(); t_cvb = Tok()
            bw = min(512, n)
            for c8 in range(8):
                whb, t_whb = whb_r.next()
                k.dma("sp", whb[:], g.WINB[:, :, O_HY + c8 * 128:O_HY + (c8 + 1) * 128], reads=[g.T_WINB], writes=[t_whb])
                for bi, b0 in enumerate(range(0, n, bw)):
                    pi_ = bi % 2
                    for kc in range(8):
                        k.op("pe", lambda en: en.matmul(PSm[pi_][:, 0:bw], lhsT=whb[:, kc, :], rhs=hT[:, kc, b0:b0 + bw], start=(kc == 0), stop=(kc == 7)), reads=[t_h, t_whb], writes=[TP[pi_]])
                    if c8 < 6:
                        k.op("act", lambda en: en.activation(out=PJ[:, b0:b0 + bw], in_=PSm[pi_][:, 0:bw], func=AF.Copy), reads=[TP[pi_]], writes=[t_pj])
                    else:
                        k.op("act", lambda en: en.activation(out=CVb[:, b0:b0 + bw], in_=PSm[pi_][:, 0:bw], func=AF.Silu), reads=[TP[pi_]], writes=[t_cvb])
                if c8 < 6:
                    k.op("dve", lambda en: en.tensor_scalar(out=CV[:], in0=PJ[:], scalar1=cw[:, c8, 1:2], scalar2=cb[:, c8:c8 + 1], op0=ALU.mult, op1=ALU.add), reads=[t_pj, t_c], writes=[t_cv])
                    k.op("dve", lambda en: en.scalar_tensor_tensor(out=CV[:, 1:n], in0=PJ[:, 0:n - 1], scalar=cw[:, c8, 0:1], in1=CV[:, 1:n], op0=ALU.mult, op1=ALU.add), reads=[t_pj, t_c, t_cv], writes=[t_cv])
                    k.op("dve", lambda en: en.scalar_tensor_tensor(out=CVb[:, 0:n - 1], in0=PJ[:, 1:n], scalar=cw[:, c8, 2:3], in1=CV[:, 0:n - 1], op0=ALU.mult, op1=ALU.add), reads=[t_pj, t_c, t_cv], writes=[t_cvb])
                    k.op("dve", lambda en: en.tensor_copy(out=CVb[:, n - 1:n], in_=CV[:, n - 1:n]), reads=[t_cv, t_cvb], writes=[t_cvb])
                nm = names[c8]
                g0 = (c8 % 2) * (128 // cpg)
                Uv = U[nm][:].rearrange("p (g i c) -> p g i c", i=ni, c=cpg)
                nb4 = min(4, ni)
                for i0 in range(0, ni, nb4):
                    pt = (i0 // nb4) % 2
                    for ii in range(nb4):
                        k.op("pe", lambda en: en.transpose(PT[pt][:, ii, :], CVb[:, i0 + ii:n:ni], g.ident[:]), reads=[t_cvb, g.T_ident], writes=[TPT[pt]])
                    k.op("act" if (i0 // nb4) % 2 else "dve",
                         (lambda en: en.activation(out=Uv[:, g0:g0 + 128 // cpg, i0:i0 + nb4, :].rearrange("p g i c -> p i g c"), in_=PT[pt][:, 0:nb4, :].rearrange("p i (g c) -> p i g c", c=cpg), func=AF.Copy)) if (i0 // nb4) % 2 else
                         (lambda en: en.tensor_copy(out=Uv[:, g0:g0 + 128 // cpg, i0:i0 + nb4, :].rearrange("p g i c -> p i g c"), in_=PT[pt][:, 0:nb4, :].rearrange("p i (g c) -> p i g c", c=cpg))),
                         reads=[TPT[pt]], writes=[t_U[nm]])
            k.barrier()
        with ExitStack() as es2:
            sb2 = lambda nm, s_, d=F32: es2.enter_context(nc.sbuf_tensor(_nm(nm), s_, d))
            U["z1"] = sb2("yUz1", [128, ncol], BF16)
            BW = [[sb2("yBW%d%d" % (bc, ri), [128, ncol], BF16) for ri in range(2)] for bc in range(2)]
            t_bw = Tok()
            mm_r = Rot([(sb2("ym1_%d" % i, [128, 512]), sb2("ym2_%d" % i, [128, 512])) for i in range(3)])
            tw_r = Rot([(sb2("yq1_%d" % i, [128, 512], BF16), sb2("yq2_%d" % i, [128, 512], BF16)) for i in range(3)])
            yy_r = Rot([(sb2("yy1_%d" % i, [128, 512], BF16), sb2("yy2_%d" % i, [128, 512], BF16)) for i in range(3)])
            G4 = sb2("yG4", [128, 4, 128], BF16)
            GI3 = sb2("yGI3", [128, 3, 256], BF16)
            k.op("dve", lambda en: en.tensor_copy(out=G4[:, 0:3, :], in_=G3[:]), reads=[t_c], writes=[t_c])
            k.op("dve", lambda en: en.tensor_scalar(out=G4[:, 3, :], in0=G3[:, 0, :], scalar1=-1.0, scalar2=None, op0=ALU.mult), reads=[t_c], writes=[t_c])
            k.op("dve", lambda en: en.tensor_copy(out=GI3[:, 0:2, :], in_=GI[:]), reads=[t_c], writes=[t_c])
            k.op("dve", lambda en: en.tensor_scalar(out=GI3[:, 2, :], in0=GI[:, 0, :], scalar1=-1.0, scalar2=None, op0=ALU.mult), reads=[t_c], writes=[t_c])
            kh_r = Rot([sb2("ykh%d" % i, [128, 512]) for i in range(2)])
            e1 = sb2("ye1", [128, 512]); e2 = sb2("ye2", [128, 512]); e3 = sb2("ye3", [128, 512]); t_e = Tok()
            gpt = 512 // (ni * cpg)

            def conv(src_nm, o, epilogue):
                Uin = U[src_nm]
                st = {}

                def S1(grp):
                    s1 = grp % 2
                    (q1, q2), t_q = tw_r.next()
                    st[grp] = (q1, q2, t_q)
                    k.op("pe", lambda en: en.matmul(PSm[s1][:], lhsT=Uin[:, grp * 128:(grp + 1) * 128], rhs=F1b[:], start=True, stop=True), reads=[t_U[src_nm], t_c], writes=[TP[s1]])
                    k.op("dve", lambda en: en.tensor_tensor(out=q1[:], in0=PSm[s1][:], in1=TW[:, 0, :], op=ALU.mult), reads=[TP[s1], t_c], writes=[t_q])
                    k.op("dve", lambda en: en.tensor_tensor(out=q2[:], in0=PSm[s1][:], in1=TW[:, 1, :], op=ALU.mult), reads=[TP[s1], t_c], writes=[t_q])

                def S2(grp):
                    sx = 2 + grp % 2
                    q1, q2, t_q = st[grp]
                    kh, t_kh = kh_r.next()
                    k.dma("sp", kh[:], KH[o, grp], reads=[g.T_KHAT], writes=[t_kh])
                    (y1, y2), t_y = yy_r.next()
                    st[grp] = (y1, y2, t_y)
                    seq = ((0, 0, q1, 0), (0, 3, q2, 1), (0, 2, q2, 0), (0, 2, q1, 1),
                           (1, 0, q2, 0), (1, 0, q1, 1), (1, 1, q1, 0), (1, 2, q2, 1))
                    for idx, (half, gi_, src, hs) in enumerate(seq):
                        k.op("pe", lambda en: en.matmul(PSm[sx][:, half * 256:(half + 1) * 256], lhsT=G4[:, gi_, :], rhs=src[:, hs * 256:(hs + 1) * 256], start=(idx == 0), stop=(idx == 7)), reads=[t_q, t_c], writes=[TP[sx]])
                    Xv = PSm[sx][:].rearrange("p (a n) -> p a n", a=2)
                    k.op("dve", lambda en: en.tensor_tensor(out=y1[:].rearrange("p (a n) -> p a n", a=2), in0=Xv, in1=kh[:, 0:256].unsqueeze(1).broadcast_to([128, 2, 256]), op=ALU.mult), reads=[TP[sx], t_kh], writes=[t_y])
                    k.op("dve", lambda en: en.tensor_tensor(out=y2[:].rearrange("p (a n) -> p a n", a=2), in0=Xv, in1=kh[:, 256:512].unsqueeze(1).broadcast_to([128, 2, 256]), op=ALU.mult), reads=[TP[sx], t_kh], writes=[t_y])

                def S3(grp):
                    y1, y2, t_y = st.pop(grp)
                    for bc in range(2):
                        pi_ = 4 + bc
                        (m1, m2), t_m = mm_r.next()
                        seq = ((y1, 0, 0), (y2, 1, 2), (y2, 0, 1), (y1, 1, 1))
                        for idx, (src, hs, ri_) in enumerate(seq):
                            k.op("pe", lambda en: en.matmul(PSm[pi_][:, 0:256], lhsT=src[:, hs * 256 + bc * 128:hs * 256 + (bc + 1) * 128], rhs=GI3[:, ri_, :], start=(idx == 0), stop=(idx == 3)), reads=[t_y, t_c], writes=[TP[pi_]])
                        k.op("dve", lambda en: en.tensor_tensor(out=m1[:, 0:256], in0=PSm[pi_][:, 0:256], in1=TWI[:, bc, 0, :], op=ALU.mult), reads=[TP[pi_], t_c], writes=[t_m])
                        k.op("dve", lambda en: en.tensor_tensor(out=m2[:, 0:256], in0=PSm[pi_][:, 0:256], in1=TWI[:, bc, 1, :], op=ALU.mult), reads=[TP[pi_], t_c, t_m], writes=[t_m])
                        k.op("pool", lambda en: en.tensor_tensor(out=BW[bc][0][:, grp * 128:(grp + 1) * 128], in0=m1[:, 0:128], in1=m2[:, 128:256], op=ALU.subtract), reads=[t_m], writes=[t_bw])
                        k.op("pool", lambda en: en.tensor_tensor(out=BW[bc][1][:, grp * 128:(grp + 1) * 128], in0=m2[:, 0:128], in1=m1[:, 128:256], op=ALU.add), reads=[t_m, t_bw], writes=[t_bw])

                for t_ in range(ng + 2):
                    if t_ < ng:
                        S1(t_)
                    if 0 <= t_ - 1 < ng:
                        S2(t_ - 1)
                    if 0 <= t_ - 2 < ng:
                        S3(t_ - 2)
                for ct in range(ncol // 512):
                    pi_ = 4 + ct % 2
                    cs = slice(ct * 512, (ct + 1) * 512)
                    idx = 0
                    for bc in range(2):
                        for ri in range(2):
                            k.op("pe", lambda en: en.matmul(PSm[pi_][:], lhsT=FI[:, bc, ri, :], rhs=BW[bc][ri][:, cs], start=(idx == 0), stop=(idx == 3)), reads=[t_bw, t_c], writes=[TP[pi_]])
                            idx += 1
                    epilogue(ct, cs, PSm[pi_], TP[pi_], o)

            def v4(ap):
                return ap.rearrange("p (g i c) -> p g i c", i=ni, c=cpg)

            def epi1(ct, cs, ps_, tps, o):
                bview = BR[:, o, ct * gpt * cpg:(ct + 1) * gpt * cpg].rearrange("p (g c) -> p g c", c=cpg).unsqueeze(2).broadcast_to([128, gpt, ni, cpg])
                k.op("pool", lambda en: en.tensor_tensor(out=v4(e1[:]), in0=v4(U["v"][:, cs]), in1=bview, op=ALU.mult), reads=[t_U["v"], t_c, t_e], writes=[t_e])
                k.op("dve", lambda en: en.tensor_tensor(out=e2[:], in0=ps_[:], in1=e1[:], op=ALU.add), reads=[tps, t_e], writes=[t_e])
                k.op("pool", lambda en: en.tensor_tensor(out=U["z1"][:, cs], in0=e2[:], in1=U["x1"][:, cs], op=ALU.mult), reads=[t_e, t_U["x1"]], writes=[t_U["z1"]])

            ZN = U["v"]

            def epi2(ct, cs, ps_, tps, o):
                bview = BR[:, o, ct * gpt * cpg:(ct + 1) * gpt * cpg].rearrange("p (g c) -> p g c", c=cpg).unsqueeze(2).broadcast_to([128, gpt, ni, cpg])
                k.op("pool", lambda en: en.tensor_tensor(out=v4(e1[:]), in0=v4(U["z1"][:, cs]), in1=bview, op=ALU.mult), reads=[t_U["z1"], t_c, t_e], writes=[t_e])
                k.op("dve", lambda en: en.tensor_tensor(out=e2[:], in0=ps_[:], in1=e1[:], op=ALU.add), reads=[tps, t_e], writes=[t_e])
                k.op("pool", lambda en: en.tensor_tensor(out=e3[:], in0=e2[:], in1=U["x2"][:, cs], op=ALU.mult), reads=[t_e, t_U["x2"]], writes=[t_e])
                outv = ZN[:].rearrange("p (i g c) -> p g i c", g=ng, c=cpg)[:, ct * gpt:(ct + 1) * gpt, :, :]
                k.op("dve", lambda en: en.tensor_tensor(out=outv, in0=v4(e3[:]), in1=v4(U["gt"][:, cs]), op=ALU.mult), reads=[t_e, t_U["gt"]], writes=[t_U["v"]])

            conv("v", 0, epi1)
            conv("z1", 1, epi2)
            ZT = U["x1"][:].rearrange("p (h t) -> p h t", h=2)
            cnt = 0
            for ch in range(2):
                nb4 = min(4, ni)
                for i0 in range(0, ni, nb4):
                    pt = cnt % 2
                    cnt += 1
                    for ii in range(nb4):
                        i = i0 + ii
                        k.op("pe", lambda en: en.transpose(PT[pt][:, ii, :], ZN[:, i * 256 + ch * 128:i * 256 + ch * 128 + 128], g.ident[:]), reads=[t_U["v"], g.T_ident], writes=[TPT[pt]])
                    outv = ZT[:, ch, :].rearrange("p (j i) -> p i j", i=ni)[:, i0:i0 + nb4, :]
                    k.op("act" if cnt % 2 else "dve",
                         (lambda en: en.activation(out=outv, in_=PT[pt][:, 0:nb4, :], func=AF.Copy)) if cnt % 2 else (lambda en: en.tensor_copy(out=outv, in_=PT[pt][:, 0:nb4, :])),
                         reads=[TPT[pt], t_U["x1"]], writes=[t_U["x1"]])
            k.dma("pool", g.CATT[:, 6:8, tok0:tok0 + n], ZT, reads=[t_U["x1"]], writes=[g.T_CATT])
            k.barrier()


TC = 16
NCH = T // TC
NDBL = 9


def ssm_host_layouts(inputs):
    f = lambda n: np.asarray(inputs[n], np.float32)
    lr, li, ls = f("ssm_lambda_re"), f("ssm_lambda_im"), f("ssm_log_step")
    br, bi, cr, ci = f("ssm_b_re"), f("ssm_b_im"), f("ssm_c_re"), f("ssm_c_im")
    lam = np.zeros((DEPTH, 128, 2, 16), np.float32)
    lsb = np.zeros((DEPTH, 128, 16), np.float32)
    Bp = np.zeros((DEPTH, 128, 2, 16, 64), np.float32)
    Cp = np.zeros((DEPTH, 128, 2, 16, 64), np.float32)
    for d in range(2):
        for q in range(8):
            for e in range(2):
                grp = 2 * q + e
                rows = slice(e * 64, (e + 1) * 64)
                dq = d * 8 + q
                lam[:, rows, 0, dq] = lr[:, d, grp, :]
                lam[:, rows, 1, dq] = li[:, d, grp, :]
                lsb[:, rows, dq] = ls[:, d, grp][:, None]
                pp = q % 2
                c0 = pp * 32 + e * 16
                Bp[:, rows, 0, dq, c0:c0 + 16] = br[:, d, grp, :, :]
                Bp[:, rows, 1, dq, c0:c0 + 16] = bi[:, d, grp, :, :]
                Cp[:, rows, 0, dq, c0:c0 + 16] = cr[:, d, grp, :, :].transpose(0, 2, 1)
                Cp[:, rows, 1, dq, c0:c0 + 16] = ci[:, d, grp, :, :].transpose(0, 2, 1)
    out = {"ssm_lam": lam, "ssm_ls": lsb, "ssm_Bp": Bp, "ssm_Cp": Cp}
    dd = f("ssm_d")
    gb = f("ssm_glu_b")
    out["ssm_cols"] = np.ascontiguousarray(np.stack([dd.reshape(DEPTH, 2, 128).transpose(0, 2, 1), gb.reshape(DEPTH, 2, 128).transpose(0, 2, 1)], -1))
    out["ssm_glu_w"] = f("ssm_glu_w")
    return out


def phase_ssm_weights(g, l):
    nc, k, W = g.nc, g.k, g.W
    PI = math.pi
    with ExitStack() as es:
        sb = lambda nm, s_, d=F32: es.enter_context(nc.sbuf_tensor(_nm(nm), s_, d))
        ps = lambda nm, s_, d=F32: es.enter_context(nc.psum_tensor(_nm(nm), s_, d))
        t = Tok()
        lam = sb("slam", [128, 2, 16]); ls = sb("sls", [128, 16])
        Bp = sb("sBp", [128, 2, 16, 64]); Cp = sb("sCp", [128, 2, 16, 64])
        k.dma("sp", lam[:], W["ssm_lam"][l], writes=[t])
        k.dma("sp", ls[:], W["ssm_ls"][l], writes=[t])
        k.dma("sp", Bp[:], W["ssm_Bp"][l], writes=[t])
        k.dma("sp", Cp[:], W["ssm_Cp"][l], writes=[t])
        sc = sb("ssc", [128, 24, 16])
        R = lambda i: sc[:, i, :]
        STEP, RHO, TH, MK, SIN, COS, AR, AI, DEN, NR, NI_, CRc, CIc, T1, T2 = range(15)

        def dv(fn, eng="dve"):
            k.op(eng, fn, reads=[t], writes=[t])

        dv(lambda en: en.activation(out=R(STEP), in_=ls[:], func=AF.Exp), "act")
        dv(lambda en: en.tensor_tensor(out=R(T1), in0=lam[:, 0, :], in1=R(STEP), op=ALU.mult))
        dv(lambda en: en.activation(out=R(RHO), in_=R(T1), func=AF.Exp), "act")
        dv(lambda en: en.tensor_tensor(out=R(TH), in0=lam[:, 1, :], in1=R(STEP), op=ALU.mult))
        for thr in (PI, 3 * PI, 5 * PI, 7 * PI):
            dv(lambda en: en.tensor_scalar(out=R(MK), in0=R(TH), scalar1=thr, scalar2=-2 * PI, op0=ALU.is_gt, op1=ALU.mult))
            if thr == PI:
                dv(lambda en: en.tensor_copy(out=R(T1), in_=R(MK)))
            else:
                dv(lambda en: en.tensor_tensor(out=R(T1), in0=R(T1), in1=R(MK), op=ALU.add))
        dv(lambda en: en.tensor_tensor(out=R(TH), in0=R(TH), in1=R(T1), op=ALU.add))
        dv(lambda en: en.activation(out=R(SIN), in_=R(TH), func=AF.Sin), "act")
        dv(lambda en: en.tensor_scalar(out=R(T2), in0=R(TH), scalar1=-1.0, scalar2=None, op0=ALU.mult))
        dv(lambda en: en.tensor_tensor(out=R(T1), in0=R(TH), in1=R(T2), op=ALU.max))
        dv(lambda en: en.tensor_scalar(out=R(T1), in0=R(T1), scalar1=-1.0, scalar2=PI / 2, op0=ALU.mult, op1=ALU.add))
        dv(lambda en: en.activation(out=R(COS), in_=R(T1), func=AF.Sin), "act")
        dv(lambda en: en.tensor_tensor(out=R(AR), in0=R(RHO), in1=R(COS), op=ALU.mult))
        dv(lambda en: en.tensor_tensor(out=R(AI), in0=R(RHO), in1=R(SIN), op=ALU.mult))
        dv(lambda en: en.tensor_tensor(out=R(DEN), in0=lam[:, 0, :], in1=lam[:, 0, :], op=ALU.mult))
        dv(lambda en: en.tensor_tensor(out=R(T1), in0=lam[:, 1, :], in1=lam[:, 1, :], op=ALU.mult))
        dv(lambda en: en.tensor_tensor(out=R(DEN), in0=R(DEN), in1=R(T1), op=ALU.add))
        dv(lambda en: en.reciprocal(out=R(DEN), in_=R(DEN)))
        dv(lambda en: en.tensor_scalar(out=R(T2), in0=R(AR), scalar1=-1.0, scalar2=None, op0=ALU.add))
        dv(lambda en: en.tensor_tensor(out=R(NR), in0=R(T2), in1=lam[:, 0, :], op=ALU.mult))
        dv(lambda en: en.tensor_tensor(out=R(T1), in0=R(AI), in1=lam[:, 1, :], op=ALU.mult))
        dv(lambda en: en.tensor_tensor(out=R(NR), in0=R(NR), in1=R(T1), op=ALU.add))
        dv(lambda en: en.tensor_tensor(out=R(NI_), in0=R(AI), in1=lam[:, 0, :], op=ALU.mult))
        dv(lambda en: en.tensor_tensor(out=R(T1), in0=R(T2), in1=lam[:, 1, :], op=ALU.mult))
        dv(lambda en: en.tensor_tensor(out=R(NI_), in0=R(NI_), in1=R(T1), op=ALU.subtract))
        dv(lambda en: en.tensor_tensor(out=R(CRc), in0=R(NR), in1=R(DEN), op=ALU.mult))
        dv(lambda en: en.tensor_tensor(out=R(CIc), in0=R(NI_), in1=R(DEN), op=ALU.mult))
        PW = sb("sPW", [128, 2, 17, 16])
        dv(lambda en: en.memset(PW[:, 0, 0, :], 1.0))
        dv(lambda en: en.memset(PW[:, 1, 0, :], 0.0))
        for n_ in range(16):
            dv(lambda en: en.tensor_tensor(out=R(T1), in0=PW[:, 0, n_, :], in1=R(AR), op=ALU.mult))
            dv(lambda en: en.tensor_tensor(out=R(T2), in0=PW[:, 1, n_, :], in1=R(AI), op=ALU.mult))
            dv(lambda en: en.tensor_tensor(out=PW[:, 0, n_ + 1, :], in0=R(T1), in1=R(T2), op=ALU.subtract))
            dv(lambda en: en.tensor_tensor(out=R(T1), in0=PW[:, 0, n_, :], in1=R(AI), op=ALU.mult))
            dv(lambda en: en.tensor_tensor(out=R(T2), in0=PW[:, 1, n_, :], in1=R(AR), op=ALU.mult))
            dv(lambda en: en.tensor_tensor(out=PW[:, 1, n_ + 1, :], in0=R(T1), in1=R(T2), op=ALU.add))
        A2 = g.lw.A2
        dv(lambda en: en.tensor_copy(out=A2[:, 0, 0, :], in_=PW[:, 0, 16, :]))
        dv(lambda en: en.tensor_copy(out=A2[:, 0, 1, :], in_=PW[:, 1, 16, :]))
        for kk in range(NDBL):
            if kk > 0:
                dv(lambda en: en.tensor_tensor(out=R(T1), in0=A2[:, kk - 1, 0, :], in1=A2[:, kk - 1, 0, :], op=ALU.mult))
                dv(lambda en: en.tensor_tensor(out=R(T2), in0=A2[:, kk - 1, 1, :], in1=A2[:, kk - 1, 1, :], op=ALU.mult))
                dv(lambda en: en.tensor_tensor(out=A2[:, kk, 0, :], in0=R(T1), in1=R(T2), op=ALU.subtract))
                dv(lambda en: en.tensor_tensor(out=R(T1), in0=A2[:, kk - 1, 0, :], in1=A2[:, kk - 1, 1, :], op=ALU.mult))
                dv(lambda en: en.tensor_scalar(out=A2[:, kk, 1, :], in0=R(T1), scalar1=2.0, scalar2=None, op0=ALU.mult))
            dv(lambda en: en.tensor_scalar(out=A2[:, kk, 2, :], in0=A2[:, kk, 1, :], scalar1=-1.0, scalar2=None, op0=ALU.mult))
        bc3 = lambda ap: ap.unsqueeze(2).broadcast_to([128, 16, 64])
        Bb = sb("sBb", [128, 2, 16, 64])
        big = [sb("sbig%d" % i, [128, 16, 64]) for i in range(4)]
        dv(lambda en: en.tensor_tensor(out=big[0][:], in0=Bp[:, 0], in1=bc3(R(CRc)), op=ALU.mult))
        dv(lambda en: en.tensor_tensor(out=big[1][:], in0=Bp[:, 1], in1=bc3(R(CIc)), op=ALU.mult))
        dv(lambda en: en.tensor_tensor(out=Bb[:, 0], in0=big[0][:], in1=big[1][:], op=ALU.subtract))
        dv(lambda en: en.tensor_tensor(out=big[0][:], in0=Bp[:, 1], in1=bc3(R(CRc)), op=ALU.mult))
        dv(lambda en: en.tensor_tensor(out=big[1][:], in0=Bp[:, 0], in1=bc3(R(CIc)), op=ALU.mult))
        dv(lambda en: en.tensor_tensor(out=Bb[:, 1], in0=big[0][:], in1=big[1][:], op=ALU.add))
        Bbb = sb("sBbb", [128, 2, 16, 64], BF16)
        dv(lambda en: en.tensor_copy(out=Bbb[:], in_=Bb[:]))
        CR = sb("sCR", [128, 2, 16, 17, 64], BF16)
        engs = ("dve", "pool")
        for n_ in range(17):
            e1 = engs[n_ % 2]
            dv(lambda en: en.tensor_tensor(out=big[0][:], in0=Cp[:, 0], in1=bc3(PW[:, 0, n_, :]), op=ALU.mult), e1)
            dv(lambda en: en.tensor_tensor(out=big[1][:], in0=Cp[:, 1], in1=bc3(PW[:, 1, n_, :]), op=ALU.mult), e1)
            dv(lambda en: en.tensor_tensor(out=CR[:, 0, :, n_, :], in0=big[0][:], in1=big[1][:], op=ALU.subtract), e1)
            dv(lambda en: en.tensor_tensor(out=big[2][:], in0=Cp[:, 0], in1=bc3(PW[:, 1, n_, :]), op=ALU.mult), e1)
            dv(lambda en: en.tensor_tensor(out=big[3][:], in0=Cp[:, 1], in1=bc3(PW[:, 0, n_, :]), op=ALU.mult), e1)
            dv(lambda en: en.tensor_tensor(out=big[2][:], in0=big[2][:], in1=big[3][:], op=ALU.add), e1)
            dv(lambda en: en.tensor_scalar(out=CR[:, 1, :, n_, :], in0=big[2][:], scalar1=-1.0, scalar2=None, op0=ALU.mult), e1)
        k.dma("pool", g.SSM_L3, CR[:], reads=[t], writes=[g.T_SSMW])
        L1W = sb("sL1W", [128, 2, 16, 2, 128], BF16)
        dv(lambda en: en.memset(L1W[:], 0.0), "pool")
        PS_ = [ps("sps%d" % i, [128, 512]) for i in range(2)]
        TPS_ = [Tok(True) for _ in range(2)]
        for d in range(2):
            for Q in range(4):
                hc, Ql = Q // 2, Q % 2
                for half in range(2):
                    first = True
                    for pp in range(2):
                        dq = d * 8 + 2 * Q + pp
                        for ri in range(2):
                            rhs = CR[:, ri, dq, half * 8:(half + 1) * 8, :].rearrange("p n c -> p (n c)")
                            k.op("pe", lambda en: en.matmul(PS_[half][Ql * 64:(Ql + 1) * 64, :], lhsT=Bbb[:, ri, dq, :], rhs=rhs, start=first, stop=(pp == 1 and ri == 1)), reads=[t], writes=[TPS_[half]])
                            first = False
                    k.op("act" if half else "dve",
                         (lambda en: en.activation(out=L1W[Ql * 64:(Ql + 1) * 64, d, half * 8:(half + 1) * 8, hc, Ql * 64:(Ql + 1) * 64], in_=PS_[half][Ql * 64:(Ql + 1) * 64, :].rearrange("p (n c) -> p n c", c=64), func=AF.Copy)) if half else
                         (lambda en: en.tensor_copy(out=L1W[Ql * 64:(Ql + 1) * 64, d, half * 8:(half + 1) * 8, hc, Ql * 64:(Ql + 1) * 64], in_=PS_[half][Ql * 64:(Ql + 1) * 64, :].rearrange("p (n c) -> p n c", c=64))),
                         reads=[TPS_[half], t], writes=[t])
        k.dma("pool", g.SSM_L1, L1W[:], reads=[t], writes=[g.T_SSMW])
        ZZ = sb("sZZ", [128, 2, 16, 64], BF16)
        PT = [ps("spt%d" % i, [128, 8, 128], BF16) for i in range(2)]
        TPT = [Tok(True) for _ in range(2)]
        sg_r = Rot([sb("ssg%d" % i, [128, 2, 2, 2, 128], BF16) for i in range(2)])
        for n_ in range(16):
            dv(lambda en: en.tensor_tensor(out=big[0][:], in0=Bb[:, 0], in1=bc3(PW[:, 0, n_, :]), op=ALU.mult))
            dv(lambda en: en.tensor_tensor(out=big[1][:], in0=Bb[:, 1], in1=bc3(PW[:, 1, n_, :]), op=ALU.mult))
            dv(lambda en: en.tensor_tensor(out=ZZ[:, 0], in0=big[0][:], in1=big[1][:], op=ALU.subtract))
            dv(lambda en: en.tensor_tensor(out=big[2][:], in0=Bb[:, 0], in1=bc3(PW[:, 1, n_, :]), op=ALU.mult), "pool")
            dv(lambda en: en.tensor_tensor(out=big[3][:], in0=Bb[:, 1], in1=bc3(PW[:, 0, n_, :]), op=ALU.mult), "pool")
            dv(lambda en: en.tensor_tensor(out=ZZ[:, 1], in0=big[2][:], in1=big[3][:], op=ALU.add), "pool")
            for d in range(2):
                sg, t_sg = sg_r.next()
                for hc in range(2):
                    pt = hc
                    for ri in range(2):
                        for Ql in range(2):
                            for pp in range(2):
                                dq = d * 8 + 2 * (hc * 2 + Ql) + pp
                                k.op("pe", lambda en: en.transpose(PT[pt][Ql * 64:(Ql + 1) * 64, ri * 2 + pp, :], ZZ[:, ri, dq, :], g.ident[:]), reads=[t, g.T_ident], writes=[TPT[pt]])
                    k.op("act" if hc else "dve",
                         (lambda en: en.activation(out=sg[:, hc].rearrange("p r q n -> p (r q) n"), in_=PT[pt][:, 0:4, :], func=AF.Copy)) if hc else
                         (lambda en: en.tensor_copy(out=sg[:, hc].rearrange("p r q n -> p (r q) n"), in_=PT[pt][:, 0:4, :])),
                         reads=[TPT[pt]], writes=[t_sg])
                k.dma("pool", g.SSM_SG[:, d, n_], sg[:], reads=[t_sg], writes=[g.T_SSMW])
        k.barrier()


def phase_ssm(g, l, s, need_ctx):
    nc, k, W, lw = g.nc, g.k, g.W, g.lw
    A2 = lw.A2
    with ExitStack() as es:
        sb = lambda nm, s_, d=F32: es.enter_context(nc.sbuf_tensor(_nm(nm), s_, d))
        ps = lambda nm, s_, d=F32: es.enter_context(nc.psum_tensor(_nm(nm), s_, d))
        PSm = [ps("mps%d" % i, [128, 512]) for i in range(8)]
        TP = [Tok(True) for _ in range(8)]
        ujm = sb("mujm", [128, 2, TC, NCH], BF16)
        gjm = sb("mgjm", [128, 2, TC, NCH], BF16)
        Sb = [[sb("mSb%d%d" % (d, ri), [128, 8, NCH], BF16) for ri in range(2)] for d in range(2)]
        t_u = Tok(); t_g = Tok(); t_sb = Tok()
        cols = sb("mcols", [128, 2, 2])
        GW = sb("mGW", [128, 2, 256], BF16)
        t_c = Tok()
        k.dma("sp", cols[:], W["ssm_cols"][l], writes=[t_c])
        with ExitStack() as es1:
            sb1 = lambda nm, s_, d=F32: es1.enter_context(nc.sbuf_tensor(_nm(nm), s_, d))
            wss = sb1("mwss", [128, 8, 512], BF16)
            gwf = sb1("mgwf", [128, 2, 256])
            t_w = Tok()
            k.dma("sp", wss[:], g.WINB[:, :, O_SU:O_SU + 512], reads=[g.T_WINB], writes=[t_w])
            k.dma("sp", gwf[:], W["ssm_glu_w"][l].rearrange("(kc p) n -> p kc n", p=128), writes=[t_w])
            k.op("dve", lambda en: en.tensor_copy(out=GW[:], in_=gwf[:]), reads=[t_w], writes=[t_c])
            hT_r = Rot([sb1("mhT%d" % i, [128, 8, 512], BF16) for i in range(2)])
            blocks = [(0, 256)] + [(256 + 512 * b, 512) for b in range(8)]
            cnt = 0
            for (t0, nb) in blocks:
                hT, t_h = hT_r.next()
                k.dma("sp", hT[:, :, 0:nb], g.HT[:, :, t0:t0 + nb], reads=[g.T_HT], writes=[t_h])
                c0, ncb = t0 // TC, nb // TC
                for cc in range(4):
                    pi_ = cnt % 4
                    cnt += 1
                    for kc in range(8):
                        k.op("pe", lambda en: en.matmul(PSm[pi_][:, 0:nb], lhsT=wss[:, kc, cc * 128:(cc + 1) * 128], rhs=hT[:, kc, 0:nb], start=(kc == 0), stop=(kc == 7)), reads=[t_w, t_h], writes=[TP[pi_]])
                    src = PSm[pi_][:, 0:nb].rearrange("p (c j) -> p j c", j=TC)
                    if cc < 2:
                        k.op("dve", lambda en: en.tensor_copy(out=ujm[:, cc, :, c0:c0 + ncb], in_=src), reads=[TP[pi_]], writes=[t_u])
                    else:
                        k.op("act", lambda en: en.activation(out=gjm[:, cc - 2, :, c0:c0 + ncb], in_=src, func=AF.Silu), reads=[TP[pi_]], writes=[t_g])
            k.barrier()
        with ExitStack() as es2:
            sb2 = lambda nm, s_, d=F32: es2.enter_context(nc.sbuf_tensor(_nm(nm), s_, d))
            SG = sb2("mSG", [128, 2, 16, 2, 2, 2, 128], BF16)
            t_sg = Tok()
            k.dma("sp", SG[:, 0], g.SSM_SG[:, 0], reads=[g.T_SSMW], writes=[t_sg])
            k.dma("sp", SG[:, 1], g.SSM_SG[:, 1], reads=[g.T_SSMW], writes=[t_sg])
            S = [[sb2("mS%d%d" % (d, ri), [128, 8, NCH]) for ri in range(2)] for d in range(2)]
            t_S = [[Tok() for q in range(8)] for d in range(2)]
            Tt = [[sb2("mT%d%d" % (i, ri), [128, NCH]) for ri in range(2)] for i in range(4)]
            t_T = [(Tok(), Tok()) for _ in range(4)]
            cnt = 0
            for d in range(2):
                for q in range(8):
                    Q, pp = q // 2, q % 2
                    hc, Ql = Q // 2, Q % 2
                    rows = slice(Ql * 64, (Ql + 1) * 64)
                    for ri in range(2):
                        pi_ = cnt % 4
                        cnt += 1
                        for i in range(TC):
                            n_ = (TC - 1 - i) if d == 0 else i
                            k.op("pe", lambda en: en.matmul(PSm[pi_][:, 0:NCH], lhsT=SG[rows, d, n_, hc, ri, pp, :], rhs=ujm[rows, hc, i, :], start=(i == 0), stop=(i == TC - 1)), reads=[t_sg, t_u], writes=[TP[pi_]])
                        if d == 0:
                            k.op("act" if ri else "dve", (lambda en: en.activation(out=S[d][ri][:, q, :], in_=PSm[pi_][:, 0:NCH], func=AF.Copy)) if ri else (lambda en: en.tensor_copy(out=S[d][ri][:, q, :], in_=PSm[pi_][:, 0:NCH])), reads=[TP[pi_]], writes=[t_S[d][q]])
                        else:
                            k.op("dve", lambda en: en.tensor_copy(out=S[d][ri][:, q, 0:256], in_=PSm[pi_][:, 16:NCH]), reads=[TP[pi_]], writes=[t_S[d][q]])
                            k.op("act", lambda en: en.activation(out=S[d][ri][:, q, 256:NCH], in_=PSm[pi_][:, 0:16], func=AF.Copy), reads=[TP[pi_]], writes=[t_S[d][q]])
            for kk in range(NDBL):
                sh = 1 << kk
                w_ = NCH - sh
                it = 0
                for d in range(2):
                    dst = slice(sh, NCH) if d == 0 else slice(0, w_)
                    srcs = slice(0, w_) if d == 0 else slice(sh, NCH)
                    for q in range(8):
                        dq = d * 8 + q
                        Ar, Ai, nAi = A2[:, kk, 0, dq:dq + 1], A2[:, kk, 1, dq:dq + 1], A2[:, kk, 2, dq:dq + 1]
                        Tr, Ti = Tt[it % 4]
                        tTr, tTi = t_T[it % 4]
                        it += 1
                        Sr, Si = S[d][0], S[d][1]
                        ts = t_S[d][q]
                        k.op("act", lambda en: en.activation(out=Tr[:, 0:w_], in_=Sr[:, q, srcs], func=AF.Copy, scale=Ar), reads=[ts, lw.tok], writes=[tTr])
                        k.op("act", lambda en: en.activation(out=Ti[:, 0:w_], in_=Si[:, q, srcs], func=AF.Copy, scale=Ar), reads=[ts, lw.tok], writes=[tTi])
                        k.op("dve", lambda en: en.scalar_tensor_tensor(out=Tr[:, 0:w_], in0=Si[:, q, srcs], scalar=nAi, in1=Tr[:, 0:w_], op0=ALU.mult, op1=ALU.add), reads=[ts, tTr, lw.tok], writes=[tTr])
                        k.op("dve", lambda en: en.scalar_tensor_tensor(out=Ti[:, 0:w_], in0=Sr[:, q, srcs], scalar=Ai, in1=Ti[:, 0:w_], op0=ALU.mult, op1=ALU.add), reads=[ts, tTi, lw.tok], writes=[tTi])
                        k.op("pool", lambda en: en.tensor_tensor(out=Sr[:, q, dst], in0=Sr[:, q, dst], in1=Tr[:, 0:w_], op=ALU.add), reads=[tTr, ts], writes=[ts])
                        k.op("dve", lambda en: en.tensor_tensor(out=Si[:, q, dst], in0=Si[:, q, dst], in1=Ti[:, 0:w_], op=ALU.add), reads=[tTi, ts], writes=[ts])
            for d in range(2):
                for ri in range(2):
                    k.op("act" if ri else "dve", (lambda en: en.activation(out=Sb[d][ri][:], in_=S[d][ri][:], func=AF.Copy)) if ri else (lambda en: en.tensor_copy(out=Sb[d][ri][:], in_=S[d][ri][:])), reads=[t_S[d][q] for q in range(8)], writes=[t_sb])
            k.barrier()
        with ExitStack() as es3:
            sb3 = lambda nm, s_, d=F32: es3.enter_context(nc.sbuf_tensor(_nm(nm), s_, d))
            L3 = sb3("mL3", [128, 2, 16, 17, 64], BF16)
            L1W = sb3("mL1W", [128, 2, 16, 2, 128], BF16)
            t_l = Tok()
            k.dma("sp", L3[:, 0], g.SSM_L3[:, 0], reads=[g.T_SSMW], writes=[t_l])
            k.dma("sp", L3[:, 1], g.SSM_L3[:, 1], reads=[g.T_SSMW], writes=[t_l])
            k.dma("sp", L1W[:], g.SSM_L1, reads=[g.T_SSMW], writes=[t_l])
            Yjm = sb3("mYjm", [128, 2, TC, NCH])
            t_y = Tok()
            cnt = 0
            for j in range(TC):
                for hc in range(2):
                    pi_ = cnt % 4
                    cnt += 1
                    yp = PSm[pi_]
                    first = True
                    for tau in range(j + 1):
                        k.op("pe", lambda en: en.matmul(yp[:, 0:NCH], lhsT=L1W[:, 0, tau, hc, :], rhs=ujm[:, hc, j - tau, :], start=first, stop=False), reads=[t_l, t_u], writes=[TP[pi_]])
                        first = False
                    for tau in range(TC - j):
                        k.op("pe", lambda en: en.matmul(yp[:, 0:NCH], lhsT=L1W[:, 1, tau, hc, :], rhs=ujm[:, hc, j + tau, :], start=False, stop=False), reads=[t_l, t_u], writes=[TP[pi_]])
                    for Ql in range(2):
                        rows = slice(Ql * 64, (Ql + 1) * 64)
                        for pp in range(2):
                            q = 2 * (2 * hc + Ql) + pp
                            for ri in range(2):
                                k.op("pe", lambda en: en.matmul(yp[rows, 1:NCH], lhsT=L3[:, ri, q, j + 1, :], rhs=Sb[0][ri][:, q, 0:NCH - 1], start=False, stop=False), reads=[t_l, t_sb], writes=[TP[pi_]])
                                k.op("pe", lambda en: en.matmul(yp[rows, 16:NCH], lhsT=L3[:, ri, 8 + q, TC - j, :], rhs=Sb[1][ri][:, q, 1:257], start=False, stop=False), reads=[t_l, t_sb], writes=[TP[pi_]])
                                lastm = (Ql == 1 and pp == 1 and ri == 1)
                                k.op("pe", lambda en: en.matmul(yp[rows, 0:15], lhsT=L3[:, ri, 8 + q, TC - j, :], rhs=Sb[1][ri][:, q, 257:NCH], start=False, stop=lastm), reads=[t_l, t_sb], writes=[TP[pi_]])
                    k.op("dve", lambda en: en.scalar_tensor_tensor(out=Yjm[:, hc, j, :], in0=ujm[:, hc, j, :], scalar=cols[:, hc, 0:1], in1=yp[:, 0:NCH], op0=ALU.mult, op1=ALU.add), reads=[TP[pi_], t_u, t_c], writes=[t_y])
            k.barrier()
            FL = TC * NCH
            Yf = [Yjm[:, hc].rearrange("p j c -> p (j c)") for hc in range(2)]
            Gf = [gjm[:, hc].rearrange("p j c -> p (j c)") for hc in range(2)]
            Zb = ujm
            Zf = [Zb[:, hc].rearrange("p j c -> p (j c)") for hc in range(2)]
            w1 = sb3("mw1", [128, 512]); w2 = sb3("mw2", [128, 512]); w3 = sb3("mw3", [128, 2, 512])
            t_w1 = Tok(); t_z = Tok(); t_w3 = Tok()
            CG = 1.5957691216057308
            pieces = [(c0, min(512, FL - c0)) for c0 in range(0, FL, 512)]
            for (c0, w_) in pieces:
                cs = slice(c0, c0 + w_)
                for hc in range(2):
                    k.op("pool", lambda en: en.tensor_tensor(out=w1[:, 0:w_], in0=Yf[hc][:, cs], in1=Yf[hc][:, cs], op=ALU.mult), reads=[t_y, t_w1], writes=[t_w1])
                    k.op("dve", lambda en: en.tensor_scalar(out=w1[:, 0:w_], in0=w1[:, 0:w_], scalar1=0.044715, scalar2=1.0, op0=ALU.mult, op1=ALU.add), reads=[t_w1], writes=[t_w1])
                    k.op("pool", lambda en: en.tensor_tensor(out=w1[:, 0:w_], in0=w1[:, 0:w_], in1=Yf[hc][:, cs], op=ALU.mult), reads=[t_w1, t_y], writes=[t_w1])
                    k.op("act", lambda en: en.activation(out=w2[:, 0:w_], in_=w1[:, 0:w_], func=AF.Sigmoid, scale=CG), reads=[t_w1], writes=[t_w1])
                    k.op("dve", lambda en: en.tensor_tensor(out=w3[:, hc, 0:w_], in0=w2[:, 0:w_], in1=Yf[hc][:, cs], op=ALU.mult), reads=[t_w1, t_y, t_w3], writes=[t_w3])
                    k.op("pool", lambda en: en.tensor_copy(out=Zf[hc][:, cs], in_=w3[:, hc, 0:w_]), reads=[t_w3, t_u], writes=[t_z])
                for oc in range(2):
                    pi_ = 4 + oc
                    for kc in range(2):
                        k.op("pe", lambda en: en.matmul(PSm[pi_][:, 0:w_], lhsT=GW[:, kc, oc * 128:(oc + 1) * 128], rhs=Zf[kc][:, cs], start=(kc == 0), stop=(kc == 1)), reads=[t_z, t_c], writes=[TP[pi_]])
                    k.op("act", lambda en: en.activation(out=w2[:, 0:w_], in_=PSm[pi_][:, 0:w_], func=AF.Sigmoid, bias=cols[:, oc, 1:2]), reads=[TP[pi_], t_c, t_w1], writes=[t_w1])
                    k.op("dve", lambda en: en.tensor_tensor(out=w2[:, 0:w_], in0=w2[:, 0:w_], in1=w3[:, oc, 0:w_], op=ALU.mult), reads=[t_w1, t_w3], writes=[t_w1])
                    k.op("pool", lambda en: en.tensor_tensor(out=Gf[oc][:, cs], in0=w2[:, 0:w_], in1=Gf[oc][:, cs], op=ALU.mult), reads=[t_w1, t_g], writes=[t_g])
            on_t = sb3("mON", [128, 2, T], BF16)
            t_on = Tok()
            for hc in range(2):
                k.op("dve" if hc else "pool", lambda en: en.tensor_copy(out=on_t[:, hc, :].rearrange("p (c j) -> p j c", j=TC), in_=gjm[:, hc]), reads=[t_g], writes=[t_on])
            if need_ctx:
                k.dma("pool", g.CATT[:, 4:6, :], on_t[:], reads=[t_on], writes=[g.T_CATT])
            else:
                k.dma("pool", g.CATT[:, 4:6, C:T], on_t[:, :, C:T], reads=[t_on], writes=[g.T_CATT])
            k.barrier()
```
